# Optimizing a Trainium2 kernel written in Bass

```python
import math
import jax
import jax.numpy as jnp
from jax import lax
import numpy as np


D_MODEL = 1024
BATCH = 4
SEQ = 4096
DEPTH = 4

A_HEADS = 8
A_HEAD_DIM = 128
IDX_HEADS = 8
IDX_DIM = 64
TOPK_MAX = 256
Q_BLOCK = 128
REL_BUCKETS = 32
REL_MAX_DIST = 128
B_HEADS = 8
B_HEAD_DIM = D_MODEL // B_HEADS
B_CHUNK = 64
C_HEADS = 4
C_QK_DIM = 256
C_V_DIM = 512
C_CHUNK = 128
N_GROUPS = 4
EXPERTS_PER_GROUP = 8
N_EXPERTS = N_GROUPS * EXPERTS_PER_GROUP
EXPERT_TOPK = 2
D_EXPERT = 512
EPS = 1e-6

A_WIDTH = A_HEADS * A_HEAD_DIM
B_WIDTH = B_HEADS * B_HEAD_DIM
C_QK_WIDTH = C_HEADS * C_QK_DIM
C_V_WIDTH = C_HEADS * C_V_DIM
IN_SPLITS = (A_WIDTH, A_WIDTH, A_WIDTH, IDX_HEADS * IDX_DIM, IDX_DIM, IDX_HEADS,
             B_WIDTH, B_WIDTH, B_WIDTH, B_WIDTH,
             C_QK_WIDTH, C_QK_WIDTH, C_V_WIDTH, C_V_WIDTH,
             D_MODEL, D_MODEL, D_MODEL)
N_IN = sum(IN_SPLITS)

kernel_name = 'hybrid_dsa_hgrn2_retnet_hmoe'


def rmsnorm(x, g=None):
    xf = x.astype(jnp.float32)
    y = xf * lax.rsqrt(jnp.mean(xf * xf, axis=-1, keepdims=True) + EPS)
    if g is not None:
        y = y * g.astype(jnp.float32)
    return y.astype(x.dtype)


def t5_bucket(rel):
    max_exact = REL_BUCKETS // 2
    relf = jnp.maximum(rel, 1).astype(jnp.float32)
    large = max_exact + (jnp.log(relf / max_exact) / math.log(REL_MAX_DIST / max_exact)
                         * (REL_BUCKETS - max_exact)).astype(jnp.int32)
    large = jnp.minimum(large, REL_BUCKETS - 1)
    return jnp.where(rel < max_exact, rel, large)


def split_cols(proj):
    offs = np.cumsum(np.array(IN_SPLITS))[:-1].tolist()
    return jnp.split(proj, offs, axis=-1)


def dsa_attention(q, k, v, q_idx, k_idx, w_idx, rel_bias):
    bsz, seq = q.shape[0], q.shape[1]
    top_k = min(TOPK_MAX, seq // 4)
    n_blk = seq // Q_BLOCK
    s_pos = jnp.arange(seq, dtype=jnp.int32)
    k_idx32 = k_idx.astype(jnp.float32)
    idx_scale = (IDX_HEADS * IDX_DIM) ** -0.5

    def to_blocks(a):
        return jnp.moveaxis(a.reshape((bsz, n_blk, Q_BLOCK) + a.shape[2:]), 1, 0)

    def one_block(args):
        qb, qib, wb, t0 = args
        t_pos = t0 + jnp.arange(Q_BLOCK, dtype=jnp.int32)
        rel = jax.nn.relu(jnp.einsum('bqhd,bsd->bqhs', qib.astype(jnp.float32), k_idx32))
        score = jnp.einsum('bqhs,bqh->bqs', rel, wb.astype(jnp.float32) * idx_scale)
        causal = s_pos[None, :] <= t_pos[:, None]
        score = jnp.where(causal[None], score, -jnp.inf)
        _, sel = lax.top_k(score, top_k)
        valid = sel <= t_pos[None, :, None]
        k_sel = jax.vmap(lambda kb, ib: kb[ib])(k, sel)
        v_sel = jax.vmap(lambda vb, ib: vb[ib])(v, sel)
        logits = jnp.einsum('bqhd,bqkhd->bqhk', qb, k_sel).astype(jnp.float32) * (A_HEAD_DIM ** -0.5)
        bucket = t5_bucket(jnp.maximum(t_pos[None, :, None] - sel, 0))
        logits = logits + jnp.transpose(rel_bias[bucket], (0, 1, 3, 2)).astype(jnp.float32)
        logits = jnp.where(valid[:, :, None, :], logits, -jnp.inf)
        p = jax.nn.softmax(logits, axis=-1).astype(v.dtype)
        return jnp.einsum('bqhk,bqkhd->bqhd', p, v_sel)

    t0s = jnp.arange(n_blk, dtype=jnp.int32) * Q_BLOCK
    out = lax.map(one_block, (to_blocks(q), to_blocks(q_idx), to_blocks(w_idx), t0s))
    return jnp.moveaxis(out, 0, 1).reshape(bsz, seq, A_WIDTH)


def hgrn2(q, f_pre, i, g_out, lb, norm_g):
    bsz, seq = q.shape[0], q.shape[1]
    nc = seq // B_CHUNK
    lb = lb.reshape(B_HEADS, B_HEAD_DIM).astype(jnp.float32)
    f = lb + (1.0 - lb) * jax.nn.sigmoid(f_pre.astype(jnp.float32))
    log_f = jnp.log(f)
    kk = 1.0 - f

    def chunks(a):
        return jnp.transpose(a.reshape(bsz, nc, B_CHUNK, B_HEADS, -1), (1, 0, 3, 2, 4))

    causal = jnp.tril(jnp.ones((B_CHUNK, B_CHUNK), dtype=bool))

    def step(state, inp):
        qt, kt, vt, gt = inp
        b = jnp.cumsum(gt, axis=2)
        diff = b[:, :, :, None, :] - b[:, :, None, :, :]
        decay = jnp.exp(jnp.where(causal[None, None, :, :, None], diff, -jnp.inf))
        attn = jnp.einsum('bhtd,bhsd,bhtsd->bhts', qt, kt, decay)
        o = (jnp.einsum('bhts,bhsv->bhtv', attn, vt)
             + jnp.einsum('bhtd,bhdv->bhtv', qt * jnp.exp(b), state))
        b_last = b[:, :, -1:, :]
        state = (jnp.exp(b_last[:, :, 0, :, None]) * state
                 + jnp.einsum('bhsd,bhsv->bhdv', kt * jnp.exp(b_last - b), vt))
        return state, o

    s0 = jnp.zeros((bsz, B_HEADS, B_HEAD_DIM, B_HEAD_DIM), jnp.float32)
    _, o = lax.scan(step, s0, (chunks(q.astype(jnp.float32)), chunks(kk),
                               chunks(i.astype(jnp.float32)), chunks(log_f)))
    o = jnp.transpose(o, (1, 0, 3, 2, 4)).reshape(bsz, seq, B_HEADS, B_HEAD_DIM)
    o = rmsnorm(o, norm_g) * jax.nn.silu(g_out.astype(jnp.float32))
    return o.reshape(bsz, seq, B_WIDTH).astype(q.dtype)


def retention(q, k, v, g):
    bsz, seq = q.shape[0], q.shape[1]
    nc = seq // C_CHUNK
    pos = jnp.arange(seq, dtype=jnp.float32)
    theta = jnp.repeat(1.0 / (10000.0 ** jnp.linspace(0.0, 1.0, C_QK_DIM // 2)), 2)
    ang = pos[:, None] * theta[None, :]
    sin = jnp.sin(ang)[None, :, None, :]
    cos = jnp.cos(ang)[None, :, None, :]

    def rot(a):
        a2 = jnp.stack([-a[..., 1::2], a[..., 0::2]], axis=-1).reshape(a.shape)
        return a * cos + a2 * sin

    qr = rot(q.astype(jnp.float32))
    kr = rot(k.astype(jnp.float32)) * (C_QK_DIM ** -0.5)
    log_gamma = jnp.log(1.0 - 2.0 ** (-5.0 - jnp.arange(C_HEADS, dtype=jnp.float32)))
    idx = jnp.arange(C_CHUNK, dtype=jnp.float32)
    causal = idx[:, None] >= idx[None, :]
    intra_decay = jnp.exp(jnp.where(causal[None], (idx[:, None] - idx[None, :])[None] * log_gamma[:, None, None], -jnp.inf))
    q_decay = jnp.exp((idx + 1.0)[None, :] * log_gamma[:, None])[..., None]
    k_decay = jnp.exp((C_CHUNK - 1.0 - idx)[None, :] * log_gamma[:, None])[..., None]
    chunk_decay = jnp.exp(C_CHUNK * log_gamma)[:, None, None]

    def chunks(a):
        return jnp.transpose(a.reshape(bsz, nc, C_CHUNK, C_HEADS, -1), (1, 0, 3, 2, 4))

    def step(state, inp):
        qt, kt, vt = inp
        attn = jnp.einsum('bhtd,bhsd->bhts', qt, kt) * intra_decay
        o = (jnp.einsum('bhts,bhsv->bhtv', attn, vt)
             + jnp.einsum('bhtd,bhdv->bhtv', qt * q_decay, state))
        state = chunk_decay * state + jnp.einsum('bhsd,bhsv->bhdv', kt * k_decay, vt)
        return state, o

    s0 = jnp.zeros((bsz, C_HEADS, C_QK_DIM, C_V_DIM), jnp.float32)
    _, o = lax.scan(step, s0, (chunks(qr), chunks(kr), chunks(v.astype(jnp.float32))))
    o = jnp.transpose(o, (1, 0, 3, 2, 4)).reshape(bsz, seq, C_HEADS, C_V_DIM)
    o = rmsnorm(o).reshape(bsz, seq, C_V_WIDTH)
    return (jax.nn.silu(g.astype(jnp.float32)) * o).astype(q.dtype)


def hier_moe(h, rg_w, rg_b, re_w, re_b, w_gate, w_up, w_down):
    bsz, seq, dm = h.shape
    x = h.reshape(-1, dm)
    group_logits = (x @ rg_w + rg_b).astype(jnp.float32)
    group_p = jax.nn.softmax(group_logits, axis=-1)
    g_sel = jnp.argmax(group_logits, axis=-1)
    g_prob = jnp.take_along_axis(group_p, g_sel[:, None], axis=-1)
    exp_logits = (x @ re_w + re_b).astype(jnp.float32).reshape(-1, N_GROUPS, EXPERTS_PER_GROUP)
    in_group = jnp.take_along_axis(exp_logits, g_sel[:, None, None], axis=1)[:, 0]
    top_v, top_i = lax.top_k(in_group, EXPERT_TOPK)
    top_w = jax.nn.softmax(top_v, axis=-1) * g_prob
    w_group = jnp.sum(jax.nn.one_hot(top_i, EXPERTS_PER_GROUP, dtype=jnp.float32) * top_w[..., None], axis=1)
    gates = (jax.nn.one_hot(g_sel, N_GROUPS, dtype=jnp.float32)[:, :, None] * w_group[:, None, :]).astype(x.dtype)
    wg = w_gate.reshape(N_GROUPS, EXPERTS_PER_GROUP, dm, D_EXPERT)
    wu = w_up.reshape(N_GROUPS, EXPERTS_PER_GROUP, dm, D_EXPERT)
    wd = w_down.reshape(N_GROUPS, EXPERTS_PER_GROUP, D_EXPERT, dm)
    y = jnp.zeros_like(x)
    for gi in range(N_GROUPS):
        hg = jax.nn.silu(jnp.einsum('nd,edf->nef', x, wg[gi])) * jnp.einsum('nd,edf->nef', x, wu[gi])
        y = y + jnp.einsum('nef,efd->nd', hg * gates[:, gi, :, None], wd[gi])
    return y.reshape(bsz, seq, dm)


def hybrid_layer(x, c, lb, rel_bias, norm1_g, norm2_g, ada_w, ada_b, w_in, hgrn_norm_g,
                 w_branch_a, w_branch_b, w_branch_c, w_out, rg_w, rg_b, re_w, re_b,
                 e_gate, e_up, e_down):
    bsz, seq, _ = x.shape
    mod = (jax.nn.silu(c) @ ada_w + ada_b)[:, None, :]
    sh1, sc1, gt1, sh2, sc2, gt2 = jnp.split(mod, 6, axis=-1)
    h = rmsnorm(x, norm1_g) * (1.0 + sc1) + sh1
    (aq, ak, av, iq, ik, iw, bq, bf, bi, bg, cq, ck, cv, cg, ga, gb, gc) = split_cols(h @ w_in)
    o_a = dsa_attention(aq.reshape(bsz, seq, A_HEADS, A_HEAD_DIM),
                        ak.reshape(bsz, seq, A_HEADS, A_HEAD_DIM),
                        av.reshape(bsz, seq, A_HEADS, A_HEAD_DIM),
                        iq.reshape(bsz, seq, IDX_HEADS, IDX_DIM), ik, iw, rel_bias)
    hs = (bsz, seq, B_HEADS, B_HEAD_DIM)
    o_b = hgrn2(bq.reshape(hs), bf.reshape(hs), bi.reshape(hs), bg.reshape(hs), lb, hgrn_norm_g)
    o_c = retention(cq.reshape(bsz, seq, C_HEADS, C_QK_DIM), ck.reshape(bsz, seq, C_HEADS, C_QK_DIM),
                    cv.reshape(bsz, seq, C_HEADS, C_V_DIM), cg)
    merged = (jax.nn.sigmoid(ga) * (o_a @ w_branch_a)
              + jax.nn.sigmoid(gb) * (o_b @ w_branch_b)
              + jax.nn.sigmoid(gc) * (o_c @ w_branch_c))
    x = x + gt1 * (merged @ w_out)
    h2 = rmsnorm(x, norm2_g) * (1.0 + sc2) + sh2
    x = x + gt2 * hier_moe(h2, rg_w, rg_b, re_w, re_b, e_gate, e_up, e_down)
    return x


def setup_inputs(seed: int = 0) -> dict:
    key = jax.random.key(seed)
    ks = jax.random.split(key, 22)
    D = D_MODEL

    def nrm(k, shape, scale):
        return jax.random.normal(k, shape, jnp.float32) * scale

    return {
        'x': nrm(ks[0], (BATCH, SEQ, D), 1.0),
        'c': nrm(ks[1], (BATCH, D), 1.0),
        'rel_bias': nrm(ks[2], (REL_BUCKETS, A_HEADS), 0.5),
        'hgrn_lb_raw': nrm(ks[3], (DEPTH, B_WIDTH), 0.5),
        'norm1_g': 1.0 + nrm(ks[4], (DEPTH, D), 0.05),
        'norm2_g': 1.0 + nrm(ks[5], (DEPTH, D), 0.05),
        'ada_w': nrm(ks[6], (DEPTH, D, 6 * D), 0.5 * D ** -0.5),
        'ada_b': nrm(ks[7], (DEPTH, 6 * D), 0.02),
        'w_in': nrm(ks[8], (DEPTH, D, N_IN), D ** -0.5),
        'hgrn_norm_g': 1.0 + nrm(ks[9], (DEPTH, B_HEAD_DIM), 0.05),
        'w_branch_a': nrm(ks[10], (DEPTH, A_WIDTH, D), A_WIDTH ** -0.5),
        'w_branch_b': nrm(ks[11], (DEPTH, B_WIDTH, D), B_WIDTH ** -0.5),
        'w_branch_c': nrm(ks[12], (DEPTH, C_V_WIDTH, D), C_V_WIDTH ** -0.5),
        'w_out': nrm(ks[13], (DEPTH, D, D), D ** -0.5),
        'router_group_w': nrm(ks[14], (DEPTH, D, N_GROUPS), D ** -0.5),
        'router_group_b': nrm(ks[15], (DEPTH, N_GROUPS), 0.01),
        'router_expert_w': nrm(ks[16], (DEPTH, D, N_EXPERTS), D ** -0.5),
        'router_expert_b': nrm(ks[17], (DEPTH, N_EXPERTS), 0.01),
        'expert_w_gate': nrm(ks[18], (DEPTH, N_EXPERTS, D, D_EXPERT), D ** -0.5),
        'expert_w_up': nrm(ks[19], (DEPTH, N_EXPERTS, D, D_EXPERT), D ** -0.5),
        'expert_w_down': nrm(ks[20], (DEPTH, N_EXPERTS, D_EXPERT, D), D_EXPERT ** -0.5),
        'final_norm_g': 1.0 + nrm(ks[21], (D,), 0.05),
    }


def reference(x, c, rel_bias, hgrn_lb_raw, norm1_g, norm2_g, ada_w, ada_b, w_in, hgrn_norm_g,
              w_branch_a, w_branch_b, w_branch_c, w_out, router_group_w, router_group_b,
              router_expert_w, router_expert_b, expert_w_gate, expert_w_up, expert_w_down,
              final_norm_g):
    lb_soft = jax.nn.softmax(hgrn_lb_raw.astype(jnp.float32), axis=0)
    lb_all = jnp.cumsum(lb_soft, axis=0) - lb_soft[0:1]
    for l in range(DEPTH):
        x = hybrid_layer(x, c, lb_all[l], rel_bias, norm1_g[l], norm2_g[l], ada_w[l], ada_b[l],
                         w_in[l], hgrn_norm_g[l], w_branch_a[l], w_branch_b[l], w_branch_c[l],
                         w_out[l], router_group_w[l], router_group_b[l], router_expert_w[l],
                         router_expert_b[l], expert_w_gate[l], expert_w_up[l], expert_w_down[l])
    return rmsnorm(x, final_norm_g)
```

```python
import contextlib
import math
import numpy as np
import concourse.bass as bass
import concourse.mybir as mybir
from concourse.bass_utils import run_bass_kernel_spmd

F32 = mybir.dt.float32
BF16 = mybir.dt.bfloat16
AF = mybir.ActivationFunctionType
ALU = mybir.AluOpType
AX = mybir.AxisListType

D = 1024
T = 4096
DEPTH = 4
NT = T // 128
N_IN = 16968
EPS = 1e-6
O_AQ, O_AK, O_AV = 0, 1024, 2048
O_IQ, O_IK, O_IW = 3072, 3584, 3648
O_BQ, O_BF, O_BI, O_BG = 3656, 4680, 5704, 6728
O_CQ, O_CK, O_CV, O_CG = 7752, 8776, 9800, 11848
O_GA, O_GB, O_GC = 13896, 14920, 15944

COMPUTE = ("pe", "act", "dve", "pool")


class Buf:
    __slots__ = ("name", "writers", "readers", "sem")

    def __init__(self, name):
        self.name = name
        self.writers = []
        self.readers = []
        self.sem = None


class Op:
    __slots__ = ("eng", "emit", "deps", "is_dma", "sem", "semval", "used", "sig")

    def __init__(self, eng, emit, is_dma):
        self.eng = eng
        self.emit = emit
        self.deps = []
        self.is_dma = is_dma
        self.sem = None
        self.semval = 0
        self.used = False
        self.sig = 0


class Ctx:
    def __init__(self, nc, stack):
        self.nc = nc
        self.eng_sem = {}
        self.eng_cnt = {}
        for e in COMPUTE:
            self.eng_sem[e] = stack.enter_context(nc.semaphore("es_" + e))
            self.eng_cnt[e] = 0
        self.pool = []
        for i in range(56):
            self.pool.append([stack.enter_context(nc.semaphore("ds%d" % i)), 0])
        self.nphase = 0


class Phase:
    def __init__(self, ctx, name):
        self.ctx = ctx
        self.nc = ctx.nc
        self.name = "%s_%d" % (name, ctx.nphase)
        ctx.nphase += 1
        self.ops = []
        self.stack = contextlib.ExitStack()
        self.sems = []
        self.nbuf = 0

    def sb(self, name, shape, dtype):
        t = self.stack.enter_context(self.nc.sbuf_tensor("%s_%s" % (self.name, name), list(shape), dtype))
        return t, Buf(name)

    def ps(self, name, shape=(128, 512), dtype=F32):
        t = self.stack.enter_context(self.nc.psum_tensor("%s_%s" % (self.name, name), list(shape), dtype))
        return t, Buf(name)

    def buf(self, name):
        return Buf(name)

    def _sem_for(self, b):
        if b.sem is None:
            b.sem = self.ctx.pool.pop()
            self.sems.append(b.sem)
        return b.sem

    def _record(self, op, reads, writes):
        deps = []
        for b in reads:
            deps.extend(b.writers)
        for b in writes:
            keep = []
            for w in b.writers:
                if op.is_dma and w.is_dma and w.sem is op.sem:
                    keep.append(w)
                else:
                    deps.append(w)
            for r in b.readers:
                deps.append(r)
            b.writers = keep + [op]
            b.readers = []
        for b in reads:
            b.readers.append(op)
        seen = set()
        for d in deps:
            if (not d.is_dma) and d.eng == "pe" and op.eng == "pe":
                continue
            if id(d) not in seen and d is not op:
                seen.add(id(d))
                d.used = True
                op.deps.append(d)
        self.ops.append(op)

    def op(self, eng, emit, reads=(), writes=()):
        o = Op(eng, emit, False)
        self._record(o, reads, writes)
        return o

    def dma(self, eng, out, in_, sbuf, reads=(), writes=(), **kw):
        o = Op(eng, None, True)
        o.sem = self._sem_for(sbuf)
        o.sem[1] += 16
        o.semval = o.sem[1]
        o.emit = lambda e: e.dma_start(out=out, in_=in_, **kw)
        self._record(o, reads, writes)
        return o

    def run(self):
        ctx = self.ctx
        nc = self.nc
        for e in COMPUTE:
            for o in self.ops:
                if o.eng == e and not o.is_dma and o.used:
                    ctx.eng_cnt[e] += 1
                    o.sig = ctx.eng_cnt[e]
        engs = {"pe": [], "act": [], "dve": [], "pool": [], "sp": []}
        for o in self.ops:
            engs[o.eng].append(o)
        final_waits = [(s[0], s[1]) for s in self.sems]

        def emit_engine(e, name):
            waited = {}
            for o in engs[name]:
                need = {}
                for d in o.deps:
                    if d.is_dma:
                        key, val = d.sem[0], d.semval
                    else:
                        if d.eng == name and name == "pe":
                            continue
                        key, val = ctx.eng_sem[d.eng], d.sig
                    if need.get(key, 0) < val:
                        need[key] = val
                for key, val in need.items():
                    if waited.get(key, 0) < val:
                        e.wait_ge(key, val)
                        waited[key] = val
                ins = o.emit(e)
                if o.is_dma:
                    ins.then_inc(o.sem[0], 16)
                elif o.used:
                    ins.then_inc(ctx.eng_sem[name], 1)
            if name == "sp":
                for s, v in final_waits:
                    e.wait_ge(s, v)

        with nc.Block() as blk:
            blk.tensor(lambda e: emit_engine(e, "pe"))
            blk.scalar(lambda e: emit_engine(e, "act"))
            blk.vector(lambda e: emit_engine(e, "dve"))
            blk.gpsimd(lambda e: emit_engine(e, "pool"))
            blk.sync(lambda e: emit_engine(e, "sp"))
        for s in self.sems:
            ctx.pool.append(s)
        self.stack.close()


def bcast_rows(ap2d, nrows):
    return bass.AP(ap2d.tensor, ap2d.offset, [[0, nrows], [1, ap2d.shape[-1]]])


def phase_mod(ctx, c, ada_w, ada_b, modbc):
    nc = ctx.nc
    ph = Phase(ctx, "mod")
    cT, cT_b = ph.sb("cT", [128, 8], F32)
    cs, cs_b = ph.sb("cs", [128, 8], F32)
    crep, crep_b = ph.sb("crep", [128, 8, 128], F32)
    wts = [ph.sb("w%d" % i, [128, 8, 512], F32) for i in range(2)]
    bia, bia_b = ph.sb("bias", [128, 6144], F32)
    outs = [ph.sb("o%d" % i, [128, 512], F32) for i in range(2)]
    pss = [ph.ps("ps%d" % i) for i in range(2)]
    ph.dma("sp", cT[:], c.rearrange("o (k p) -> p (o k)", p=128), cT_b, writes=[cT_b],
           allow_slow_non_contiguous=True)
    ph.op("act", lambda e: e.activation(out=cs[:], in_=cT[:], func=AF.Silu), [cT_b], [cs_b])
    ph.op("dve", lambda e: e.tensor_copy(out=crep[:], in_=cs[:].unsqueeze(2).broadcast_to([128, 8, 128])),
          [cs_b], [crep_b])
    n = 0
    for l in range(DEPTH):
        ph.dma("sp", bia[:], bcast_rows(ada_b[l:l + 1, :], 128), bia_b, writes=[bia_b])
        for seg in (1, 4):
            ph.op("pool", lambda e, seg=seg: e.tensor_scalar_add(out=bia[:, seg * 1024:(seg + 1) * 1024],
                                                                 in0=bia[:, seg * 1024:(seg + 1) * 1024], scalar1=1.0),
                  [bia_b], [bia_b])
        for j in range(12):
            w, w_b = wts[n % 2]
            o, o_b = outs[n % 2]
            p, p_b = pss[n % 2]
            n += 1
            ph.dma("sp", w[:], ada_w[l, :, j * 512:(j + 1) * 512].rearrange("(k p) n -> p k n", p=128), w_b,
                   writes=[w_b])
            for k in range(8):
                ph.op("pe", lambda e, k=k, w=w, p=p: e.matmul(p[:], crep[:, k, :], w[:, k, :], start=(k == 0),
                                                               stop=(k == 7)),
                      [crep_b, w_b], [p_b])
            ph.op("dve", lambda e, o=o, p=p, j=j: e.tensor_tensor(out=o[:], in0=p[:], in1=bia[:, j * 512:(j + 1) * 512],
                                                                   op=ALU.add), [p_b, bia_b], [o_b])
            ph.dma("sp", modbc[l, :, j * 512:(j + 1) * 512], o[:], o_b, reads=[o_b])
    ph.run()


def emit_norm_tiles(ph, xsrc, hT, hT_b, G, S, GS_b, ident, ident_b, ntiles, h32T=None, h32T_b=None):
    xs = [ph.sb("x%d" % i, [128, D], F32) for i in range(2)]
    sq, sq_b = ph.sb("sq", [128, D], BF16)
    hs = [ph.sb("h%d" % i, [128, D], BF16) for i in range(2)]
    st = [ph.sb("st%d" % i, [128, 4], F32) for i in range(2)]
    pts = [ph.ps("pt%d" % i, [128, 8, 128], BF16) for i in range(2)]
    for t in range(ntiles):
        x, x_b = xs[t % 2]
        h, h_b = hs[t % 2]
        s, s_b = st[t % 2]
        pt, pt_b = pts[t % 2]
        ph.dma("sp", x[:], xsrc[t * 128:(t + 1) * 128, :], x_b, writes=[x_b])
        ph.op("act", lambda e, x=x, s=s: e.activation(out=sq[:], in_=x[:], func=AF.Square, accum_out=s[:, 0:1]),
              [x_b], [sq_b, s_b])
        ph.op("dve", lambda e, s=s: e.tensor_scalar(out=s[:, 1:2], in0=s[:, 0:1], scalar1=1.0 / D, scalar2=EPS,
                                                    op0=ALU.mult, op1=ALU.add), [s_b], [s_b])
        ph.op("act", lambda e, s=s: e.activation(out=s[:, 2:3], in_=s[:, 1:2], func=AF.Sqrt), [s_b], [s_b])
        ph.op("dve", lambda e, s=s: e.reciprocal(out=s[:, 3:4], in_=s[:, 2:3]), [s_b], [s_b])
        ph.op("dve", lambda e, x=x, s=s: e.scalar_tensor_tensor(out=x[:], in0=x[:], scalar=s[:, 3:4], in1=G[:],
                                                                op0=ALU.mult, op1=ALU.mult), [x_b, s_b, GS_b], [x_b])
        if S is not None:
            ph.op("pool", lambda e, x=x, h=h: e.tensor_tensor(out=h[:], in0=x[:], in1=S[:], op=ALU.add),
                  [x_b, GS_b], [h_b])
        else:
            ph.op("pool", lambda e, x=x, h=h: e.tensor_copy(out=h[:], in_=x[:]), [x_b], [h_b])
        for k in range(8):
            ph.op("pe", lambda e, k=k, h=h, pt=pt: e.transpose(pt[:, k, :], h[:, k * 128:(k + 1) * 128], ident[:]),
                  [h_b, ident_b], [pt_b])
        ph.op("act", lambda e, pt=pt, t=t: e.copy(out=hT[:, :, t * 128:(t + 1) * 128], in_=pt[:]), [pt_b], [hT_b])


def make_ident(ph, dtype=BF16):
    nc = ph.nc
    idf, idf_b = ph.sb("identf", [128, 128], F32)
    ident, ident_b = ph.sb("ident", [128, 128], dtype)
    ph.op("pool", lambda e: e.memset(idf[:], 1.0), [], [idf_b])
    ph.op("pool", lambda e: e.affine_select(out=idf[:], in_=idf[:], pattern=[[-1, 128]], compare_op=ALU.is_equal,
                                            fill=0.0, base=0, channel_multiplier=1), [idf_b], [idf_b])
    ph.op("pool", lambda e: e.tensor_copy(out=ident[:], in_=idf[:]), [idf_b], [ident_b])
    return ident, ident_b


def phase_proj(ctx, xres, modbc, l, norm_g, w_in_l, jobs, seg_scale, seg_shift):
    nc = ctx.nc
    ph = Phase(ctx, "proj")
    ident, ident_b = make_ident(ph)
    G, GS_b = ph.sb("G", [128, D], F32)
    S, _ = ph.sb("S", [128, D], F32)
    ng, _ = ph.sb("ng", [128, D], F32)
    hT, hT_b = ph.sb("hT", [128, 8, T], BF16)
    ph.dma("sp", G[:], modbc[l, :, seg_scale * D:(seg_scale + 1) * D], GS_b, writes=[GS_b])
    ph.dma("sp", S[:], modbc[l, :, seg_shift * D:(seg_shift + 1) * D], GS_b, writes=[GS_b])
    ph.dma("sp", ng[:], bcast_rows(norm_g[l:l + 1, :], 128), GS_b, writes=[GS_b])
    ph.op("dve", lambda e: e.tensor_tensor(out=G[:], in0=G[:], in1=ng[:], op=ALU.mult), [GS_b], [GS_b])
    emit_norm_tiles(ph, xres, hT, hT_b, G, S, GS_b, ident, ident_b, NT)

    wts = [ph.sb("w%d" % i, [128, 8, 512], BF16) for i in range(3)]
    pss = [ph.ps("pp%d" % i) for i in range(4)]
    stg = [ph.sb("sg%d" % i, [128, 512], F32) for i in range(4)]
    stgb = [ph.sb("sgb%d" % i, [128, 512], BF16) for i in range(4)]
    nw = 0
    ne = 0
    for (c0, ncols, kind, dst_fn, dtype, func) in jobs:
        step = 512 if kind == "tok" else 128
        for cc in range(c0, c0 + ncols, step):
            n = min(step, c0 + ncols - cc)
            w, w_b = wts[nw % 3]
            nw += 1
            ph.dma("pool", w[:, :, 0:n], w_in_l[:, cc:cc + n].rearrange("(k p) n -> p k n", p=128), w_b,
                   writes=[w_b])
            nchunk = NT if kind == "tok" else T // 512
            for ci in range(nchunk):
                p, p_b = pss[ne % 4]
                if dtype == F32:
                    sg, sg_b = stg[ne % 4]
                else:
                    sg, sg_b = stgb[ne % 4]
                evac_eng = "act" if (func is not None or ne % 2 == 0) else "dve"
                ne += 1
                if kind == "tok":
                    for k in range(8):
                        ph.op("pe", lambda e, k=k, w=w, p=p, ci=ci, n=n: e.matmul(
                            p[:, 0:n], hT[:, k, ci * 128:(ci + 1) * 128], w[:, k, 0:n], start=(k == 0), stop=(k == 7)),
                              [hT_b, w_b], [p_b])
                    po, so = p[:, 0:n], sg[:, 0:n]
                    dst = dst_fn(ci * 128, cc - c0, n)
                else:
                    for k in range(8):
                        ph.op("pe", lambda e, k=k, w=w, p=p, ci=ci, n=n: e.matmul(
                            p[0:n, :], w[:, k, 0:n], hT[:, k, ci * 512:(ci + 1) * 512], start=(k == 0), stop=(k == 7)),
                              [hT_b, w_b], [p_b])
                    po, so = p[0:n, :], sg[0:n, :]
                    dst = dst_fn(cc - c0, n, ci * 512)
                if evac_eng == "act":
                    f = func if func is not None else AF.Copy
                    ph.op("act", lambda e, po=po, so=so, f=f: e.activation(out=so, in_=po, func=f), [p_b], [sg_b])
                else:
                    ph.op("dve", lambda e, po=po, so=so: e.tensor_copy(out=so, in_=po), [p_b], [sg_b])
                ph.dma("sp", dst, so, sg_b, reads=[sg_b])
    ph.run()


INPUT_SPECS = [
    ("x", [T, D]), ("c", [1, D]), ("rel_bias", [32, 8]), ("hgrn_lb_raw", [DEPTH, D]), ("norm1_g", [DEPTH, D]),
    ("norm2_g", [DEPTH, D]), ("ada_w", [DEPTH, D, 6 * D]), ("ada_b", [DEPTH, 6 * D]), ("w_in", [DEPTH, D, N_IN]),
    ("hgrn_norm_g", [DEPTH, 128]), ("w_branch_a", [DEPTH, 1024, D]), ("w_branch_b", [DEPTH, 1024, D]),
    ("w_branch_c", [DEPTH, 2048, D]), ("w_out", [DEPTH, D, D]), ("router_group_w", [DEPTH, D, 4]),
    ("router_group_b", [DEPTH, 4]), ("router_expert_w", [DEPTH, D, 32]), ("router_expert_b", [DEPTH, 32]),
    ("expert_w_gate", [DEPTH, 32, D, 512]), ("expert_w_up", [DEPTH, 32, D, 512]), ("expert_w_down", [DEPTH, 32, 512, D]),
    ("final_norm_g", [1, D]), ("oh_tab", [32, 384]), ("cosT", [T, 256]), ("sinS", [T, 256]), ("cdec", [128, 8]),
]


def build_program(depth=DEPTH, debug=False):
    nc = bass.Bass("TRN2", target_bir_lowering=False)
    I = {}
    for name, shape in INPUT_SPECS:
        I[name] = nc.dram_tensor(name, shape, F32, kind="ExternalInput").ap()
    y = nc.dram_tensor("y", [T, D], F32, kind="ExternalOutput").ap()

    def scr(name, shape, dt):
        return nc.dram_tensor(name, shape, dt, kind="ExternalOutput" if debug else "Internal").ap()

    xres = scr("xres", [T, D], F32)
    modbc = scr("modbc", [DEPTH, 128, 6 * D], F32)
    s_aqT = scr("s_aqT", [1024, T], BF16)
    s_akT = scr("s_akT", [1024, T], BF16)
    s_av = scr("s_av", [T, 1024], BF16)
    s_iqT = scr("s_iqT", [512, T], BF16)
    s_ikT = scr("s_ikT", [64, T], BF16)
    s_iw = scr("s_iw", [T, 8], F32)
    s_bqT = scr("s_bqT", [1024, T], BF16)
    s_bfT = scr("s_bfT", [1024, T], F32)
    s_bi = scr("s_bi", [T, 1024], BF16)
    s_bg = scr("s_bg", [T, 1024], BF16)
    s_cq = scr("s_cq", [T, 1024], F32)
    s_ck = scr("s_ck", [T, 1024], F32)
    s_cv = scr("s_cv", [T, 2048], BF16)
    s_cg = scr("s_cg", [T, 2048], BF16)
    s_gT = scr("s_gT", [3072, T], BF16)
    s_mb = scr("s_mb", [T, T], BF16)
    s_tab = scr("s_tab", [128, 3072], BF16)
    s_oa = scr("s_oa", [T, 1024], BF16)
    s_ob = scr("s_ob", [T, 1024], BF16)
    s_oc = scr("s_oc", [T, 2048], BF16)
    s_hT = scr("s_hT", [128, 8, TH], BF16)
    s_gates = scr("s_gates", [T, 32], F32)

    def tok(dst):
        return lambda t0, c0, n: dst[t0:t0 + 128, c0:c0 + n]

    def feat(dst):
        return lambda c0, n, t0: dst[c0:c0 + n, t0:t0 + 512]

    jobs = [
        (O_AQ, 1024, "feat", feat(s_aqT), BF16, None),
        (O_AK, 1024, "feat", feat(s_akT), BF16, None),
        (O_AV, 1024, "tok", tok(s_av), BF16, None),
        (O_IQ, 512, "feat", feat(s_iqT), BF16, None),
        (O_IK, 64, "feat", feat(s_ikT), BF16, None),
        (O_IW, 8, "tok", tok(s_iw), F32, None),
        (O_BQ, 1024, "feat", feat(s_bqT), BF16, None),
        (O_BF, 1024, "feat", feat(s_bfT), F32, None),
        (O_BI, 1024, "tok", tok(s_bi), BF16, None),
        (O_BG, 1024, "tok", tok(s_bg), BF16, AF.Silu),
        (O_CQ, 1024, "tok", tok(s_cq), F32, None),
        (O_CK, 1024, "tok", tok(s_ck), F32, None),
        (O_CV, 2048, "tok", tok(s_cv), BF16, None),
        (O_CG, 2048, "tok", tok(s_cg), BF16, AF.Silu),
        (O_GA, 3072, "feat", feat(s_gT), BF16, AF.Sigmoid),
    ]
    with contextlib.ExitStack() as stack:
        ctx = Ctx(nc, stack)
        phase_copy(ctx, I["x"], xres, T)
        phase_mod(ctx, I["c"], I["ada_w"], I["ada_b"], modbc)
        phase_bias_tab(ctx, I["rel_bias"], I["oh_tab"], s_tab)
        for l in range(depth):
            phase_proj(ctx, xres, modbc, l, I["norm1_g"], I["w_in"][l], jobs, 1, 0)
            phase_a1(ctx, s_iqT, s_ikT, s_iw, s_mb)
            for hg in range(2):
                phase_a2(ctx, hg, s_aqT, s_akT, s_av, s_mb, s_tab, s_oa)
            phase_b(ctx, l, I["hgrn_lb_raw"], s_bqT, s_bfT, s_bi, s_bg, I["hgrn_norm_g"], s_ob)
            phase_c(ctx, s_cq, s_ck, s_cv, s_cg, I["cosT"], I["sinS"], I["cdec"], s_oc)
            phase_merge(ctx, l, modbc, s_oa, s_ob, s_oc, s_gT, I["w_branch_a"], I["w_branch_b"], I["w_branch_c"],
                        I["w_out"], xres)
            for hh in range(2):
                sg = s_gates[hh * TH:(hh + 1) * TH, :]
                phase_route(ctx, l, hh, xres, modbc, I["norm2_g"], I["router_group_w"], I["router_group_b"],
                            I["router_expert_w"], I["router_expert_b"], s_hT, sg)
                phase_experts(ctx, l, hh, xres, modbc, s_hT, sg, I["expert_w_gate"], I["expert_w_up"], I["expert_w_down"])
        phase_final(ctx, xres, I["final_norm_g"], y)
    return nc


_PROGRAM = None


def kernel(**inputs):
    global _PROGRAM
    if _PROGRAM is None:
        _PROGRAM = build_program()
    nc = _PROGRAM
    f = lambda a: np.ascontiguousarray(np.asarray(a, dtype=np.float32))
    cos, sinS, dec = make_ret_tables()
    shared = {k: f(inputs[k]) for k in ("rel_bias", "hgrn_lb_raw", "norm1_g", "norm2_g", "ada_w", "ada_b", "w_in",
                                        "hgrn_norm_g", "w_branch_a", "w_branch_b", "w_branch_c", "w_out",
                                        "router_group_w", "router_group_b", "router_expert_w", "router_expert_b",
                                        "expert_w_gate", "expert_w_up", "expert_w_down")}
    shared["final_norm_g"] = f(inputs["final_norm_g"]).reshape(1, D)
    shared["oh_tab"] = make_oh_tab()
    shared["cosT"] = cos
    shared["sinS"] = sinS
    shared["cdec"] = dec
    x = f(inputs["x"])
    c = f(inputs["c"])
    in_maps = []
    for core in range(8):
        b = core % 4
        m = dict(shared)
        m["x"] = x[b]
        m["c"] = c[b:b + 1]
        in_maps.append(m)
    res = run_bass_kernel_spmd(nc, in_maps, core_ids=list(range(8)))
    out = np.stack([np.asarray(res.results[b]["y"], dtype=np.float32) for b in range(4)], axis=0)
    return out


TOPK = 256
NBIS = 16
MASKV = -30000.0


def phase_a1(ctx, s_iqT, s_ikT, s_iw, s_mb):
    ph = Phase(ctx, "a1")
    ikT, ikT_b = ph.sb("ikT", [64, T], BF16)
    ph.dma("sp", ikT[:], s_ikT[:, :], ikT_b, writes=[ikT_b])
    pw, pw_b = ph.sb("pw", [128, NBIS], F32)
    for k in range(NBIS):
        ph.op("pool", lambda e, k=k: e.memset(pw[:, k:k + 1], 0.5 ** (k + 1)), [], [pw_b])
    cm, cm_b = ph.sb("cm", [128, 128], F32)
    ph.op("pool", lambda e: e.memset(cm[:], 0.0), [], [cm_b])
    ph.op("pool", lambda e: e.affine_select(out=cm[:], in_=cm[:], pattern=[[-1, 128]], compare_op=ALU.is_ge, fill=-1e30,
                                            base=0, channel_multiplier=1), [cm_b], [cm_b])
    iqs = [ph.sb("iq%d" % i, [64, 8, 128], BF16) for i in range(2)]
    ws = [ph.sb("w%d" % i, [128, 8], F32) for i in range(2)]
    Is = [ph.sb("I%d" % i, [128, T], F32) for i in range(2)]
    tmps = [ph.sb("tmp%d" % i, [128, 512], F32) for i in range(3)]
    junk, junk_b = ph.sb("junk", [128, T], BF16)
    mbs = [ph.sb("mb%d" % i, [128, T], BF16) for i in range(2)]
    sts = [ph.sb("st%d" % i, [128, 8 + NBIS], F32) for i in range(2)]
    pss = [ph.ps("ps%d" % i) for i in range(4)]
    npz = 0
    ntm = 0
    for i in range(NT):
        S = (i + 1) * 128
        iq, iq_b = iqs[i % 2]
        w, w_b = ws[i % 2]
        I, I_b = Is[i % 2]
        mb, mb_b = mbs[i % 2]
        st, st_b = sts[i % 2]
        ph.dma("sp", iq[:], s_iqT[:, i * 128:(i + 1) * 128].rearrange("(h d) t -> d h t", d=64), iq_b, writes=[iq_b])
        ph.dma("sp", w[:], s_iw[i * 128:(i + 1) * 128, :], w_b, writes=[w_b])
        ph.op("pool", lambda e, w=w: e.tensor_scalar(out=w[:], in0=w[:], scalar1=512.0 ** -0.5, scalar2=None,
                                                      op0=ALU.mult), [w_b], [w_b])
        for c0 in range(0, S, 512):
            n = min(512, S - c0)
            for h in range(8):
                p, p_b = pss[npz % 4]
                npz += 1
                ph.op("pe", lambda e, p=p, iq=iq, h=h, c0=c0, n=n: e.matmul(p[:, 0:n], iq[:, h, :], ikT[:, c0:c0 + n],
                                                                             start=True, stop=True),
                      [iq_b, ikT_b], [p_b])
                if h == 0:
                    ph.op("dve", lambda e, p=p, I=I, w=w, c0=c0, n=n: e.tensor_scalar(
                        out=I[:, c0:c0 + n], in0=p[:, 0:n], scalar1=0.0, scalar2=w[:, 0:1], op0=ALU.max, op1=ALU.mult),
                          [p_b, w_b], [I_b])
                else:
                    tm, tm_b = tmps[ntm % 3]
                    ntm += 1
                    ph.op("dve", lambda e, p=p, tm=tm, w=w, h=h, n=n: e.tensor_scalar(
                        out=tm[:, 0:n], in0=p[:, 0:n], scalar1=0.0, scalar2=w[:, h:h + 1], op0=ALU.max, op1=ALU.mult),
                          [p_b, w_b], [tm_b])
                    ph.op("pool", lambda e, tm=tm, I=I, c0=c0, n=n: e.tensor_tensor(
                        out=I[:, c0:c0 + n], in0=I[:, c0:c0 + n], in1=tm[:, 0:n], op=ALU.add), [tm_b, I_b], [I_b])
        ph.op("pool", lambda e, I=I, i=i: e.tensor_tensor(out=I[:, i * 128:(i + 1) * 128], in0=I[:, i * 128:(i + 1) * 128],
                                                           in1=cm[:], op=ALU.add), [I_b, cm_b], [I_b])
        if i < 2:
            ph.op("dve", lambda e, st=st: e.memset(st[:, 0:1], -1e29), [], [st_b])
        else:
            ph.op("dve", lambda e, st=st, I=I, i=i: e.tensor_reduce(out=st[:, 0:1], in_=I[:, 0:i * 128], axis=AX.X,
                                                                   op=ALU.min), [I_b], [st_b])
            ph.op("dve", lambda e, st=st, I=I, S=S: e.tensor_reduce(out=st[:, 1:2], in_=I[:, 0:S], axis=AX.X,
                                                                   op=ALU.max), [I_b], [st_b])
            ph.op("dve", lambda e, st=st: e.tensor_tensor(out=st[:, 2:3], in0=st[:, 1:2], in1=st[:, 0:1],
                                                          op=ALU.subtract), [st_b], [st_b])
            ph.op("dve", lambda e, st=st: e.tensor_scalar(out=st[:, 8:8 + NBIS], in0=pw[:], scalar1=st[:, 2:3],
                                                          scalar2=None, op0=ALU.mult), [st_b, pw_b], [st_b])
            for k in range(NBIS):
                ph.op("dve", lambda e, st=st, k=k: e.tensor_tensor(out=st[:, 3:4], in0=st[:, 0:1], in1=st[:, 8 + k:9 + k],
                                                                   op=ALU.add), [st_b], [st_b])
                ph.op("dve", lambda e, st=st, I=I, S=S: e.tensor_scalar(out=junk[:, 0:S], in0=I[:, 0:S],
                                                                        scalar1=st[:, 3:4], scalar2=None, op0=ALU.is_ge,
                                                                        op1=ALU.add, accum_out=st[:, 4:5]),
                      [I_b, st_b], [junk_b, st_b])
                ph.op("dve", lambda e, st=st, k=k: e.scalar_tensor_tensor(out=st[:, 5:6], in0=st[:, 4:5],
                                                                          scalar=TOPK - 0.5, in1=st[:, 8 + k:9 + k],
                                                                          op0=ALU.is_ge, op1=ALU.mult), [st_b], [st_b])
                ph.op("dve", lambda e, st=st: e.tensor_tensor(out=st[:, 0:1], in0=st[:, 0:1], in1=st[:, 5:6],
                                                              op=ALU.add), [st_b], [st_b])
        ph.op("dve", lambda e, mb=mb, I=I, st=st, S=S: e.tensor_scalar(out=mb[:, 0:S], in0=I[:, 0:S], scalar1=st[:, 0:1],
                                                                       scalar2=MASKV, op0=ALU.is_lt, op1=ALU.mult),
              [I_b, st_b], [mb_b])
        ph.dma("sp", s_mb[i * 128:(i + 1) * 128, 0:S], mb[:, 0:S], mb_b, reads=[mb_b])
    ph.run()


def phase_bias_tab(ctx, rel_bias, oh_tab, s_tab):
    ph = Phase(ctx, "btab")
    rb, rb_b = ph.sb("rb", [32, 8], F32)
    r31, r31_b = ph.sb("r31", [32, 8], F32)
    rep, rep_b = ph.sb("rep", [32, 8, 128], F32)
    oh, oh_b = ph.sb("oh", [32, 384], F32)
    tb, tb_b = ph.sb("tb", [128, 8, 384], BF16)
    pss = [ph.ps("p%d" % i) for i in range(2)]
    ph.dma("sp", rb[:], rel_bias[:, :], rb_b, writes=[rb_b])
    ph.dma("sp", r31[:], bcast_rows(rel_bias[31:32, :], 32), r31_b, writes=[r31_b])
    ph.dma("sp", oh[:], oh_tab[:, :], oh_b, writes=[oh_b])
    ph.op("dve", lambda e: e.tensor_tensor(out=rb[:], in0=rb[:], in1=r31[:], op=ALU.subtract), [rb_b, r31_b], [rb_b])
    ph.op("dve", lambda e: e.tensor_scalar(out=rb[:], in0=rb[:], scalar1=math.sqrt(128.0), scalar2=None, op0=ALU.mult),
          [rb_b], [rb_b])
    ph.op("dve", lambda e: e.tensor_copy(out=rep[:], in_=rb[:].unsqueeze(2).broadcast_to([32, 8, 128])), [rb_b], [rep_b])
    for h in range(8):
        p, p_b = pss[h % 2]
        ph.op("pe", lambda e, p=p, h=h: e.matmul(p[:, 0:384], rep[:, h, :], oh[:], start=True, stop=True),
              [rep_b, oh_b], [p_b])
        ph.op("act", lambda e, p=p, h=h: e.copy(out=tb[:, h, :], in_=p[:, 0:384]), [p_b], [tb_b])
    ph.dma("sp", s_tab[:, :], tb[:].rearrange("p h u -> p (h u)"), tb_b, reads=[tb_b])
    ph.run()


def phase_a2(ctx, hg, s_aqT, s_akT, s_av, s_mb, s_tab, s_oa):
    ph = Phase(ctx, "a2")
    ident, ident_b = make_ident(ph)
    kT, kT_b = ph.sb("kT", [128, 4, T], BF16)
    v, v_b = ph.sb("v", [128, NT, 4, 129], BF16)
    bt, bt_b = ph.sb("bt", [128, 2, 4, 128], BF16)
    r0 = hg * 512
    for h in range(4):
        ph.dma("sp", kT[:, h, :], s_akT[r0 + h * 128:r0 + (h + 1) * 128, :], kT_b, writes=[kT_b])
        ph.dma("sp", v[:, :, h, 0:128], s_av[:, r0 + h * 128:r0 + (h + 1) * 128].rearrange("(j p) d -> p j d", p=128),
               v_b, writes=[v_b])
        for pat in range(2):
            src = bass.AP(s_tab.tensor, s_tab.offset + (hg * 4 + h) * 384 + 255 - 128 * pat, [[3071, 128], [1, 128]])
            ph.dma("sp", bt[:, pat, h, :], src, bt_b, writes=[bt_b])
    ph.op("pool", lambda e: e.memset(v[:, :, :, 128:129], 1.0), [], [v_b])
    qs = [ph.sb("q%d" % i, [128, 4, 128], BF16) for i in range(2)]
    mbs = [ph.sb("mb%d" % i, [128, T], BF16) for i in range(2)]
    pts = [ph.sb("pt%d" % i, [128, 4, 128], BF16) for i in range(3)]
    sts = [ph.ps("st%d" % i) for i in range(2)]
    ops = [ph.ps("o%d" % i) for i in range(4)]
    rcs = [ph.sb("rc%d" % i, [128, 4], F32) for i in range(2)]
    outs = [ph.sb("out%d" % i, [128, 512], BF16) for i in range(2)]
    npt = 0
    for i in range(NT):
        S = (i + 1) * 128
        q, q_b = qs[i % 2]
        mb, mb_b = mbs[i % 2]
        rc, rc_b = rcs[i % 2]
        out, out_b = outs[i % 2]
        ph.dma("sp", q[:], s_aqT[r0:r0 + 512, i * 128:(i + 1) * 128].rearrange("(h d) t -> d h t", d=128), q_b,
               writes=[q_b])
        ph.dma("sp", mb[:, 0:S], s_mb[i * 128:(i + 1) * 128, 0:S], mb_b, writes=[mb_b])
        for j in range(i + 1):
            st, st_b = sts[j % 2]
            pt, pt_b = pts[npt % 3]
            npt += 1
            near = (i - j) <= 1
            for h in range(4):
                ph.op("pe", lambda e, st=st, h=h, j=j, q=q: e.matmul(st[:, h * 128:(h + 1) * 128],
                                                                   kT[:, h, j * 128:(j + 1) * 128], q[:, h, :],
                                                                   start=True, stop=False), [kT_b, q_b], [st_b])
                ph.op("pe", lambda e, st=st, h=h, j=j, mb=mb, near=near: e.matmul(
                    st[:, h * 128:(h + 1) * 128], mb[:, j * 128:(j + 1) * 128], ident[:], start=False, stop=not near),
                      [mb_b, ident_b], [st_b])
                if near:
                    ph.op("pe", lambda e, st=st, h=h, i=i, j=j: e.matmul(st[:, h * 128:(h + 1) * 128], bt[:, i - j, h, :],
                                                                       ident[:], start=False, stop=True),
                          [bt_b, ident_b], [st_b])
            ph.op("act", lambda e, st=st, pt=pt: e.activation(out=pt[:], in_=st[:], func=AF.Exp, scale=128.0 ** -0.5),
                  [st_b], [pt_b])
            for h in range(4):
                o, o_b = ops[h]
                ph.op("pe", lambda e, o=o, pt=pt, h=h, j=j, i=i: e.matmul(o[:, 0:129], pt[:, h, :], v[:, j, h, :],
                                                                        start=(j == 0), stop=(j == i)),
                      [pt_b, v_b], [o_b])
        for h in range(4):
            o, o_b = ops[h]
            ph.op("dve", lambda e, o=o, rc=rc, h=h: e.reciprocal(out=rc[:, h:h + 1], in_=o[:, 128:129]), [o_b], [rc_b])
            ph.op("dve", lambda e, o=o, rc=rc, h=h, out=out: e.tensor_scalar(out=out[:, h * 128:(h + 1) * 128],
                                                                            in0=o[:, 0:128], scalar1=rc[:, h:h + 1],
                                                                            scalar2=None, op0=ALU.mult),
                  [o_b, rc_b], [out_b])
        ph.dma("sp", s_oa[i * 128:(i + 1) * 128, r0:r0 + 512], out[:], out_b, reads=[out_b])
    ph.run()


def phase_b(ctx, l, lb_raw, s_bqT, s_bfT, s_bi, s_bg, hgrn_norm_g, s_ob):
    ph = Phase(ctx, "hg")
    ident, ident_b = make_ident(ph)
    lbr, lbr_b = ph.sb("lbr", [128, DEPTH, 8], F32)
    lb, lb_b = ph.sb("lb", [128, 8], F32)
    oml, oml_b = ph.sb("oml", [128, 8], F32)
    ssum, ssum_b = ph.sb("ssum", [128, 8], F32)
    ph.dma("sp", lbr[:], lb_raw.rearrange("l (h p) -> p l h", p=128), lbr_b, writes=[lbr_b],
           allow_slow_non_contiguous=True)
    ph.op("act", lambda e: e.activation(out=lbr[:], in_=lbr[:], func=AF.Exp), [lbr_b], [lbr_b])
    ph.op("dve", lambda e: e.tensor_tensor(out=ssum[:], in0=lbr[:, 0, :], in1=lbr[:, 1, :], op=ALU.add), [lbr_b], [ssum_b])
    ph.op("dve", lambda e: e.tensor_tensor(out=ssum[:], in0=ssum[:], in1=lbr[:, 2, :], op=ALU.add), [lbr_b, ssum_b], [ssum_b])
    ph.op("dve", lambda e: e.tensor_tensor(out=ssum[:], in0=ssum[:], in1=lbr[:, 3, :], op=ALU.add), [lbr_b, ssum_b], [ssum_b])
    ph.op("dve", lambda e: e.reciprocal(out=ssum[:], in_=ssum[:]), [ssum_b], [ssum_b])
    ph.op("dve", lambda e: e.memset(lb[:], 0.0), [], [lb_b])
    for m in range(1, l + 1):
        ph.op("dve", lambda e, m=m: e.tensor_tensor(out=lb[:], in0=lb[:], in1=lbr[:, m, :], op=ALU.add), [lbr_b, lb_b], [lb_b])
    ph.op("dve", lambda e: e.tensor_tensor(out=lb[:], in0=lb[:], in1=ssum[:], op=ALU.mult), [lb_b, ssum_b], [lb_b])
    ph.op("dve", lambda e: e.tensor_scalar(out=oml[:], in0=lb[:], scalar1=-1.0, scalar2=1.0, op0=ALU.mult, op1=ALU.add),
          [lb_b], [oml_b])
    ng, ng_b = ph.sb("ng", [64, 128], F32)
    ph.dma("sp", ng[:], bcast_rows(hgrn_norm_g[l:l + 1, :], 64), ng_b, writes=[ng_b])
    ones, ones_b = ph.sb("ones", [128, T], BF16)
    ph.op("pool", lambda e: e.memset(ones[:], 1.0), [], [ones_b])
    zer, zer_b = ph.sb("zer", [128, 32], BF16)
    ph.op("pool", lambda e: e.memset(zer[:], 0.0), [], [zer_b])
    m01, m01_b = ph.sb("m01", [64, 64], F32)
    ph.op("pool", lambda e: e.memset(m01[:], 1.0), [], [m01_b])
    ph.op("pool", lambda e: e.affine_select(out=m01[:], in_=m01[:], pattern=[[1, 64]], compare_op=ALU.is_ge, fill=0.0,
                                            base=0, channel_multiplier=-1), [m01_b], [m01_b])
    NC = T // 64
    fT, fT_b = ph.sb("fT", [128, T], F32)
    Bc, Bc_b = ph.sb("Bc", [128, T], F32)
    E, E_b = ph.sb("E", [128, T], F32)
    qT, qT_b = ph.sb("qT", [128, T], BF16)
    qd, qd_b = ph.sb("qd", [128, T], BF16)
    kd, kd_b = ph.sb("kd", [128, T], BF16)
    kdtm, kdtm_b = ph.sb("kdtm", [64, NC, 128], BF16)
    iv, iv_b = ph.sb("iv", [64, NC, 128], BF16)
    oall, oall_b = ph.sb("oall", [64, NC, 128], F32)
    osb, osb_b = ph.sb("osb", [64, NC, 128], BF16)
    sc, sc_b = ph.sb("sc", [128, 4, NC], F32)
    S, S_b = ph.sb("S", [128, 128], F32)
    Sps = [ph.sb("Sp%d" % i, [128, 128], BF16) for i in range(2)]
    tmpu, tmpu_b = ph.sb("tmpu", [128, 128], F32)
    atms = [ph.sb("atm%d" % i, [64, 64], BF16) for i in range(2)]
    rs, rs_b = ph.sb("rs", [64, 2, NC], F32)
    psA = [ph.ps("pA%d" % i) for i in range(2)]
    psO = [ph.ps("pO%d" % i) for i in range(2)]
    psU = [ph.ps("pU%d" % i) for i in range(2)]
    psT = [ph.ps("pT%d" % i, [128, 4, 128], BF16) for i in range(2)]
    for h in range(8):
        r0 = h * 128
        ph.dma("sp", fT[:], s_bfT[r0:r0 + 128, :], fT_b, writes=[fT_b])
        ph.dma("sp", qT[:], s_bqT[r0:r0 + 128, :], qT_b, writes=[qT_b])
        ph.dma("sp", iv[:], s_bi[:, r0:r0 + 128].rearrange("(c p) v -> p c v", p=64), iv_b, writes=[iv_b])
        ph.dma("sp", osb[:], s_bg[:, r0:r0 + 128].rearrange("(c p) v -> p c v", p=64), osb_b, writes=[osb_b])
        ph.op("act", lambda e: e.activation(out=fT[:], in_=fT[:], func=AF.Sigmoid), [fT_b], [fT_b])
        ph.op("dve", lambda e, h=h: e.tensor_scalar(out=fT[:], in0=fT[:], scalar1=oml[:, h:h + 1], scalar2=lb[:, h:h + 1],
                                                    op0=ALU.mult, op1=ALU.add), [fT_b, oml_b, lb_b], [fT_b])
        ph.op("act", lambda e: e.activation(out=Bc[:], in_=fT[:], func=AF.Ln), [fT_b], [Bc_b])
        ph.op("pool", lambda e: e.tensor_scalar(out=fT[:], in0=fT[:], scalar1=-1.0, scalar2=1.0, op0=ALU.mult,
                                                op1=ALU.add), [fT_b, Bc_b], [fT_b])
        ph.op("dve", lambda e: e.tensor_tensor_scan(out=Bc[:], data0=ones[:], data1=Bc[:], initial=0.0, op0=ALU.mult,
                                                    op1=ALU.add), [Bc_b, ones_b], [Bc_b])
        Bc3 = Bc[:].rearrange("p (c s) -> p c s", s=64)
        ph.op("dve", lambda e: e.memset(sc[:, 0, 0:1], 0.0), [], [sc_b])
        ph.op("dve", lambda e, Bc3=Bc3: e.tensor_copy(out=sc[:, 0, 1:NC], in_=Bc3[:, 0:NC - 1, 63]), [Bc_b], [sc_b])
        ph.op("dve", lambda e, Bc3=Bc3: e.tensor_tensor(out=sc[:, 1, :], in0=Bc3[:, :, 63], in1=sc[:, 0, :],
                                                        op=ALU.subtract), [Bc_b, sc_b], [sc_b])
        ph.op("dve", lambda e, Bc3=Bc3: e.tensor_tensor(out=sc[:, 2, :], in0=Bc3[:, :, 31], in1=sc[:, 0, :],
                                                        op=ALU.subtract), [Bc_b, sc_b], [sc_b])
        ph.op("dve", lambda e, Bc3=Bc3: e.tensor_tensor(out=sc[:, 3, :], in0=Bc3[:, :, 63], in1=Bc3[:, :, 31],
                                                        op=ALU.subtract), [Bc_b, sc_b], [sc_b])
        ph.op("act", lambda e: e.activation(out=sc[:, 1:4, :], in_=sc[:, 1:4, :], func=AF.Exp), [sc_b], [sc_b])
        ph.op("dve", lambda e, Bc3=Bc3: e.tensor_copy(out=E[:, 0:NC], in_=Bc3[:, :, 31]), [Bc_b], [E_b])
        ph.op("dve", lambda e, Bc3=Bc3: e.tensor_tensor(out=Bc3, in0=Bc3, in1=E[:, 0:NC].unsqueeze(2).broadcast_to(
            [128, NC, 64]), op=ALU.subtract), [Bc_b, E_b], [Bc_b])
        ph.op("act", lambda e: e.activation(out=E[:], in_=Bc[:], func=AF.Exp), [Bc_b], [E_b])
        ph.op("dve", lambda e: e.tensor_tensor(out=qd[:], in0=qT[:], in1=E[:], op=ALU.mult), [qT_b, E_b], [qd_b])
        ph.op("act", lambda e: e.activation(out=E[:], in_=Bc[:], func=AF.Exp, scale=-1.0), [Bc_b, qd_b], [E_b])
        ph.op("dve", lambda e: e.tensor_tensor(out=kd[:], in0=fT[:], in1=E[:], op=ALU.mult), [fT_b, E_b], [kd_b])
        for c4 in range(NC // 4):
            pT, pT_b = psT[c4 % 2]
            for u in range(4):
                c = c4 * 4 + u
                ph.op("pe", lambda e, pT=pT, u=u, c=c: e.transpose(pT[0:64, u, :], kd[:, c * 64:(c + 1) * 64], ident[:]),
                      [kd_b, ident_b], [pT_b])
            ph.op("act", lambda e, pT=pT, c4=c4: e.copy(out=kdtm[:, c4 * 4:(c4 + 1) * 4, :], in_=pT[0:64, :, :]),
                  [pT_b], [kdtm_b])
        for c in range(NC):
            pA, pA_b = psA[c % 2]
            pO, pO_b = psO[c % 2]
            pU, pU_b = psU[c % 2]
            atm, atm_b = atms[c % 2]
            Sp, Sp_b = Sps[c % 2]
            Spn, Spn_b = Sps[(c + 1) % 2]
            cs = slice(c * 64, (c + 1) * 64)
            c0 = c * 64
            ph.op("pe", lambda e, pA=pA, c0=c0: e.matmul(pA[0:32, 0:64], kd[:, c0:c0 + 32], qd[:, c0:c0 + 64], start=True,
                                                         stop=True), [kd_b, qd_b], [pA_b])
            ph.op("pe", lambda e, pA=pA, c0=c0: e.matmul(pA[32:64, 32:64], kd[:, c0 + 32:c0 + 64], qd[:, c0 + 32:c0 + 64],
                                                         start=True, stop=True), [kd_b, qd_b], [pA_b])
            ph.op("pe", lambda e, pA=pA, c0=c0: e.matmul(pA[32:64, 0:32], zer[:], qd[:, c0:c0 + 32], start=True, stop=True),
                  [zer_b, qd_b], [pA_b])
            ph.op("dve", lambda e, pA=pA, atm=atm: e.tensor_tensor(out=atm[:], in0=pA[0:64, 0:64], in1=m01[:], op=ALU.mult),
                  [pA_b, m01_b], [atm_b])
            ph.op("pe", lambda e, pO=pO, atm=atm, c=c: e.matmul(pO[0:64, 0:128], atm[:], iv[:, c, :], start=True,
                                                               stop=(c == 0)), [atm_b, iv_b], [pO_b])
            if c > 0:
                ph.op("pe", lambda e, pO=pO, cs=cs, Sp=Sp: e.matmul(pO[0:64, 0:128], qd[:, cs], Sp[:], start=False,
                                                                   stop=True), [qd_b, Sp_b], [pO_b])
            ph.op("act", lambda e, pO=pO, c=c: e.copy(out=oall[:, c, :], in_=pO[0:64, 0:128]), [pO_b], [oall_b])
            if c < NC - 1:
                ph.op("pe", lambda e, pU=pU, c=c: e.matmul(pU[:, 0:128], kdtm[:, c, :], iv[:, c, :], start=True, stop=True),
                      [kdtm_b, iv_b], [pU_b])
                if c == 0:
                    ph.op("dve", lambda e, pU=pU, c=c: e.tensor_scalar(out=S[:], in0=pU[:, 0:128], scalar1=sc[:, 3, c:c + 1],
                                                                       scalar2=None, op0=ALU.mult), [pU_b, sc_b], [S_b])
                else:
                    ph.op("dve", lambda e, pU=pU, c=c: e.tensor_scalar(out=tmpu[:], in0=pU[:, 0:128],
                                                                       scalar1=sc[:, 3, c:c + 1], scalar2=None,
                                                                       op0=ALU.mult), [pU_b, sc_b], [tmpu_b])
                    ph.op("dve", lambda e, c=c: e.scalar_tensor_tensor(out=S[:], in0=S[:], scalar=sc[:, 1, c:c + 1],
                                                                       in1=tmpu[:], op0=ALU.mult, op1=ALU.add),
                          [S_b, tmpu_b, sc_b], [S_b])
                ph.op("dve", lambda e, Spn=Spn, c=c: e.tensor_scalar(out=Spn[:], in0=S[:], scalar1=sc[:, 2, c + 1:c + 2],
                                                                     scalar2=None, op0=ALU.mult), [S_b, sc_b], [Spn_b])
        oflat = oall[:].rearrange("p c v -> p (c v)")
        ph.op("act", lambda e: e.activation(out=kdtm[:], in_=oall[:], func=AF.Square), [oall_b], [kdtm_b])
        ph.op("dve", lambda e: e.tensor_reduce(out=rs[:, 0, :], in_=kdtm[:], axis=AX.X, op=ALU.add), [kdtm_b], [rs_b])
        ph.op("dve", lambda e: e.tensor_scalar(out=rs[:, 0, :], in0=rs[:, 0, :], scalar1=1.0 / 128, scalar2=EPS,
                                               op0=ALU.mult, op1=ALU.add), [rs_b], [rs_b])
        ph.op("act", lambda e: e.activation(out=rs[:, 0, :], in_=rs[:, 0, :], func=AF.Sqrt), [rs_b], [rs_b])
        ph.op("dve", lambda e: e.reciprocal(out=rs[:, 1, :], in_=rs[:, 0, :]), [rs_b], [rs_b])
        ph.op("dve", lambda e: e.tensor_tensor(out=oall[:], in0=oall[:], in1=rs[:, 1, :].unsqueeze(2).broadcast_to(
            [64, NC, 128]), op=ALU.mult), [oall_b, rs_b], [oall_b])
        ph.op("pool", lambda e: e.tensor_tensor(out=oall[:], in0=oall[:], in1=ng[:].unsqueeze(1).broadcast_to(
            [64, NC, 128]), op=ALU.mult), [oall_b, ng_b], [oall_b])
        ph.op("dve", lambda e: e.tensor_tensor(out=osb[:], in0=oall[:], in1=osb[:], op=ALU.mult), [oall_b, osb_b], [osb_b])
        ph.dma("sp", s_ob[:, r0:r0 + 128].rearrange("(c p) v -> p c v", p=64), osb[:], osb_b, reads=[osb_b])
    ph.run()


def phase_c(ctx, s_cq, s_ck, s_cv, s_cg, cosT, sinS, cdec, s_oc):
    ph = Phase(ctx, "ret")
    ident, ident_b = make_ident(ph)
    dec, dec_b = ph.sb("dec", [128, 8], F32)
    ph.dma("sp", dec[:], cdec[:, :], dec_b, writes=[dec_b])
    m01, m01_b = ph.sb("m01", [128, 128], F32)
    ph.op("pool", lambda e: e.memset(m01[:], 1.0), [], [m01_b])
    ph.op("pool", lambda e: e.affine_select(out=m01[:], in_=m01[:], pattern=[[1, 128]], compare_op=ALU.is_ge, fill=0.0,
                                            base=0, channel_multiplier=-1), [m01_b], [m01_b])
    qrdT, qrdT_b = ph.sb("qrdT", [128, 2, T], BF16)
    krnT, krnT_b = ph.sb("krnT", [128, 2, T], BF16)
    krn, krn_b = ph.sb("krn", [128, NT, 256], BF16)
    v, v_b = ph.sb("v", [128, NT, 512], BF16)
    ins = [[ph.sb("in%d_%d" % (a, i), [128, 256], F32) for i in range(2)] for a in range(2)]
    cs_ = [ph.sb("cos%d" % i, [128, 256], F32) for i in range(2)]
    sn_ = [ph.sb("sin%d" % i, [128, 256], F32) for i in range(2)]
    t1s = [ph.sb("t1_%d" % i, [128, 256], F32) for i in range(2)]
    t2s = [ph.sb("t2_%d" % i, [128, 256], F32) for i in range(2)]
    qtm = [ph.sb("qtm%d" % i, [128, 256], BF16) for i in range(2)]
    W = [ph.sb("W%d" % i, [128, 512], F32) for i in range(2)]
    Wb = [ph.sb("Wb%d" % i, [128, 512], BF16) for i in range(2)]
    atms = [ph.sb("atm%d" % i, [128, 128], BF16) for i in range(2)]
    gts = [ph.sb("g%d" % i, [128, 512], BF16) for i in range(2)]
    outs = [ph.sb("o%d" % i, [128, 512], BF16) for i in range(2)]
    junk, junk_b = ph.sb("junk", [128, 512], BF16)
    sts = [ph.sb("st%d" % i, [128, 4], F32) for i in range(2)]
    psT = [ph.ps("pT%d" % i, [128, 2, 128], BF16) for i in range(2)]
    psA = [ph.ps("pA%d" % i) for i in range(1)]
    psO = [ph.ps("pO%d" % i) for i in range(2)]
    psU = [ph.ps("pU%d" % i) for i in range(2)]
    for h in range(4):
        gam = 1.0 - 2.0 ** (-5.0 - h)
        g128 = gam ** 128
        ph.dma("sp", v[:], s_cv[:, h * 512:(h + 1) * 512].rearrange("(c p) d -> p c d", p=128), v_b, writes=[v_b])
        for i in range(NT):
            rows = slice(i * 128, (i + 1) * 128)
            co, co_b = cs_[i % 2]
            sn, sn_b = sn_[i % 2]
            ph.dma("sp", co[:], cosT[rows, :], co_b, writes=[co_b])
            ph.dma("sp", sn[:], sinS[rows, :], sn_b, writes=[sn_b])
            for a, src in enumerate((s_cq, s_ck)):
                x, x_b = ins[a][i % 2]
                t1, t1_b = t1s[a]
                t2, t2_b = t2s[a]
                ph.dma("sp", x[:], src[rows, h * 256:(h + 1) * 256], x_b, writes=[x_b])
                x3 = x[:].rearrange("p (i two) -> p i two", two=2)
                s3 = sn[:].rearrange("p (i two) -> p i two", two=2)
                t23 = t2[:].rearrange("p (i two) -> p i two", two=2)
                ph.op("dve", lambda e, t1=t1, x=x, co=co: e.tensor_tensor(out=t1[:], in0=x[:], in1=co[:], op=ALU.mult),
                      [x_b, co_b], [t1_b])
                ph.op("pool", lambda e, t23=t23, x3=x3, s3=s3: e.tensor_tensor(out=t23[:, :, 0], in0=x3[:, :, 1],
                                                                               in1=s3[:, :, 0], op=ALU.mult),
                      [x_b, sn_b], [t2_b])
                ph.op("pool", lambda e, t23=t23, x3=x3, s3=s3: e.tensor_tensor(out=t23[:, :, 1], in0=x3[:, :, 0],
                                                                               in1=s3[:, :, 1], op=ALU.mult),
                      [x_b, sn_b], [t2_b])
                ph.op("dve", lambda e, t1=t1, t2=t2: e.tensor_tensor(out=t1[:], in0=t1[:], in1=t2[:], op=ALU.add),
                      [t1_b, t2_b], [t1_b])
                pT, pT_b = psT[a]
                if a == 0:
                    dst, dst_b = qtm[i % 2]
                    dstap = dst[:]
                else:
                    dst_b = krn_b
                    dstap = krn[:, i, :]
                ph.op("act", lambda e, dstap=dstap, t1=t1, a=a, h=h: e.activation(out=dstap, in_=t1[:], func=AF.Copy,
                                                                                  scale=dec[:, a * 4 + h:a * 4 + h + 1]),
                      [t1_b, dec_b], [dst_b])
                for kc in range(2):
                    ph.op("pe", lambda e, pT=pT, kc=kc, dstap=dstap: e.transpose(pT[:, kc, :],
                                                                                 dstap[:, kc * 128:(kc + 1) * 128], ident[:]),
                          [dst_b, ident_b], [pT_b])
                tgt, tgt_b = (qrdT, qrdT_b) if a == 0 else (krnT, krnT_b)
                ph.op("act", lambda e, tgt=tgt, pT=pT, i=i: e.copy(out=tgt[:, :, i * 128:(i + 1) * 128], in_=pT[:]),
                      [pT_b], [tgt_b])
        for c in range(NT):
            cs = slice(c * 128, (c + 1) * 128)
            pA, pA_b = psA[0]
            pO, pO_b = psO[c % 2]
            atm, atm_b = atms[c % 2]
            g, g_b = gts[c % 2]
            out, out_b = outs[c % 2]
            st, st_b = sts[c % 2]
            ph.dma("sp", g[:], s_cg[cs, h * 512:(h + 1) * 512], g_b, writes=[g_b])
            for kc in range(2):
                ph.op("pe", lambda e, pA=pA, kc=kc, cs=cs: e.matmul(pA[:, 0:128], krnT[:, kc, cs], qrdT[:, kc, cs],
                                                                   start=(kc == 0), stop=(kc == 1)),
                      [krnT_b, qrdT_b], [pA_b])
            ph.op("dve", lambda e, pA=pA, atm=atm: e.tensor_tensor(out=atm[:], in0=pA[:, 0:128], in1=m01[:], op=ALU.mult),
                  [pA_b, m01_b], [atm_b])
            ph.op("pe", lambda e, pO=pO, atm=atm, c=c: e.matmul(pO[:], atm[:], v[:, c, :], start=True, stop=(c == 0)),
                  [atm_b, v_b], [pO_b])
            if c > 0:
                for kc in range(2):
                    ph.op("pe", lambda e, pO=pO, kc=kc, cs=cs: e.matmul(pO[:], qrdT[:, kc, cs], Wb[kc][0][:], start=False,
                                                                       stop=(kc == 1)), [qrdT_b, Wb[kc][1]], [pO_b])
            if c < NT - 1:
                for kc in range(2):
                    pU, pU_b = psU[kc]
                    ph.op("pe", lambda e, pU=pU, kc=kc, c=c: e.matmul(pU[:], krn[:, c, kc * 128:(kc + 1) * 128], v[:, c, :],
                                                                     start=True, stop=True), [krn_b, v_b], [pU_b])
                    Wk, Wk_b = W[kc]
                    if c == 0:
                        ph.op("dve", lambda e, Wk=Wk, pU=pU: e.tensor_copy(out=Wk[:], in_=pU[:]), [pU_b], [Wk_b])
                    else:
                        ph.op("dve", lambda e, Wk=Wk, pU=pU, g128=g128: e.scalar_tensor_tensor(
                            out=Wk[:], in0=Wk[:], scalar=g128, in1=pU[:], op0=ALU.mult, op1=ALU.add), [Wk_b, pU_b], [Wk_b])
                    ph.op("act", lambda e, Wk=Wk, kc=kc, g128=g128: e.activation(out=Wb[kc][0][:], in_=Wk[:], func=AF.Copy,
                                                                                 scale=g128), [Wk_b], [Wb[kc][1]])
            ph.op("act", lambda e, pO=pO, st=st: e.activation(out=junk[:], in_=pO[:], func=AF.Square,
                                                             accum_out=st[:, 0:1]), [pO_b], [junk_b, st_b])
            ph.op("dve", lambda e, st=st: e.tensor_scalar(out=st[:, 1:2], in0=st[:, 0:1], scalar1=1.0 / 512, scalar2=EPS,
                                                          op0=ALU.mult, op1=ALU.add), [st_b], [st_b])
            ph.op("act", lambda e, st=st: e.activation(out=st[:, 2:3], in_=st[:, 1:2], func=AF.Sqrt), [st_b], [st_b])
            ph.op("dve", lambda e, st=st: e.reciprocal(out=st[:, 3:4], in_=st[:, 2:3]), [st_b], [st_b])
            ph.op("dve", lambda e, pO=pO, st=st, g=g, out=out: e.scalar_tensor_tensor(
                out=out[:], in0=pO[:], scalar=st[:, 3:4], in1=g[:], op0=ALU.mult, op1=ALU.mult), [pO_b, st_b, g_b], [out_b])
            ph.dma("sp", s_oc[cs, h * 512:(h + 1) * 512], out[:], out_b, reads=[out_b])
    ph.run()


def phase_merge(ctx, l, modbc, s_oa, s_ob, s_oc, s_gT, w_a, w_b, w_c, w_out, xres):
    ph = Phase(ctx, "mrg")
    ident, ident_b = make_ident(ph)
    Wbr, Wbr_b = ph.sb("Wbr", [128, 32, D], BF16)
    wo, wo_b = ph.sb("wo", [128, 8, D], BF16)
    gt, gt_b = ph.sb("gt", [128, D], F32)
    for kc in range(8):
        ph.dma("pool", Wbr[:, kc, :], w_a[l, kc * 128:(kc + 1) * 128, :], Wbr_b, writes=[Wbr_b])
        ph.dma("pool", Wbr[:, 8 + kc, :], w_b[l, kc * 128:(kc + 1) * 128, :], Wbr_b, writes=[Wbr_b])
        ph.dma("pool", wo[:, kc, :], w_out[l, kc * 128:(kc + 1) * 128, :], wo_b, writes=[wo_b])
    for kc in range(16):
        ph.dma("pool", Wbr[:, 16 + kc, :], w_c[l, kc * 128:(kc + 1) * 128, :], Wbr_b, writes=[Wbr_b])
    ph.dma("sp", gt[:], modbc[l, :, 2 * D:3 * D], gt_b, writes=[gt_b])
    oin = [ph.sb("oin%d" % i, [128, 4096], BF16) for i in range(2)]
    oT, oT_b = ph.sb("oT", [128, 32, 512], BF16)
    gs = [ph.sb("gs%d" % i, [128, 3, 512], BF16) for i in range(2)]
    tt1 = [ph.sb("tt%d" % i, [128, 512], F32) for i in range(3)]
    mT, mT_b = ph.sb("mT", [128, 8, 512], BF16)
    xs = [ph.sb("x%d" % i, [128, D], F32) for i in range(2)]
    ty = [ph.sb("ty%d" % i, [128, 512], F32) for i in range(2)]
    psT = [ph.ps("pT%d" % i, [128, 8, 128], BF16) for i in range(2)]
    psB = [ph.ps("pB%d" % i) for i in range(3)]
    psY = [ph.ps("pY%d" % i) for i in range(2)]
    nT = 0
    ny = 0
    ng = 0
    for g in range(T // 512):
        for tt in range(4):
            rows = slice(g * 512 + tt * 128, g * 512 + (tt + 1) * 128)
            o, o_b = oin[tt % 2]
            ph.dma("sp", o[:, 0:1024], s_oa[rows, :], o_b, writes=[o_b])
            ph.dma("sp", o[:, 1024:2048], s_ob[rows, :], o_b, writes=[o_b])
            ph.dma("sp", o[:, 2048:4096], s_oc[rows, :], o_b, writes=[o_b])
            for k8 in range(4):
                pT, pT_b = psT[nT % 2]
                nT += 1
                for u in range(8):
                    kc = k8 * 8 + u
                    ph.op("pe", lambda e, pT=pT, u=u, o=o, kc=kc: e.transpose(pT[:, u, :], o[:, kc * 128:(kc + 1) * 128],
                                                                           ident[:]), [o_b, ident_b], [pT_b])
                ph.op("act", lambda e, pT=pT, k8=k8, tt=tt: e.copy(out=oT[:, k8 * 8:(k8 + 1) * 8, tt * 128:(tt + 1) * 128],
                                                                 in_=pT[:]), [pT_b], [oT_b])
        for dc in range(8):
            gsb, gsb_b = gs[ng % 2]
            ng += 1
            ph.dma("sp", gsb[:], s_gT.rearrange("(b r) t -> r b t", b=3)[dc * 128:(dc + 1) * 128, :, g * 512:(g + 1) * 512],
                   gsb_b, writes=[gsb_b])
            for br, (k0, k1) in enumerate(((0, 8), (8, 16), (16, 32))):
                pB, pB_b = psB[br]
                for kc in range(k0, k1):
                    ph.op("pe", lambda e, pB=pB, kc=kc, dc=dc, k0=k0, k1=k1: e.matmul(
                        pB[:], Wbr[:, kc, dc * 128:(dc + 1) * 128], oT[:, kc, :], start=(kc == k0), stop=(kc == k1 - 1)),
                          [Wbr_b, oT_b], [pB_b])
                t, t_b = tt1[br]
                ph.op("dve", lambda e, t=t, pB=pB, gsb=gsb, br=br: e.tensor_tensor(out=t[:], in0=pB[:], in1=gsb[:, br, :],
                                                                                  op=ALU.mult), [pB_b, gsb_b], [t_b])
            ph.op("pool", lambda e: e.tensor_tensor(out=tt1[0][0][:], in0=tt1[0][0][:], in1=tt1[1][0][:], op=ALU.add),
                  [tt1[0][1], tt1[1][1]], [tt1[0][1]])
            ph.op("pool", lambda e, dc=dc: e.tensor_tensor(out=mT[:, dc, :], in0=tt1[0][0][:], in1=tt1[2][0][:], op=ALU.add),
                  [tt1[0][1], tt1[2][1]], [mT_b])
        for tt in range(4):
            rows = slice(g * 512 + tt * 128, g * 512 + (tt + 1) * 128)
            x, x_b = xs[tt % 2]
            ph.dma("sp", x[:], xres[rows, :], x_b, writes=[x_b])
            for dh in range(2):
                pY, pY_b = psY[ny % 2]
                t, t_b = ty[ny % 2]
                ny += 1
                for dc in range(8):
                    ph.op("pe", lambda e, pY=pY, dc=dc, tt=tt, dh=dh: e.matmul(
                        pY[:], mT[:, dc, tt * 128:(tt + 1) * 128], wo[:, dc, dh * 512:(dh + 1) * 512], start=(dc == 0),
                        stop=(dc == 7)), [mT_b, wo_b], [pY_b])
                ph.op("dve", lambda e, t=t, pY=pY, dh=dh: e.tensor_tensor(out=t[:], in0=pY[:],
                                                                          in1=gt[:, dh * 512:(dh + 1) * 512], op=ALU.mult),
                      [pY_b, gt_b], [t_b])
                ph.op("pool", lambda e, t=t, x=x, dh=dh: e.tensor_tensor(out=x[:, dh * 512:(dh + 1) * 512],
                                                                         in0=x[:, dh * 512:(dh + 1) * 512], in1=t[:],
                                                                         op=ALU.add), [t_b, x_b], [x_b])
            ph.dma("sp", xres[rows, :], x[:], x_b, reads=[x_b])
    ph.run()


TH = 2048
NTH = TH // 128


def phase_route(ctx, l, hh, xres, modbc, norm_g, rg_w, rg_b, re_w, re_b, s_hT, s_gates):
    ph = Phase(ctx, "rt")
    identb, identb_b = make_ident(ph)
    idf, idf_b = ph.sb("identf32", [128, 128], F32)
    ph.op("pool", lambda e: e.memset(idf[:], 1.0), [], [idf_b])
    ph.op("pool", lambda e: e.affine_select(out=idf[:], in_=idf[:], pattern=[[-1, 128]], compare_op=ALU.is_equal,
                                            fill=0.0, base=0, channel_multiplier=1), [idf_b], [idf_b])
    G, GS_b = ph.sb("G", [128, D], F32)
    S, _ = ph.sb("S", [128, D], F32)
    ng, _ = ph.sb("ng", [128, D], F32)
    ph.dma("sp", G[:], modbc[l, :, 4 * D:5 * D], GS_b, writes=[GS_b])
    ph.dma("sp", S[:], modbc[l, :, 3 * D:4 * D], GS_b, writes=[GS_b])
    ph.dma("sp", ng[:], bcast_rows(norm_g[l:l + 1, :], 128), GS_b, writes=[GS_b])
    ph.op("dve", lambda e: e.tensor_tensor(out=G[:], in0=G[:], in1=ng[:], op=ALU.mult), [GS_b], [GS_b])
    rw, rw_b = ph.sb("rw", [128, 8, 36], F32)
    rb, rb_b = ph.sb("rb", [128, 36], F32)
    ph.dma("sp", rw[:, :, 0:4], rg_w[l].rearrange("(k p) n -> p k n", p=128), rw_b, writes=[rw_b])
    ph.dma("sp", rw[:, :, 4:36], re_w[l].rearrange("(k p) n -> p k n", p=128), rw_b, writes=[rw_b])
    ph.dma("sp", rb[:, 0:4], bcast_rows(rg_b[l:l + 1, :], 128), rb_b, writes=[rb_b])
    ph.dma("sp", rb[:, 4:36], bcast_rows(re_b[l:l + 1, :], 128), rb_b, writes=[rb_b])
    hT, hT_b = ph.sb("hT", [128, 8, TH], BF16)
    gates, gates_b = ph.sb("gates", [128, NTH, 32], F32)
    xs = [ph.sb("x%d" % i, [128, D], F32) for i in range(2)]
    sq, sq_b = ph.sb("sq", [128, D], BF16)
    hs = [ph.sb("h%d" % i, [128, D], BF16) for i in range(2)]
    st = [ph.sb("st%d" % i, [128, 4], F32) for i in range(2)]
    h32T = [ph.sb("h32T%d" % i, [128, 8, 128], F32) for i in range(2)]
    lgs = [ph.sb("lg%d" % i, [128, 36], F32) for i in range(2)]
    rts = [ph.sb("r%d" % i, [128, 64], F32) for i in range(2)]
    pts = [ph.ps("pt%d" % i, [128, 8, 128], BF16) for i in range(2)]
    pfs = [ph.ps("pf%d" % i, [128, 4, 128], F32) for i in range(2)]
    pls = [ph.ps("pl%d" % i) for i in range(2)]
    for t in range(NTH):
        x, x_b = xs[t % 2]
        h, h_b = hs[t % 2]
        s, s_b = st[t % 2]
        pt, pt_b = pts[t % 2]
        hf, hf_b = h32T[t % 2]
        pl, pl_b = pls[t % 2]
        lg, lg_b = lgs[t % 2]
        r, r_b = rts[t % 2]
        row0 = hh * TH + t * 128
        ph.dma("sp", x[:], xres[row0:row0 + 128, :], x_b, writes=[x_b])
        ph.op("act", lambda e, x=x, s=s: e.activation(out=sq[:], in_=x[:], func=AF.Square, accum_out=s[:, 0:1]),
              [x_b], [sq_b, s_b])
        ph.op("dve", lambda e, s=s: e.tensor_scalar(out=s[:, 1:2], in0=s[:, 0:1], scalar1=1.0 / D, scalar2=EPS,
                                                    op0=ALU.mult, op1=ALU.add), [s_b], [s_b])
        ph.op("act", lambda e, s=s: e.activation(out=s[:, 2:3], in_=s[:, 1:2], func=AF.Sqrt), [s_b], [s_b])
        ph.op("dve", lambda e, s=s: e.reciprocal(out=s[:, 3:4], in_=s[:, 2:3]), [s_b], [s_b])
        ph.op("dve", lambda e, x=x, s=s: e.scalar_tensor_tensor(out=x[:], in0=x[:], scalar=s[:, 3:4], in1=G[:],
                                                                op0=ALU.mult, op1=ALU.mult), [x_b, s_b, GS_b], [x_b])
        ph.op("pool", lambda e, x=x: e.tensor_tensor(out=x[:], in0=x[:], in1=S[:], op=ALU.add), [x_b, GS_b], [x_b])
        ph.op("pool", lambda e, x=x, h=h: e.tensor_copy(out=h[:], in_=x[:]), [x_b], [h_b])
        for k in range(8):
            ph.op("pe", lambda e, k=k, h=h, pt=pt: e.transpose(pt[:, k, :], h[:, k * 128:(k + 1) * 128], identb[:]),
                  [h_b, identb_b], [pt_b])
        ph.op("act", lambda e, pt=pt, t=t: e.copy(out=hT[:, :, t * 128:(t + 1) * 128], in_=pt[:]), [pt_b], [hT_b])
        for half in range(2):
            pf, pf_b = pfs[half]
            for u in range(4):
                k = half * 4 + u
                ph.op("pe", lambda e, pf=pf, u=u, k=k, x=x: e.transpose(pf[:, u, :], x[:, k * 128:(k + 1) * 128], idf[:]),
                      [x_b, idf_b], [pf_b])
            ph.op("dve", lambda e, pf=pf, hf=hf, half=half: e.tensor_copy(out=hf[:, half * 4:(half + 1) * 4, :], in_=pf[:]),
                  [pf_b], [hf_b])
        for k in range(8):
            ph.op("pe", lambda e, pl=pl, hf=hf, k=k: e.matmul(pl[:, 0:36], hf[:, k, :], rw[:, k, :], start=(k == 0),
                                                             stop=(k == 7)), [hf_b, rw_b], [pl_b])
        ph.op("dve", lambda e, lg=lg, pl=pl: e.tensor_tensor(out=lg[:], in0=pl[:, 0:36], in1=rb[:], op=ALU.add),
              [pl_b, rb_b], [lg_b])
        def dv(fn, r=r, lg=lg):
            ph.op("dve", fn, [r_b, lg_b], [r_b])
        dv(lambda e, r=r, lg=lg: e.tensor_reduce(out=r[:, 0:1], in_=lg[:, 0:4], axis=AX.X, op=ALU.max))
        dv(lambda e, r=r, lg=lg: e.tensor_scalar(out=r[:, 16:20], in0=lg[:, 0:4], scalar1=r[:, 0:1], scalar2=None,
                                                 op0=ALU.is_equal))
        dv(lambda e, r=r: e.tensor_scalar(out=r[:, 1:2], in0=r[:, 0:1], scalar1=-1.0, scalar2=None, op0=ALU.mult))
        ph.op("act", lambda e, r=r, lg=lg: e.activation(out=r[:, 20:24], in_=lg[:, 0:4], func=AF.Exp, bias=r[:, 1:2]),
              [r_b, lg_b], [r_b])
        dv(lambda e, r=r: e.tensor_reduce(out=r[:, 2:3], in_=r[:, 20:24], axis=AX.X, op=ALU.add))
        dv(lambda e, r=r: e.reciprocal(out=r[:, 3:4], in_=r[:, 2:3]))
        dv(lambda e, r=r, lg=lg: e.tensor_scalar(out=r[:, 24:32], in0=lg[:, 4:12], scalar1=r[:, 16:17], scalar2=None,
                                                 op0=ALU.mult))
        for g in range(1, 4):
            dv(lambda e, r=r, lg=lg, g=g: e.scalar_tensor_tensor(out=r[:, 24:32], in0=lg[:, 4 + 8 * g:12 + 8 * g],
                                                                 scalar=r[:, 16 + g:17 + g], in1=r[:, 24:32],
                                                                 op0=ALU.mult, op1=ALU.add))
        dv(lambda e, r=r: e.tensor_reduce(out=r[:, 4:5], in_=r[:, 24:32], axis=AX.X, op=ALU.max))
        dv(lambda e, r=r: e.tensor_scalar(out=r[:, 32:40], in0=r[:, 24:32], scalar1=r[:, 4:5], scalar2=None,
                                          op0=ALU.is_equal))
        dv(lambda e, r=r: e.scalar_tensor_tensor(out=r[:, 40:48], in0=r[:, 32:40], scalar=-1e30, in1=r[:, 24:32],
                                                 op0=ALU.mult, op1=ALU.add))
        dv(lambda e, r=r: e.tensor_reduce(out=r[:, 5:6], in_=r[:, 40:48], axis=AX.X, op=ALU.max))
        dv(lambda e, r=r: e.tensor_scalar(out=r[:, 48:56], in0=r[:, 40:48], scalar1=r[:, 5:6], scalar2=None,
                                          op0=ALU.is_equal))
        dv(lambda e, r=r: e.tensor_tensor(out=r[:, 6:7], in0=r[:, 5:6], in1=r[:, 4:5], op=ALU.subtract))
        ph.op("act", lambda e, r=r: e.activation(out=r[:, 7:8], in_=r[:, 6:7], func=AF.Exp), [r_b], [r_b])
        dv(lambda e, r=r: e.tensor_scalar(out=r[:, 8:9], in0=r[:, 7:8], scalar1=1.0, scalar2=None, op0=ALU.add))
        dv(lambda e, r=r: e.reciprocal(out=r[:, 9:10], in_=r[:, 8:9]))
        dv(lambda e, r=r: e.tensor_tensor(out=r[:, 10:11], in0=r[:, 9:10], in1=r[:, 3:4], op=ALU.mult))
        dv(lambda e, r=r: e.tensor_tensor(out=r[:, 11:12], in0=r[:, 10:11], in1=r[:, 7:8], op=ALU.mult))
        dv(lambda e, r=r: e.tensor_scalar(out=r[:, 56:64], in0=r[:, 32:40], scalar1=r[:, 10:11], scalar2=None,
                                          op0=ALU.mult))
        dv(lambda e, r=r: e.scalar_tensor_tensor(out=r[:, 56:64], in0=r[:, 48:56], scalar=r[:, 11:12], in1=r[:, 56:64],
                                                 op0=ALU.mult, op1=ALU.add))
        for g in range(4):
            ph.op("dve", lambda e, r=r, g=g, t=t: e.tensor_scalar(out=gates[:, t, g * 8:(g + 1) * 8], in0=r[:, 56:64],
                                                                  scalar1=r[:, 16 + g:17 + g], scalar2=None, op0=ALU.mult),
                  [r_b], [gates_b])
    for k in range(8):
        ph.dma("sp", s_hT[:, k, :], hT[:, k, :], hT_b, reads=[hT_b])
    ph.dma("sp", s_gates.rearrange("(t p) e -> p t e", p=128), gates[:], gates_b, reads=[gates_b])
    ph.run()


def phase_experts(ctx, l, hh, xres, modbc, s_hT, s_gates, e_gate, e_up, e_down, n_exp=32):
    ph = Phase(ctx, "ex")
    hT, hT_b = ph.sb("hT", [128, 8, TH], BF16)
    gates, gates_b = ph.sb("gates", [128, NTH, 32], F32)
    gt, gt_b = ph.sb("gt", [128, D], F32)
    yacc, yacc_b = ph.sb("yacc", [128, NTH, D], F32)
    for k in range(8):
        ph.dma("sp", hT[:, k, :], s_hT[:, k, :], hT_b, writes=[hT_b])
    ph.dma("sp", gates[:], s_gates.rearrange("(t p) e -> p t e", p=128), gates_b, writes=[gates_b])
    ph.dma("sp", gt[:], modbc[l, :, 5 * D:6 * D], gt_b, writes=[gt_b])
    wg = [ph.sb("wg%d" % i, [128, 8, 512], BF16) for i in range(2)]
    wu = [ph.sb("wu%d" % i, [128, 8, 512], BF16) for i in range(2)]
    wd = [ph.sb("wd%d" % i, [128, 4, D], BF16) for i in range(2)]
    sgs = [ph.sb("sg%d" % i, [128, 512], F32) for i in range(2)]
    hgs = [ph.sb("hg%d" % i, [128, 4, 512], BF16) for i in range(2)]
    psG = [ph.ps("pG%d" % i) for i in range(2)]
    psU = [ph.ps("pU%d" % i) for i in range(2)]
    psY = [ph.ps("pY%d" % i) for i in range(4)]
    nf = 0
    ny = 0
    nh = 0
    for ex in range(n_exp):
        g_, g_b = wg[ex % 2]
        u_, u_b = wu[ex % 2]
        d_, d_b = wd[ex % 2]
        ph.dma("pool", g_[:], e_gate[l, ex].rearrange("(k p) f -> p k f", p=128), g_b, writes=[g_b])
        ph.dma("pool", u_[:], e_up[l, ex].rearrange("(k p) f -> p k f", p=128), u_b, writes=[u_b])
        ph.dma("pool", d_[:], e_down[l, ex].rearrange("(k p) f -> p k f", p=128), d_b, writes=[d_b])
        for tg in range(TH // 512):
            ts = slice(tg * 512, (tg + 1) * 512)
            hg, hg_b = hgs[nh % 2]
            nh += 1
            for fc in range(4):
                pG, pG_b = psG[nf % 2]
                pU, pU_b = psU[nf % 2]
                sg, sg_b = sgs[nf % 2]
                nf += 1
                for k in range(8):
                    ph.op("pe", lambda e, pG=pG, g_=g_, k=k, fc=fc, ts=ts: e.matmul(
                        pG[:], g_[:, k, fc * 128:(fc + 1) * 128], hT[:, k, ts], start=(k == 0), stop=(k == 7)),
                          [g_b, hT_b], [pG_b])
                for k in range(8):
                    ph.op("pe", lambda e, pU=pU, u_=u_, k=k, fc=fc, ts=ts: e.matmul(
                        pU[:], u_[:, k, fc * 128:(fc + 1) * 128], hT[:, k, ts], start=(k == 0), stop=(k == 7)),
                          [u_b, hT_b], [pU_b])
                ph.op("act", lambda e, sg=sg, pG=pG: e.activation(out=sg[:], in_=pG[:], func=AF.Silu), [pG_b], [sg_b])
                ph.op("dve", lambda e, hg=hg, fc=fc, sg=sg, pU=pU: e.tensor_tensor(out=hg[:, fc, :], in0=pU[:], in1=sg[:],
                                                                                  op=ALU.mult), [pU_b, sg_b], [hg_b])
            for tt in range(4):
                tile = tg * 4 + tt
                for dh in range(2):
                    pY, pY_b = psY[ny % 4]
                    ny += 1
                    for fc in range(4):
                        ph.op("pe", lambda e, pY=pY, hg=hg, fc=fc, tt=tt, d_=d_, dh=dh: e.matmul(
                            pY[:], hg[:, fc, tt * 128:(tt + 1) * 128], d_[:, fc, dh * 512:(dh + 1) * 512], start=(fc == 0),
                            stop=(fc == 3)), [hg_b, d_b], [pY_b])
                    ya = yacc[:, tile, dh * 512:(dh + 1) * 512]
                    if ex == 0:
                        ph.op("dve", lambda e, ya=ya, pY=pY, tile=tile, ex=ex: e.tensor_scalar(
                            out=ya, in0=pY[:], scalar1=gates[:, tile, ex:ex + 1], scalar2=None, op0=ALU.mult),
                              [pY_b, gates_b], [yacc_b])
                    else:
                        ph.op("dve", lambda e, ya=ya, pY=pY, tile=tile, ex=ex: e.scalar_tensor_tensor(
                            out=ya, in0=pY[:], scalar=gates[:, tile, ex:ex + 1], in1=ya, op0=ALU.mult, op1=ALU.add),
                              [pY_b, gates_b, yacc_b], [yacc_b])
    xs = [ph.sb("x%d" % i, [128, D], F32) for i in range(2)]
    for t in range(NTH):
        x, x_b = xs[t % 2]
        row0 = hh * TH + t * 128
        ph.dma("sp", x[:], xres[row0:row0 + 128, :], x_b, writes=[x_b])
        ph.op("pool", lambda e, t=t: e.tensor_tensor(out=yacc[:, t, :], in0=yacc[:, t, :], in1=gt[:], op=ALU.mult),
              [yacc_b, gt_b], [yacc_b])
        ph.op("pool", lambda e, x=x, t=t: e.tensor_tensor(out=x[:], in0=x[:], in1=yacc[:, t, :], op=ALU.add),
              [x_b, yacc_b], [x_b])
        ph.dma("sp", xres[row0:row0 + 128, :], x[:], x_b, reads=[x_b])
    ph.run()


def phase_final(ctx, xres, final_g, y):
    ph = Phase(ctx, "fin")
    G, G_b = ph.sb("G", [128, D], F32)
    ph.dma("sp", G[:], bcast_rows(final_g, 128), G_b, writes=[G_b])
    xs = [ph.sb("x%d" % i, [128, D], F32) for i in range(3)]
    sq, sq_b = ph.sb("sq", [128, D], BF16)
    st = [ph.sb("st%d" % i, [128, 4], F32) for i in range(2)]
    for t in range(NT):
        x, x_b = xs[t % 3]
        s, s_b = st[t % 2]
        ph.dma("sp", x[:], xres[t * 128:(t + 1) * 128, :], x_b, writes=[x_b])
        ph.op("act", lambda e, x=x, s=s: e.activation(out=sq[:], in_=x[:], func=AF.Square, accum_out=s[:, 0:1]),
              [x_b], [sq_b, s_b])
        ph.op("dve", lambda e, s=s: e.tensor_scalar(out=s[:, 1:2], in0=s[:, 0:1], scalar1=1.0 / D, scalar2=EPS,
                                                    op0=ALU.mult, op1=ALU.add), [s_b], [s_b])
        ph.op("act", lambda e, s=s: e.activation(out=s[:, 2:3], in_=s[:, 1:2], func=AF.Sqrt), [s_b], [s_b])
        ph.op("dve", lambda e, s=s: e.reciprocal(out=s[:, 3:4], in_=s[:, 2:3]), [s_b], [s_b])
        ph.op("dve", lambda e, x=x, s=s: e.scalar_tensor_tensor(out=x[:], in0=x[:], scalar=s[:, 3:4], in1=G[:],
                                                                op0=ALU.mult, op1=ALU.mult), [x_b, s_b, G_b], [x_b])
        ph.dma("sp", y[t * 128:(t + 1) * 128, :], x[:], x_b, reads=[x_b])
    ph.run()


def make_oh_tab():
    u = np.arange(384)
    rel = np.maximum(255 - u, 0)
    relf = np.maximum(rel, 1).astype(np.float32)
    large = 16 + (np.log(relf / np.float32(16)) / np.float32(math.log(8)) * np.float32(16)).astype(np.int32)
    large = np.minimum(large, 31)
    bucket = np.where(rel < 16, rel, large)
    oh = np.zeros((32, 384), np.float32)
    oh[bucket, u] = 1.0
    return oh


def make_ret_tables():
    pos = np.arange(T, dtype=np.float32)
    theta = (np.float32(1.0) / (np.float32(10000.0) ** np.linspace(0.0, 1.0, 128, dtype=np.float32))).astype(np.float32)
    theta = np.repeat(theta, 2)
    ang = (pos[:, None] * theta[None, :]).astype(np.float32)
    cos = np.cos(ang.astype(np.float64)).astype(np.float32)
    sin = np.sin(ang.astype(np.float64)).astype(np.float32)
    sinS = sin.copy()
    sinS[:, 0::2] = -sin[:, 0::2]
    p = np.arange(128, dtype=np.float64)
    dec = np.zeros((128, 8), np.float32)
    for h in range(4):
        gam = 1.0 - 2.0 ** (-5.0 - h)
        dec[:, h] = gam ** (p + 1)
        dec[:, 4 + h] = gam ** (-(p + 1)) / 16.0
    return cos, sinS, dec


def phase_copy(ctx, src, dst, nrows):
    ph = Phase(ctx, "cp")
    b = ph.buf("cpbuf")
    step = 512
    for r in range(0, nrows, step):
        ph.dma("sp", dst[r:r + step, :], src[r:r + step, :], b)
    ph.run()
```

```python
import contextlib
import math
import numpy as np
import concourse.bass as bass
import concourse.mybir as mybir
from concourse.bass_utils import run_bass_kernel_spmd

F32 = mybir.dt.float32
BF16 = mybir.dt.bfloat16
AF = mybir.ActivationFunctionType
ALU = mybir.AluOpType
AX = mybir.AxisListType

D = 1024
T = 4096
DEPTH = 4
NT = T // 128
N_IN = 16968
EPS = 1e-6
O_AQ, O_AK, O_AV = 0, 1024, 2048
O_IQ, O_IK, O_IW = 3072, 3584, 3648
O_BQ, O_BF, O_BI, O_BG = 3656, 4680, 5704, 6728
O_CQ, O_CK, O_CV, O_CG = 7752, 8776, 9800, 11848
O_GA, O_GB, O_GC = 13896, 14920, 15944

COMPUTE = ("pe", "act", "dve", "pool")


class Buf:
    __slots__ = ("name", "writers", "readers", "sem")

    def __init__(self, name):
        self.name = name
        self.writers = []
        self.readers = []
        self.sem = None


class Op:
    __slots__ = ("eng", "emit", "deps", "is_dma", "sem", "semval", "used", "sig")

    def __init__(self, eng, emit, is_dma):
        self.eng = eng
        self.emit = emit
        self.deps = []
        self.is_dma = is_dma
        self.sem = None
        self.semval = 0
        self.used = False
        self.sig = 0


class Ctx:
    def __init__(self, nc, stack):
        self.nc = nc
        self.eng_sem = {}
        self.eng_cnt = {}
        for e in COMPUTE:
            self.eng_sem[e] = stack.enter_context(nc.semaphore("es_" + e))
            self.eng_cnt[e] = 0
        self.pool = []
        for i in range(56):
            self.pool.append([stack.enter_context(nc.semaphore("ds%d" % i)), 0])
        self.nphase = 0


class Phase:
    def __init__(self, ctx, name):
        self.ctx = ctx
        self.nc = ctx.nc
        self.name = "%s_%d" % (name, ctx.nphase)
        ctx.nphase += 1
        self.ops = []
        self.stack = contextlib.ExitStack()
        self.sems = []
        self.nbuf = 0

    def sb(self, name, shape, dtype):
        t = self.stack.enter_context(self.nc.sbuf_tensor("%s_%s" % (self.name, name), list(shape), dtype))
        return t, Buf(name)

    def ps(self, name, shape=(128, 512), dtype=F32):
        t = self.stack.enter_context(self.nc.psum_tensor("%s_%s" % (self.name, name), list(shape), dtype))
        return t, Buf(name)

    def buf(self, name):
        return Buf(name)

    def _sem_for(self, b):
        if b.sem is None:
            b.sem = self.ctx.pool.pop()
            self.sems.append(b.sem)
        return b.sem

    def _record(self, op, reads, writes):
        deps = []
        for b in reads:
            deps.extend(b.writers)
        for b in writes:
            keep = []
            for w in b.writers:
                if op.is_dma and w.is_dma and w.sem is op.sem:
                    keep.append(w)
                else:
                    deps.append(w)
            for r in b.readers:
                deps.append(r)
            b.writers = keep + [op]
            b.readers = []
        for b in reads:
            b.readers.append(op)
        seen = set()
        for d in deps:
            if (not d.is_dma) and d.eng == "pe" and op.eng == "pe":
                continue
            if id(d) not in seen and d is not op:
                seen.add(id(d))
                d.used = True
                op.deps.append(d)
        self.ops.append(op)

    def op(self, eng, emit, reads=(), writes=()):
        o = Op(eng, emit, False)
        self._record(o, reads, writes)
        return o

    def dma(self, eng, out, in_, sbuf, reads=(), writes=(), **kw):
        o = Op(eng, None, True)
        o.sem = self._sem_for(sbuf)
        o.sem[1] += 16
        o.semval = o.sem[1]
        o.emit = lambda e: e.dma_start(out=out, in_=in_, **kw)
        self._record(o, reads, writes)
        return o

    def run(self):
        ctx = self.ctx
        nc = self.nc
        for e in COMPUTE:
            for o in self.ops:
                if o.eng == e and not o.is_dma and o.used:
                    ctx.eng_cnt[e] += 1
                    o.sig = ctx.eng_cnt[e]
        engs = {"pe": [], "act": [], "dve": [], "pool": [], "sp": []}
        for o in self.ops:
            engs[o.eng].append(o)
        final_waits = [(s[0], s[1]) for s in self.sems]

        def emit_engine(e, name):
            waited = {}
            for o in engs[name]:
                need = {}
                for d in o.deps:
                    if d.is_dma:
                        key, val = d.sem[0], d.semval
                    else:
                        if d.eng == name and name == "pe":
                            continue
                        key, val = ctx.eng_sem[d.eng], d.sig
                    if need.get(key, 0) < val:
                        need[key] = val
                for key, val in need.items():
                    if waited.get(key, 0) < val:
                        e.wait_ge(key, val)
                        waited[key] = val
                ins = o.emit(e)
                if o.is_dma:
                    ins.then_inc(o.sem[0], 16)
                elif o.used:
                    ins.then_inc(ctx.eng_sem[name], 1)
            if name == "sp":
                for s, v in final_waits:
                    e.wait_ge(s, v)

        with nc.Block() as blk:
            blk.tensor(lambda e: emit_engine(e, "pe"))
            blk.scalar(lambda e: emit_engine(e, "act"))
            blk.vector(lambda e: emit_engine(e, "dve"))
            blk.gpsimd(lambda e: emit_engine(e, "pool"))
            blk.sync(lambda e: emit_engine(e, "sp"))
        for s in self.sems:
            ctx.pool.append(s)
        self.stack.close()


def bcast_rows(ap2d, nrows):
    return bass.AP(ap2d.tensor, ap2d.offset, [[0, nrows], [1, ap2d.shape[-1]]])


def phase_mod(ctx, c, ada_w, ada_b, modbc):
    nc = ctx.nc
    ph = Phase(ctx, "mod")
    cT, cT_b = ph.sb("cT", [128, 8], F32)
    cs, cs_b = ph.sb("cs", [128, 8], F32)
    crep, crep_b = ph.sb("crep", [128, 8, 128], F32)
    wts = [ph.sb("w%d" % i, [128, 8, 512], F32) for i in range(2)]
    bia, bia_b = ph.sb("bias", [128, 6144], F32)
    outs = [ph.sb("o%d" % i, [128, 512], F32) for i in range(2)]
    pss = [ph.ps("ps%d" % i) for i in range(2)]
    ph.dma("sp", cT[:], c.rearrange("o (k p) -> p (o k)", p=128), cT_b, writes=[cT_b],
           allow_slow_non_contiguous=True)
    ph.op("act", lambda e: e.activation(out=cs[:], in_=cT[:], func=AF.Silu), [cT_b], [cs_b])
    ph.op("dve", lambda e: e.tensor_copy(out=crep[:], in_=cs[:].unsqueeze(2).broadcast_to([128, 8, 128])),
          [cs_b], [crep_b])
    n = 0
    for l in range(DEPTH):
        ph.dma("sp", bia[:], bcast_rows(ada_b[l:l + 1, :], 128), bia_b, writes=[bia_b])
        for seg in (1, 4):
            ph.op("pool", lambda e, seg=seg: e.tensor_scalar_add(out=bia[:, seg * 1024:(seg + 1) * 1024],
                                                                 in0=bia[:, seg * 1024:(seg + 1) * 1024], scalar1=1.0),
                  [bia_b], [bia_b])
        for j in range(12):
            w, w_b = wts[n % 2]
            o, o_b = outs[n % 2]
            p, p_b = pss[n % 2]
            n += 1
            ph.dma("sp", w[:], ada_w[l, :, j * 512:(j + 1) * 512].rearrange("(k p) n -> p k n", p=128), w_b,
                   writes=[w_b])
            for k in range(8):
                ph.op("pe", lambda e, k=k, w=w, p=p: e.matmul(p[:], crep[:, k, :], w[:, k, :], start=(k == 0),
                                                               stop=(k == 7)),
                      [crep_b, w_b], [p_b])
            ph.op("dve", lambda e, o=o, p=p, j=j: e.tensor_tensor(out=o[:], in0=p[:], in1=bia[:, j * 512:(j + 1) * 512],
                                                                   op=ALU.add), [p_b, bia_b], [o_b])
            ph.dma("sp", modbc[l, :, j * 512:(j + 1) * 512], o[:], o_b, reads=[o_b])
    ph.run()


def emit_norm_tiles(ph, xsrc, hT, hT_b, G, S, GS_b, ident, ident_b, ntiles, h32T=None, h32T_b=None):
    xs = [ph.sb("x%d" % i, [128, D], F32) for i in range(2)]
    sq, sq_b = ph.sb("sq", [128, D], BF16)
    hs = [ph.sb("h%d" % i, [128, D], BF16) for i in range(2)]
    st = [ph.sb("st%d" % i, [128, 4], F32) for i in range(2)]
    pts = [ph.ps("pt%d" % i, [128, 8, 128], BF16) for i in range(2)]
    for t in range(ntiles):
        x, x_b = xs[t % 2]
        h, h_b = hs[t % 2]
        s, s_b = st[t % 2]
        pt, pt_b = pts[t % 2]
        ph.dma("sp", x[:], xsrc[t * 128:(t + 1) * 128, :], x_b, writes=[x_b])
        ph.op("act", lambda e, x=x, s=s: e.activation(out=sq[:], in_=x[:], func=AF.Square, accum_out=s[:, 0:1]),
              [x_b], [sq_b, s_b])
        ph.op("dve", lambda e, s=s: e.tensor_scalar(out=s[:, 1:2], in0=s[:, 0:1], scalar1=1.0 / D, scalar2=EPS,
                                                    op0=ALU.mult, op1=ALU.add), [s_b], [s_b])
        ph.op("act", lambda e, s=s: e.activation(out=s[:, 2:3], in_=s[:, 1:2], func=AF.Sqrt), [s_b], [s_b])
        ph.op("dve", lambda e, s=s: e.reciprocal(out=s[:, 3:4], in_=s[:, 2:3]), [s_b], [s_b])
        ph.op("dve", lambda e, x=x, s=s: e.scalar_tensor_tensor(out=x[:], in0=x[:], scalar=s[:, 3:4], in1=G[:],
                                                                op0=ALU.mult, op1=ALU.mult), [x_b, s_b, GS_b], [x_b])
        if S is not None:
            ph.op("pool", lambda e, x=x, h=h: e.tensor_tensor(out=h[:], in0=x[:], in1=S[:], op=ALU.add),
                  [x_b, GS_b], [h_b])
        else:
            ph.op("pool", lambda e, x=x, h=h: e.tensor_copy(out=h[:], in_=x[:]), [x_b], [h_b])
        for k in range(8):
            ph.op("pe", lambda e, k=k, h=h, pt=pt: e.transpose(pt[:, k, :], h[:, k * 128:(k + 1) * 128], ident[:]),
                  [h_b, ident_b], [pt_b])
        ph.op("act", lambda e, pt=pt, t=t: e.copy(out=hT[:, :, t * 128:(t + 1) * 128], in_=pt[:]), [pt_b], [hT_b])


def make_ident(ph, dtype=BF16):
    nc = ph.nc
    idf, idf_b = ph.sb("identf", [128, 128], F32)
    ident, ident_b = ph.sb("ident", [128, 128], dtype)
    ph.op("pool", lambda e: e.memset(idf[:], 1.0), [], [idf_b])
    ph.op("pool", lambda e: e.affine_select(out=idf[:], in_=idf[:], pattern=[[-1, 128]], compare_op=ALU.is_equal,
                                            fill=0.0, base=0, channel_multiplier=1), [idf_b], [idf_b])
    ph.op("pool", lambda e: e.tensor_copy(out=ident[:], in_=idf[:]), [idf_b], [ident_b])
    return ident, ident_b


def phase_proj(ctx, xres, modbc, l, norm_g, w_in_l, jobs, seg_scale, seg_shift):
    nc = ctx.nc
    ph = Phase(ctx, "proj")
    ident, ident_b = make_ident(ph)
    G, GS_b = ph.sb("G", [128, D], F32)
    S, _ = ph.sb("S", [128, D], F32)
    ng, _ = ph.sb("ng", [128, D], F32)
    hT, hT_b = ph.sb("hT", [128, 8, T], BF16)
    ph.dma("sp", G[:], modbc[l, :, seg_scale * D:(seg_scale + 1) * D], GS_b, writes=[GS_b])
    ph.dma("sp", S[:], modbc[l, :, seg_shift * D:(seg_shift + 1) * D], GS_b, writes=[GS_b])
    ph.dma("sp", ng[:], bcast_rows(norm_g[l:l + 1, :], 128), GS_b, writes=[GS_b])
    ph.op("dve", lambda e: e.tensor_tensor(out=G[:], in0=G[:], in1=ng[:], op=ALU.mult), [GS_b], [GS_b])
    emit_norm_tiles(ph, xres, hT, hT_b, G, S, GS_b, ident, ident_b, NT)

    wts = [ph.sb("w%d" % i, [128, 8, 512], BF16) for i in range(3)]
    pss = [ph.ps("pp%d" % i) for i in range(4)]
    stg = [ph.sb("sg%d" % i, [128, 512], F32) for i in range(4)]
    stgb = [ph.sb("sgb%d" % i, [128, 512], BF16) for i in range(4)]
    nw = 0
    ne = 0
    for (c0, ncols, kind, dst_fn, dtype, func) in jobs:
        step = 512 if kind == "tok" else 128
        for cc in range(c0, c0 + ncols, step):
            n = min(step, c0 + ncols - cc)
            w, w_b = wts[nw % 3]
            nw += 1
            ph.dma("pool", w[:, :, 0:n], w_in_l[:, cc:cc + n].rearrange("(k p) n -> p k n", p=128), w_b,
                   writes=[w_b])
            nchunk = NT if kind == "tok" else T // 512
            for ci in range(nchunk):
                p, p_b = pss[ne % 4]
                if dtype == F32:
                    sg, sg_b = stg[ne % 4]
                else:
                    sg, sg_b = stgb[ne % 4]
                evac_eng = "act" if (func is not None or ne % 2 == 0) else "dve"
                ne += 1
                if kind == "tok":
                    for k in range(8):
                        ph.op("pe", lambda e, k=k, w=w, p=p, ci=ci, n=n: e.matmul(
                            p[:, 0:n], hT[:, k, ci * 128:(ci + 1) * 128], w[:, k, 0:n], start=(k == 0), stop=(k == 7)),
                              [hT_b, w_b], [p_b])
                    po, so = p[:, 0:n], sg[:, 0:n]
                    dst = dst_fn(ci * 128, cc - c0, n)
                else:
                    for k in range(8):
                        ph.op("pe", lambda e, k=k, w=w, p=p, ci=ci, n=n: e.matmul(
                            p[0:n, :], w[:, k, 0:n], hT[:, k, ci * 512:(ci + 1) * 512], start=(k == 0), stop=(k == 7)),
                              [hT_b, w_b], [p_b])
                    po, so = p[0:n, :], sg[0:n, :]
                    dst = dst_fn(cc - c0, n, ci * 512)
                if evac_eng == "act":
                    f = func if func is not None else AF.Copy
                    ph.op("act", lambda e, po=po, so=so, f=f: e.activation(out=so, in_=po, func=f), [p_b], [sg_b])
                else:
                    ph.op("dve", lambda e, po=po, so=so: e.tensor_copy(out=so, in_=po), [p_b], [sg_b])
                ph.dma("sp", dst, so, sg_b, reads=[sg_b])
    ph.run()


INPUT_SPECS = [
    ("x", [T, D]), ("c", [1, D]), ("rel_bias", [32, 8]), ("hgrn_lb_raw", [DEPTH, D]), ("norm1_g", [DEPTH, D]),
    ("norm2_g", [DEPTH, D]), ("ada_w", [DEPTH, D, 6 * D]), ("ada_b", [DEPTH, 6 * D]), ("w_in", [DEPTH, D, N_IN]),
    ("hgrn_norm_g", [DEPTH, 128]), ("w_branch_a", [DEPTH, 1024, D]), ("w_branch_b", [DEPTH, 1024, D]),
    ("w_branch_c", [DEPTH, 2048, D]), ("w_out", [DEPTH, D, D]), ("router_group_w", [DEPTH, D, 4]),
    ("router_group_b", [DEPTH, 4]), ("router_expert_w", [DEPTH, D, 32]), ("router_expert_b", [DEPTH, 32]),
    ("expert_w_gate", [DEPTH, 32, D, 512]), ("expert_w_up", [DEPTH, 32, D, 512]), ("expert_w_down", [DEPTH, 32, 512, D]),
    ("final_norm_g", [1, D]), ("oh_tab", [32, 384]), ("cosT", [T, 256]), ("sinS", [T, 256]), ("cdec", [128, 8]),
]


def build_program(depth=DEPTH, debug=False):
    nc = bass.Bass("TRN2", target_bir_lowering=False)
    I = {}
    for name, shape in INPUT_SPECS:
        I[name] = nc.dram_tensor(name, shape, F32, kind="ExternalInput").ap()
    y = nc.dram_tensor("y", [T, D], F32, kind="ExternalOutput").ap()

    def scr(name, shape, dt):
        return nc.dram_tensor(name, shape, dt, kind="ExternalOutput" if debug else "Internal").ap()

    xres = scr("xres", [T, D], F32)
    modbc = scr("modbc", [DEPTH, 128, 6 * D], F32)
    s_aqT = scr("s_aqT", [1024, T], BF16)
    s_akT = scr("s_akT", [1024, T], BF16)
    s_av = scr("s_av", [T, 1024], BF16)
    s_iqT = scr("s_iqT", [512, T], BF16)
    s_ikT = scr("s_ikT", [64, T], BF16)
    s_iw = scr("s_iw", [T, 8], F32)
    s_bqT = scr("s_bqT", [1024, T], BF16)
    s_bfT = scr("s_bfT", [1024, T], F32)
    s_bi = scr("s_bi", [T, 1024], BF16)
    s_bg = scr("s_bg", [T, 1024], BF16)
    s_cq = scr("s_cq", [T, 1024], F32)
    s_ck = scr("s_ck", [T, 1024], F32)
    s_cv = scr("s_cv", [T, 2048], BF16)
    s_cg = scr("s_cg", [T, 2048], BF16)
    s_gT = scr("s_gT", [3072, T], BF16)
    s_mT = scr("s_mT", [NT, 128, NT, 128], BF16)
    s_tab = scr("s_tab", [128, 3072], BF16)
    s_oa = scr("s_oa", [T, 1024], BF16)
    s_ob = scr("s_ob", [T, 1024], BF16)
    s_oc = scr("s_oc", [T, 2048], BF16)
    s_hT = scr("s_hT", [128, 8, TH], BF16)
    s_gates = scr("s_gates", [T, 32], F32)

    def tok(dst):
        return lambda t0, c0, n: dst[t0:t0 + 128, c0:c0 + n]

    def feat(dst):
        return lambda c0, n, t0: dst[c0:c0 + n, t0:t0 + 512]

    jobs = [
        (O_AQ, 1024, "feat", feat(s_aqT), BF16, None),
        (O_AK, 1024, "feat", feat(s_akT), BF16, None),
        (O_AV, 1024, "tok", tok(s_av), BF16, None),
        (O_IQ, 512, "feat", feat(s_iqT), BF16, None),
        (O_IK, 64, "feat", feat(s_ikT), BF16, None),
        (O_IW, 8, "tok", tok(s_iw), F32, None),
        (O_BQ, 1024, "feat", feat(s_bqT), BF16, None),
        (O_BF, 1024, "feat", feat(s_bfT), F32, None),
        (O_BI, 1024, "tok", tok(s_bi), BF16, None),
        (O_BG, 1024, "tok", tok(s_bg), BF16, AF.Silu),
        (O_CQ, 1024, "tok", tok(s_cq), F32, None),
        (O_CK, 1024, "tok", tok(s_ck), F32, None),
        (O_CV, 2048, "tok", tok(s_cv), BF16, None),
        (O_CG, 2048, "tok", tok(s_cg), BF16, AF.Silu),
        (O_GA, 3072, "feat", feat(s_gT), BF16, AF.Sigmoid),
    ]
    with contextlib.ExitStack() as stack:
        ctx = Ctx(nc, stack)
        phase_copy(ctx, I["x"], xres, T)
        phase_mod(ctx, I["c"], I["ada_w"], I["ada_b"], modbc)
        phase_bias_tab(ctx, I["rel_bias"], I["oh_tab"], s_tab)
        for l in range(depth):
            phase_proj(ctx, xres, modbc, l, I["norm1_g"], I["w_in"][l], jobs, 1, 0)
            phase_a1(ctx, s_iqT, s_ikT, s_iw, s_mT)
            for hg in range(2):
                phase_a2(ctx, hg, s_aqT, s_akT, s_av, s_mT, s_tab, s_oa)
            phase_b(ctx, l, I["hgrn_lb_raw"], s_bqT, s_bfT, s_bi, s_bg, I["hgrn_norm_g"], s_ob)
            phase_c(ctx, s_cq, s_ck, s_cv, s_cg, I["cosT"], I["sinS"], I["cdec"], s_oc)
            phase_merge(ctx, l, modbc, s_oa, s_ob, s_oc, s_gT, I["w_branch_a"], I["w_branch_b"], I["w_branch_c"],
                        I["w_out"], xres)
            for hh in range(2):
                sg = s_gates[hh * TH:(hh + 1) * TH, :]
                phase_route(ctx, l, hh, xres, modbc, I["norm2_g"], I["router_group_w"], I["router_group_b"],
                            I["router_expert_w"], I["router_expert_b"], s_hT, sg)
                phase_experts(ctx, l, hh, xres, modbc, s_hT, sg, I["expert_w_gate"], I["expert_w_up"], I["expert_w_down"])
        phase_final(ctx, xres, I["final_norm_g"], y)
    return nc


_PROGRAM = None


def kernel(**inputs):
    global _PROGRAM
    if _PROGRAM is None:
        _PROGRAM = build_program()
    nc = _PROGRAM
    f = lambda a: np.ascontiguousarray(np.asarray(a, dtype=np.float32))
    cos, sinS, dec = make_ret_tables()
    shared = {k: f(inputs[k]) for k in ("rel_bias", "hgrn_lb_raw", "norm1_g", "norm2_g", "ada_w", "ada_b", "w_in",
                                        "hgrn_norm_g", "w_branch_a", "w_branch_b", "w_branch_c", "w_out",
                                        "router_group_w", "router_group_b", "router_expert_w", "router_expert_b",
                                        "expert_w_gate", "expert_w_up", "expert_w_down")}
    shared["final_norm_g"] = f(inputs["final_norm_g"]).reshape(1, D)
    shared["oh_tab"] = make_oh_tab()
    shared["cosT"] = cos
    shared["sinS"] = sinS
    shared["cdec"] = dec
    x = f(inputs["x"])
    c = f(inputs["c"])
    in_maps = []
    for core in range(8):
        b = core % 4
        m = dict(shared)
        m["x"] = x[b]
        m["c"] = c[b:b + 1]
        in_maps.append(m)
    res = run_bass_kernel_spmd(nc, in_maps, core_ids=list(range(8)))
    out = np.stack([np.asarray(res.results[b]["y"], dtype=np.float32) for b in range(4)], axis=0)
    return out


TOPK = 256
NBIS = 16
MASKV = -30000.0


def phase_a1(ctx, s_iqT, s_ikT, s_iw, s_mT):
    ph = Phase(ctx, "a1")
    ident, ident_b = make_ident(ph)
    ikT, ikT_b = ph.sb("ikT", [64, T], BF16)
    ph.dma("sp", ikT[:], s_ikT[:, :], ikT_b, writes=[ikT_b])
    pw, pw_b = ph.sb("pw", [128, NBIS], F32)
    for k in range(NBIS):
        ph.op("pool", lambda e, k=k: e.memset(pw[:, k:k + 1], 0.5 ** (k + 1)), [], [pw_b])
    cm, cm_b = ph.sb("cm", [128, 128], F32)
    ph.op("pool", lambda e: e.memset(cm[:], 0.0), [], [cm_b])
    ph.op("pool", lambda e: e.affine_select(out=cm[:], in_=cm[:], pattern=[[-1, 128]], compare_op=ALU.is_ge, fill=-1e30,
                                            base=0, channel_multiplier=1), [cm_b], [cm_b])
    iqs = [ph.sb("iq%d" % i, [64, 8, 128], BF16) for i in range(4)]
    ws = [ph.sb("w%d" % i, [128, 24], F32) for i in range(4)]
    Is = [ph.sb("I%d" % i, [128, T], F32) for i in range(4)]
    rls = [ph.sb("rl%d" % i, [128, 512], F32) for i in range(4)]
    junk, junk_b = ph.sb("junk", [128, T], BF16)
    junk2, junk2_b = ph.sb("junk2", [128, T], BF16)
    m01s = [ph.sb("m01_%d" % i, [128, T], BF16) for i in range(2)]
    mTs = [ph.sb("mT%d" % i, [128, NT, 128], BF16) for i in range(2)]
    sts = [ph.sb("st%d" % i, [128, 8 + NBIS], F32) for i in range(4)]
    pss = [ph.ps("ps%d" % i) for i in range(4)]
    ptr = [ph.ps("ptr%d" % i, [128, 4, 128], BF16) for i in range(2)]
    cnt = {"pz": 0, "rl": 0, "tr": 0}

    def gen_indexer(i):
        S = (i + 1) * 128
        iq, iq_b = iqs[i % 4]
        w, w_b = ws[i % 4]
        I, I_b = Is[i % 4]
        ph.dma("sp", iq[:], s_iqT[:, i * 128:(i + 1) * 128].rearrange("(h d) t -> d h t", d=64), iq_b, writes=[iq_b])
        ph.dma("sp", w[:, 0:8], s_iw[i * 128:(i + 1) * 128, :], w_b, writes=[w_b])
        ph.op("pool", lambda e: e.tensor_scalar(out=w[:, 0:8], in0=w[:, 0:8], scalar1=512.0 ** -0.5, scalar2=None,
                                                op0=ALU.mult), [w_b], [w_b])
        ph.op("pool", lambda e: e.tensor_scalar(out=w[:, 8:16], in0=w[:, 0:8], scalar1=-1.0, scalar2=None, op0=ALU.mult),
              [w_b], [w_b])
        ph.op("dve", lambda e: e.tensor_tensor(out=w[:, 8:16], in0=w[:, 8:16], in1=w[:, 0:8], op=ALU.max), [w_b], [w_b])
        ph.op("pool", lambda e: e.tensor_scalar(out=w[:, 16:24], in0=w[:, 0:8], scalar1=0.0, scalar2=2.0, op0=ALU.is_ge,
                                                op1=ALU.mult), [w_b], [w_b])
        ph.op("pool", lambda e: e.tensor_scalar(out=w[:, 16:24], in0=w[:, 16:24], scalar1=-1.0, scalar2=None, op0=ALU.add),
              [w_b], [w_b])
        yield
        for c0 in range(0, S, 512):
            n = min(512, S - c0)
            for h in range(8):
                p, p_b = pss[cnt["pz"] % 4]
                cnt["pz"] += 1
                rl, rl_b = rls[cnt["rl"] % 4]
                cnt["rl"] += 1
                ph.op("pe", lambda e, p=p, h=h, c0=c0, n=n: e.matmul(p[:, 0:n], iq[:, h, :], ikT[:, c0:c0 + n], start=True, stop=True),
                      [iq_b, ikT_b], [p_b])
                ph.op("act", lambda e, p=p, rl=rl, h=h, n=n: e.activation(out=rl[:, 0:n], in_=p[:, 0:n], func=AF.Relu,
                                                                     scale=w[:, 8 + h:9 + h]), [p_b, w_b], [rl_b])
                if h == 0:
                    ph.op("dve", lambda e, rl=rl, h=h, c0=c0, n=n: e.tensor_scalar(out=I[:, c0:c0 + n], in0=rl[:, 0:n],
                                                                       scalar1=w[:, 16 + h:17 + h], scalar2=None,
                                                                       op0=ALU.mult), [rl_b, w_b], [I_b])
                else:
                    ph.op("dve", lambda e, rl=rl, h=h, c0=c0, n=n: e.scalar_tensor_tensor(out=I[:, c0:c0 + n], in0=rl[:, 0:n],
                                                                              scalar=w[:, 16 + h:17 + h],
                                                                              in1=I[:, c0:c0 + n], op0=ALU.mult,
                                                                              op1=ALU.add), [rl_b, w_b, I_b], [I_b])
                if h % 2 == 1:
                    yield
        ph.op("pool", lambda e: e.tensor_tensor(out=I[:, i * 128:(i + 1) * 128], in0=I[:, i * 128:(i + 1) * 128],
                                                in1=cm[:], op=ALU.add), [I_b, cm_b], [I_b])
        yield

    def gen_select(i):
        S = (i + 1) * 128
        I, I_b = Is[i % 4]
        m01, m01_b = m01s[i % 2]
        mT, mT_b = mTs[i % 2]
        st, st_b = sts[i % 4]
        if i < 2:
            ph.op("dve", lambda e: e.memset(st[:, 0:1], -1e29), [], [st_b])
        else:
            on_act = (i % 2 == 1)
            ph.op("dve", lambda e: e.tensor_reduce(out=st[:, 0:1], in_=I[:, 0:i * 128], axis=AX.X, op=ALU.min),
                  [I_b], [st_b])
            ph.op("dve", lambda e: e.tensor_reduce(out=st[:, 1:2], in_=I[:, 0:S], axis=AX.X, op=ALU.max), [I_b], [st_b])
            ph.op("dve", lambda e: e.tensor_tensor(out=st[:, 2:3], in0=st[:, 1:2], in1=st[:, 0:1], op=ALU.subtract),
                  [st_b], [st_b])
            ph.op("dve", lambda e: e.tensor_scalar(out=st[:, 8:8 + NBIS], in0=pw[:], scalar1=st[:, 2:3], scalar2=None,
                                                   op0=ALU.mult), [st_b, pw_b], [st_b])
            yield
            for k in range(NBIS):
                if on_act:
                    ph.op("dve", lambda e, k=k: e.tensor_scalar(out=st[:, 3:4], in0=st[:, 0:1], scalar1=st[:, 8 + k:9 + k],
                                                                scalar2=-1.0, op0=ALU.add, op1=ALU.mult), [st_b], [st_b])
                    ph.op("act", lambda e: e.activation(out=junk2[:, 0:S], in_=I[:, 0:S], func=AF.Sign, bias=st[:, 3:4],
                                                        accum_out=st[:, 4:5]), [I_b, st_b], [junk2_b, st_b])
                    thresh = 2.0 * (TOPK - 0.5) - S
                else:
                    ph.op("dve", lambda e, k=k: e.tensor_tensor(out=st[:, 3:4], in0=st[:, 0:1], in1=st[:, 8 + k:9 + k],
                                                                op=ALU.add), [st_b], [st_b])
                    ph.op("dve", lambda e: e.tensor_scalar(out=junk[:, 0:S], in0=I[:, 0:S], scalar1=st[:, 3:4],
                                                           scalar2=None, op0=ALU.is_ge, op1=ALU.add,
                                                           accum_out=st[:, 4:5]), [I_b, st_b], [junk_b, st_b])
                    thresh = TOPK - 0.5
                ph.op("dve", lambda e, k=k, thresh=thresh: e.scalar_tensor_tensor(
                    out=st[:, 5:6], in0=st[:, 4:5], scalar=thresh, in1=st[:, 8 + k:9 + k], op0=ALU.is_ge, op1=ALU.mult),
                      [st_b], [st_b])
                ph.op("dve", lambda e: e.tensor_tensor(out=st[:, 0:1], in0=st[:, 0:1], in1=st[:, 5:6], op=ALU.add),
                      [st_b], [st_b])
                yield
        ph.op("dve", lambda e: e.tensor_scalar(out=m01[:, 0:S], in0=I[:, 0:S], scalar1=st[:, 0:1], scalar2=None,
                                               op0=ALU.is_ge), [I_b, st_b], [m01_b])
        yield
        for j0 in range(0, i + 1, 4):
            nj = min(4, i + 1 - j0)
            pt, pt_b = ptr[cnt["tr"] % 2]
            cnt["tr"] += 1
            for u in range(nj):
                j = j0 + u
                ph.op("pe", lambda e, pt=pt, u=u, j=j: e.transpose(pt[:, u, :], m01[:, j * 128:(j + 1) * 128], ident[:]),
                      [m01_b, ident_b], [pt_b])
            ph.op("dve", lambda e, pt=pt, j0=j0, nj=nj: e.tensor_copy(out=mT[:, j0:j0 + nj, :], in_=pt[:, 0:nj, :]),
                  [pt_b], [mT_b])
            yield
        ph.dma("sp", s_mT[i, :, 0:i + 1, :], mT[:, 0:i + 1, :], mT_b, reads=[mT_b])

    def merge(gens):
        state = [[g, max(n, 1), 0, False] for g, n in gens]
        while any(not st_[3] for st_ in state):
            best = None
            for st_ in state:
                if st_[3]:
                    continue
                frac = st_[2] / st_[1]
                if best is None or frac < best[0]:
                    best = (frac, st_)
            st_ = best[1]
            try:
                next(st_[0])
                st_[2] += 1
            except StopIteration:
                st_[3] = True

    def n_sel(i):
        return (NBIS + 4 if i >= 2 else 3) + (i + 4) // 4

    def n_idx(i):
        return 2 + 4 * (((i + 1) * 128 + 511) // 512)

    merge([(gen_indexer(0), n_idx(0)), (gen_indexer(1), n_idx(1))])
    for m in range(NT // 2):
        gens = [(gen_select(2 * m), n_sel(2 * m)), (gen_select(2 * m + 1), n_sel(2 * m + 1))]
        if 2 * m + 2 < NT:
            gens.append((gen_indexer(2 * m + 2), n_idx(2 * m + 2)))
            gens.append((gen_indexer(2 * m + 3), n_idx(2 * m + 3)))
        merge(gens)
    ph.run()


def phase_bias_tab(ctx, rel_bias, oh_tab, s_tab):
    ph = Phase(ctx, "btab")
    rb, rb_b = ph.sb("rb", [32, 8], F32)
    r31, r31_b = ph.sb("r31", [32, 8], F32)
    rep, rep_b = ph.sb("rep", [32, 8, 128], F32)
    oh, oh_b = ph.sb("oh", [32, 384], F32)
    tb, tb_b = ph.sb("tb", [128, 8, 384], BF16)
    pss = [ph.ps("p%d" % i) for i in range(2)]
    ph.dma("sp", rb[:], rel_bias[:, :], rb_b, writes=[rb_b])
    ph.dma("sp", r31[:], bcast_rows(rel_bias[31:32, :], 32), r31_b, writes=[r31_b])
    ph.dma("sp", oh[:], oh_tab[:, :], oh_b, writes=[oh_b])
    ph.op("dve", lambda e: e.tensor_tensor(out=rb[:], in0=rb[:], in1=r31[:], op=ALU.subtract), [rb_b, r31_b], [rb_b])
    ph.op("dve", lambda e: e.tensor_scalar(out=rb[:], in0=rb[:], scalar1=math.sqrt(128.0), scalar2=None, op0=ALU.mult),
          [rb_b], [rb_b])
    ph.op("dve", lambda e: e.tensor_copy(out=rep[:], in_=rb[:].unsqueeze(2).broadcast_to([32, 8, 128])), [rb_b], [rep_b])
    for h in range(8):
        p, p_b = pss[h % 2]
        ph.op("pe", lambda e, p=p, h=h: e.matmul(p[:, 0:384], rep[:, h, :], oh[:], start=True, stop=True),
              [rep_b, oh_b], [p_b])
        ph.op("act", lambda e, p=p, h=h: e.copy(out=tb[:, h, :], in_=p[:, 0:384]), [p_b], [tb_b])
    ph.dma("sp", s_tab[:, :], tb[:].rearrange("p h u -> p (h u)"), tb_b, reads=[tb_b])
    ph.run()


def phase_a2(ctx, hg, s_aqT, s_akT, s_av, s_mT, s_tab, s_oa):
    ph = Phase(ctx, "a2")
    ident, ident_b = make_ident(ph)
    kT, kT_b = ph.sb("kT", [128, 4, T], BF16)
    v, v_b = ph.sb("v", [128, NT, 4, 129], BF16)
    bt, bt_b = ph.sb("bt", [128, 2, 4, 128], BF16)
    r0 = hg * 512
    for h in range(4):
        ph.dma("sp", kT[:, h, :], s_akT[r0 + h * 128:r0 + (h + 1) * 128, :], kT_b, writes=[kT_b])
        ph.dma("sp", v[:, :, h, 0:128], s_av[:, r0 + h * 128:r0 + (h + 1) * 128].rearrange("(j p) d -> p j d", p=128),
               v_b, writes=[v_b])
        for pat in range(2):
            src = bass.AP(s_tab.tensor, s_tab.offset + (hg * 4 + h) * 384 + 255 - 128 * pat, [[3071, 128], [1, 128]])
            ph.dma("sp", bt[:, pat, h, :], src, bt_b, writes=[bt_b])
    ph.op("pool", lambda e: e.memset(v[:, :, :, 128:129], 1.0), [], [v_b])
    qs = [ph.sb("q%d" % i, [128, 4, 128], BF16) for i in range(2)]
    mbs = [ph.sb("mb%d" % i, [128, NT, 128], BF16) for i in range(2)]
    pts = [ph.sb("pt%d" % i, [128, 4, 128], BF16) for i in range(3)]
    sts = [ph.ps("st%d" % i) for i in range(3)]
    ops = [ph.ps("o%d" % i) for i in range(4)]
    rcs = [ph.sb("rc%d" % i, [128, 4], F32) for i in range(2)]
    outs = [ph.sb("out%d" % i, [128, 512], BF16) for i in range(2)]
    sts3 = sts
    pairs = [(i, j) for i in range(NT) for j in range(i + 1)]

    def emit_loads(i):
        q, q_b = qs[i % 2]
        mb, mb_b = mbs[i % 2]
        ph.dma("sp", q[:], s_aqT[r0:r0 + 512, i * 128:(i + 1) * 128].rearrange("(h d) t -> d h t", d=128), q_b,
               writes=[q_b])
        ph.dma("sp", mb[:, 0:i + 1, :], s_mT[i, :, 0:i + 1, :], mb_b, writes=[mb_b])

    def emit_qk(idx):
        i, j = pairs[idx]
        q, q_b = qs[i % 2]
        st, st_b = sts3[idx % 3]
        near = (i - j) <= 1
        for h in range(4):
            ph.op("pe", lambda e, h=h: e.matmul(st[:, h * 128:(h + 1) * 128], kT[:, h, j * 128:(j + 1) * 128], q[:, h, :],
                                                start=True, stop=not near), [kT_b, q_b], [st_b])
            if near:
                ph.op("pe", lambda e, h=h: e.matmul(st[:, h * 128:(h + 1) * 128], bt[:, i - j, h, :], ident[:], start=False,
                                                    stop=True), [bt_b, ident_b], [st_b])

    def emit_soft(idx):
        i, j = pairs[idx]
        mb, mb_b = mbs[i % 2]
        st, st_b = sts3[idx % 3]
        pt, pt_b = pts[idx % 3]
        ph.op("act", lambda e: e.activation(out=pt[:], in_=st[:], func=AF.Exp, scale=128.0 ** -0.5), [st_b], [pt_b])
        ph.op("dve", lambda e: e.tensor_tensor(out=pt[:], in0=pt[:], in1=mb[:, j:j + 1, :].broadcast_to([128, 4, 128]),
                                               op=ALU.mult), [pt_b, mb_b], [pt_b])

    def emit_pv(idx):
        i, j = pairs[idx]
        pt, pt_b = pts[idx % 3]
        for h in range(4):
            o, o_b = ops[h]
            ph.op("pe", lambda e, o=o, h=h: e.matmul(o[:, 0:129], pt[:, h, :], v[:, j, h, :], start=(j == 0), stop=(j == i)),
                  [pt_b, v_b], [o_b])
        if j == i:
            rc, rc_b = rcs[i % 2]
            out, out_b = outs[i % 2]
            for h in range(4):
                o, o_b = ops[h]
                ph.op("dve", lambda e, o=o, h=h: e.reciprocal(out=rc[:, h:h + 1], in_=o[:, 128:129]), [o_b], [rc_b])
                ph.op("dve", lambda e, o=o, h=h: e.tensor_scalar(out=out[:, h * 128:(h + 1) * 128], in0=o[:, 0:128],
                                                                 scalar1=rc[:, h:h + 1], scalar2=None, op0=ALU.mult),
                      [o_b, rc_b], [out_b])
            ph.dma("sp", s_oa[i * 128:(i + 1) * 128, r0:r0 + 512], out[:], out_b, reads=[out_b])

    emit_loads(0)
    emit_qk(0)
    if len(pairs) > 1:
        emit_loads(1)
        emit_qk(1)
    for idx in range(len(pairs)):
        i, j = pairs[idx]
        if j == 0 and i >= 1 and i + 1 < NT:
            emit_loads(i + 1)
        emit_soft(idx)
        if idx + 2 < len(pairs):
            emit_qk(idx + 2)
        emit_pv(idx)
    ph.run()


def phase_b(ctx, l, lb_raw, s_bqT, s_bfT, s_bi, s_bg, hgrn_norm_g, s_ob):
    ph = Phase(ctx, "hg")
    ident, ident_b = make_ident(ph)
    lbr, lbr_b = ph.sb("lbr", [128, DEPTH, 8], F32)
    lb, lb_b = ph.sb("lb", [128, 8], F32)
    oml, oml_b = ph.sb("oml", [128, 8], F32)
    ssum, ssum_b = ph.sb("ssum", [128, 8], F32)
    ph.dma("sp", lbr[:], lb_raw.rearrange("l (h p) -> p l h", p=128), lbr_b, writes=[lbr_b],
           allow_slow_non_contiguous=True)
    ph.op("act", lambda e: e.activation(out=lbr[:], in_=lbr[:], func=AF.Exp), [lbr_b], [lbr_b])
    ph.op("dve", lambda e: e.tensor_tensor(out=ssum[:], in0=lbr[:, 0, :], in1=lbr[:, 1, :], op=ALU.add), [lbr_b], [ssum_b])
    ph.op("dve", lambda e: e.tensor_tensor(out=ssum[:], in0=ssum[:], in1=lbr[:, 2, :], op=ALU.add), [lbr_b, ssum_b], [ssum_b])
    ph.op("dve", lambda e: e.tensor_tensor(out=ssum[:], in0=ssum[:], in1=lbr[:, 3, :], op=ALU.add), [lbr_b, ssum_b], [ssum_b])
    ph.op("dve", lambda e: e.reciprocal(out=ssum[:], in_=ssum[:]), [ssum_b], [ssum_b])
    ph.op("dve", lambda e: e.memset(lb[:], 0.0), [], [lb_b])
    for m in range(1, l + 1):
        ph.op("dve", lambda e, m=m: e.tensor_tensor(out=lb[:], in0=lb[:], in1=lbr[:, m, :], op=ALU.add), [lbr_b, lb_b], [lb_b])
    ph.op("dve", lambda e: e.tensor_tensor(out=lb[:], in0=lb[:], in1=ssum[:], op=ALU.mult), [lb_b, ssum_b], [lb_b])
    ph.op("dve", lambda e: e.tensor_scalar(out=oml[:], in0=lb[:], scalar1=-1.0, scalar2=1.0, op0=ALU.mult, op1=ALU.add),
          [lb_b], [oml_b])
    ng, ng_b = ph.sb("ng", [64, 128], F32)
    ph.dma("sp", ng[:], bcast_rows(hgrn_norm_g[l:l + 1, :], 64), ng_b, writes=[ng_b])
    ones, ones_b = ph.sb("ones", [128, T], BF16)
    ph.op("pool", lambda e: e.memset(ones[:], 1.0), [], [ones_b])
    zer, zer_b = ph.sb("zer", [128, 32], BF16)
    ph.op("pool", lambda e: e.memset(zer[:], 0.0), [], [zer_b])
    m01, m01_b = ph.sb("m01", [64, 64], F32)
    ph.op("pool", lambda e: e.memset(m01[:], 1.0), [], [m01_b])
    ph.op("pool", lambda e: e.affine_select(out=m01[:], in_=m01[:], pattern=[[1, 64]], compare_op=ALU.is_ge, fill=0.0,
                                            base=0, channel_multiplier=-1), [m01_b], [m01_b])
    NC = T // 64
    fT, fT_b = ph.sb("fT", [128, T], F32)
    Bc, Bc_b = ph.sb("Bc", [128, T], F32)
    E, E_b = ph.sb("E", [128, T], F32)
    qT, qT_b = ph.sb("qT", [128, T], BF16)
    qd, qd_b = ph.sb("qd", [128, T], BF16)
    kd, kd_b = ph.sb("kd", [128, T], BF16)
    kdtm, kdtm_b = ph.sb("kdtm", [64, NC, 128], BF16)
    iv, iv_b = ph.sb("iv", [64, NC, 128], BF16)
    oall, oall_b = ph.sb("oall", [64, NC, 128], F32)
    osb, osb_b = ph.sb("osb", [64, NC, 128], BF16)
    sc, sc_b = ph.sb("sc", [128, 4, NC], F32)
    S, S_b = ph.sb("S", [128, 128], F32)
    Sps = [ph.sb("Sp%d" % i, [128, 128], BF16) for i in range(2)]
    tmpu, tmpu_b = ph.sb("tmpu", [128, 128], F32)
    atms = [ph.sb("atm%d" % i, [64, 64], BF16) for i in range(2)]
    rs, rs_b = ph.sb("rs", [64, 2, NC], F32)
    psA = [ph.ps("pA%d" % i) for i in range(3)]
    psO = [ph.ps("pO%d" % i) for i in range(2)]
    psU = [ph.ps("pU%d" % i) for i in range(2)]
    psT = [ph.ps("pT%d" % i, [128, 4, 128], BF16) for i in range(1)]
    for h in range(8):
        r0 = h * 128
        ph.dma("sp", fT[:], s_bfT[r0:r0 + 128, :], fT_b, writes=[fT_b])
        ph.dma("sp", qT[:], s_bqT[r0:r0 + 128, :], qT_b, writes=[qT_b])
        ph.dma("sp", iv[:], s_bi[:, r0:r0 + 128].rearrange("(c p) v -> p c v", p=64), iv_b, writes=[iv_b])
        ph.dma("sp", osb[:], s_bg[:, r0:r0 + 128].rearrange("(c p) v -> p c v", p=64), osb_b, writes=[osb_b])
        ph.op("act", lambda e: e.activation(out=fT[:], in_=fT[:], func=AF.Sigmoid), [fT_b], [fT_b])
        ph.op("dve", lambda e, h=h: e.tensor_scalar(out=fT[:], in0=fT[:], scalar1=oml[:, h:h + 1], scalar2=lb[:, h:h + 1],
                                                    op0=ALU.mult, op1=ALU.add), [fT_b, oml_b, lb_b], [fT_b])
        ph.op("act", lambda e: e.activation(out=Bc[:], in_=fT[:], func=AF.Ln), [fT_b], [Bc_b])
        ph.op("pool", lambda e: e.tensor_scalar(out=fT[:], in0=fT[:], scalar1=-1.0, scalar2=1.0, op0=ALU.mult,
                                                op1=ALU.add), [fT_b, Bc_b], [fT_b])
        ph.op("dve", lambda e: e.tensor_tensor_scan(out=Bc[:], data0=ones[:], data1=Bc[:], initial=0.0, op0=ALU.mult,
                                                    op1=ALU.add), [Bc_b, ones_b], [Bc_b])
        Bc3 = Bc[:].rearrange("p (c s) -> p c s", s=64)
        ph.op("dve", lambda e: e.memset(sc[:, 0, 0:1], 0.0), [], [sc_b])
        ph.op("dve", lambda e, Bc3=Bc3: e.tensor_copy(out=sc[:, 0, 1:NC], in_=Bc3[:, 0:NC - 1, 63]), [Bc_b], [sc_b])
        ph.op("dve", lambda e, Bc3=Bc3: e.tensor_tensor(out=sc[:, 1, :], in0=Bc3[:, :, 63], in1=sc[:, 0, :],
                                                        op=ALU.subtract), [Bc_b, sc_b], [sc_b])
        ph.op("dve", lambda e, Bc3=Bc3: e.tensor_tensor(out=sc[:, 2, :], in0=Bc3[:, :, 31], in1=sc[:, 0, :],
                                                        op=ALU.subtract), [Bc_b, sc_b], [sc_b])
        ph.op("dve", lambda e, Bc3=Bc3: e.tensor_tensor(out=sc[:, 3, :], in0=Bc3[:, :, 63], in1=Bc3[:, :, 31],
                                                        op=ALU.subtract), [Bc_b, sc_b], [sc_b])
        ph.op("act", lambda e: e.activation(out=sc[:, 1:4, :], in_=sc[:, 1:4, :], func=AF.Exp), [sc_b], [sc_b])
        ph.op("dve", lambda e, Bc3=Bc3: e.tensor_copy(out=E[:, 0:NC], in_=Bc3[:, :, 31]), [Bc_b], [E_b])
        ph.op("dve", lambda e, Bc3=Bc3: e.tensor_tensor(out=Bc3, in0=Bc3, in1=E[:, 0:NC].unsqueeze(2).broadcast_to(
            [128, NC, 64]), op=ALU.subtract), [Bc_b, E_b], [Bc_b])
        ph.op("act", lambda e: e.activation(out=E[:], in_=Bc[:], func=AF.Exp), [Bc_b], [E_b])
        ph.op("dve", lambda e: e.tensor_tensor(out=qd[:], in0=qT[:], in1=E[:], op=ALU.mult), [qT_b, E_b], [qd_b])
        ph.op("act", lambda e: e.activation(out=E[:], in_=Bc[:], func=AF.Exp, scale=-1.0), [Bc_b, qd_b], [E_b])
        ph.op("dve", lambda e: e.tensor_tensor(out=kd[:], in0=fT[:], in1=E[:], op=ALU.mult), [fT_b, E_b], [kd_b])
        for c4 in range(NC // 4):
            pT, pT_b = psT[0]
            for u in range(4):
                c = c4 * 4 + u
                ph.op("pe", lambda e, pT=pT, u=u, c=c: e.transpose(pT[0:64, u, :], kd[:, c * 64:(c + 1) * 64], ident[:]),
                      [kd_b, ident_b], [pT_b])
            ph.op("act", lambda e, pT=pT, c4=c4: e.copy(out=kdtm[:, c4 * 4:(c4 + 1) * 4, :], in_=pT[0:64, :, :]),
                  [pT_b], [kdtm_b])
        def emit_A(c):
            pA, pA_b = psA[c % 3]
            c0 = c * 64
            ph.op("pe", lambda e: e.matmul(pA[0:32, 0:64], kd[:, c0:c0 + 32], qd[:, c0:c0 + 64], start=True, stop=True),
                  [kd_b, qd_b], [pA_b])
            ph.op("pe", lambda e: e.matmul(pA[32:64, 32:64], kd[:, c0 + 32:c0 + 64], qd[:, c0 + 32:c0 + 64], start=True,
                                           stop=True), [kd_b, qd_b], [pA_b])
            ph.op("pe", lambda e: e.matmul(pA[32:64, 0:32], zer[:], qd[:, c0:c0 + 32], start=True, stop=True),
                  [zer_b, qd_b], [pA_b])

        def emit_atm(c):
            pA, pA_b = psA[c % 3]
            atm, atm_b = atms[c % 2]
            ph.op("dve", lambda e: e.tensor_tensor(out=atm[:], in0=pA[0:64, 0:64], in1=m01[:], op=ALU.mult),
                  [pA_b, m01_b], [atm_b])

        def emit_U(c):
            pU, pU_b = psU[c % 2]
            ph.op("pe", lambda e: e.matmul(pU[:, 0:128], kdtm[:, c, :], iv[:, c, :], start=True, stop=True),
                  [kdtm_b, iv_b], [pU_b])

        emit_A(0)
        emit_A(1)
        emit_U(0)
        emit_atm(0)
        for c in range(NC):
            pO, pO_b = psO[c % 2]
            pU, pU_b = psU[c % 2]
            atm, atm_b = atms[c % 2]
            Sp, Sp_b = Sps[c % 2]
            Spn, Spn_b = Sps[(c + 1) % 2]
            cs = slice(c * 64, (c + 1) * 64)
            ph.op("pe", lambda e, pO=pO, atm=atm, c=c: e.matmul(pO[0:64, 0:128], atm[:], iv[:, c, :], start=True,
                                                               stop=(c == 0)), [atm_b, iv_b], [pO_b])
            if c > 0:
                ph.op("pe", lambda e, pO=pO, cs=cs, Sp=Sp: e.matmul(pO[0:64, 0:128], qd[:, cs], Sp[:], start=False,
                                                                   stop=True), [qd_b, Sp_b], [pO_b])
            ph.op("act", lambda e, pO=pO, c=c: e.copy(out=oall[:, c, :], in_=pO[0:64, 0:128]), [pO_b], [oall_b])
            if c + 2 < NC:
                emit_A(c + 2)
            if c + 1 < NC - 1:
                emit_U(c + 1)
            if c < NC - 1:
                if c == 0:
                    ph.op("dve", lambda e, pU=pU, c=c: e.tensor_scalar(out=S[:], in0=pU[:, 0:128], scalar1=sc[:, 3, c:c + 1],
                                                                       scalar2=None, op0=ALU.mult), [pU_b, sc_b], [S_b])
                else:
                    ph.op("dve", lambda e, pU=pU, c=c: e.tensor_scalar(out=tmpu[:], in0=pU[:, 0:128],
                                                                       scalar1=sc[:, 3, c:c + 1], scalar2=None,
                                                                       op0=ALU.mult), [pU_b, sc_b], [tmpu_b])
                    ph.op("dve", lambda e, c=c: e.scalar_tensor_tensor(out=S[:], in0=S[:], scalar=sc[:, 1, c:c + 1],
                                                                       in1=tmpu[:], op0=ALU.mult, op1=ALU.add),
                          [S_b, tmpu_b, sc_b], [S_b])
                ph.op("dve", lambda e, Spn=Spn, c=c: e.tensor_scalar(out=Spn[:], in0=S[:], scalar1=sc[:, 2, c + 1:c + 2],
                                                                     scalar2=None, op0=ALU.mult), [S_b, sc_b], [Spn_b])
                emit_atm(c + 1)
        oflat = oall[:].rearrange("p c v -> p (c v)")
        ph.op("act", lambda e: e.activation(out=kdtm[:], in_=oall[:], func=AF.Square), [oall_b], [kdtm_b])
        ph.op("dve", lambda e: e.tensor_reduce(out=rs[:, 0, :], in_=kdtm[:], axis=AX.X, op=ALU.add), [kdtm_b], [rs_b])
        ph.op("dve", lambda e: e.tensor_scalar(out=rs[:, 0, :], in0=rs[:, 0, :], scalar1=1.0 / 128, scalar2=EPS,
                                               op0=ALU.mult, op1=ALU.add), [rs_b], [rs_b])
        ph.op("act", lambda e: e.activation(out=rs[:, 0, :], in_=rs[:, 0, :], func=AF.Sqrt), [rs_b], [rs_b])
        ph.op("dve", lambda e: e.reciprocal(out=rs[:, 1, :], in_=rs[:, 0, :]), [rs_b], [rs_b])
        ph.op("dve", lambda e: e.tensor_tensor(out=oall[:], in0=oall[:], in1=rs[:, 1, :].unsqueeze(2).broadcast_to(
            [64, NC, 128]), op=ALU.mult), [oall_b, rs_b], [oall_b])
        ph.op("pool", lambda e: e.tensor_tensor(out=oall[:], in0=oall[:], in1=ng[:].unsqueeze(1).broadcast_to(
            [64, NC, 128]), op=ALU.mult), [oall_b, ng_b], [oall_b])
        ph.op("dve", lambda e: e.tensor_tensor(out=osb[:], in0=oall[:], in1=osb[:], op=ALU.mult), [oall_b, osb_b], [osb_b])
        ph.dma("sp", s_ob[:, r0:r0 + 128].rearrange("(c p) v -> p c v", p=64), osb[:], osb_b, reads=[osb_b])
    ph.run()


def phase_c(ctx, s_cq, s_ck, s_cv, s_cg, cosT, sinS, cdec, s_oc):
    ph = Phase(ctx, "ret")
    ident, ident_b = make_ident(ph)
    dec, dec_b = ph.sb("dec", [128, 8], F32)
    ph.dma("sp", dec[:], cdec[:, :], dec_b, writes=[dec_b])
    m01, m01_b = ph.sb("m01", [128, 128], F32)
    ph.op("pool", lambda e: e.memset(m01[:], 1.0), [], [m01_b])
    ph.op("pool", lambda e: e.affine_select(out=m01[:], in_=m01[:], pattern=[[1, 128]], compare_op=ALU.is_ge, fill=0.0,
                                            base=0, channel_multiplier=-1), [m01_b], [m01_b])
    qrdT, qrdT_b = ph.sb("qrdT", [128, 2, T], BF16)
    krnT, krnT_b = ph.sb("krnT", [128, 2, T], BF16)
    krn, krn_b = ph.sb("krn", [128, NT, 256], BF16)
    v, v_b = ph.sb("v", [128, NT, 512], BF16)
    ins = [[ph.sb("in%d_%d" % (a, i), [128, 256], F32) for i in range(2)] for a in range(2)]
    cs_ = [ph.sb("cos%d" % i, [128, 256], F32) for i in range(2)]
    sn_ = [ph.sb("sin%d" % i, [128, 256], F32) for i in range(2)]
    t1s = [ph.sb("t1_%d" % i, [128, 256], F32) for i in range(2)]
    t2s = [ph.sb("t2_%d" % i, [128, 256], F32) for i in range(2)]
    qtm = [ph.sb("qtm%d" % i, [128, 256], BF16) for i in range(2)]
    W = [ph.sb("W%d" % i, [128, 512], F32) for i in range(2)]
    Wb = [ph.sb("Wb%d" % i, [128, 512], BF16) for i in range(2)]
    atms = [ph.sb("atm%d" % i, [128, 128], BF16) for i in range(2)]
    gts = [ph.sb("g%d" % i, [128, 512], BF16) for i in range(2)]
    outs = [ph.sb("o%d" % i, [128, 512], BF16) for i in range(2)]
    junk, junk_b = ph.sb("junk", [128, 512], BF16)
    sts = [ph.sb("st%d" % i, [128, 4], F32) for i in range(2)]
    psT = [ph.ps("pT%d" % i, [128, 2, 128], BF16) for i in range(2)]
    psA = [ph.ps("pA%d" % i) for i in range(1)]
    psO = [ph.ps("pO%d" % i) for i in range(2)]
    psU = [ph.ps("pU%d" % i) for i in range(2)]
    for h in range(4):
        gam = 1.0 - 2.0 ** (-5.0 - h)
        g128 = gam ** 128
        ph.dma("sp", v[:], s_cv[:, h * 512:(h + 1) * 512].rearrange("(c p) d -> p c d", p=128), v_b, writes=[v_b])
        for i in range(NT):
            rows = slice(i * 128, (i + 1) * 128)
            co, co_b = cs_[i % 2]
            sn, sn_b = sn_[i % 2]
            ph.dma("sp", co[:], cosT[rows, :], co_b, writes=[co_b])
            ph.dma("sp", sn[:], sinS[rows, :], sn_b, writes=[sn_b])
            for a, src in enumerate((s_cq, s_ck)):
                x, x_b = ins[a][i % 2]
                t1, t1_b = t1s[a]
                t2, t2_b = t2s[a]
                ph.dma("sp", x[:], src[rows, h * 256:(h + 1) * 256], x_b, writes=[x_b])
                x3 = x[:].rearrange("p (i two) -> p i two", two=2)
                s3 = sn[:].rearrange("p (i two) -> p i two", two=2)
                t23 = t2[:].rearrange("p (i two) -> p i two", two=2)
                ph.op("dve", lambda e, t1=t1, x=x, co=co: e.tensor_tensor(out=t1[:], in0=x[:], in1=co[:], op=ALU.mult),
                      [x_b, co_b], [t1_b])
                ph.op("pool", lambda e, t23=t23, x3=x3, s3=s3: e.tensor_tensor(out=t23[:, :, 0], in0=x3[:, :, 1],
                                                                               in1=s3[:, :, 0], op=ALU.mult),
                      [x_b, sn_b], [t2_b])
                ph.op("pool", lambda e, t23=t23, x3=x3, s3=s3: e.tensor_tensor(out=t23[:, :, 1], in0=x3[:, :, 0],
                                                                               in1=s3[:, :, 1], op=ALU.mult),
                      [x_b, sn_b], [t2_b])
                ph.op("dve", lambda e, t1=t1, t2=t2: e.tensor_tensor(out=t1[:], in0=t1[:], in1=t2[:], op=ALU.add),
                      [t1_b, t2_b], [t1_b])
                pT, pT_b = psT[a]
                if a == 0:
                    dst, dst_b = qtm[i % 2]
                    dstap = dst[:]
                else:
                    dst_b = krn_b
                    dstap = krn[:, i, :]
                ph.op("act", lambda e, dstap=dstap, t1=t1, a=a, h=h: e.activation(out=dstap, in_=t1[:], func=AF.Copy,
                                                                                  scale=dec[:, a * 4 + h:a * 4 + h + 1]),
                      [t1_b, dec_b], [dst_b])
                for kc in range(2):
                    ph.op("pe", lambda e, pT=pT, kc=kc, dstap=dstap: e.transpose(pT[:, kc, :],
                                                                                 dstap[:, kc * 128:(kc + 1) * 128], ident[:]),
                          [dst_b, ident_b], [pT_b])
                tgt, tgt_b = (qrdT, qrdT_b) if a == 0 else (krnT, krnT_b)
                ph.op("act", lambda e, tgt=tgt, pT=pT, i=i: e.copy(out=tgt[:, :, i * 128:(i + 1) * 128], in_=pT[:]),
                      [pT_b], [tgt_b])
        for c in range(NT):
            cs = slice(c * 128, (c + 1) * 128)
            pA, pA_b = psA[0]
            pO, pO_b = psO[c % 2]
            atm, atm_b = atms[c % 2]
            g, g_b = gts[c % 2]
            out, out_b = outs[c % 2]
            st, st_b = sts[c % 2]
            ph.dma("sp", g[:], s_cg[cs, h * 512:(h + 1) * 512], g_b, writes=[g_b])
            for kc in range(2):
                ph.op("pe", lambda e, pA=pA, kc=kc, cs=cs: e.matmul(pA[:, 0:128], krnT[:, kc, cs], qrdT[:, kc, cs],
                                                                   start=(kc == 0), stop=(kc == 1)),
                      [krnT_b, qrdT_b], [pA_b])
            ph.op("dve", lambda e, pA=pA, atm=atm: e.tensor_tensor(out=atm[:], in0=pA[:, 0:128], in1=m01[:], op=ALU.mult),
                  [pA_b, m01_b], [atm_b])
            ph.op("pe", lambda e, pO=pO, atm=atm, c=c: e.matmul(pO[:], atm[:], v[:, c, :], start=True, stop=(c == 0)),
                  [atm_b, v_b], [pO_b])
            if c > 0:
                for kc in range(2):
                    ph.op("pe", lambda e, pO=pO, kc=kc, cs=cs: e.matmul(pO[:], qrdT[:, kc, cs], Wb[kc][0][:], start=False,
                                                                       stop=(kc == 1)), [qrdT_b, Wb[kc][1]], [pO_b])
            if c < NT - 1:
                for kc in range(2):
                    pU, pU_b = psU[kc]
                    ph.op("pe", lambda e, pU=pU, kc=kc, c=c: e.matmul(pU[:], krn[:, c, kc * 128:(kc + 1) * 128], v[:, c, :],
                                                                     start=True, stop=True), [krn_b, v_b], [pU_b])
                    Wk, Wk_b = W[kc]
                    if c == 0:
                        ph.op("dve", lambda e, Wk=Wk, pU=pU: e.tensor_copy(out=Wk[:], in_=pU[:]), [pU_b], [Wk_b])
                    else:
                        ph.op("dve", lambda e, Wk=Wk, pU=pU, g128=g128: e.scalar_tensor_tensor(
                            out=Wk[:], in0=Wk[:], scalar=g128, in1=pU[:], op0=ALU.mult, op1=ALU.add), [Wk_b, pU_b], [Wk_b])
                    ph.op("act", lambda e, Wk=Wk, kc=kc, g128=g128: e.activation(out=Wb[kc][0][:], in_=Wk[:], func=AF.Copy,
                                                                                 scale=g128), [Wk_b], [Wb[kc][1]])
            ph.op("act", lambda e, pO=pO, st=st: e.activation(out=junk[:], in_=pO[:], func=AF.Square,
                                                             accum_out=st[:, 0:1]), [pO_b], [junk_b, st_b])
            ph.op("dve", lambda e, st=st: e.tensor_scalar(out=st[:, 1:2], in0=st[:, 0:1], scalar1=1.0 / 512, scalar2=EPS,
                                                          op0=ALU.mult, op1=ALU.add), [st_b], [st_b])
            ph.op("act", lambda e, st=st: e.activation(out=st[:, 2:3], in_=st[:, 1:2], func=AF.Sqrt), [st_b], [st_b])
            ph.op("dve", lambda e, st=st: e.reciprocal(out=st[:, 3:4], in_=st[:, 2:3]), [st_b], [st_b])
            ph.op("dve", lambda e, pO=pO, st=st, g=g, out=out: e.scalar_tensor_tensor(
                out=out[:], in0=pO[:], scalar=st[:, 3:4], in1=g[:], op0=ALU.mult, op1=ALU.mult), [pO_b, st_b, g_b], [out_b])
            ph.dma("sp", s_oc[cs, h * 512:(h + 1) * 512], out[:], out_b, reads=[out_b])
    ph.run()


def phase_merge(ctx, l, modbc, s_oa, s_ob, s_oc, s_gT, w_a, w_b, w_c, w_out, xres):
    ph = Phase(ctx, "mrg")
    ident, ident_b = make_ident(ph)
    Wbr, Wbr_b = ph.sb("Wbr", [128, 32, D], BF16)
    wo, wo_b = ph.sb("wo", [128, 8, D], BF16)
    gt, gt_b = ph.sb("gt", [128, D], F32)
    for kc in range(8):
        ph.dma("pool", Wbr[:, kc, :], w_a[l, kc * 128:(kc + 1) * 128, :], Wbr_b, writes=[Wbr_b])
        ph.dma("pool", Wbr[:, 8 + kc, :], w_b[l, kc * 128:(kc + 1) * 128, :], Wbr_b, writes=[Wbr_b])
        ph.dma("pool", wo[:, kc, :], w_out[l, kc * 128:(kc + 1) * 128, :], wo_b, writes=[wo_b])
    for kc in range(16):
        ph.dma("pool", Wbr[:, 16 + kc, :], w_c[l, kc * 128:(kc + 1) * 128, :], Wbr_b, writes=[Wbr_b])
    ph.dma("sp", gt[:], modbc[l, :, 2 * D:3 * D], gt_b, writes=[gt_b])
    oin = [ph.sb("oin%d" % i, [128, 4096], BF16) for i in range(2)]
    oT, oT_b = ph.sb("oT", [128, 32, 512], BF16)
    gs = [ph.sb("gs%d" % i, [128, 3, 512], BF16) for i in range(2)]
    tt1 = [ph.sb("tt%d" % i, [128, 512], F32) for i in range(3)]
    mT, mT_b = ph.sb("mT", [128, 8, 512], BF16)
    xs = [ph.sb("x%d" % i, [128, D], F32) for i in range(2)]
    ty = [ph.sb("ty%d" % i, [128, 512], F32) for i in range(2)]
    psT = [ph.ps("pT%d" % i, [128, 8, 128], BF16) for i in range(2)]
    psB = [ph.ps("pB%d" % i) for i in range(3)]
    psY = [ph.ps("pY%d" % i) for i in range(2)]
    nT = 0
    ny = 0
    ng = 0
    for g in range(T // 512):
        for tt in range(4):
            rows = slice(g * 512 + tt * 128, g * 512 + (tt + 1) * 128)
            o, o_b = oin[tt % 2]
            ph.dma("sp", o[:, 0:1024], s_oa[rows, :], o_b, writes=[o_b])
            ph.dma("sp", o[:, 1024:2048], s_ob[rows, :], o_b, writes=[o_b])
            ph.dma("sp", o[:, 2048:4096], s_oc[rows, :], o_b, writes=[o_b])
            for k8 in range(4):
                pT, pT_b = psT[nT % 2]
                nT += 1
                for u in range(8):
                    kc = k8 * 8 + u
                    ph.op("pe", lambda e, pT=pT, u=u, o=o, kc=kc: e.transpose(pT[:, u, :], o[:, kc * 128:(kc + 1) * 128],
                                                                           ident[:]), [o_b, ident_b], [pT_b])
                ph.op("act", lambda e, pT=pT, k8=k8, tt=tt: e.copy(out=oT[:, k8 * 8:(k8 + 1) * 8, tt * 128:(tt + 1) * 128],
                                                                 in_=pT[:]), [pT_b], [oT_b])
        for dc in range(8):
            gsb, gsb_b = gs[ng % 2]
            ng += 1
            ph.dma("sp", gsb[:], s_gT.rearrange("(b r) t -> r b t", b=3)[dc * 128:(dc + 1) * 128, :, g * 512:(g + 1) * 512],
                   gsb_b, writes=[gsb_b])
            for br, (k0, k1) in enumerate(((0, 8), (8, 16), (16, 32))):
                pB, pB_b = psB[br]
                for kc in range(k0, k1):
                    ph.op("pe", lambda e, pB=pB, kc=kc, dc=dc, k0=k0, k1=k1: e.matmul(
                        pB[:], Wbr[:, kc, dc * 128:(dc + 1) * 128], oT[:, kc, :], start=(kc == k0), stop=(kc == k1 - 1)),
                          [Wbr_b, oT_b], [pB_b])
                t, t_b = tt1[br]
                ph.op("dve", lambda e, t=t, pB=pB, gsb=gsb, br=br: e.tensor_tensor(out=t[:], in0=pB[:], in1=gsb[:, br, :],
                                                                                  op=ALU.mult), [pB_b, gsb_b], [t_b])
            ph.op("pool", lambda e: e.tensor_tensor(out=tt1[0][0][:], in0=tt1[0][0][:], in1=tt1[1][0][:], op=ALU.add),
                  [tt1[0][1], tt1[1][1]], [tt1[0][1]])
            ph.op("pool", lambda e, dc=dc: e.tensor_tensor(out=mT[:, dc, :], in0=tt1[0][0][:], in1=tt1[2][0][:], op=ALU.add),
                  [tt1[0][1], tt1[2][1]], [mT_b])
        for tt in range(4):
            rows = slice(g * 512 + tt * 128, g * 512 + (tt + 1) * 128)
            x, x_b = xs[tt % 2]
            ph.dma("sp", x[:], xres[rows, :], x_b, writes=[x_b])
            for dh in range(2):
                pY, pY_b = psY[ny % 2]
                t, t_b = ty[ny % 2]
                ny += 1
                for dc in range(8):
                    ph.op("pe", lambda e, pY=pY, dc=dc, tt=tt, dh=dh: e.matmul(
                        pY[:], mT[:, dc, tt * 128:(tt + 1) * 128], wo[:, dc, dh * 512:(dh + 1) * 512], start=(dc == 0),
                        stop=(dc == 7)), [mT_b, wo_b], [pY_b])
                ph.op("dve", lambda e, t=t, pY=pY, dh=dh: e.tensor_tensor(out=t[:], in0=pY[:],
                                                                          in1=gt[:, dh * 512:(dh + 1) * 512], op=ALU.mult),
                      [pY_b, gt_b], [t_b])
                ph.op("pool", lambda e, t=t, x=x, dh=dh: e.tensor_tensor(out=x[:, dh * 512:(dh + 1) * 512],
                                                                         in0=x[:, dh * 512:(dh + 1) * 512], in1=t[:],
                                                                         op=ALU.add), [t_b, x_b], [x_b])
            ph.dma("sp", xres[rows, :], x[:], x_b, reads=[x_b])
    ph.run()


TH = 2048
NTH = TH // 128


def phase_route(ctx, l, hh, xres, modbc, norm_g, rg_w, rg_b, re_w, re_b, s_hT, s_gates):
    ph = Phase(ctx, "rt")
    identb, identb_b = make_ident(ph)
    idf, idf_b = ph.sb("identf32", [128, 128], F32)
    ph.op("pool", lambda e: e.memset(idf[:], 1.0), [], [idf_b])
    ph.op("pool", lambda e: e.affine_select(out=idf[:], in_=idf[:], pattern=[[-1, 128]], compare_op=ALU.is_equal,
                                            fill=0.0, base=0, channel_multiplier=1), [idf_b], [idf_b])
    G, GS_b = ph.sb("G", [128, D], F32)
    S, _ = ph.sb("S", [128, D], F32)
    ng, _ = ph.sb("ng", [128, D], F32)
    ph.dma("sp", G[:], modbc[l, :, 4 * D:5 * D], GS_b, writes=[GS_b])
    ph.dma("sp", S[:], modbc[l, :, 3 * D:4 * D], GS_b, writes=[GS_b])
    ph.dma("sp", ng[:], bcast_rows(norm_g[l:l + 1, :], 128), GS_b, writes=[GS_b])
    ph.op("dve", lambda e: e.tensor_tensor(out=G[:], in0=G[:], in1=ng[:], op=ALU.mult), [GS_b], [GS_b])
    rw, rw_b = ph.sb("rw", [128, 8, 36], F32)
    rb, rb_b = ph.sb("rb", [128, 36], F32)
    ph.dma("sp", rw[:, :, 0:4], rg_w[l].rearrange("(k p) n -> p k n", p=128), rw_b, writes=[rw_b])
    ph.dma("sp", rw[:, :, 4:36], re_w[l].rearrange("(k p) n -> p k n", p=128), rw_b, writes=[rw_b])
    ph.dma("sp", rb[:, 0:4], bcast_rows(rg_b[l:l + 1, :], 128), rb_b, writes=[rb_b])
    ph.dma("sp", rb[:, 4:36], bcast_rows(re_b[l:l + 1, :], 128), rb_b, writes=[rb_b])
    hT, hT_b = ph.sb("hT", [128, 8, TH], BF16)
    gates, gates_b = ph.sb("gates", [128, NTH, 32], F32)
    xs = [ph.sb("x%d" % i, [128, D], F32) for i in range(2)]
    sq, sq_b = ph.sb("sq", [128, D], BF16)
    hs = [ph.sb("h%d" % i, [128, D], BF16) for i in range(2)]
    st = [ph.sb("st%d" % i, [128, 4], F32) for i in range(2)]
    h32T = [ph.sb("h32T%d" % i, [128, 8, 128], F32) for i in range(2)]
    lgs = [ph.sb("lg%d" % i, [128, 36], F32) for i in range(2)]
    rts = [ph.sb("r%d" % i, [128, 64], F32) for i in range(2)]
    pts = [ph.ps("pt%d" % i, [128, 8, 128], BF16) for i in range(2)]
    pfs = [ph.ps("pf%d" % i, [128, 4, 128], F32) for i in range(2)]
    pls = [ph.ps("pl%d" % i) for i in range(2)]
    for t in range(NTH):
        x, x_b = xs[t % 2]
        h, h_b = hs[t % 2]
        s, s_b = st[t % 2]
        pt, pt_b = pts[t % 2]
        hf, hf_b = h32T[t % 2]
        pl, pl_b = pls[t % 2]
        lg, lg_b = lgs[t % 2]
        r, r_b = rts[t % 2]
        row0 = hh * TH + t * 128
        ph.dma("sp", x[:], xres[row0:row0 + 128, :], x_b, writes=[x_b])
        ph.op("act", lambda e, x=x, s=s: e.activation(out=sq[:], in_=x[:], func=AF.Square, accum_out=s[:, 0:1]),
              [x_b], [sq_b, s_b])
        ph.op("dve", lambda e, s=s: e.tensor_scalar(out=s[:, 1:2], in0=s[:, 0:1], scalar1=1.0 / D, scalar2=EPS,
                                                    op0=ALU.mult, op1=ALU.add), [s_b], [s_b])
        ph.op("act", lambda e, s=s: e.activation(out=s[:, 2:3], in_=s[:, 1:2], func=AF.Sqrt), [s_b], [s_b])
        ph.op("dve", lambda e, s=s: e.reciprocal(out=s[:, 3:4], in_=s[:, 2:3]), [s_b], [s_b])
        ph.op("dve", lambda e, x=x, s=s: e.scalar_tensor_tensor(out=x[:], in0=x[:], scalar=s[:, 3:4], in1=G[:],
                                                                op0=ALU.mult, op1=ALU.mult), [x_b, s_b, GS_b], [x_b])
        ph.op("pool", lambda e, x=x: e.tensor_tensor(out=x[:], in0=x[:], in1=S[:], op=ALU.add), [x_b, GS_b], [x_b])
        ph.op("pool", lambda e, x=x, h=h: e.tensor_copy(out=h[:], in_=x[:]), [x_b], [h_b])
        for k in range(8):
            ph.op("pe", lambda e, k=k, h=h, pt=pt: e.transpose(pt[:, k, :], h[:, k * 128:(k + 1) * 128], identb[:]),
                  [h_b, identb_b], [pt_b])
        ph.op("act", lambda e, pt=pt, t=t: e.copy(out=hT[:, :, t * 128:(t + 1) * 128], in_=pt[:]), [pt_b], [hT_b])
        for half in range(2):
            pf, pf_b = pfs[half]
            for u in range(4):
                k = half * 4 + u
                ph.op("pe", lambda e, pf=pf, u=u, k=k, x=x: e.transpose(pf[:, u, :], x[:, k * 128:(k + 1) * 128], idf[:]),
                      [x_b, idf_b], [pf_b])
            ph.op("dve", lambda e, pf=pf, hf=hf, half=half: e.tensor_copy(out=hf[:, half * 4:(half + 1) * 4, :], in_=pf[:]),
                  [pf_b], [hf_b])
        for k in range(8):
            ph.op("pe", lambda e, pl=pl, hf=hf, k=k: e.matmul(pl[:, 0:36], hf[:, k, :], rw[:, k, :], start=(k == 0),
                                                             stop=(k == 7)), [hf_b, rw_b], [pl_b])
        ph.op("dve", lambda e, lg=lg, pl=pl: e.tensor_tensor(out=lg[:], in0=pl[:, 0:36], in1=rb[:], op=ALU.add),
              [pl_b, rb_b], [lg_b])
        def dv(fn, r=r, lg=lg):
            ph.op("dve", fn, [r_b, lg_b], [r_b])
        dv(lambda e, r=r, lg=lg: e.tensor_reduce(out=r[:, 0:1], in_=lg[:, 0:4], axis=AX.X, op=ALU.max))
        dv(lambda e, r=r, lg=lg: e.tensor_scalar(out=r[:, 16:20], in0=lg[:, 0:4], scalar1=r[:, 0:1], scalar2=None,
                                                 op0=ALU.is_equal))
        dv(lambda e, r=r: e.tensor_scalar(out=r[:, 1:2], in0=r[:, 0:1], scalar1=-1.0, scalar2=None, op0=ALU.mult))
        ph.op("act", lambda e, r=r, lg=lg: e.activation(out=r[:, 20:24], in_=lg[:, 0:4], func=AF.Exp, bias=r[:, 1:2]),
              [r_b, lg_b], [r_b])
        dv(lambda e, r=r: e.tensor_reduce(out=r[:, 2:3], in_=r[:, 20:24], axis=AX.X, op=ALU.add))
        dv(lambda e, r=r: e.reciprocal(out=r[:, 3:4], in_=r[:, 2:3]))
        dv(lambda e, r=r, lg=lg: e.tensor_scalar(out=r[:, 24:32], in0=lg[:, 4:12], scalar1=r[:, 16:17], scalar2=None,
                                                 op0=ALU.mult))
        for g in range(1, 4):
            dv(lambda e, r=r, lg=lg, g=g: e.scalar_tensor_tensor(out=r[:, 24:32], in0=lg[:, 4 + 8 * g:12 + 8 * g],
                                                                 scalar=r[:, 16 + g:17 + g], in1=r[:, 24:32],
                                                                 op0=ALU.mult, op1=ALU.add))
        dv(lambda e, r=r: e.tensor_reduce(out=r[:, 4:5], in_=r[:, 24:32], axis=AX.X, op=ALU.max))
        dv(lambda e, r=r: e.tensor_scalar(out=r[:, 32:40], in0=r[:, 24:32], scalar1=r[:, 4:5], scalar2=None,
                                          op0=ALU.is_equal))
        dv(lambda e, r=r: e.scalar_tensor_tensor(out=r[:, 40:48], in0=r[:, 32:40], scalar=-1e30, in1=r[:, 24:32],
                                                 op0=ALU.mult, op1=ALU.add))
        dv(lambda e, r=r: e.tensor_reduce(out=r[:, 5:6], in_=r[:, 40:48], axis=AX.X, op=ALU.max))
        dv(lambda e, r=r: e.tensor_scalar(out=r[:, 48:56], in0=r[:, 40:48], scalar1=r[:, 5:6], scalar2=None,
                                          op0=ALU.is_equal))
        dv(lambda e, r=r: e.tensor_tensor(out=r[:, 6:7], in0=r[:, 5:6], in1=r[:, 4:5], op=ALU.subtract))
        ph.op("act", lambda e, r=r: e.activation(out=r[:, 7:8], in_=r[:, 6:7], func=AF.Exp), [r_b], [r_b])
        dv(lambda e, r=r: e.tensor_scalar(out=r[:, 8:9], in0=r[:, 7:8], scalar1=1.0, scalar2=None, op0=ALU.add))
        dv(lambda e, r=r: e.reciprocal(out=r[:, 9:10], in_=r[:, 8:9]))
        dv(lambda e, r=r: e.tensor_tensor(out=r[:, 10:11], in0=r[:, 9:10], in1=r[:, 3:4], op=ALU.mult))
        dv(lambda e, r=r: e.tensor_tensor(out=r[:, 11:12], in0=r[:, 10:11], in1=r[:, 7:8], op=ALU.mult))
        dv(lambda e, r=r: e.tensor_scalar(out=r[:, 56:64], in0=r[:, 32:40], scalar1=r[:, 10:11], scalar2=None,
                                          op0=ALU.mult))
        dv(lambda e, r=r: e.scalar_tensor_tensor(out=r[:, 56:64], in0=r[:, 48:56], scalar=r[:, 11:12], in1=r[:, 56:64],
                                                 op0=ALU.mult, op1=ALU.add))
        for g in range(4):
            ph.op("dve", lambda e, r=r, g=g, t=t: e.tensor_scalar(out=gates[:, t, g * 8:(g + 1) * 8], in0=r[:, 56:64],
                                                                  scalar1=r[:, 16 + g:17 + g], scalar2=None, op0=ALU.mult),
                  [r_b], [gates_b])
    for k in range(8):
        ph.dma("sp", s_hT[:, k, :], hT[:, k, :], hT_b, reads=[hT_b])
    ph.dma("sp", s_gates.rearrange("(t p) e -> p t e", p=128), gates[:], gates_b, reads=[gates_b])
    ph.run()


def phase_experts(ctx, l, hh, xres, modbc, s_hT, s_gates, e_gate, e_up, e_down, n_exp=32):
    ph = Phase(ctx, "ex")
    hT, hT_b = ph.sb("hT", [128, 8, TH], BF16)
    gates, gates_b = ph.sb("gates", [128, NTH, 32], F32)
    gt, gt_b = ph.sb("gt", [128, D], F32)
    yacc, yacc_b = ph.sb("yacc", [128, NTH, D], F32)
    for k in range(8):
        ph.dma("sp", hT[:, k, :], s_hT[:, k, :], hT_b, writes=[hT_b])
    ph.dma("sp", gates[:], s_gates.rearrange("(t p) e -> p t e", p=128), gates_b, writes=[gates_b])
    ph.dma("sp", gt[:], modbc[l, :, 5 * D:6 * D], gt_b, writes=[gt_b])
    wg = [ph.sb("wg%d" % i, [128, 8, 512], BF16) for i in range(2)]
    wu = [ph.sb("wu%d" % i, [128, 8, 512], BF16) for i in range(2)]
    wd = [ph.sb("wd%d" % i, [128, 4, D], BF16) for i in range(2)]
    sgs = [ph.sb("sg%d" % i, [128, 512], F32) for i in range(2)]
    hgs = [ph.sb("hg%d" % i, [128, 4, 512], BF16) for i in range(2)]
    psG = [ph.ps("pG%d" % i) for i in range(2)]
    psU = [ph.ps("pU%d" % i) for i in range(2)]
    psY = [ph.ps("pY%d" % i) for i in range(4)]
    nf = 0
    ny = 0
    nh = 0
    for ex in range(n_exp):
        g_, g_b = wg[ex % 2]
        u_, u_b = wu[ex % 2]
        d_, d_b = wd[ex % 2]
        ph.dma("pool", g_[:], e_gate[l, ex].rearrange("(k p) f -> p k f", p=128), g_b, writes=[g_b])
        ph.dma("pool", u_[:], e_up[l, ex].rearrange("(k p) f -> p k f", p=128), u_b, writes=[u_b])
        ph.dma("pool", d_[:], e_down[l, ex].rearrange("(k p) f -> p k f", p=128), d_b, writes=[d_b])
        for tg in range(TH // 512):
            ts = slice(tg * 512, (tg + 1) * 512)
            hg, hg_b = hgs[nh % 2]
            nh += 1
            for fc in range(4):
                pG, pG_b = psG[nf % 2]
                pU, pU_b = psU[nf % 2]
                sg, sg_b = sgs[nf % 2]
                nf += 1
                for k in range(8):
                    ph.op("pe", lambda e, pG=pG, g_=g_, k=k, fc=fc, ts=ts: e.matmul(
                        pG[:], g_[:, k, fc * 128:(fc + 1) * 128], hT[:, k, ts], start=(k == 0), stop=(k == 7)),
                          [g_b, hT_b], [pG_b])
                for k in range(8):
                    ph.op("pe", lambda e, pU=pU, u_=u_, k=k, fc=fc, ts=ts: e.matmul(
                        pU[:], u_[:, k, fc * 128:(fc + 1) * 128], hT[:, k, ts], start=(k == 0), stop=(k == 7)),
                          [u_b, hT_b], [pU_b])
                ph.op("act", lambda e, sg=sg, pG=pG: e.activation(out=sg[:], in_=pG[:], func=AF.Silu), [pG_b], [sg_b])
                ph.op("dve", lambda e, hg=hg, fc=fc, sg=sg, pU=pU: e.tensor_tensor(out=hg[:, fc, :], in0=pU[:], in1=sg[:],
                                                                                  op=ALU.mult), [pU_b, sg_b], [hg_b])
            for tt in range(4):
                tile = tg * 4 + tt
                for dh in range(2):
                    pY, pY_b = psY[ny % 4]
                    ny += 1
                    for fc in range(4):
                        ph.op("pe", lambda e, pY=pY, hg=hg, fc=fc, tt=tt, d_=d_, dh=dh: e.matmul(
                            pY[:], hg[:, fc, tt * 128:(tt + 1) * 128], d_[:, fc, dh * 512:(dh + 1) * 512], start=(fc == 0),
                            stop=(fc == 3)), [hg_b, d_b], [pY_b])
                    ya = yacc[:, tile, dh * 512:(dh + 1) * 512]
                    if ex == 0:
                        ph.op("dve", lambda e, ya=ya, pY=pY, tile=tile, ex=ex: e.tensor_scalar(
                            out=ya, in0=pY[:], scalar1=gates[:, tile, ex:ex + 1], scalar2=None, op0=ALU.mult),
                              [pY_b, gates_b], [yacc_b])
                    else:
                        ph.op("dve", lambda e, ya=ya, pY=pY, tile=tile, ex=ex: e.scalar_tensor_tensor(
                            out=ya, in0=pY[:], scalar=gates[:, tile, ex:ex + 1], in1=ya, op0=ALU.mult, op1=ALU.add),
                              [pY_b, gates_b, yacc_b], [yacc_b])
    xs = [ph.sb("x%d" % i, [128, D], F32) for i in range(2)]
    for t in range(NTH):
        x, x_b = xs[t % 2]
        row0 = hh * TH + t * 128
        ph.dma("sp", x[:], xres[row0:row0 + 128, :], x_b, writes=[x_b])
        ph.op("pool", lambda e, t=t: e.tensor_tensor(out=yacc[:, t, :], in0=yacc[:, t, :], in1=gt[:], op=ALU.mult),
              [yacc_b, gt_b], [yacc_b])
        ph.op("pool", lambda e, x=x, t=t: e.tensor_tensor(out=x[:], in0=x[:], in1=yacc[:, t, :], op=ALU.add),
              [x_b, yacc_b], [x_b])
        ph.dma("sp", xres[row0:row0 + 128, :], x[:], x_b, reads=[x_b])
    ph.run()


def phase_final(ctx, xres, final_g, y):
    ph = Phase(ctx, "fin")
    G, G_b = ph.sb("G", [128, D], F32)
    ph.dma("sp", G[:], bcast_rows(final_g, 128), G_b, writes=[G_b])
    xs = [ph.sb("x%d" % i, [128, D], F32) for i in range(3)]
    sq, sq_b = ph.sb("sq", [128, D], BF16)
    st = [ph.sb("st%d" % i, [128, 4], F32) for i in range(2)]
    for t in range(NT):
        x, x_b = xs[t % 3]
        s, s_b = st[t % 2]
        ph.dma("sp", x[:], xres[t * 128:(t + 1) * 128, :], x_b, writes=[x_b])
        ph.op("act", lambda e, x=x, s=s: e.activation(out=sq[:], in_=x[:], func=AF.Square, accum_out=s[:, 0:1]),
              [x_b], [sq_b, s_b])
        ph.op("dve", lambda e, s=s: e.tensor_scalar(out=s[:, 1:2], in0=s[:, 0:1], scalar1=1.0 / D, scalar2=EPS,
                                                    op0=ALU.mult, op1=ALU.add), [s_b], [s_b])
        ph.op("act", lambda e, s=s: e.activation(out=s[:, 2:3], in_=s[:, 1:2], func=AF.Sqrt), [s_b], [s_b])
        ph.op("dve", lambda e, s=s: e.reciprocal(out=s[:, 3:4], in_=s[:, 2:3]), [s_b], [s_b])
        ph.op("dve", lambda e, x=x, s=s: e.scalar_tensor_tensor(out=x[:], in0=x[:], scalar=s[:, 3:4], in1=G[:],
                                                                op0=ALU.mult, op1=ALU.mult), [x_b, s_b, G_b], [x_b])
        ph.dma("sp", y[t * 128:(t + 1) * 128, :], x[:], x_b, reads=[x_b])
    ph.run()


def make_oh_tab():
    u = np.arange(384)
    rel = np.maximum(255 - u, 0)
    relf = np.maximum(rel, 1).astype(np.float32)
    large = 16 + (np.log(relf / np.float32(16)) / np.float32(math.log(8)) * np.float32(16)).astype(np.int32)
    large = np.minimum(large, 31)
    bucket = np.where(rel < 16, rel, large)
    oh = np.zeros((32, 384), np.float32)
    oh[bucket, u] = 1.0
    return oh


def make_ret_tables():
    pos = np.arange(T, dtype=np.float32)
    theta = (np.float32(1.0) / (np.float32(10000.0) ** np.linspace(0.0, 1.0, 128, dtype=np.float32))).astype(np.float32)
    theta = np.repeat(theta, 2)
    ang = (pos[:, None] * theta[None, :]).astype(np.float32)
    cos = np.cos(ang.astype(np.float64)).astype(np.float32)
    sin = np.sin(ang.astype(np.float64)).astype(np.float32)
    sinS = sin.copy()
    sinS[:, 0::2] = -sin[:, 0::2]
    p = np.arange(128, dtype=np.float64)
    dec = np.zeros((128, 8), np.float32)
    for h in range(4):
        gam = 1.0 - 2.0 ** (-5.0 - h)
        dec[:, h] = gam ** (p + 1)
        dec[:, 4 + h] = gam ** (-(p + 1)) / 16.0
    return cos, sinS, dec


def phase_copy(ctx, src, dst, nrows):
    ph = Phase(ctx, "cp")
    b = ph.buf("cpbuf")
    step = 512
    for r in range(0, nrows, step):
        ph.dma("sp", dst[r:r + step, :], src[r:r + step, :], b)
    ph.run()
```

```python
import contextlib
import math
import numpy as np
import concourse.bass as bass
import concourse.mybir as mybir
from concourse.bass_utils import run_bass_kernel_spmd

F32 = mybir.dt.float32
BF16 = mybir.dt.bfloat16
AF = mybir.ActivationFunctionType
ALU = mybir.AluOpType
AX = mybir.AxisListType

D = 1024
T = 4096
DEPTH = 4
NT = T // 128
N_IN = 16968
EPS = 1e-6
O_AQ, O_AK, O_AV = 0, 1024, 2048
O_IQ, O_IK, O_IW = 3072, 3584, 3648
O_BQ, O_BF, O_BI, O_BG = 3656, 4680, 5704, 6728
O_CQ, O_CK, O_CV, O_CG = 7752, 8776, 9800, 11848
O_GA, O_GB, O_GC = 13896, 14920, 15944

COMPUTE = ("pe", "act", "dve", "pool")


class Buf:
    __slots__ = ("name", "writers", "readers", "sem")

    def __init__(self, name):
        self.name = name
        self.writers = []
        self.readers = []
        self.sem = None


class Op:
    __slots__ = ("eng", "emit", "deps", "is_dma", "sem", "semval", "used", "sig")

    def __init__(self, eng, emit, is_dma):
        self.eng = eng
        self.emit = emit
        self.deps = []
        self.is_dma = is_dma
        self.sem = None
        self.semval = 0
        self.used = False
        self.sig = 0


class Ctx:
    def __init__(self, nc, stack):
        self.nc = nc
        self.eng_sem = {}
        self.eng_cnt = {}
        for e in COMPUTE:
            self.eng_sem[e] = stack.enter_context(nc.semaphore("es_" + e))
            self.eng_cnt[e] = 0
        self.pool = []
        for i in range(56):
            self.pool.append([stack.enter_context(nc.semaphore("ds%d" % i)), 0])
        self.nphase = 0


class Phase:
    def __init__(self, ctx, name):
        self.ctx = ctx
        self.nc = ctx.nc
        self.name = "%s_%d" % (name, ctx.nphase)
        ctx.nphase += 1
        self.ops = []
        self.stack = contextlib.ExitStack()
        self.sems = []
        self.nbuf = 0

    def sb(self, name, shape, dtype):
        t = self.stack.enter_context(self.nc.sbuf_tensor("%s_%s" % (self.name, name), list(shape), dtype))
        return t, Buf(name)

    def ps(self, name, shape=(128, 512), dtype=F32):
        t = self.stack.enter_context(self.nc.psum_tensor("%s_%s" % (self.name, name), list(shape), dtype))
        return t, Buf(name)

    def buf(self, name):
        return Buf(name)

    def _sem_for(self, b):
        if b.sem is None:
            b.sem = self.ctx.pool.pop()
            self.sems.append(b.sem)
        return b.sem

    def _record(self, op, reads, writes):
        deps = []
        for b in reads:
            deps.extend(b.writers)
        for b in writes:
            keep = []
            for w in b.writers:
                if op.is_dma and w.is_dma and w.sem is op.sem:
                    keep.append(w)
                else:
                    deps.append(w)
            for r in b.readers:
                deps.append(r)
            b.writers = keep + [op]
            b.readers = []
        for b in reads:
            b.readers.append(op)
        seen = set()
        for d in deps:
            if (not d.is_dma) and d.eng == "pe" and op.eng == "pe":
                continue
            if id(d) not in seen and d is not op:
                seen.add(id(d))
                d.used = True
                op.deps.append(d)
        self.ops.append(op)

    def op(self, eng, emit, reads=(), writes=()):
        o = Op(eng, emit, False)
        self._record(o, reads, writes)
        return o

    def dma(self, eng, out, in_, sbuf, reads=(), writes=(), **kw):
        o = Op(eng, None, True)
        o.sem = self._sem_for(sbuf)
        o.sem[1] += 16
        o.semval = o.sem[1]
        o.emit = lambda e: e.dma_start(out=out, in_=in_, **kw)
        self._record(o, reads, writes)
        return o

    def run(self):
        ctx = self.ctx
        nc = self.nc
        for e in COMPUTE:
            for o in self.ops:
                if o.eng == e and not o.is_dma and o.used:
                    ctx.eng_cnt[e] += 1
                    o.sig = ctx.eng_cnt[e]
        engs = {"pe": [], "act": [], "dve": [], "pool": [], "sp": []}
        for o in self.ops:
            engs[o.eng].append(o)
        final_waits = [(s[0], s[1]) for s in self.sems]

        def emit_engine(e, name):
            waited = {}
            for o in engs[name]:
                need = {}
                for d in o.deps:
                    if d.is_dma:
                        key, val = d.sem[0], d.semval
                    else:
                        if d.eng == name and name == "pe":
                            continue
                        key, val = ctx.eng_sem[d.eng], d.sig
                    if need.get(key, 0) < val:
                        need[key] = val
                for key, val in need.items():
                    if waited.get(key, 0) < val:
                        e.wait_ge(key, val)
                        waited[key] = val
                ins = o.emit(e)
                if o.is_dma:
                    ins.then_inc(o.sem[0], 16)
                elif o.used:
                    ins.then_inc(ctx.eng_sem[name], 1)
            if name == "sp":
                for s, v in final_waits:
                    e.wait_ge(s, v)

        with nc.Block() as blk:
            blk.tensor(lambda e: emit_engine(e, "pe"))
            blk.scalar(lambda e: emit_engine(e, "act"))
            blk.vector(lambda e: emit_engine(e, "dve"))
            blk.gpsimd(lambda e: emit_engine(e, "pool"))
            blk.sync(lambda e: emit_engine(e, "sp"))
        for s in self.sems:
            ctx.pool.append(s)
        self.stack.close()


def bcast_rows(ap2d, nrows):
    return bass.AP(ap2d.tensor, ap2d.offset, [[0, nrows], [1, ap2d.shape[-1]]])


def phase_mod(ctx, c, ada_w, ada_b, modbc):
    nc = ctx.nc
    ph = Phase(ctx, "mod")
    cT, cT_b = ph.sb("cT", [128, 8], F32)
    cs, cs_b = ph.sb("cs", [128, 8], F32)
    crep, crep_b = ph.sb("crep", [128, 8, 128], F32)
    wts = [ph.sb("w%d" % i, [128, 8, 512], F32) for i in range(2)]
    bia, bia_b = ph.sb("bias", [128, 6144], F32)
    outs = [ph.sb("o%d" % i, [128, 512], F32) for i in range(2)]
    pss = [ph.ps("ps%d" % i) for i in range(2)]
    ph.dma("sp", cT[:], c.rearrange("o (k p) -> p (o k)", p=128), cT_b, writes=[cT_b],
           allow_slow_non_contiguous=True)
    ph.op("act", lambda e: e.activation(out=cs[:], in_=cT[:], func=AF.Silu), [cT_b], [cs_b])
    ph.op("dve", lambda e: e.tensor_copy(out=crep[:], in_=cs[:].unsqueeze(2).broadcast_to([128, 8, 128])),
          [cs_b], [crep_b])
    n = 0
    for l in range(DEPTH):
        ph.dma("sp", bia[:], bcast_rows(ada_b[l:l + 1, :], 128), bia_b, writes=[bia_b])
        for seg in (1, 4):
            ph.op("pool", lambda e, seg=seg: e.tensor_scalar_add(out=bia[:, seg * 1024:(seg + 1) * 1024],
                                                                 in0=bia[:, seg * 1024:(seg + 1) * 1024], scalar1=1.0),
                  [bia_b], [bia_b])
        for j in range(12):
            w, w_b = wts[n % 2]
            o, o_b = outs[n % 2]
            p, p_b = pss[n % 2]
            n += 1
            ph.dma("sp", w[:], ada_w[l, :, j * 512:(j + 1) * 512].rearrange("(k p) n -> p k n", p=128), w_b,
                   writes=[w_b])
            for k in range(8):
                ph.op("pe", lambda e, k=k, w=w, p=p: e.matmul(p[:], crep[:, k, :], w[:, k, :], start=(k == 0),
                                                               stop=(k == 7)),
                      [crep_b, w_b], [p_b])
            ph.op("dve", lambda e, o=o, p=p, j=j: e.tensor_tensor(out=o[:], in0=p[:], in1=bia[:, j * 512:(j + 1) * 512],
                                                                   op=ALU.add), [p_b, bia_b], [o_b])
            ph.dma("sp", modbc[l, :, j * 512:(j + 1) * 512], o[:], o_b, reads=[o_b])
    ph.run()


def emit_norm_tiles(ph, xsrc, hT, hT_b, G, S, GS_b, ident, ident_b, ntiles, h32T=None, h32T_b=None):
    xs = [ph.sb("x%d" % i, [128, D], F32) for i in range(2)]
    sq, sq_b = ph.sb("sq", [128, D], BF16)
    hs = [ph.sb("h%d" % i, [128, D], BF16) for i in range(2)]
    st = [ph.sb("st%d" % i, [128, 4], F32) for i in range(2)]
    pts = [ph.ps("pt%d" % i, [128, 8, 128], BF16) for i in range(2)]
    for t in range(ntiles):
        x, x_b = xs[t % 2]
        h, h_b = hs[t % 2]
        s, s_b = st[t % 2]
        pt, pt_b = pts[t % 2]
        ph.dma("sp", x[:], xsrc[t * 128:(t + 1) * 128, :], x_b, writes=[x_b])
        ph.op("act", lambda e, x=x, s=s: e.activation(out=sq[:], in_=x[:], func=AF.Square, accum_out=s[:, 0:1]),
              [x_b], [sq_b, s_b])
        ph.op("dve", lambda e, s=s: e.tensor_scalar(out=s[:, 1:2], in0=s[:, 0:1], scalar1=1.0 / D, scalar2=EPS,
                                                    op0=ALU.mult, op1=ALU.add), [s_b], [s_b])
        ph.op("act", lambda e, s=s: e.activation(out=s[:, 2:3], in_=s[:, 1:2], func=AF.Sqrt), [s_b], [s_b])
        ph.op("dve", lambda e, s=s: e.reciprocal(out=s[:, 3:4], in_=s[:, 2:3]), [s_b], [s_b])
        ph.op("dve", lambda e, x=x, s=s: e.scalar_tensor_tensor(out=x[:], in0=x[:], scalar=s[:, 3:4], in1=G[:],
                                                                op0=ALU.mult, op1=ALU.mult), [x_b, s_b, GS_b], [x_b])
        if S is not None:
            ph.op("pool", lambda e, x=x, h=h: e.tensor_tensor(out=h[:], in0=x[:], in1=S[:], op=ALU.add),
                  [x_b, GS_b], [h_b])
        else:
            ph.op("pool", lambda e, x=x, h=h: e.tensor_copy(out=h[:], in_=x[:]), [x_b], [h_b])
        for k in range(8):
            ph.op("pe", lambda e, k=k, h=h, pt=pt: e.transpose(pt[:, k, :], h[:, k * 128:(k + 1) * 128], ident[:]),
                  [h_b, ident_b], [pt_b])
        ph.op("act", lambda e, pt=pt, t=t: e.copy(out=hT[:, :, t * 128:(t + 1) * 128], in_=pt[:]), [pt_b], [hT_b])


def make_ident(ph, dtype=BF16):
    nc = ph.nc
    idf, idf_b = ph.sb("identf", [128, 128], F32)
    ident, ident_b = ph.sb("ident", [128, 128], dtype)
    ph.op("pool", lambda e: e.memset(idf[:], 1.0), [], [idf_b])
    ph.op("pool", lambda e: e.affine_select(out=idf[:], in_=idf[:], pattern=[[-1, 128]], compare_op=ALU.is_equal,
                                            fill=0.0, base=0, channel_multiplier=1), [idf_b], [idf_b])
    ph.op("pool", lambda e: e.tensor_copy(out=ident[:], in_=idf[:]), [idf_b], [ident_b])
    return ident, ident_b


def phase_proj(ctx, xres, modbc, l, norm_g, w_in_l, jobs, seg_scale, seg_shift):
    nc = ctx.nc
    ph = Phase(ctx, "proj")
    ident, ident_b = make_ident(ph)
    G, GS_b = ph.sb("G", [128, D], F32)
    S, _ = ph.sb("S", [128, D], F32)
    ng, _ = ph.sb("ng", [128, D], F32)
    hT, hT_b = ph.sb("hT", [128, 8, T], BF16)
    ph.dma("sp", G[:], modbc[l, :, seg_scale * D:(seg_scale + 1) * D], GS_b, writes=[GS_b])
    ph.dma("sp", S[:], modbc[l, :, seg_shift * D:(seg_shift + 1) * D], GS_b, writes=[GS_b])
    ph.dma("sp", ng[:], bcast_rows(norm_g[l:l + 1, :], 128), GS_b, writes=[GS_b])
    ph.op("dve", lambda e: e.tensor_tensor(out=G[:], in0=G[:], in1=ng[:], op=ALU.mult), [GS_b], [GS_b])
    emit_norm_tiles(ph, xres, hT, hT_b, G, S, GS_b, ident, ident_b, NT)

    wts = [ph.sb("w%d" % i, [128, 8, 512], BF16) for i in range(3)]
    pss = [ph.ps("pp%d" % i) for i in range(4)]
    stg = [ph.sb("sg%d" % i, [128, 512], F32) for i in range(4)]
    stgb = [ph.sb("sgb%d" % i, [128, 512], BF16) for i in range(4)]
    nw = 0
    ne = 0
    for (c0, ncols, kind, dst_fn, dtype, func) in jobs:
        step = 512 if kind == "tok" else 128
        for cc in range(c0, c0 + ncols, step):
            n = min(step, c0 + ncols - cc)
            w, w_b = wts[nw % 3]
            nw += 1
            ph.dma("pool", w[:, :, 0:n], w_in_l[:, cc:cc + n].rearrange("(k p) n -> p k n", p=128), w_b,
                   writes=[w_b])
            nchunk = NT if kind == "tok" else T // 512
            for ci in range(nchunk):
                p, p_b = pss[ne % 4]
                if dtype == F32:
                    sg, sg_b = stg[ne % 4]
                else:
                    sg, sg_b = stgb[ne % 4]
                evac_eng = "act" if (func is not None or ne % 2 == 0) else "dve"
                ne += 1
                if kind == "tok":
                    for k in range(8):
                        ph.op("pe", lambda e, k=k, w=w, p=p, ci=ci, n=n: e.matmul(
                            p[:, 0:n], hT[:, k, ci * 128:(ci + 1) * 128], w[:, k, 0:n], start=(k == 0), stop=(k == 7)),
                              [hT_b, w_b], [p_b])
                    po, so = p[:, 0:n], sg[:, 0:n]
                    dst = dst_fn(ci * 128, cc - c0, n)
                else:
                    for k in range(8):
                        ph.op("pe", lambda e, k=k, w=w, p=p, ci=ci, n=n: e.matmul(
                            p[0:n, :], w[:, k, 0:n], hT[:, k, ci * 512:(ci + 1) * 512], start=(k == 0), stop=(k == 7)),
                              [hT_b, w_b], [p_b])
                    po, so = p[0:n, :], sg[0:n, :]
                    dst = dst_fn(cc - c0, n, ci * 512)
                if evac_eng == "act":
                    f = func if func is not None else AF.Copy
                    ph.op("act", lambda e, po=po, so=so, f=f: e.activation(out=so, in_=po, func=f), [p_b], [sg_b])
                else:
                    ph.op("dve", lambda e, po=po, so=so: e.tensor_copy(out=so, in_=po), [p_b], [sg_b])
                ph.dma("sp", dst, so, sg_b, reads=[sg_b])
    ph.run()


INPUT_SPECS = [
    ("x", [T, D]), ("c", [1, D]), ("rel_bias", [32, 8]), ("hgrn_lb_raw", [DEPTH, D]), ("norm1_g", [DEPTH, D]),
    ("norm2_g", [DEPTH, D]), ("ada_w", [DEPTH, D, 6 * D]), ("ada_b", [DEPTH, 6 * D]), ("w_in", [DEPTH, D, N_IN]),
    ("hgrn_norm_g", [DEPTH, 128]), ("w_branch_a", [DEPTH, 1024, D]), ("w_branch_b", [DEPTH, 1024, D]),
    ("w_branch_c", [DEPTH, 2048, D]), ("w_out", [DEPTH, D, D]), ("router_group_w", [DEPTH, D, 4]),
    ("router_group_b", [DEPTH, 4]), ("router_expert_w", [DEPTH, D, 32]), ("router_expert_b", [DEPTH, 32]),
    ("expert_w_gate", [DEPTH, 32, D, 512]), ("expert_w_up", [DEPTH, 32, D, 512]), ("expert_w_down", [DEPTH, 32, 512, D]),
    ("final_norm_g", [1, D]), ("oh_tab", [32, 384]), ("cosT", [T, 256]), ("sinS", [T, 256]), ("cdec", [128, 8]),
]


def build_program(depth=DEPTH, debug=False):
    nc = bass.Bass("TRN2", target_bir_lowering=False)
    I = {}
    for name, shape in INPUT_SPECS:
        I[name] = nc.dram_tensor(name, shape, F32, kind="ExternalInput").ap()
    y = nc.dram_tensor("y", [T, D], F32, kind="ExternalOutput").ap()

    def scr(name, shape, dt):
        return nc.dram_tensor(name, shape, dt, kind="ExternalOutput" if debug else "Internal").ap()

    xres = scr("xres", [T, D], F32)
    modbc = scr("modbc", [DEPTH, 128, 6 * D], F32)
    s_aqT = scr("s_aqT", [1024, T], BF16)
    s_akT = scr("s_akT", [1024, T], BF16)
    s_av = scr("s_av", [T, 1024], BF16)
    s_iqT = scr("s_iqT", [512, T], BF16)
    s_ikT = scr("s_ikT", [64, T], BF16)
    s_iw = scr("s_iw", [T, 8], F32)
    s_bqT = scr("s_bqT", [1024, T], BF16)
    s_bfT = scr("s_bfT", [1024, T], F32)
    s_bi = scr("s_bi", [T, 1024], BF16)
    s_bg = scr("s_bg", [T, 1024], BF16)
    s_cq = scr("s_cq", [T, 1024], F32)
    s_ck = scr("s_ck", [T, 1024], F32)
    s_cv = scr("s_cv", [T, 2048], BF16)
    s_cg = scr("s_cg", [T, 2048], BF16)
    s_gT = scr("s_gT", [3072, T], BF16)
    s_mT = scr("s_mT", [NT, 128, NT, 128], BF16)
    s_tab = scr("s_tab", [128, 3072], BF16)
    s_oa = scr("s_oa", [T, 1024], BF16)
    s_ob = scr("s_ob", [T, 1024], BF16)
    s_oc = scr("s_oc", [T, 2048], BF16)
    s_hT = scr("s_hT", [128, 8, TH], BF16)
    s_gates = scr("s_gates", [T, 32], F32)

    def tok(dst):
        return lambda t0, c0, n: dst[t0:t0 + 128, c0:c0 + n]

    def feat(dst):
        return lambda c0, n, t0: dst[c0:c0 + n, t0:t0 + 512]

    jobs = [
        (O_AQ, 1024, "feat", feat(s_aqT), BF16, None),
        (O_AK, 1024, "feat", feat(s_akT), BF16, None),
        (O_AV, 1024, "tok", tok(s_av), BF16, None),
        (O_IQ, 512, "feat", feat(s_iqT), BF16, None),
        (O_IK, 64, "feat", feat(s_ikT), BF16, None),
        (O_IW, 8, "tok", tok(s_iw), F32, None),
        (O_BQ, 1024, "feat", feat(s_bqT), BF16, None),
        (O_BF, 1024, "feat", feat(s_bfT), F32, None),
        (O_BI, 1024, "tok", tok(s_bi), BF16, None),
        (O_BG, 1024, "tok", tok(s_bg), BF16, AF.Silu),
        (O_CQ, 1024, "tok", tok(s_cq), F32, None),
        (O_CK, 1024, "tok", tok(s_ck), F32, None),
        (O_CV, 2048, "tok", tok(s_cv), BF16, None),
        (O_CG, 2048, "tok", tok(s_cg), BF16, AF.Silu),
        (O_GA, 3072, "feat", feat(s_gT), BF16, AF.Sigmoid),
    ]
    with contextlib.ExitStack() as stack:
        ctx = Ctx(nc, stack)
        phase_copy(ctx, I["x"], xres, T)
        phase_mod(ctx, I["c"], I["ada_w"], I["ada_b"], modbc)
        phase_bias_tab(ctx, I["rel_bias"], I["oh_tab"], s_tab)
        for l in range(depth):
            phase_proj(ctx, xres, modbc, l, I["norm1_g"], I["w_in"][l], jobs, 1, 0)
            phase_a1(ctx, s_iqT, s_ikT, s_iw, s_mT)
            for hg in range(2):
                phase_a2(ctx, hg, s_aqT, s_akT, s_av, s_mT, s_tab, s_oa)
            phase_b(ctx, l, I["hgrn_lb_raw"], s_bqT, s_bfT, s_bi, s_bg, I["hgrn_norm_g"], s_ob)
            phase_c(ctx, s_cq, s_ck, s_cv, s_cg, I["cosT"], I["sinS"], I["cdec"], s_oc)
            phase_merge(ctx, l, modbc, s_oa, s_ob, s_oc, s_gT, I["w_branch_a"], I["w_branch_b"], I["w_branch_c"],
                        I["w_out"], xres)
            for hh in range(2):
                sg = s_gates[hh * TH:(hh + 1) * TH, :]
                phase_route(ctx, l, hh, xres, modbc, I["norm2_g"], I["router_group_w"], I["router_group_b"],
                            I["router_expert_w"], I["router_expert_b"], s_hT, sg)
                phase_experts(ctx, l, hh, xres, modbc, s_hT, sg, I["expert_w_gate"], I["expert_w_up"], I["expert_w_down"])
        phase_final(ctx, xres, I["final_norm_g"], y)
    return nc


_PROGRAM = None


def kernel(**inputs):
    global _PROGRAM
    if _PROGRAM is None:
        _PROGRAM = build_program()
    nc = _PROGRAM
    f = lambda a: np.ascontiguousarray(np.asarray(a, dtype=np.float32))
    cos, sinS, dec = make_ret_tables()
    shared = {k: f(inputs[k]) for k in ("rel_bias", "hgrn_lb_raw", "norm1_g", "norm2_g", "ada_w", "ada_b", "w_in",
                                        "hgrn_norm_g", "w_branch_a", "w_branch_b", "w_branch_c", "w_out",
                                        "router_group_w", "router_group_b", "router_expert_w", "router_expert_b",
                                        "expert_w_gate", "expert_w_up", "expert_w_down")}
    shared["final_norm_g"] = f(inputs["final_norm_g"]).reshape(1, D)
    shared["oh_tab"] = make_oh_tab()
    shared["cosT"] = cos
    shared["sinS"] = sinS
    shared["cdec"] = dec
    x = f(inputs["x"])
    c = f(inputs["c"])
    active = {0: 0, 1: 1, 4: 2, 5: 3}
    idle = {k: np.zeros_like(v) for k, v in shared.items()}
    idle["x"] = np.zeros_like(x[0])
    idle["c"] = np.zeros_like(c[0:1])
    in_maps = []
    for core in range(8):
        if core in active:
            b = active[core]
            m = dict(shared)
            m["x"] = x[b]
            m["c"] = c[b:b + 1]
        else:
            m = idle
        in_maps.append(m)
    res = run_bass_kernel_spmd(nc, in_maps, core_ids=list(range(8)))
    core_of = {b: core for core, b in active.items()}
    out = np.stack([np.asarray(res.results[core_of[b]]["y"], dtype=np.float32) for b in range(4)], axis=0)
    return out


TOPK = 256
NBIS = 16
MASKV = -30000.0


def phase_a1(ctx, s_iqT, s_ikT, s_iw, s_mT):
    ph = Phase(ctx, "a1")
    ident, ident_b = make_ident(ph)
    ikT, ikT_b = ph.sb("ikT", [64, T], BF16)
    ph.dma("sp", ikT[:], s_ikT[:, :], ikT_b, writes=[ikT_b])
    pw, pw_b = ph.sb("pw", [128, NBIS], F32)
    for k in range(NBIS):
        ph.op("pool", lambda e, k=k: e.memset(pw[:, k:k + 1], 0.5 ** (k + 1)), [], [pw_b])
    cm, cm_b = ph.sb("cm", [128, 128], F32)
    ph.op("pool", lambda e: e.memset(cm[:], 0.0), [], [cm_b])
    ph.op("pool", lambda e: e.affine_select(out=cm[:], in_=cm[:], pattern=[[-1, 128]], compare_op=ALU.is_ge, fill=-1e30,
                                            base=0, channel_multiplier=1), [cm_b], [cm_b])
    iqs = [ph.sb("iq%d" % i, [64, 8, 128], BF16) for i in range(4)]
    ws = [ph.sb("w%d" % i, [128, 24], F32) for i in range(4)]
    Is = [ph.sb("I%d" % i, [128, T], F32) for i in range(4)]
    rls = [ph.sb("rl%d" % i, [128, 512], F32) for i in range(4)]
    junk, junk_b = ph.sb("junk", [128, T], BF16)
    junk2, junk2_b = ph.sb("junk2", [128, T], BF16)
    m01s = [ph.sb("m01_%d" % i, [128, T], BF16) for i in range(2)]
    mTs = [ph.sb("mT%d" % i, [128, NT, 128], BF16) for i in range(2)]
    sts = [ph.sb("st%d" % i, [128, 8 + NBIS], F32) for i in range(4)]
    pss = [ph.ps("ps%d" % i) for i in range(4)]
    ptr = [ph.ps("ptr%d" % i, [128, 4, 128], BF16) for i in range(2)]
    cnt = {"pz": 0, "rl": 0, "tr": 0}

    def gen_indexer(i):
        S = (i + 1) * 128
        iq, iq_b = iqs[i % 4]
        w, w_b = ws[i % 4]
        I, I_b = Is[i % 4]
        ph.dma("sp", iq[:], s_iqT[:, i * 128:(i + 1) * 128].rearrange("(h d) t -> d h t", d=64), iq_b, writes=[iq_b])
        ph.dma("sp", w[:, 0:8], s_iw[i * 128:(i + 1) * 128, :], w_b, writes=[w_b])
        ph.op("pool", lambda e: e.tensor_scalar(out=w[:, 0:8], in0=w[:, 0:8], scalar1=512.0 ** -0.5, scalar2=None,
                                                op0=ALU.mult), [w_b], [w_b])
        ph.op("pool", lambda e: e.tensor_scalar(out=w[:, 8:16], in0=w[:, 0:8], scalar1=-1.0, scalar2=None, op0=ALU.mult),
              [w_b], [w_b])
        ph.op("dve", lambda e: e.tensor_tensor(out=w[:, 8:16], in0=w[:, 8:16], in1=w[:, 0:8], op=ALU.max), [w_b], [w_b])
        ph.op("pool", lambda e: e.tensor_scalar(out=w[:, 16:24], in0=w[:, 0:8], scalar1=0.0, scalar2=2.0, op0=ALU.is_ge,
                                                op1=ALU.mult), [w_b], [w_b])
        ph.op("pool", lambda e: e.tensor_scalar(out=w[:, 16:24], in0=w[:, 16:24], scalar1=-1.0, scalar2=None, op0=ALU.add),
              [w_b], [w_b])
        yield
        for c0 in range(0, S, 512):
            n = min(512, S - c0)
            for h in range(8):
                p, p_b = pss[cnt["pz"] % 4]
                cnt["pz"] += 1
                rl, rl_b = rls[cnt["rl"] % 4]
                cnt["rl"] += 1
                ph.op("pe", lambda e, p=p, h=h, c0=c0, n=n: e.matmul(p[:, 0:n], iq[:, h, :], ikT[:, c0:c0 + n], start=True, stop=True),
                      [iq_b, ikT_b], [p_b])
                ph.op("act", lambda e, p=p, rl=rl, h=h, n=n: e.activation(out=rl[:, 0:n], in_=p[:, 0:n], func=AF.Relu,
                                                                     scale=w[:, 8 + h:9 + h]), [p_b, w_b], [rl_b])
                if h == 0:
                    ph.op("dve", lambda e, rl=rl, h=h, c0=c0, n=n: e.tensor_scalar(out=I[:, c0:c0 + n], in0=rl[:, 0:n],
                                                                       scalar1=w[:, 16 + h:17 + h], scalar2=None,
                                                                       op0=ALU.mult), [rl_b, w_b], [I_b])
                else:
                    ph.op("dve", lambda e, rl=rl, h=h, c0=c0, n=n: e.scalar_tensor_tensor(out=I[:, c0:c0 + n], in0=rl[:, 0:n],
                                                                              scalar=w[:, 16 + h:17 + h],
                                                                              in1=I[:, c0:c0 + n], op0=ALU.mult,
                                                                              op1=ALU.add), [rl_b, w_b, I_b], [I_b])
                if h % 2 == 1:
                    yield
        ph.op("pool", lambda e: e.tensor_tensor(out=I[:, i * 128:(i + 1) * 128], in0=I[:, i * 128:(i + 1) * 128],
                                                in1=cm[:], op=ALU.add), [I_b, cm_b], [I_b])
        yield

    def gen_select(i):
        S = (i + 1) * 128
        I, I_b = Is[i % 4]
        m01, m01_b = m01s[i % 2]
        mT, mT_b = mTs[i % 2]
        st, st_b = sts[i % 4]
        if i < 2:
            ph.op("dve", lambda e: e.memset(st[:, 0:1], -1e29), [], [st_b])
        else:
            on_act = (i % 2 == 1)
            ph.op("dve", lambda e: e.tensor_reduce(out=st[:, 0:1], in_=I[:, 0:i * 128], axis=AX.X, op=ALU.min),
                  [I_b], [st_b])
            ph.op("dve", lambda e: e.tensor_reduce(out=st[:, 1:2], in_=I[:, 0:S], axis=AX.X, op=ALU.max), [I_b], [st_b])
            ph.op("dve", lambda e: e.tensor_tensor(out=st[:, 2:3], in0=st[:, 1:2], in1=st[:, 0:1], op=ALU.subtract),
                  [st_b], [st_b])
            ph.op("dve", lambda e: e.tensor_scalar(out=st[:, 8:8 + NBIS], in0=pw[:], scalar1=st[:, 2:3], scalar2=None,
                                                   op0=ALU.mult), [st_b, pw_b], [st_b])
            yield
            for k in range(NBIS):
                if on_act:
                    ph.op("dve", lambda e, k=k: e.tensor_scalar(out=st[:, 3:4], in0=st[:, 0:1], scalar1=st[:, 8 + k:9 + k],
                                                                scalar2=-1.0, op0=ALU.add, op1=ALU.mult), [st_b], [st_b])
                    ph.op("act", lambda e: e.activation(out=junk2[:, 0:S], in_=I[:, 0:S], func=AF.Sign, bias=st[:, 3:4],
                                                        accum_out=st[:, 4:5]), [I_b, st_b], [junk2_b, st_b])
                    thresh = 2.0 * (TOPK - 0.5) - S
                else:
                    ph.op("dve", lambda e, k=k: e.tensor_tensor(out=st[:, 3:4], in0=st[:, 0:1], in1=st[:, 8 + k:9 + k],
                                                                op=ALU.add), [st_b], [st_b])
                    ph.op("dve", lambda e: e.tensor_scalar(out=junk[:, 0:S], in0=I[:, 0:S], scalar1=st[:, 3:4],
                                                           scalar2=None, op0=ALU.is_ge, op1=ALU.add,
                                                           accum_out=st[:, 4:5]), [I_b, st_b], [junk_b, st_b])
                    thresh = TOPK - 0.5
                ph.op("dve", lambda e, k=k, thresh=thresh: e.scalar_tensor_tensor(
                    out=st[:, 5:6], in0=st[:, 4:5], scalar=thresh, in1=st[:, 8 + k:9 + k], op0=ALU.is_ge, op1=ALU.mult),
                      [st_b], [st_b])
                ph.op("dve", lambda e: e.tensor_tensor(out=st[:, 0:1], in0=st[:, 0:1], in1=st[:, 5:6], op=ALU.add),
                      [st_b], [st_b])
                yield
        ph.op("dve", lambda e: e.tensor_scalar(out=m01[:, 0:S], in0=I[:, 0:S], scalar1=st[:, 0:1], scalar2=None,
                                               op0=ALU.is_ge), [I_b, st_b], [m01_b])
        yield
        for j0 in range(0, i + 1, 4):
            nj = min(4, i + 1 - j0)
            pt, pt_b = ptr[cnt["tr"] % 2]
            cnt["tr"] += 1
            for u in range(nj):
                j = j0 + u
                ph.op("pe", lambda e, pt=pt, u=u, j=j: e.transpose(pt[:, u, :], m01[:, j * 128:(j + 1) * 128], ident[:]),
                      [m01_b, ident_b], [pt_b])
            ph.op("dve", lambda e, pt=pt, j0=j0, nj=nj: e.tensor_copy(out=mT[:, j0:j0 + nj, :], in_=pt[:, 0:nj, :]),
                  [pt_b], [mT_b])
            yield
        ph.dma("sp", s_mT[i, :, 0:i + 1, :], mT[:, 0:i + 1, :], mT_b, reads=[mT_b])

    def merge(gens):
        state = [[g, max(n, 1), 0, False] for g, n in gens]
        while any(not st_[3] for st_ in state):
            best = None
            for st_ in state:
                if st_[3]:
                    continue
                frac = st_[2] / st_[1]
                if best is None or frac < best[0]:
                    best = (frac, st_)
            st_ = best[1]
            try:
                next(st_[0])
                st_[2] += 1
            except StopIteration:
                st_[3] = True

    def n_sel(i):
        return (NBIS + 4 if i >= 2 else 3) + (i + 4) // 4

    def n_idx(i):
        return 2 + 4 * (((i + 1) * 128 + 511) // 512)

    merge([(gen_indexer(0), n_idx(0)), (gen_indexer(1), n_idx(1))])
    for m in range(NT // 2):
        gens = [(gen_select(2 * m), n_sel(2 * m)), (gen_select(2 * m + 1), n_sel(2 * m + 1))]
        if 2 * m + 2 < NT:
            gens.append((gen_indexer(2 * m + 2), n_idx(2 * m + 2)))
            gens.append((gen_indexer(2 * m + 3), n_idx(2 * m + 3)))
        merge(gens)
    ph.run()


def phase_bias_tab(ctx, rel_bias, oh_tab, s_tab):
    ph = Phase(ctx, "btab")
    rb, rb_b = ph.sb("rb", [32, 8], F32)
    r31, r31_b = ph.sb("r31", [32, 8], F32)
    rep, rep_b = ph.sb("rep", [32, 8, 128], F32)
    oh, oh_b = ph.sb("oh", [32, 384], F32)
    tb, tb_b = ph.sb("tb", [128, 8, 384], BF16)
    pss = [ph.ps("p%d" % i) for i in range(2)]
    ph.dma("sp", rb[:], rel_bias[:, :], rb_b, writes=[rb_b])
    ph.dma("sp", r31[:], bcast_rows(rel_bias[31:32, :], 32), r31_b, writes=[r31_b])
    ph.dma("sp", oh[:], oh_tab[:, :], oh_b, writes=[oh_b])
    ph.op("dve", lambda e: e.tensor_tensor(out=rb[:], in0=rb[:], in1=r31[:], op=ALU.subtract), [rb_b, r31_b], [rb_b])
    ph.op("dve", lambda e: e.tensor_scalar(out=rb[:], in0=rb[:], scalar1=math.sqrt(128.0), scalar2=None, op0=ALU.mult),
          [rb_b], [rb_b])
    ph.op("dve", lambda e: e.tensor_copy(out=rep[:], in_=rb[:].unsqueeze(2).broadcast_to([32, 8, 128])), [rb_b], [rep_b])
    for h in range(8):
        p, p_b = pss[h % 2]
        ph.op("pe", lambda e, p=p, h=h: e.matmul(p[:, 0:384], rep[:, h, :], oh[:], start=True, stop=True),
              [rep_b, oh_b], [p_b])
        ph.op("act", lambda e, p=p, h=h: e.copy(out=tb[:, h, :], in_=p[:, 0:384]), [p_b], [tb_b])
    ph.dma("sp", s_tab[:, :], tb[:].rearrange("p h u -> p (h u)"), tb_b, reads=[tb_b])
    ph.run()


def phase_a2(ctx, hg, s_aqT, s_akT, s_av, s_mT, s_tab, s_oa):
    ph = Phase(ctx, "a2")
    ident, ident_b = make_ident(ph)
    kT, kT_b = ph.sb("kT", [128, 4, T], BF16)
    v, v_b = ph.sb("v", [128, NT, 4, 129], BF16)
    bt, bt_b = ph.sb("bt", [128, 2, 4, 128], BF16)
    r0 = hg * 512
    for h in range(4):
        ph.dma("sp", kT[:, h, :], s_akT[r0 + h * 128:r0 + (h + 1) * 128, :], kT_b, writes=[kT_b])
        ph.dma("sp", v[:, :, h, 0:128], s_av[:, r0 + h * 128:r0 + (h + 1) * 128].rearrange("(j p) d -> p j d", p=128),
               v_b, writes=[v_b])
        for pat in range(2):
            src = bass.AP(s_tab.tensor, s_tab.offset + (hg * 4 + h) * 384 + 255 - 128 * pat, [[3071, 128], [1, 128]])
            ph.dma("sp", bt[:, pat, h, :], src, bt_b, writes=[bt_b])
    ph.op("pool", lambda e: e.memset(v[:, :, :, 128:129], 1.0), [], [v_b])
    qs = [ph.sb("q%d" % i, [128, 4, 128], BF16) for i in range(2)]
    mbs = [ph.sb("mb%d" % i, [128, NT, 128], BF16) for i in range(2)]
    pts = [ph.sb("pt%d" % i, [128, 4, 128], BF16) for i in range(3)]
    sts = [ph.ps("st%d" % i) for i in range(3)]
    ops = [ph.ps("o%d" % i) for i in range(4)]
    rcs = [ph.sb("rc%d" % i, [128, 4], F32) for i in range(2)]
    outs = [ph.sb("out%d" % i, [128, 512], BF16) for i in range(2)]
    sts3 = sts
    pairs = [(i, j) for i in range(NT) for j in range(i + 1)]

    def emit_loads(i):
        q, q_b = qs[i % 2]
        mb, mb_b = mbs[i % 2]
        ph.dma("sp", q[:], s_aqT[r0:r0 + 512, i * 128:(i + 1) * 128].rearrange("(h d) t -> d h t", d=128), q_b,
               writes=[q_b])
        ph.dma("sp", mb[:, 0:i + 1, :], s_mT[i, :, 0:i + 1, :], mb_b, writes=[mb_b])

    def emit_qk(idx):
        i, j = pairs[idx]
        q, q_b = qs[i % 2]
        st, st_b = sts3[idx % 3]
        near = (i - j) <= 1
        for h in range(4):
            ph.op("pe", lambda e, h=h: e.matmul(st[:, h * 128:(h + 1) * 128], kT[:, h, j * 128:(j + 1) * 128], q[:, h, :],
                                                start=True, stop=not near), [kT_b, q_b], [st_b])
            if near:
                ph.op("pe", lambda e, h=h: e.matmul(st[:, h * 128:(h + 1) * 128], bt[:, i - j, h, :], ident[:], start=False,
                                                    stop=True), [bt_b, ident_b], [st_b])

    def emit_soft(idx):
        i, j = pairs[idx]
        mb, mb_b = mbs[i % 2]
        st, st_b = sts3[idx % 3]
        pt, pt_b = pts[idx % 3]
        ph.op("act", lambda e: e.activation(out=pt[:], in_=st[:], func=AF.Exp, scale=128.0 ** -0.5), [st_b], [pt_b])
        ph.op("dve", lambda e: e.tensor_tensor(out=pt[:], in0=pt[:], in1=mb[:, j:j + 1, :].broadcast_to([128, 4, 128]),
                                               op=ALU.mult), [pt_b, mb_b], [pt_b])

    def emit_pv(idx):
        i, j = pairs[idx]
        pt, pt_b = pts[idx % 3]
        for h in range(4):
            o, o_b = ops[h]
            ph.op("pe", lambda e, o=o, h=h: e.matmul(o[:, 0:129], pt[:, h, :], v[:, j, h, :], start=(j == 0), stop=(j == i)),
                  [pt_b, v_b], [o_b])
        if j == i:
            rc, rc_b = rcs[i % 2]
            out, out_b = outs[i % 2]
            for h in range(4):
                o, o_b = ops[h]
                ph.op("dve", lambda e, o=o, h=h: e.reciprocal(out=rc[:, h:h + 1], in_=o[:, 128:129]), [o_b], [rc_b])
                ph.op("dve", lambda e, o=o, h=h: e.tensor_scalar(out=out[:, h * 128:(h + 1) * 128], in0=o[:, 0:128],
                                                                 scalar1=rc[:, h:h + 1], scalar2=None, op0=ALU.mult),
                      [o_b, rc_b], [out_b])
            ph.dma("sp", s_oa[i * 128:(i + 1) * 128, r0:r0 + 512], out[:], out_b, reads=[out_b])

    emit_loads(0)
    emit_qk(0)
    if len(pairs) > 1:
        emit_loads(1)
        emit_qk(1)
    for idx in range(len(pairs)):
        i, j = pairs[idx]
        if j == 0 and i >= 1 and i + 1 < NT:
            emit_loads(i + 1)
        emit_soft(idx)
        if idx + 2 < len(pairs):
            emit_qk(idx + 2)
        emit_pv(idx)
    ph.run()


def phase_b(ctx, l, lb_raw, s_bqT, s_bfT, s_bi, s_bg, hgrn_norm_g, s_ob):
    ph = Phase(ctx, "hg")
    ident, ident_b = make_ident(ph)
    lbr, lbr_b = ph.sb("lbr", [128, DEPTH, 8], F32)
    lb, lb_b = ph.sb("lb", [128, 8], F32)
    oml, oml_b = ph.sb("oml", [128, 8], F32)
    ssum, ssum_b = ph.sb("ssum", [128, 8], F32)
    ph.dma("sp", lbr[:], lb_raw.rearrange("l (h p) -> p l h", p=128), lbr_b, writes=[lbr_b],
           allow_slow_non_contiguous=True)
    ph.op("act", lambda e: e.activation(out=lbr[:], in_=lbr[:], func=AF.Exp), [lbr_b], [lbr_b])
    ph.op("dve", lambda e: e.tensor_tensor(out=ssum[:], in0=lbr[:, 0, :], in1=lbr[:, 1, :], op=ALU.add), [lbr_b], [ssum_b])
    ph.op("dve", lambda e: e.tensor_tensor(out=ssum[:], in0=ssum[:], in1=lbr[:, 2, :], op=ALU.add), [lbr_b, ssum_b], [ssum_b])
    ph.op("dve", lambda e: e.tensor_tensor(out=ssum[:], in0=ssum[:], in1=lbr[:, 3, :], op=ALU.add), [lbr_b, ssum_b], [ssum_b])
    ph.op("dve", lambda e: e.reciprocal(out=ssum[:], in_=ssum[:]), [ssum_b], [ssum_b])
    ph.op("dve", lambda e: e.memset(lb[:], 0.0), [], [lb_b])
    for m in range(1, l + 1):
        ph.op("dve", lambda e, m=m: e.tensor_tensor(out=lb[:], in0=lb[:], in1=lbr[:, m, :], op=ALU.add), [lbr_b, lb_b], [lb_b])
    ph.op("dve", lambda e: e.tensor_tensor(out=lb[:], in0=lb[:], in1=ssum[:], op=ALU.mult), [lb_b, ssum_b], [lb_b])
    ph.op("dve", lambda e: e.tensor_scalar(out=oml[:], in0=lb[:], scalar1=-1.0, scalar2=1.0, op0=ALU.mult, op1=ALU.add),
          [lb_b], [oml_b])
    ng, ng_b = ph.sb("ng", [64, 128], F32)
    ph.dma("sp", ng[:], bcast_rows(hgrn_norm_g[l:l + 1, :], 64), ng_b, writes=[ng_b])
    ones, ones_b = ph.sb("ones", [128, T], BF16)
    ph.op("pool", lambda e: e.memset(ones[:], 1.0), [], [ones_b])
    zer, zer_b = ph.sb("zer", [128, 32], BF16)
    ph.op("pool", lambda e: e.memset(zer[:], 0.0), [], [zer_b])
    m01, m01_b = ph.sb("m01", [64, 64], F32)
    ph.op("pool", lambda e: e.memset(m01[:], 1.0), [], [m01_b])
    ph.op("pool", lambda e: e.affine_select(out=m01[:], in_=m01[:], pattern=[[1, 64]], compare_op=ALU.is_ge, fill=0.0,
                                            base=0, channel_multiplier=-1), [m01_b], [m01_b])
    NC = T // 64
    fT, fT_b = ph.sb("fT", [128, T], F32)
    Bc, Bc_b = ph.sb("Bc", [128, T], F32)
    E, E_b = ph.sb("E", [128, T], F32)
    qT, qT_b = ph.sb("qT", [128, T], BF16)
    qd, qd_b = ph.sb("qd", [128, T], BF16)
    kd, kd_b = ph.sb("kd", [128, T], BF16)
    kdtm, kdtm_b = ph.sb("kdtm", [64, NC, 128], BF16)
    iv, iv_b = ph.sb("iv", [64, NC, 128], BF16)
    oall, oall_b = ph.sb("oall", [64, NC, 128], F32)
    osb, osb_b = ph.sb("osb", [64, NC, 128], BF16)
    sc, sc_b = ph.sb("sc", [128, 4, NC], F32)
    S, S_b = ph.sb("S", [128, 128], F32)
    Sps = [ph.sb("Sp%d" % i, [128, 128], BF16) for i in range(2)]
    tmpu, tmpu_b = ph.sb("tmpu", [128, 128], F32)
    atms = [ph.sb("atm%d" % i, [64, 64], BF16) for i in range(2)]
    rs, rs_b = ph.sb("rs", [64, 2, NC], F32)
    psA = [ph.ps("pA%d" % i) for i in range(3)]
    psO = [ph.ps("pO%d" % i) for i in range(2)]
    psU = [ph.ps("pU%d" % i) for i in range(2)]
    psT = [ph.ps("pT%d" % i, [128, 4, 128], BF16) for i in range(1)]
    for h in range(8):
        r0 = h * 128
        ph.dma("sp", fT[:], s_bfT[r0:r0 + 128, :], fT_b, writes=[fT_b])
        ph.dma("sp", qT[:], s_bqT[r0:r0 + 128, :], qT_b, writes=[qT_b])
        ph.dma("sp", iv[:], s_bi[:, r0:r0 + 128].rearrange("(c p) v -> p c v", p=64), iv_b, writes=[iv_b])
        ph.dma("sp", osb[:], s_bg[:, r0:r0 + 128].rearrange("(c p) v -> p c v", p=64), osb_b, writes=[osb_b])
        ph.op("act", lambda e: e.activation(out=fT[:], in_=fT[:], func=AF.Sigmoid), [fT_b], [fT_b])
        ph.op("dve", lambda e, h=h: e.tensor_scalar(out=fT[:], in0=fT[:], scalar1=oml[:, h:h + 1], scalar2=lb[:, h:h + 1],
                                                    op0=ALU.mult, op1=ALU.add), [fT_b, oml_b, lb_b], [fT_b])
        ph.op("act", lambda e: e.activation(out=Bc[:], in_=fT[:], func=AF.Ln), [fT_b], [Bc_b])
        ph.op("pool", lambda e: e.tensor_scalar(out=fT[:], in0=fT[:], scalar1=-1.0, scalar2=1.0, op0=ALU.mult,
                                                op1=ALU.add), [fT_b, Bc_b], [fT_b])
        ph.op("dve", lambda e: e.tensor_tensor_scan(out=Bc[:], data0=ones[:], data1=Bc[:], initial=0.0, op0=ALU.mult,
                                                    op1=ALU.add), [Bc_b, ones_b], [Bc_b])
        Bc3 = Bc[:].rearrange("p (c s) -> p c s", s=64)
        ph.op("dve", lambda e: e.memset(sc[:, 0, 0:1], 0.0), [], [sc_b])
        ph.op("dve", lambda e, Bc3=Bc3: e.tensor_copy(out=sc[:, 0, 1:NC], in_=Bc3[:, 0:NC - 1, 63]), [Bc_b], [sc_b])
        ph.op("dve", lambda e, Bc3=Bc3: e.tensor_tensor(out=sc[:, 1, :], in0=Bc3[:, :, 63], in1=sc[:, 0, :],
                                                        op=ALU.subtract), [Bc_b, sc_b], [sc_b])
        ph.op("dve", lambda e, Bc3=Bc3: e.tensor_tensor(out=sc[:, 2, :], in0=Bc3[:, :, 31], in1=sc[:, 0, :],
                                                        op=ALU.subtract), [Bc_b, sc_b], [sc_b])
        ph.op("dve", lambda e, Bc3=Bc3: e.tensor_tensor(out=sc[:, 3, :], in0=Bc3[:, :, 63], in1=Bc3[:, :, 31],
                                                        op=ALU.subtract), [Bc_b, sc_b], [sc_b])
        ph.op("act", lambda e: e.activation(out=sc[:, 1:4, :], in_=sc[:, 1:4, :], func=AF.Exp), [sc_b], [sc_b])
        ph.op("dve", lambda e, Bc3=Bc3: e.tensor_copy(out=E[:, 0:NC], in_=Bc3[:, :, 31]), [Bc_b], [E_b])
        ph.op("dve", lambda e, Bc3=Bc3: e.tensor_tensor(out=Bc3, in0=Bc3, in1=E[:, 0:NC].unsqueeze(2).broadcast_to(
            [128, NC, 64]), op=ALU.subtract), [Bc_b, E_b], [Bc_b])
        ph.op("act", lambda e: e.activation(out=E[:], in_=Bc[:], func=AF.Exp), [Bc_b], [E_b])
        ph.op("dve", lambda e: e.tensor_tensor(out=qd[:], in0=qT[:], in1=E[:], op=ALU.mult), [qT_b, E_b], [qd_b])
        ph.op("act", lambda e: e.activation(out=E[:], in_=Bc[:], func=AF.Exp, scale=-1.0), [Bc_b, qd_b], [E_b])
        ph.op("dve", lambda e: e.tensor_tensor(out=kd[:], in0=fT[:], in1=E[:], op=ALU.mult), [fT_b, E_b], [kd_b])
        for c4 in range(NC // 4):
            pT, pT_b = psT[0]
            for u in range(4):
                c = c4 * 4 + u
                ph.op("pe", lambda e, pT=pT, u=u, c=c: e.transpose(pT[0:64, u, :], kd[:, c * 64:(c + 1) * 64], ident[:]),
                      [kd_b, ident_b], [pT_b])
            ph.op("act", lambda e, pT=pT, c4=c4: e.copy(out=kdtm[:, c4 * 4:(c4 + 1) * 4, :], in_=pT[0:64, :, :]),
                  [pT_b], [kdtm_b])
        def emit_A(c):
            pA, pA_b = psA[c % 3]
            c0 = c * 64
            ph.op("pe", lambda e: e.matmul(pA[0:32, 0:64], kd[:, c0:c0 + 32], qd[:, c0:c0 + 64], start=True, stop=True),
                  [kd_b, qd_b], [pA_b])
            ph.op("pe", lambda e: e.matmul(pA[32:64, 32:64], kd[:, c0 + 32:c0 + 64], qd[:, c0 + 32:c0 + 64], start=True,
                                           stop=True), [kd_b, qd_b], [pA_b])
            ph.op("pe", lambda e: e.matmul(pA[32:64, 0:32], zer[:], qd[:, c0:c0 + 32], start=True, stop=True),
                  [zer_b, qd_b], [pA_b])

        def emit_atm(c):
            pA, pA_b = psA[c % 3]
            atm, atm_b = atms[c % 2]
            ph.op("dve", lambda e: e.tensor_tensor(out=atm[:], in0=pA[0:64, 0:64], in1=m01[:], op=ALU.mult),
                  [pA_b, m01_b], [atm_b])

        def emit_U(c):
            pU, pU_b = psU[c % 2]
            ph.op("pe", lambda e: e.matmul(pU[:, 0:128], kdtm[:, c, :], iv[:, c, :], start=True, stop=True),
                  [kdtm_b, iv_b], [pU_b])

        emit_A(0)
        emit_A(1)
        emit_U(0)
        emit_atm(0)
        for c in range(NC):
            pO, pO_b = psO[c % 2]
            pU, pU_b = psU[c % 2]
            atm, atm_b = atms[c % 2]
            Sp, Sp_b = Sps[c % 2]
            Spn, Spn_b = Sps[(c + 1) % 2]
            cs = slice(c * 64, (c + 1) * 64)
            ph.op("pe", lambda e, pO=pO, atm=atm, c=c: e.matmul(pO[0:64, 0:128], atm[:], iv[:, c, :], start=True,
                                                               stop=(c == 0)), [atm_b, iv_b], [pO_b])
            if c > 0:
                ph.op("pe", lambda e, pO=pO, cs=cs, Sp=Sp: e.matmul(pO[0:64, 0:128], qd[:, cs], Sp[:], start=False,
                                                                   stop=True), [qd_b, Sp_b], [pO_b])
            ph.op("act", lambda e, pO=pO, c=c: e.copy(out=oall[:, c, :], in_=pO[0:64, 0:128]), [pO_b], [oall_b])
            if c + 2 < NC:
                emit_A(c + 2)
            if c + 1 < NC - 1:
                emit_U(c + 1)
            if c < NC - 1:
                if c == 0:
                    ph.op("dve", lambda e, pU=pU, c=c: e.tensor_scalar(out=S[:], in0=pU[:, 0:128], scalar1=sc[:, 3, c:c + 1],
                                                                       scalar2=None, op0=ALU.mult), [pU_b, sc_b], [S_b])
                else:
                    ph.op("dve", lambda e, pU=pU, c=c: e.tensor_scalar(out=tmpu[:], in0=pU[:, 0:128],
                                                                       scalar1=sc[:, 3, c:c + 1], scalar2=None,
                                                                       op0=ALU.mult), [pU_b, sc_b], [tmpu_b])
                    ph.op("dve", lambda e, c=c: e.scalar_tensor_tensor(out=S[:], in0=S[:], scalar=sc[:, 1, c:c + 1],
                                                                       in1=tmpu[:], op0=ALU.mult, op1=ALU.add),
                          [S_b, tmpu_b, sc_b], [S_b])
                ph.op("dve", lambda e, Spn=Spn, c=c: e.tensor_scalar(out=Spn[:], in0=S[:], scalar1=sc[:, 2, c + 1:c + 2],
                                                                     scalar2=None, op0=ALU.mult), [S_b, sc_b], [Spn_b])
                emit_atm(c + 1)
        oflat = oall[:].rearrange("p c v -> p (c v)")
        ph.op("act", lambda e: e.activation(out=kdtm[:], in_=oall[:], func=AF.Square), [oall_b], [kdtm_b])
        ph.op("dve", lambda e: e.tensor_reduce(out=rs[:, 0, :], in_=kdtm[:], axis=AX.X, op=ALU.add), [kdtm_b], [rs_b])
        ph.op("dve", lambda e: e.tensor_scalar(out=rs[:, 0, :], in0=rs[:, 0, :], scalar1=1.0 / 128, scalar2=EPS,
                                               op0=ALU.mult, op1=ALU.add), [rs_b], [rs_b])
        ph.op("act", lambda e: e.activation(out=rs[:, 0, :], in_=rs[:, 0, :], func=AF.Sqrt), [rs_b], [rs_b])
        ph.op("dve", lambda e: e.reciprocal(out=rs[:, 1, :], in_=rs[:, 0, :]), [rs_b], [rs_b])
        ph.op("dve", lambda e: e.tensor_tensor(out=oall[:], in0=oall[:], in1=rs[:, 1, :].unsqueeze(2).broadcast_to(
            [64, NC, 128]), op=ALU.mult), [oall_b, rs_b], [oall_b])
        ph.op("pool", lambda e: e.tensor_tensor(out=oall[:], in0=oall[:], in1=ng[:].unsqueeze(1).broadcast_to(
            [64, NC, 128]), op=ALU.mult), [oall_b, ng_b], [oall_b])
        ph.op("dve", lambda e: e.tensor_tensor(out=osb[:], in0=oall[:], in1=osb[:], op=ALU.mult), [oall_b, osb_b], [osb_b])
        ph.dma("sp", s_ob[:, r0:r0 + 128].rearrange("(c p) v -> p c v", p=64), osb[:], osb_b, reads=[osb_b])
    ph.run()


def phase_c(ctx, s_cq, s_ck, s_cv, s_cg, cosT, sinS, cdec, s_oc):
    ph = Phase(ctx, "ret")
    ident, ident_b = make_ident(ph)
    dec, dec_b = ph.sb("dec", [128, 8], F32)
    ph.dma("sp", dec[:], cdec[:, :], dec_b, writes=[dec_b])
    m01, m01_b = ph.sb("m01", [128, 128], F32)
    ph.op("pool", lambda e: e.memset(m01[:], 1.0), [], [m01_b])
    ph.op("pool", lambda e: e.affine_select(out=m01[:], in_=m01[:], pattern=[[1, 128]], compare_op=ALU.is_ge, fill=0.0,
                                            base=0, channel_multiplier=-1), [m01_b], [m01_b])
    qrdT, qrdT_b = ph.sb("qrdT", [128, 2, T], BF16)
    krnT, krnT_b = ph.sb("krnT", [128, 2, T], BF16)
    krn, krn_b = ph.sb("krn", [128, NT, 256], BF16)
    v, v_b = ph.sb("v", [128, NT, 512], BF16)
    ins = [[ph.sb("in%d_%d" % (a, i), [128, 256], F32) for i in range(2)] for a in range(2)]
    cs_ = [ph.sb("cos%d" % i, [128, 256], F32) for i in range(2)]
    sn_ = [ph.sb("sin%d" % i, [128, 256], F32) for i in range(2)]
    t1s = [ph.sb("t1_%d" % i, [128, 256], F32) for i in range(2)]
    t2s = [ph.sb("t2_%d" % i, [128, 256], F32) for i in range(2)]
    qtm = [ph.sb("qtm%d" % i, [128, 256], BF16) for i in range(2)]
    W = [ph.sb("W%d" % i, [128, 512], F32) for i in range(2)]
    Wb = [ph.sb("Wb%d" % i, [128, 512], BF16) for i in range(2)]
    atms = [ph.sb("atm%d" % i, [128, 128], BF16) for i in range(2)]
    gts = [ph.sb("g%d" % i, [128, 512], BF16) for i in range(2)]
    outs = [ph.sb("o%d" % i, [128, 512], BF16) for i in range(2)]
    junk, junk_b = ph.sb("junk", [128, 512], BF16)
    sts = [ph.sb("st%d" % i, [128, 4], F32) for i in range(2)]
    psT = [ph.ps("pT%d" % i, [128, 2, 128], BF16) for i in range(2)]
    psA = [ph.ps("pA%d" % i) for i in range(1)]
    psO = [ph.ps("pO%d" % i) for i in range(2)]
    psU = [ph.ps("pU%d" % i) for i in range(2)]
    for h in range(4):
        gam = 1.0 - 2.0 ** (-5.0 - h)
        g128 = gam ** 128
        ph.dma("sp", v[:], s_cv[:, h * 512:(h + 1) * 512].rearrange("(c p) d -> p c d", p=128), v_b, writes=[v_b])
        for i in range(NT):
            rows = slice(i * 128, (i + 1) * 128)
            co, co_b = cs_[i % 2]
            sn, sn_b = sn_[i % 2]
            ph.dma("sp", co[:], cosT[rows, :], co_b, writes=[co_b])
            ph.dma("sp", sn[:], sinS[rows, :], sn_b, writes=[sn_b])
            for a, src in enumerate((s_cq, s_ck)):
                x, x_b = ins[a][i % 2]
                t1, t1_b = t1s[a]
                t2, t2_b = t2s[a]
                ph.dma("sp", x[:], src[rows, h * 256:(h + 1) * 256], x_b, writes=[x_b])
                x3 = x[:].rearrange("p (i two) -> p i two", two=2)
                s3 = sn[:].rearrange("p (i two) -> p i two", two=2)
                t23 = t2[:].rearrange("p (i two) -> p i two", two=2)
                ph.op("dve", lambda e, t1=t1, x=x, co=co: e.tensor_tensor(out=t1[:], in0=x[:], in1=co[:], op=ALU.mult),
                      [x_b, co_b], [t1_b])
                ph.op("pool", lambda e, t23=t23, x3=x3, s3=s3: e.tensor_tensor(out=t23[:, :, 0], in0=x3[:, :, 1],
                                                                               in1=s3[:, :, 0], op=ALU.mult),
                      [x_b, sn_b], [t2_b])
                ph.op("pool", lambda e, t23=t23, x3=x3, s3=s3: e.tensor_tensor(out=t23[:, :, 1], in0=x3[:, :, 0],
                                                                               in1=s3[:, :, 1], op=ALU.mult),
                      [x_b, sn_b], [t2_b])
                ph.op("dve", lambda e, t1=t1, t2=t2: e.tensor_tensor(out=t1[:], in0=t1[:], in1=t2[:], op=ALU.add),
                      [t1_b, t2_b], [t1_b])
                pT, pT_b = psT[a]
                if a == 0:
                    dst, dst_b = qtm[i % 2]
                    dstap = dst[:]
                else:
                    dst_b = krn_b
                    dstap = krn[:, i, :]
                ph.op("act", lambda e, dstap=dstap, t1=t1, a=a, h=h: e.activation(out=dstap, in_=t1[:], func=AF.Copy,
                                                                                  scale=dec[:, a * 4 + h:a * 4 + h + 1]),
                      [t1_b, dec_b], [dst_b])
                for kc in range(2):
                    ph.op("pe", lambda e, pT=pT, kc=kc, dstap=dstap: e.transpose(pT[:, kc, :],
                                                                                 dstap[:, kc * 128:(kc + 1) * 128], ident[:]),
                          [dst_b, ident_b], [pT_b])
                tgt, tgt_b = (qrdT, qrdT_b) if a == 0 else (krnT, krnT_b)
                ph.op("act", lambda e, tgt=tgt, pT=pT, i=i: e.copy(out=tgt[:, :, i * 128:(i + 1) * 128], in_=pT[:]),
                      [pT_b], [tgt_b])
        for c in range(NT):
            cs = slice(c * 128, (c + 1) * 128)
            pA, pA_b = psA[0]
            pO, pO_b = psO[c % 2]
            atm, atm_b = atms[c % 2]
            g, g_b = gts[c % 2]
            out, out_b = outs[c % 2]
            st, st_b = sts[c % 2]
            ph.dma("sp", g[:], s_cg[cs, h * 512:(h + 1) * 512], g_b, writes=[g_b])
            for kc in range(2):
                ph.op("pe", lambda e, pA=pA, kc=kc, cs=cs: e.matmul(pA[:, 0:128], krnT[:, kc, cs], qrdT[:, kc, cs],
                                                                   start=(kc == 0), stop=(kc == 1)),
                      [krnT_b, qrdT_b], [pA_b])
            ph.op("dve", lambda e, pA=pA, atm=atm: e.tensor_tensor(out=atm[:], in0=pA[:, 0:128], in1=m01[:], op=ALU.mult),
                  [pA_b, m01_b], [atm_b])
            ph.op("pe", lambda e, pO=pO, atm=atm, c=c: e.matmul(pO[:], atm[:], v[:, c, :], start=True, stop=(c == 0)),
                  [atm_b, v_b], [pO_b])
            if c > 0:
                for kc in range(2):
                    ph.op("pe", lambda e, pO=pO, kc=kc, cs=cs: e.matmul(pO[:], qrdT[:, kc, cs], Wb[kc][0][:], start=False,
                                                                       stop=(kc == 1)), [qrdT_b, Wb[kc][1]], [pO_b])
            if c < NT - 1:
                for kc in range(2):
                    pU, pU_b = psU[kc]
                    ph.op("pe", lambda e, pU=pU, kc=kc, c=c: e.matmul(pU[:], krn[:, c, kc * 128:(kc + 1) * 128], v[:, c, :],
                                                                     start=True, stop=True), [krn_b, v_b], [pU_b])
                    Wk, Wk_b = W[kc]
                    if c == 0:
                        ph.op("dve", lambda e, Wk=Wk, pU=pU: e.tensor_copy(out=Wk[:], in_=pU[:]), [pU_b], [Wk_b])
                    else:
                        ph.op("dve", lambda e, Wk=Wk, pU=pU, g128=g128: e.scalar_tensor_tensor(
                            out=Wk[:], in0=Wk[:], scalar=g128, in1=pU[:], op0=ALU.mult, op1=ALU.add), [Wk_b, pU_b], [Wk_b])
                    ph.op("act", lambda e, Wk=Wk, kc=kc, g128=g128: e.activation(out=Wb[kc][0][:], in_=Wk[:], func=AF.Copy,
                                                                                 scale=g128), [Wk_b], [Wb[kc][1]])
            ph.op("act", lambda e, pO=pO, st=st: e.activation(out=junk[:], in_=pO[:], func=AF.Square,
                                                             accum_out=st[:, 0:1]), [pO_b], [junk_b, st_b])
            ph.op("dve", lambda e, st=st: e.tensor_scalar(out=st[:, 1:2], in0=st[:, 0:1], scalar1=1.0 / 512, scalar2=EPS,
                                                          op0=ALU.mult, op1=ALU.add), [st_b], [st_b])
            ph.op("act", lambda e, st=st: e.activation(out=st[:, 2:3], in_=st[:, 1:2], func=AF.Sqrt), [st_b], [st_b])
            ph.op("dve", lambda e, st=st: e.reciprocal(out=st[:, 3:4], in_=st[:, 2:3]), [st_b], [st_b])
            ph.op("dve", lambda e, pO=pO, st=st, g=g, out=out: e.scalar_tensor_tensor(
                out=out[:], in0=pO[:], scalar=st[:, 3:4], in1=g[:], op0=ALU.mult, op1=ALU.mult), [pO_b, st_b, g_b], [out_b])
            ph.dma("sp", s_oc[cs, h * 512:(h + 1) * 512], out[:], out_b, reads=[out_b])
    ph.run()


def phase_merge(ctx, l, modbc, s_oa, s_ob, s_oc, s_gT, w_a, w_b, w_c, w_out, xres):
    ph = Phase(ctx, "mrg")
    ident, ident_b = make_ident(ph)
    Wbr, Wbr_b = ph.sb("Wbr", [128, 32, D], BF16)
    wo, wo_b = ph.sb("wo", [128, 8, D], BF16)
    gt, gt_b = ph.sb("gt", [128, D], F32)
    for kc in range(8):
        ph.dma("pool", Wbr[:, kc, :], w_a[l, kc * 128:(kc + 1) * 128, :], Wbr_b, writes=[Wbr_b])
        ph.dma("pool", Wbr[:, 8 + kc, :], w_b[l, kc * 128:(kc + 1) * 128, :], Wbr_b, writes=[Wbr_b])
        ph.dma("pool", wo[:, kc, :], w_out[l, kc * 128:(kc + 1) * 128, :], wo_b, writes=[wo_b])
    for kc in range(16):
        ph.dma("pool", Wbr[:, 16 + kc, :], w_c[l, kc * 128:(kc + 1) * 128, :], Wbr_b, writes=[Wbr_b])
    ph.dma("sp", gt[:], modbc[l, :, 2 * D:3 * D], gt_b, writes=[gt_b])
    oin = [ph.sb("oin%d" % i, [128, 4096], BF16) for i in range(2)]
    oT, oT_b = ph.sb("oT", [128, 32, 512], BF16)
    gs = [ph.sb("gs%d" % i, [128, 3, 512], BF16) for i in range(2)]
    tt1 = [ph.sb("tt%d" % i, [128, 512], F32) for i in range(3)]
    mT, mT_b = ph.sb("mT", [128, 8, 512], BF16)
    xs = [ph.sb("x%d" % i, [128, D], F32) for i in range(2)]
    ty = [ph.sb("ty%d" % i, [128, 512], F32) for i in range(2)]
    psT = [ph.ps("pT%d" % i, [128, 8, 128], BF16) for i in range(2)]
    psB = [ph.ps("pB%d" % i) for i in range(3)]
    psY = [ph.ps("pY%d" % i) for i in range(2)]
    nT = 0
    ny = 0
    ng = 0
    for g in range(T // 512):
        for tt in range(4):
            rows = slice(g * 512 + tt * 128, g * 512 + (tt + 1) * 128)
            o, o_b = oin[tt % 2]
            ph.dma("sp", o[:, 0:1024], s_oa[rows, :], o_b, writes=[o_b])
            ph.dma("sp", o[:, 1024:2048], s_ob[rows, :], o_b, writes=[o_b])
            ph.dma("sp", o[:, 2048:4096], s_oc[rows, :], o_b, writes=[o_b])
            for k8 in range(4):
                pT, pT_b = psT[nT % 2]
                nT += 1
                for u in range(8):
                    kc = k8 * 8 + u
                    ph.op("pe", lambda e, pT=pT, u=u, o=o, kc=kc: e.transpose(pT[:, u, :], o[:, kc * 128:(kc + 1) * 128],
                                                                           ident[:]), [o_b, ident_b], [pT_b])
                ph.op("act", lambda e, pT=pT, k8=k8, tt=tt: e.copy(out=oT[:, k8 * 8:(k8 + 1) * 8, tt * 128:(tt + 1) * 128],
                                                                 in_=pT[:]), [pT_b], [oT_b])
        for dc in range(8):
            gsb, gsb_b = gs[ng % 2]
            ng += 1
            ph.dma("sp", gsb[:], s_gT.rearrange("(b r) t -> r b t", b=3)[dc * 128:(dc + 1) * 128, :, g * 512:(g + 1) * 512],
                   gsb_b, writes=[gsb_b])
            for br, (k0, k1) in enumerate(((0, 8), (8, 16), (16, 32))):
                pB, pB_b = psB[br]
                for kc in range(k0, k1):
                    ph.op("pe", lambda e, pB=pB, kc=kc, dc=dc, k0=k0, k1=k1: e.matmul(
                        pB[:], Wbr[:, kc, dc * 128:(dc + 1) * 128], oT[:, kc, :], start=(kc == k0), stop=(kc == k1 - 1)),
                          [Wbr_b, oT_b], [pB_b])
                t, t_b = tt1[br]
                ph.op("dve", lambda e, t=t, pB=pB, gsb=gsb, br=br: e.tensor_tensor(out=t[:], in0=pB[:], in1=gsb[:, br, :],
                                                                                  op=ALU.mult), [pB_b, gsb_b], [t_b])
            ph.op("pool", lambda e: e.tensor_tensor(out=tt1[0][0][:], in0=tt1[0][0][:], in1=tt1[1][0][:], op=ALU.add),
                  [tt1[0][1], tt1[1][1]], [tt1[0][1]])
            ph.op("pool", lambda e, dc=dc: e.tensor_tensor(out=mT[:, dc, :], in0=tt1[0][0][:], in1=tt1[2][0][:], op=ALU.add),
                  [tt1[0][1], tt1[2][1]], [mT_b])
        for tt in range(4):
            rows = slice(g * 512 + tt * 128, g * 512 + (tt + 1) * 128)
            x, x_b = xs[tt % 2]
            ph.dma("sp", x[:], xres[rows, :], x_b, writes=[x_b])
            for dh in range(2):
                pY, pY_b = psY[ny % 2]
                t, t_b = ty[ny % 2]
                ny += 1
                for dc in range(8):
                    ph.op("pe", lambda e, pY=pY, dc=dc, tt=tt, dh=dh: e.matmul(
                        pY[:], mT[:, dc, tt * 128:(tt + 1) * 128], wo[:, dc, dh * 512:(dh + 1) * 512], start=(dc == 0),
                        stop=(dc == 7)), [mT_b, wo_b], [pY_b])
                ph.op("dve", lambda e, t=t, pY=pY, dh=dh: e.tensor_tensor(out=t[:], in0=pY[:],
                                                                          in1=gt[:, dh * 512:(dh + 1) * 512], op=ALU.mult),
                      [pY_b, gt_b], [t_b])
                ph.op("pool", lambda e, t=t, x=x, dh=dh: e.tensor_tensor(out=x[:, dh * 512:(dh + 1) * 512],
                                                                         in0=x[:, dh * 512:(dh + 1) * 512], in1=t[:],
                                                                         op=ALU.add), [t_b, x_b], [x_b])
            ph.dma("sp", xres[rows, :], x[:], x_b, reads=[x_b])
    ph.run()


TH = 2048
NTH = TH // 128


def phase_route(ctx, l, hh, xres, modbc, norm_g, rg_w, rg_b, re_w, re_b, s_hT, s_gates):
    ph = Phase(ctx, "rt")
    identb, identb_b = make_ident(ph)
    idf, idf_b = ph.sb("identf32", [128, 128], F32)
    ph.op("pool", lambda e: e.memset(idf[:], 1.0), [], [idf_b])
    ph.op("pool", lambda e: e.affine_select(out=idf[:], in_=idf[:], pattern=[[-1, 128]], compare_op=ALU.is_equal,
                                            fill=0.0, base=0, channel_multiplier=1), [idf_b], [idf_b])
    G, GS_b = ph.sb("G", [128, D], F32)
    S, _ = ph.sb("S", [128, D], F32)
    ng, _ = ph.sb("ng", [128, D], F32)
    ph.dma("sp", G[:], modbc[l, :, 4 * D:5 * D], GS_b, writes=[GS_b])
    ph.dma("sp", S[:], modbc[l, :, 3 * D:4 * D], GS_b, writes=[GS_b])
    ph.dma("sp", ng[:], bcast_rows(norm_g[l:l + 1, :], 128), GS_b, writes=[GS_b])
    ph.op("dve", lambda e: e.tensor_tensor(out=G[:], in0=G[:], in1=ng[:], op=ALU.mult), [GS_b], [GS_b])
    rw, rw_b = ph.sb("rw", [128, 8, 36], F32)
    rb, rb_b = ph.sb("rb", [128, 36], F32)
    ph.dma("sp", rw[:, :, 0:4], rg_w[l].rearrange("(k p) n -> p k n", p=128), rw_b, writes=[rw_b])
    ph.dma("sp", rw[:, :, 4:36], re_w[l].rearrange("(k p) n -> p k n", p=128), rw_b, writes=[rw_b])
    ph.dma("sp", rb[:, 0:4], bcast_rows(rg_b[l:l + 1, :], 128), rb_b, writes=[rb_b])
    ph.dma("sp", rb[:, 4:36], bcast_rows(re_b[l:l + 1, :], 128), rb_b, writes=[rb_b])
    hT, hT_b = ph.sb("hT", [128, 8, TH], BF16)
    gates, gates_b = ph.sb("gates", [128, NTH, 32], F32)
    xs = [ph.sb("x%d" % i, [128, D], F32) for i in range(2)]
    sq, sq_b = ph.sb("sq", [128, D], BF16)
    hs = [ph.sb("h%d" % i, [128, D], BF16) for i in range(2)]
    st = [ph.sb("st%d" % i, [128, 4], F32) for i in range(2)]
    h32T = [ph.sb("h32T%d" % i, [128, 8, 128], F32) for i in range(2)]
    lgs = [ph.sb("lg%d" % i, [128, 36], F32) for i in range(2)]
    rts = [ph.sb("r%d" % i, [128, 64], F32) for i in range(2)]
    pts = [ph.ps("pt%d" % i, [128, 8, 128], BF16) for i in range(2)]
    pfs = [ph.ps("pf%d" % i, [128, 4, 128], F32) for i in range(2)]
    pls = [ph.ps("pl%d" % i) for i in range(2)]
    for t in range(NTH):
        x, x_b = xs[t % 2]
        h, h_b = hs[t % 2]
        s, s_b = st[t % 2]
        pt, pt_b = pts[t % 2]
        hf, hf_b = h32T[t % 2]
        pl, pl_b = pls[t % 2]
        lg, lg_b = lgs[t % 2]
        r, r_b = rts[t % 2]
        row0 = hh * TH + t * 128
        ph.dma("sp", x[:], xres[row0:row0 + 128, :], x_b, writes=[x_b])
        ph.op("act", lambda e, x=x, s=s: e.activation(out=sq[:], in_=x[:], func=AF.Square, accum_out=s[:, 0:1]),
              [x_b], [sq_b, s_b])
        ph.op("dve", lambda e, s=s: e.tensor_scalar(out=s[:, 1:2], in0=s[:, 0:1], scalar1=1.0 / D, scalar2=EPS,
                                                    op0=ALU.mult, op1=ALU.add), [s_b], [s_b])
        ph.op("act", lambda e, s=s: e.activation(out=s[:, 2:3], in_=s[:, 1:2], func=AF.Sqrt), [s_b], [s_b])
        ph.op("dve", lambda e, s=s: e.reciprocal(out=s[:, 3:4], in_=s[:, 2:3]), [s_b], [s_b])
        ph.op("dve", lambda e, x=x, s=s: e.scalar_tensor_tensor(out=x[:], in0=x[:], scalar=s[:, 3:4], in1=G[:],
                                                                op0=ALU.mult, op1=ALU.mult), [x_b, s_b, GS_b], [x_b])
        ph.op("pool", lambda e, x=x: e.tensor_tensor(out=x[:], in0=x[:], in1=S[:], op=ALU.add), [x_b, GS_b], [x_b])
        ph.op("pool", lambda e, x=x, h=h: e.tensor_copy(out=h[:], in_=x[:]), [x_b], [h_b])
        for k in range(8):
            ph.op("pe", lambda e, k=k, h=h, pt=pt: e.transpose(pt[:, k, :], h[:, k * 128:(k + 1) * 128], identb[:]),
                  [h_b, identb_b], [pt_b])
        ph.op("act", lambda e, pt=pt, t=t: e.copy(out=hT[:, :, t * 128:(t + 1) * 128], in_=pt[:]), [pt_b], [hT_b])
        for half in range(2):
            pf, pf_b = pfs[half]
            for u in range(4):
                k = half * 4 + u
                ph.op("pe", lambda e, pf=pf, u=u, k=k, x=x: e.transpose(pf[:, u, :], x[:, k * 128:(k + 1) * 128], idf[:]),
                      [x_b, idf_b], [pf_b])
            ph.op("dve", lambda e, pf=pf, hf=hf, half=half: e.tensor_copy(out=hf[:, half * 4:(half + 1) * 4, :], in_=pf[:]),
                  [pf_b], [hf_b])
        for k in range(8):
            ph.op("pe", lambda e, pl=pl, hf=hf, k=k: e.matmul(pl[:, 0:36], hf[:, k, :], rw[:, k, :], start=(k == 0),
                                                             stop=(k == 7)), [hf_b, rw_b], [pl_b])
        ph.op("dve", lambda e, lg=lg, pl=pl: e.tensor_tensor(out=lg[:], in0=pl[:, 0:36], in1=rb[:], op=ALU.add),
              [pl_b, rb_b], [lg_b])
        def dv(fn, r=r, lg=lg):
            ph.op("dve", fn, [r_b, lg_b], [r_b])
        dv(lambda e, r=r, lg=lg: e.tensor_reduce(out=r[:, 0:1], in_=lg[:, 0:4], axis=AX.X, op=ALU.max))
        dv(lambda e, r=r, lg=lg: e.tensor_scalar(out=r[:, 16:20], in0=lg[:, 0:4], scalar1=r[:, 0:1], scalar2=None,
                                                 op0=ALU.is_equal))
        dv(lambda e, r=r: e.tensor_scalar(out=r[:, 1:2], in0=r[:, 0:1], scalar1=-1.0, scalar2=None, op0=ALU.mult))
        ph.op("act", lambda e, r=r, lg=lg: e.activation(out=r[:, 20:24], in_=lg[:, 0:4], func=AF.Exp, bias=r[:, 1:2]),
              [r_b, lg_b], [r_b])
        dv(lambda e, r=r: e.tensor_reduce(out=r[:, 2:3], in_=r[:, 20:24], axis=AX.X, op=ALU.add))
        dv(lambda e, r=r: e.reciprocal(out=r[:, 3:4], in_=r[:, 2:3]))
        dv(lambda e, r=r, lg=lg: e.tensor_scalar(out=r[:, 24:32], in0=lg[:, 4:12], scalar1=r[:, 16:17], scalar2=None,
                                                 op0=ALU.mult))
        for g in range(1, 4):
            dv(lambda e, r=r, lg=lg, g=g: e.scalar_tensor_tensor(out=r[:, 24:32], in0=lg[:, 4 + 8 * g:12 + 8 * g],
                                                                 scalar=r[:, 16 + g:17 + g], in1=r[:, 24:32],
                                                                 op0=ALU.mult, op1=ALU.add))
        dv(lambda e, r=r: e.tensor_reduce(out=r[:, 4:5], in_=r[:, 24:32], axis=AX.X, op=ALU.max))
        dv(lambda e, r=r: e.tensor_scalar(out=r[:, 32:40], in0=r[:, 24:32], scalar1=r[:, 4:5], scalar2=None,
                                          op0=ALU.is_equal))
        dv(lambda e, r=r: e.scalar_tensor_tensor(out=r[:, 40:48], in0=r[:, 32:40], scalar=-1e30, in1=r[:, 24:32],
                                                 op0=ALU.mult, op1=ALU.add))
        dv(lambda e, r=r: e.tensor_reduce(out=r[:, 5:6], in_=r[:, 40:48], axis=AX.X, op=ALU.max))
        dv(lambda e, r=r: e.tensor_scalar(out=r[:, 48:56], in0=r[:, 40:48], scalar1=r[:, 5:6], scalar2=None,
                                          op0=ALU.is_equal))
        dv(lambda e, r=r: e.tensor_tensor(out=r[:, 6:7], in0=r[:, 5:6], in1=r[:, 4:5], op=ALU.subtract))
        ph.op("act", lambda e, r=r: e.activation(out=r[:, 7:8], in_=r[:, 6:7], func=AF.Exp), [r_b], [r_b])
        dv(lambda e, r=r: e.tensor_scalar(out=r[:, 8:9], in0=r[:, 7:8], scalar1=1.0, scalar2=None, op0=ALU.add))
        dv(lambda e, r=r: e.reciprocal(out=r[:, 9:10], in_=r[:, 8:9]))
        dv(lambda e, r=r: e.tensor_tensor(out=r[:, 10:11], in0=r[:, 9:10], in1=r[:, 3:4], op=ALU.mult))
        dv(lambda e, r=r: e.tensor_tensor(out=r[:, 11:12], in0=r[:, 10:11], in1=r[:, 7:8], op=ALU.mult))
        dv(lambda e, r=r: e.tensor_scalar(out=r[:, 56:64], in0=r[:, 32:40], scalar1=r[:, 10:11], scalar2=None,
                                          op0=ALU.mult))
        dv(lambda e, r=r: e.scalar_tensor_tensor(out=r[:, 56:64], in0=r[:, 48:56], scalar=r[:, 11:12], in1=r[:, 56:64],
                                                 op0=ALU.mult, op1=ALU.add))
        for g in range(4):
            ph.op("dve", lambda e, r=r, g=g, t=t: e.tensor_scalar(out=gates[:, t, g * 8:(g + 1) * 8], in0=r[:, 56:64],
                                                                  scalar1=r[:, 16 + g:17 + g], scalar2=None, op0=ALU.mult),
                  [r_b], [gates_b])
    for k in range(8):
        ph.dma("sp", s_hT[:, k, :], hT[:, k, :], hT_b, reads=[hT_b])
    ph.dma("sp", s_gates.rearrange("(t p) e -> p t e", p=128), gates[:], gates_b, reads=[gates_b])
    ph.run()


def phase_experts(ctx, l, hh, xres, modbc, s_hT, s_gates, e_gate, e_up, e_down, n_exp=32):
    ph = Phase(ctx, "ex")
    hT, hT_b = ph.sb("hT", [128, 8, TH], BF16)
    gates, gates_b = ph.sb("gates", [128, NTH, 32], F32)
    gt, gt_b = ph.sb("gt", [128, D], F32)
    yacc, yacc_b = ph.sb("yacc", [128, NTH, D], F32)
    for k in range(8):
        ph.dma("sp", hT[:, k, :], s_hT[:, k, :], hT_b, writes=[hT_b])
    ph.dma("sp", gates[:], s_gates.rearrange("(t p) e -> p t e", p=128), gates_b, writes=[gates_b])
    ph.dma("sp", gt[:], modbc[l, :, 5 * D:6 * D], gt_b, writes=[gt_b])
    wg = [ph.sb("wg%d" % i, [128, 8, 512], BF16) for i in range(2)]
    wu = [ph.sb("wu%d" % i, [128, 8, 512], BF16) for i in range(2)]
    wd = [ph.sb("wd%d" % i, [128, 4, D], BF16) for i in range(2)]
    sgs = [ph.sb("sg%d" % i, [128, 512], F32) for i in range(2)]
    hgs = [ph.sb("hg%d" % i, [128, 4, 512], BF16) for i in range(2)]
    psG = [ph.ps("pG%d" % i) for i in range(2)]
    psU = [ph.ps("pU%d" % i) for i in range(2)]
    psY = [ph.ps("pY%d" % i) for i in range(4)]
    nf = 0
    ny = 0
    nh = 0
    for ex in range(n_exp):
        g_, g_b = wg[ex % 2]
        u_, u_b = wu[ex % 2]
        d_, d_b = wd[ex % 2]
        ph.dma("pool", g_[:], e_gate[l, ex].rearrange("(k p) f -> p k f", p=128), g_b, writes=[g_b])
        ph.dma("pool", u_[:], e_up[l, ex].rearrange("(k p) f -> p k f", p=128), u_b, writes=[u_b])
        ph.dma("pool", d_[:], e_down[l, ex].rearrange("(k p) f -> p k f", p=128), d_b, writes=[d_b])
        for tg in range(TH // 512):
            ts = slice(tg * 512, (tg + 1) * 512)
            hg, hg_b = hgs[nh % 2]
            nh += 1
            for fc in range(4):
                pG, pG_b = psG[nf % 2]
                pU, pU_b = psU[nf % 2]
                sg, sg_b = sgs[nf % 2]
                nf += 1
                for k in range(8):
                    ph.op("pe", lambda e, pG=pG, g_=g_, k=k, fc=fc, ts=ts: e.matmul(
                        pG[:], g_[:, k, fc * 128:(fc + 1) * 128], hT[:, k, ts], start=(k == 0), stop=(k == 7)),
                          [g_b, hT_b], [pG_b])
                for k in range(8):
                    ph.op("pe", lambda e, pU=pU, u_=u_, k=k, fc=fc, ts=ts: e.matmul(
                        pU[:], u_[:, k, fc * 128:(fc + 1) * 128], hT[:, k, ts], start=(k == 0), stop=(k == 7)),
                          [u_b, hT_b], [pU_b])
                ph.op("act", lambda e, sg=sg, pG=pG: e.activation(out=sg[:], in_=pG[:], func=AF.Silu), [pG_b], [sg_b])
                ph.op("dve", lambda e, hg=hg, fc=fc, sg=sg, pU=pU: e.tensor_tensor(out=hg[:, fc, :], in0=pU[:], in1=sg[:],
                                                                                  op=ALU.mult), [pU_b, sg_b], [hg_b])
            for tt in range(4):
                tile = tg * 4 + tt
                for dh in range(2):
                    pY, pY_b = psY[ny % 4]
                    ny += 1
                    for fc in range(4):
                        ph.op("pe", lambda e, pY=pY, hg=hg, fc=fc, tt=tt, d_=d_, dh=dh: e.matmul(
                            pY[:], hg[:, fc, tt * 128:(tt + 1) * 128], d_[:, fc, dh * 512:(dh + 1) * 512], start=(fc == 0),
                            stop=(fc == 3)), [hg_b, d_b], [pY_b])
                    ya = yacc[:, tile, dh * 512:(dh + 1) * 512]
                    if ex == 0:
                        ph.op("dve", lambda e, ya=ya, pY=pY, tile=tile, ex=ex: e.tensor_scalar(
                            out=ya, in0=pY[:], scalar1=gates[:, tile, ex:ex + 1], scalar2=None, op0=ALU.mult),
                              [pY_b, gates_b], [yacc_b])
                    else:
                        ph.op("dve", lambda e, ya=ya, pY=pY, tile=tile, ex=ex: e.scalar_tensor_tensor(
                            out=ya, in0=pY[:], scalar=gates[:, tile, ex:ex + 1], in1=ya, op0=ALU.mult, op1=ALU.add),
                              [pY_b, gates_b, yacc_b], [yacc_b])
    xs = [ph.sb("x%d" % i, [128, D], F32) for i in range(2)]
    for t in range(NTH):
        x, x_b = xs[t % 2]
        row0 = hh * TH + t * 128
        ph.dma("sp", x[:], xres[row0:row0 + 128, :], x_b, writes=[x_b])
        ph.op("pool", lambda e, t=t: e.tensor_tensor(out=yacc[:, t, :], in0=yacc[:, t, :], in1=gt[:], op=ALU.mult),
              [yacc_b, gt_b], [yacc_b])
        ph.op("pool", lambda e, x=x, t=t: e.tensor_tensor(out=x[:], in0=x[:], in1=yacc[:, t, :], op=ALU.add),
              [x_b, yacc_b], [x_b])
        ph.dma("sp", xres[row0:row0 + 128, :], x[:], x_b, reads=[x_b])
    ph.run()


def phase_final(ctx, xres, final_g, y):
    ph = Phase(ctx, "fin")
    G, G_b = ph.sb("G", [128, D], F32)
    ph.dma("sp", G[:], bcast_rows(final_g, 128), G_b, writes=[G_b])
    xs = [ph.sb("x%d" % i, [128, D], F32) for i in range(3)]
    sq, sq_b = ph.sb("sq", [128, D], BF16)
    st = [ph.sb("st%d" % i, [128, 4], F32) for i in range(2)]
    for t in range(NT):
        x, x_b = xs[t % 3]
        s, s_b = st[t % 2]
        ph.dma("sp", x[:], xres[t * 128:(t + 1) * 128, :], x_b, writes=[x_b])
        ph.op("act", lambda e, x=x, s=s: e.activation(out=sq[:], in_=x[:], func=AF.Square, accum_out=s[:, 0:1]),
              [x_b], [sq_b, s_b])
        ph.op("dve", lambda e, s=s: e.tensor_scalar(out=s[:, 1:2], in0=s[:, 0:1], scalar1=1.0 / D, scalar2=EPS,
                                                    op0=ALU.mult, op1=ALU.add), [s_b], [s_b])
        ph.op("act", lambda e, s=s: e.activation(out=s[:, 2:3], in_=s[:, 1:2], func=AF.Sqrt), [s_b], [s_b])
        ph.op("dve", lambda e, s=s: e.reciprocal(out=s[:, 3:4], in_=s[:, 2:3]), [s_b], [s_b])
        ph.op("dve", lambda e, x=x, s=s: e.scalar_tensor_tensor(out=x[:], in0=x[:], scalar=s[:, 3:4], in1=G[:],
                                                                op0=ALU.mult, op1=ALU.mult), [x_b, s_b, G_b], [x_b])
        ph.dma("sp", y[t * 128:(t + 1) * 128, :], x[:], x_b, reads=[x_b])
    ph.run()


def make_oh_tab():
    u = np.arange(384)
    rel = np.maximum(255 - u, 0)
    relf = np.maximum(rel, 1).astype(np.float32)
    large = 16 + (np.log(relf / np.float32(16)) / np.float32(math.log(8)) * np.float32(16)).astype(np.int32)
    large = np.minimum(large, 31)
    bucket = np.where(rel < 16, rel, large)
    oh = np.zeros((32, 384), np.float32)
    oh[bucket, u] = 1.0
    return oh


def make_ret_tables():
    pos = np.arange(T, dtype=np.float32)
    theta = (np.float32(1.0) / (np.float32(10000.0) ** np.linspace(0.0, 1.0, 128, dtype=np.float32))).astype(np.float32)
    theta = np.repeat(theta, 2)
    ang = (pos[:, None] * theta[None, :]).astype(np.float32)
    cos = np.cos(ang.astype(np.float64)).astype(np.float32)
    sin = np.sin(ang.astype(np.float64)).astype(np.float32)
    sinS = sin.copy()
    sinS[:, 0::2] = -sin[:, 0::2]
    p = np.arange(128, dtype=np.float64)
    dec = np.zeros((128, 8), np.float32)
    for h in range(4):
        gam = 1.0 - 2.0 ** (-5.0 - h)
        dec[:, h] = gam ** (p + 1)
        dec[:, 4 + h] = gam ** (-(p + 1)) / 16.0
    return cos, sinS, dec


def phase_copy(ctx, src, dst, nrows):
    ph = Phase(ctx, "cp")
    b = ph.buf("cpbuf")
    step = 512
    for r in range(0, nrows, step):
        ph.dma("sp", dst[r:r + step, :], src[r:r + step, :], b)
    ph.run()
```

```python
import contextlib
import math
import numpy as np
import concourse.bass as bass
import concourse.mybir as mybir
from concourse.bass_utils import run_bass_kernel_spmd

F32 = mybir.dt.float32
BF16 = mybir.dt.bfloat16
AF = mybir.ActivationFunctionType
ALU = mybir.AluOpType
AX = mybir.AxisListType

D = 1024
T = 4096
DEPTH = 4
NT = T // 128
N_IN = 16968
EPS = 1e-6
O_AQ, O_AK, O_AV = 0, 1024, 2048
O_IQ, O_IK, O_IW = 3072, 3584, 3648
O_BQ, O_BF, O_BI, O_BG = 3656, 4680, 5704, 6728
O_CQ, O_CK, O_CV, O_CG = 7752, 8776, 9800, 11848
O_GA, O_GB, O_GC = 13896, 14920, 15944

COMPUTE = ("pe", "act", "dve", "pool")


class Buf:
    __slots__ = ("name", "writers", "readers", "sem")

    def __init__(self, name):
        self.name = name
        self.writers = []
        self.readers = []
        self.sem = None


class Op:
    __slots__ = ("eng", "emit", "deps", "is_dma", "sem", "semval", "used", "sig")

    def __init__(self, eng, emit, is_dma):
        self.eng = eng
        self.emit = emit
        self.deps = []
        self.is_dma = is_dma
        self.sem = None
        self.semval = 0
        self.used = False
        self.sig = 0


class Ctx:
    def __init__(self, nc, stack):
        self.nc = nc
        self.eng_sem = {}
        self.eng_cnt = {}
        for e in COMPUTE:
            self.eng_sem[e] = stack.enter_context(nc.semaphore("es_" + e))
            self.eng_cnt[e] = 0
        self.pool = []
        for i in range(56):
            self.pool.append([stack.enter_context(nc.semaphore("ds%d" % i)), 0])
        self.nphase = 0


class Phase:
    def __init__(self, ctx, name):
        self.ctx = ctx
        self.nc = ctx.nc
        self.name = "%s_%d" % (name, ctx.nphase)
        ctx.nphase += 1
        self.ops = []
        self.stack = contextlib.ExitStack()
        self.sems = []
        self.nbuf = 0

    def sb(self, name, shape, dtype):
        t = self.stack.enter_context(self.nc.sbuf_tensor("%s_%s" % (self.name, name), list(shape), dtype))
        return t, Buf(name)

    def ps(self, name, shape=(128, 512), dtype=F32):
        t = self.stack.enter_context(self.nc.psum_tensor("%s_%s" % (self.name, name), list(shape), dtype))
        return t, Buf(name)

    def buf(self, name):
        return Buf(name)

    def _sem_for(self, b):
        if b.sem is None:
            b.sem = self.ctx.pool.pop()
            self.sems.append(b.sem)
        return b.sem

    def _record(self, op, reads, writes):
        deps = []
        for b in reads:
            deps.extend(b.writers)
        for b in writes:
            keep = []
            for w in b.writers:
                if op.is_dma and w.is_dma and w.sem is op.sem:
                    keep.append(w)
                else:
                    deps.append(w)
            for r in b.readers:
                deps.append(r)
            b.writers = keep + [op]
            b.readers = []
        for b in reads:
            b.readers.append(op)
        seen = set()
        for d in deps:
            if (not d.is_dma) and d.eng == "pe" and op.eng == "pe":
                continue
            if id(d) not in seen and d is not op:
                seen.add(id(d))
                d.used = True
                op.deps.append(d)
        self.ops.append(op)

    def op(self, eng, emit, reads=(), writes=()):
        o = Op(eng, emit, False)
        self._record(o, reads, writes)
        return o

    def dma(self, eng, out, in_, sbuf, reads=(), writes=(), **kw):
        o = Op(eng, None, True)
        o.sem = self._sem_for(sbuf)
        o.sem[1] += 16
        o.semval = o.sem[1]
        o.emit = lambda e: e.dma_start(out=out, in_=in_, **kw)
        self._record(o, reads, writes)
        return o

    def run(self):
        ctx = self.ctx
        nc = self.nc
        for e in COMPUTE:
            for o in self.ops:
                if o.eng == e and not o.is_dma and o.used:
                    ctx.eng_cnt[e] += 1
                    o.sig = ctx.eng_cnt[e]
        engs = {"pe": [], "act": [], "dve": [], "pool": [], "sp": []}
        for o in self.ops:
            engs[o.eng].append(o)
        final_waits = [(s[0], s[1]) for s in self.sems]

        def emit_engine(e, name):
            waited = {}
            for o in engs[name]:
                need = {}
                for d in o.deps:
                    if d.is_dma:
                        key, val = d.sem[0], d.semval
                    else:
                        if d.eng == name and name == "pe":
                            continue
                        key, val = ctx.eng_sem[d.eng], d.sig
                    if need.get(key, 0) < val:
                        need[key] = val
                for key, val in need.items():
                    if waited.get(key, 0) < val:
                        e.wait_ge(key, val)
                        waited[key] = val
                ins = o.emit(e)
                if o.is_dma:
                    ins.then_inc(o.sem[0], 16)
                elif o.used:
                    ins.then_inc(ctx.eng_sem[name], 1)
            if name == "sp":
                for s, v in final_waits:
                    e.wait_ge(s, v)

        with nc.Block() as blk:
            blk.tensor(lambda e: emit_engine(e, "pe"))
            blk.scalar(lambda e: emit_engine(e, "act"))
            blk.vector(lambda e: emit_engine(e, "dve"))
            blk.gpsimd(lambda e: emit_engine(e, "pool"))
            blk.sync(lambda e: emit_engine(e, "sp"))
        for s in self.sems:
            ctx.pool.append(s)
        self.stack.close()


def bcast_rows(ap2d, nrows):
    return bass.AP(ap2d.tensor, ap2d.offset, [[0, nrows], [1, ap2d.shape[-1]]])


def phase_mod(ctx, c, ada_w, ada_b, modbc):
    nc = ctx.nc
    ph = Phase(ctx, "mod")
    cT, cT_b = ph.sb("cT", [128, 8], F32)
    cs, cs_b = ph.sb("cs", [128, 8], F32)
    crep, crep_b = ph.sb("crep", [128, 8, 128], F32)
    wts = [ph.sb("w%d" % i, [128, 8, 512], F32) for i in range(2)]
    bia, bia_b = ph.sb("bias", [128, 6144], F32)
    outs = [ph.sb("o%d" % i, [128, 512], F32) for i in range(2)]
    pss = [ph.ps("ps%d" % i) for i in range(2)]
    ph.dma("sp", cT[:], c.rearrange("o (k p) -> p (o k)", p=128), cT_b, writes=[cT_b],
           allow_slow_non_contiguous=True)
    ph.op("act", lambda e: e.activation(out=cs[:], in_=cT[:], func=AF.Silu), [cT_b], [cs_b])
    ph.op("dve", lambda e: e.tensor_copy(out=crep[:], in_=cs[:].unsqueeze(2).broadcast_to([128, 8, 128])),
          [cs_b], [crep_b])
    n = 0
    for l in range(DEPTH):
        ph.dma("sp", bia[:], bcast_rows(ada_b[l:l + 1, :], 128), bia_b, writes=[bia_b])
        for seg in (1, 4):
            ph.op("pool", lambda e, seg=seg: e.tensor_scalar_add(out=bia[:, seg * 1024:(seg + 1) * 1024],
                                                                 in0=bia[:, seg * 1024:(seg + 1) * 1024], scalar1=1.0),
                  [bia_b], [bia_b])
        for j in range(12):
            w, w_b = wts[n % 2]
            o, o_b = outs[n % 2]
            p, p_b = pss[n % 2]
            n += 1
            ph.dma("sp", w[:], ada_w[l, :, j * 512:(j + 1) * 512].rearrange("(k p) n -> p k n", p=128), w_b,
                   writes=[w_b])
            for k in range(8):
                ph.op("pe", lambda e, k=k, w=w, p=p: e.matmul(p[:], crep[:, k, :], w[:, k, :], start=(k == 0),
                                                               stop=(k == 7)),
                      [crep_b, w_b], [p_b])
            ph.op("dve", lambda e, o=o, p=p, j=j: e.tensor_tensor(out=o[:], in0=p[:], in1=bia[:, j * 512:(j + 1) * 512],
                                                                   op=ALU.add), [p_b, bia_b], [o_b])
            ph.dma("sp", modbc[l, :, j * 512:(j + 1) * 512], o[:], o_b, reads=[o_b])
    ph.run()


def emit_norm_tiles(ph, xsrc, hT, hT_b, G, S, GS_b, ident, ident_b, ntiles, h32T=None, h32T_b=None):
    xs = [ph.sb("x%d" % i, [128, D], F32) for i in range(2)]
    sq, sq_b = ph.sb("sq", [128, D], BF16)
    hs = [ph.sb("h%d" % i, [128, D], BF16) for i in range(2)]
    st = [ph.sb("st%d" % i, [128, 4], F32) for i in range(2)]
    pts = [ph.ps("pt%d" % i, [128, 8, 128], BF16) for i in range(2)]
    for t in range(ntiles):
        x, x_b = xs[t % 2]
        h, h_b = hs[t % 2]
        s, s_b = st[t % 2]
        pt, pt_b = pts[t % 2]
        ph.dma("sp", x[:], xsrc[t * 128:(t + 1) * 128, :], x_b, writes=[x_b])
        ph.op("act", lambda e, x=x, s=s: e.activation(out=sq[:], in_=x[:], func=AF.Square, accum_out=s[:, 0:1]),
              [x_b], [sq_b, s_b])
        ph.op("dve", lambda e, s=s: e.tensor_scalar(out=s[:, 1:2], in0=s[:, 0:1], scalar1=1.0 / D, scalar2=EPS,
                                                    op0=ALU.mult, op1=ALU.add), [s_b], [s_b])
        ph.op("act", lambda e, s=s: e.activation(out=s[:, 2:3], in_=s[:, 1:2], func=AF.Sqrt), [s_b], [s_b])
        ph.op("dve", lambda e, s=s: e.reciprocal(out=s[:, 3:4], in_=s[:, 2:3]), [s_b], [s_b])
        ph.op("dve", lambda e, x=x, s=s: e.scalar_tensor_tensor(out=x[:], in0=x[:], scalar=s[:, 3:4], in1=G[:],
                                                                op0=ALU.mult, op1=ALU.mult), [x_b, s_b, GS_b], [x_b])
        if S is not None:
            ph.op("pool", lambda e, x=x, h=h: e.tensor_tensor(out=h[:], in0=x[:], in1=S[:], op=ALU.add),
                  [x_b, GS_b], [h_b])
        else:
            ph.op("pool", lambda e, x=x, h=h: e.tensor_copy(out=h[:], in_=x[:]), [x_b], [h_b])
        for k in range(8):
            ph.op("pe", lambda e, k=k, h=h, pt=pt: e.transpose(pt[:, k, :], h[:, k * 128:(k + 1) * 128], ident[:]),
                  [h_b, ident_b], [pt_b])
        ph.op("act", lambda e, pt=pt, t=t: e.copy(out=hT[:, :, t * 128:(t + 1) * 128], in_=pt[:]), [pt_b], [hT_b])


def make_ident(ph, dtype=BF16):
    nc = ph.nc
    idf, idf_b = ph.sb("identf", [128, 128], F32)
    ident, ident_b = ph.sb("ident", [128, 128], dtype)
    ph.op("pool", lambda e: e.memset(idf[:], 1.0), [], [idf_b])
    ph.op("pool", lambda e: e.affine_select(out=idf[:], in_=idf[:], pattern=[[-1, 128]], compare_op=ALU.is_equal,
                                            fill=0.0, base=0, channel_multiplier=1), [idf_b], [idf_b])
    ph.op("pool", lambda e: e.tensor_copy(out=ident[:], in_=idf[:]), [idf_b], [ident_b])
    return ident, ident_b


def phase_proj(ctx, xres, modbc, l, norm_g, w_in_l, jobs, seg_scale, seg_shift):
    nc = ctx.nc
    ph = Phase(ctx, "proj")
    ident, ident_b = make_ident(ph)
    G, GS_b = ph.sb("G", [128, D], F32)
    S, _ = ph.sb("S", [128, D], F32)
    ng, _ = ph.sb("ng", [128, D], F32)
    hT, hT_b = ph.sb("hT", [128, 8, T], BF16)
    ph.dma("sp", G[:], modbc[l, :, seg_scale * D:(seg_scale + 1) * D], GS_b, writes=[GS_b])
    ph.dma("sp", S[:], modbc[l, :, seg_shift * D:(seg_shift + 1) * D], GS_b, writes=[GS_b])
    ph.dma("sp", ng[:], bcast_rows(norm_g[l:l + 1, :], 128), GS_b, writes=[GS_b])
    ph.op("dve", lambda e: e.tensor_tensor(out=G[:], in0=G[:], in1=ng[:], op=ALU.mult), [GS_b], [GS_b])
    emit_norm_tiles(ph, xres, hT, hT_b, G, S, GS_b, ident, ident_b, NT)

    wts = [ph.sb("w%d" % i, [128, 8, 512], BF16) for i in range(3)]
    pss = [ph.ps("pp%d" % i) for i in range(4)]
    stg = [ph.sb("sg%d" % i, [128, 512], F32) for i in range(4)]
    stgb = [ph.sb("sgb%d" % i, [128, 512], BF16) for i in range(4)]
    nw = 0
    ne = 0
    for (c0, ncols, kind, dst_fn, dtype, func) in jobs:
        step = 512 if kind == "tok" else 128
        for cc in range(c0, c0 + ncols, step):
            n = min(step, c0 + ncols - cc)
            w, w_b = wts[nw % 3]
            nw += 1
            ph.dma("pool", w[:, :, 0:n], w_in_l[:, cc:cc + n].rearrange("(k p) n -> p k n", p=128), w_b,
                   writes=[w_b])
            nchunk = NT if kind == "tok" else T // 512
            for ci in range(nchunk):
                p, p_b = pss[ne % 4]
                if dtype == F32:
                    sg, sg_b = stg[ne % 4]
                else:
                    sg, sg_b = stgb[ne % 4]
                evac_eng = "act" if (func is not None or ne % 2 == 0) else "dve"
                ne += 1
                if kind == "tok":
                    for k in range(8):
                        ph.op("pe", lambda e, k=k, w=w, p=p, ci=ci, n=n: e.matmul(
                            p[:, 0:n], hT[:, k, ci * 128:(ci + 1) * 128], w[:, k, 0:n], start=(k == 0), stop=(k == 7)),
                              [hT_b, w_b], [p_b])
                    po, so = p[:, 0:n], sg[:, 0:n]
                    dst = dst_fn(ci * 128, cc - c0, n)
                else:
                    for k in range(8):
                        ph.op("pe", lambda e, k=k, w=w, p=p, ci=ci, n=n: e.matmul(
                            p[0:n, :], w[:, k, 0:n], hT[:, k, ci * 512:(ci + 1) * 512], start=(k == 0), stop=(k == 7)),
                              [hT_b, w_b], [p_b])
                    po, so = p[0:n, :], sg[0:n, :]
                    dst = dst_fn(cc - c0, n, ci * 512)
                if evac_eng == "act":
                    f = func if func is not None else AF.Copy
                    ph.op("act", lambda e, po=po, so=so, f=f: e.activation(out=so, in_=po, func=f), [p_b], [sg_b])
                else:
                    ph.op("dve", lambda e, po=po, so=so: e.tensor_copy(out=so, in_=po), [p_b], [sg_b])
                ph.dma("sp", dst, so, sg_b, reads=[sg_b])
    ph.run()


INPUT_SPECS = [
    ("x", [T, D]), ("c", [1, D]), ("rel_bias", [32, 8]), ("hgrn_lb_raw", [DEPTH, D]), ("norm1_g", [DEPTH, D]),
    ("norm2_g", [DEPTH, D]), ("ada_w", [DEPTH, D, 6 * D]), ("ada_b", [DEPTH, 6 * D]), ("w_in", [DEPTH, D, N_IN]),
    ("hgrn_norm_g", [DEPTH, 128]), ("w_branch_a", [DEPTH, 1024, D]), ("w_branch_b", [DEPTH, 1024, D]),
    ("w_branch_c", [DEPTH, 2048, D]), ("w_out", [DEPTH, D, D]), ("router_group_w", [DEPTH, D, 4]),
    ("router_group_b", [DEPTH, 4]), ("router_expert_w", [DEPTH, D, 32]), ("router_expert_b", [DEPTH, 32]),
    ("expert_w_gate", [DEPTH, 32, D, 512]), ("expert_w_up", [DEPTH, 32, D, 512]), ("expert_w_down", [DEPTH, 32, 512, D]),
    ("final_norm_g", [1, D]), ("oh_tab", [32, 384]), ("cosT", [T, 256]), ("sinS", [T, 256]), ("cdec", [128, 8]),
]


def build_program(depth=DEPTH, debug=False):
    nc = bass.Bass("TRN2", target_bir_lowering=False)
    I = {}
    for name, shape in INPUT_SPECS:
        I[name] = nc.dram_tensor(name, shape, F32, kind="ExternalInput").ap()
    y = nc.dram_tensor("y", [T, D], F32, kind="ExternalOutput").ap()

    def scr(name, shape, dt):
        return nc.dram_tensor(name, shape, dt, kind="ExternalOutput" if debug else "Internal").ap()

    xres = scr("xres", [T, D], F32)
    modbc = scr("modbc", [DEPTH, 128, 6 * D], F32)
    s_aqT = scr("s_aqT", [1024, T], BF16)
    s_akT = scr("s_akT", [1024, T], BF16)
    s_av = scr("s_av", [T, 1024], BF16)
    s_iqT = scr("s_iqT", [512, T], BF16)
    s_ikT = scr("s_ikT", [64, T], BF16)
    s_iw = scr("s_iw", [T, 8], F32)
    s_bqT = scr("s_bqT", [1024, T], BF16)
    s_bfT = scr("s_bfT", [1024, T], F32)
    s_bi = scr("s_bi", [T, 1024], BF16)
    s_bg = scr("s_bg", [T, 1024], BF16)
    s_cq = scr("s_cq", [T, 1024], F32)
    s_ck = scr("s_ck", [T, 1024], F32)
    s_cv = scr("s_cv", [T, 2048], BF16)
    s_cg = scr("s_cg", [T, 2048], BF16)
    s_gT = scr("s_gT", [3072, T], BF16)
    s_mT = scr("s_mT", [NT, 128, NT, 128], BF16)
    s_tab = scr("s_tab", [128, 3072], BF16)
    s_oa = scr("s_oa", [T, 1024], BF16)
    s_ob = scr("s_ob", [T, 1024], BF16)
    s_oc = scr("s_oc", [T, 2048], BF16)
    s_hT = scr("s_hT", [128, 8, TH], BF16)
    s_gates = scr("s_gates", [T, 32], F32)

    def tok(dst):
        return lambda t0, c0, n: dst[t0:t0 + 128, c0:c0 + n]

    def feat(dst):
        return lambda c0, n, t0: dst[c0:c0 + n, t0:t0 + 512]

    jobs = [
        (O_AQ, 1024, "feat", feat(s_aqT), BF16, None),
        (O_AK, 1024, "feat", feat(s_akT), BF16, None),
        (O_AV, 1024, "tok", tok(s_av), BF16, None),
        (O_IQ, 512, "feat", feat(s_iqT), BF16, None),
        (O_IK, 64, "feat", feat(s_ikT), BF16, None),
        (O_IW, 8, "tok", tok(s_iw), F32, None),
        (O_BQ, 1024, "feat", feat(s_bqT), BF16, None),
        (O_BF, 1024, "feat", feat(s_bfT), F32, None),
        (O_BI, 1024, "tok", tok(s_bi), BF16, None),
        (O_BG, 1024, "tok", tok(s_bg), BF16, AF.Silu),
        (O_CQ, 1024, "tok", tok(s_cq), F32, None),
        (O_CK, 1024, "tok", tok(s_ck), F32, None),
        (O_CV, 2048, "tok", tok(s_cv), BF16, None),
        (O_CG, 2048, "tok", tok(s_cg), BF16, AF.Silu),
        (O_GA, 3072, "feat", feat(s_gT), BF16, AF.Sigmoid),
    ]
    with contextlib.ExitStack() as stack:
        ctx = Ctx(nc, stack)
        phase_copy(ctx, I["x"], xres, T)
        phase_mod(ctx, I["c"], I["ada_w"], I["ada_b"], modbc)
        phase_bias_tab(ctx, I["rel_bias"], I["oh_tab"], s_tab)
        for l in range(depth):
            phase_proj(ctx, xres, modbc, l, I["norm1_g"], I["w_in"][l], jobs, 1, 0)
            phase_a1(ctx, s_iqT, s_ikT, s_iw, s_mT)
            for hg in range(2):
                phase_a2(ctx, hg, s_aqT, s_akT, s_av, s_mT, s_tab, s_oa)
            phase_b(ctx, l, I["hgrn_lb_raw"], s_bqT, s_bfT, s_bi, s_bg, I["hgrn_norm_g"], s_ob)
            phase_c(ctx, s_cq, s_ck, s_cv, s_cg, I["cosT"], I["sinS"], I["cdec"], s_oc)
            phase_merge(ctx, l, modbc, s_oa, s_ob, s_oc, s_gT, I["w_branch_a"], I["w_branch_b"], I["w_branch_c"],
                        I["w_out"], xres)
            for hh in range(2):
                sg = s_gates[hh * TH:(hh + 1) * TH, :]
                phase_route(ctx, l, hh, xres, modbc, I["norm2_g"], I["router_group_w"], I["router_group_b"],
                            I["router_expert_w"], I["router_expert_b"], s_hT, sg)
                phase_experts(ctx, l, hh, xres, modbc, s_hT, sg, I["expert_w_gate"], I["expert_w_up"], I["expert_w_down"])
        phase_final(ctx, xres, I["final_norm_g"], y)
    return nc


_PROGRAM = None


def kernel(**inputs):
    global _PROGRAM
    if _PROGRAM is None:
        _PROGRAM = build_program()
    nc = _PROGRAM
    f = lambda a: np.ascontiguousarray(np.asarray(a, dtype=np.float32))
    cos, sinS, dec = make_ret_tables()
    shared = {k: f(inputs[k]) for k in ("rel_bias", "hgrn_lb_raw", "norm1_g", "norm2_g", "ada_w", "ada_b", "w_in",
                                        "hgrn_norm_g", "w_branch_a", "w_branch_b", "w_branch_c", "w_out",
                                        "router_group_w", "router_group_b", "router_expert_w", "router_expert_b",
                                        "expert_w_gate", "expert_w_up", "expert_w_down")}
    shared["final_norm_g"] = f(inputs["final_norm_g"]).reshape(1, D)
    shared["oh_tab"] = make_oh_tab()
    shared["cosT"] = cos
    shared["sinS"] = sinS
    shared["cdec"] = dec
    x = f(inputs["x"])
    c = f(inputs["c"])
    in_maps = []
    for core in range(8):
        b = core % 4
        m = dict(shared)
        m["x"] = x[b]
        m["c"] = c[b:b + 1]
        in_maps.append(m)
    res = run_bass_kernel_spmd(nc, in_maps, core_ids=list(range(8)))
    out = np.stack([np.asarray(res.results[b]["y"], dtype=np.float32) for b in range(4)], axis=0)
    return out


TOPK = 256
NBIS = 16
MASKV = -30000.0


def phase_a1(ctx, s_iqT, s_ikT, s_iw, s_mT):
    ph = Phase(ctx, "a1")
    ident, ident_b = make_ident(ph)
    ikT, ikT_b = ph.sb("ikT", [64, T], BF16)
    ph.dma("sp", ikT[:], s_ikT[:, :], ikT_b, writes=[ikT_b])
    pw, pw_b = ph.sb("pw", [128, NBIS], F32)
    for k in range(NBIS):
        ph.op("pool", lambda e, k=k: e.memset(pw[:, k:k + 1], 0.5 ** (k + 1)), [], [pw_b])
    cm, cm_b = ph.sb("cm", [128, 128], F32)
    ph.op("pool", lambda e: e.memset(cm[:], 0.0), [], [cm_b])
    ph.op("pool", lambda e: e.affine_select(out=cm[:], in_=cm[:], pattern=[[-1, 128]], compare_op=ALU.is_ge, fill=-1e30,
                                            base=0, channel_multiplier=1), [cm_b], [cm_b])
    iqs = [ph.sb("iq%d" % i, [64, 8, 128], BF16) for i in range(4)]
    ws = [ph.sb("w%d" % i, [128, 24], F32) for i in range(4)]
    Is = [ph.sb("I%d" % i, [128, T], F32) for i in range(4)]
    rls = [ph.sb("rl%d" % i, [128, 512], F32) for i in range(4)]
    junk, junk_b = ph.sb("junk", [128, T], BF16)
    junk2, junk2_b = ph.sb("junk2", [128, T], BF16)
    m01s = [ph.sb("m01_%d" % i, [128, T], BF16) for i in range(2)]
    mTs = [ph.sb("mT%d" % i, [128, NT, 128], BF16) for i in range(2)]
    sts = [ph.sb("st%d" % i, [128, 8 + NBIS], F32) for i in range(4)]
    pss = [ph.ps("ps%d" % i) for i in range(4)]
    ptr = [ph.ps("ptr%d" % i, [128, 4, 128], BF16) for i in range(2)]
    cnt = {"pz": 0, "rl": 0, "tr": 0}

    def gen_indexer(i):
        S = (i + 1) * 128
        iq, iq_b = iqs[i % 4]
        w, w_b = ws[i % 4]
        I, I_b = Is[i % 4]
        ph.dma("sp", iq[:], s_iqT[:, i * 128:(i + 1) * 128].rearrange("(h d) t -> d h t", d=64), iq_b, writes=[iq_b])
        ph.dma("sp", w[:, 0:8], s_iw[i * 128:(i + 1) * 128, :], w_b, writes=[w_b])
        ph.op("pool", lambda e: e.tensor_scalar(out=w[:, 0:8], in0=w[:, 0:8], scalar1=512.0 ** -0.5, scalar2=None,
                                                op0=ALU.mult), [w_b], [w_b])
        ph.op("pool", lambda e: e.tensor_scalar(out=w[:, 8:16], in0=w[:, 0:8], scalar1=-1.0, scalar2=None, op0=ALU.mult),
              [w_b], [w_b])
        ph.op("dve", lambda e: e.tensor_tensor(out=w[:, 8:16], in0=w[:, 8:16], in1=w[:, 0:8], op=ALU.max), [w_b], [w_b])
        ph.op("pool", lambda e: e.tensor_scalar(out=w[:, 16:24], in0=w[:, 0:8], scalar1=0.0, scalar2=2.0, op0=ALU.is_ge,
                                                op1=ALU.mult), [w_b], [w_b])
        ph.op("pool", lambda e: e.tensor_scalar(out=w[:, 16:24], in0=w[:, 16:24], scalar1=-1.0, scalar2=None, op0=ALU.add),
              [w_b], [w_b])
        yield
        for c0 in range(0, S, 512):
            n = min(512, S - c0)
            for h in range(8):
                p, p_b = pss[cnt["pz"] % 4]
                cnt["pz"] += 1
                rl, rl_b = rls[cnt["rl"] % 4]
                cnt["rl"] += 1
                ph.op("pe", lambda e, p=p, h=h, c0=c0, n=n: e.matmul(p[:, 0:n], iq[:, h, :], ikT[:, c0:c0 + n], start=True, stop=True),
                      [iq_b, ikT_b], [p_b])
                ph.op("act", lambda e, p=p, rl=rl, h=h, n=n: e.activation(out=rl[:, 0:n], in_=p[:, 0:n], func=AF.Relu,
                                                                     scale=w[:, 8 + h:9 + h]), [p_b, w_b], [rl_b])
                if h == 0:
                    ph.op("dve", lambda e, rl=rl, h=h, c0=c0, n=n: e.tensor_scalar(out=I[:, c0:c0 + n], in0=rl[:, 0:n],
                                                                       scalar1=w[:, 16 + h:17 + h], scalar2=None,
                                                                       op0=ALU.mult), [rl_b, w_b], [I_b])
                else:
                    ph.op("dve", lambda e, rl=rl, h=h, c0=c0, n=n: e.scalar_tensor_tensor(out=I[:, c0:c0 + n], in0=rl[:, 0:n],
                                                                              scalar=w[:, 16 + h:17 + h],
                                                                              in1=I[:, c0:c0 + n], op0=ALU.mult,
                                                                              op1=ALU.add), [rl_b, w_b, I_b], [I_b])
                if h % 2 == 1:
                    yield
        ph.op("pool", lambda e: e.tensor_tensor(out=I[:, i * 128:(i + 1) * 128], in0=I[:, i * 128:(i + 1) * 128],
                                                in1=cm[:], op=ALU.add), [I_b, cm_b], [I_b])
        yield

    def gen_select(i):
        S = (i + 1) * 128
        I, I_b = Is[i % 4]
        m01, m01_b = m01s[i % 2]
        mT, mT_b = mTs[i % 2]
        st, st_b = sts[i % 4]
        if i < 2:
            ph.op("dve", lambda e: e.memset(st[:, 0:1], -1e29), [], [st_b])
        else:
            on_act = (i % 2 == 1)
            ph.op("dve", lambda e: e.tensor_reduce(out=st[:, 0:1], in_=I[:, 0:i * 128], axis=AX.X, op=ALU.min),
                  [I_b], [st_b])
            ph.op("dve", lambda e: e.tensor_reduce(out=st[:, 1:2], in_=I[:, 0:S], axis=AX.X, op=ALU.max), [I_b], [st_b])
            ph.op("dve", lambda e: e.tensor_tensor(out=st[:, 2:3], in0=st[:, 1:2], in1=st[:, 0:1], op=ALU.subtract),
                  [st_b], [st_b])
            ph.op("dve", lambda e: e.tensor_scalar(out=st[:, 8:8 + NBIS], in0=pw[:], scalar1=st[:, 2:3], scalar2=None,
                                                   op0=ALU.mult), [st_b, pw_b], [st_b])
            yield
            for k in range(NBIS):
                if on_act:
                    ph.op("dve", lambda e, k=k: e.tensor_scalar(out=st[:, 3:4], in0=st[:, 0:1], scalar1=st[:, 8 + k:9 + k],
                                                                scalar2=-1.0, op0=ALU.add, op1=ALU.mult), [st_b], [st_b])
                    ph.op("act", lambda e: e.activation(out=junk2[:, 0:S], in_=I[:, 0:S], func=AF.Sign, bias=st[:, 3:4],
                                                        accum_out=st[:, 4:5]), [I_b, st_b], [junk2_b, st_b])
                    thresh = 2.0 * (TOPK - 0.5) - S
                else:
                    ph.op("dve", lambda e, k=k: e.tensor_tensor(out=st[:, 3:4], in0=st[:, 0:1], in1=st[:, 8 + k:9 + k],
                                                                op=ALU.add), [st_b], [st_b])
                    ph.op("dve", lambda e: e.tensor_scalar(out=junk[:, 0:S], in0=I[:, 0:S], scalar1=st[:, 3:4],
                                                           scalar2=None, op0=ALU.is_ge, op1=ALU.add,
                                                           accum_out=st[:, 4:5]), [I_b, st_b], [junk_b, st_b])
                    thresh = TOPK - 0.5
                ph.op("dve", lambda e, k=k, thresh=thresh: e.scalar_tensor_tensor(
                    out=st[:, 5:6], in0=st[:, 4:5], scalar=thresh, in1=st[:, 8 + k:9 + k], op0=ALU.is_ge, op1=ALU.mult),
                      [st_b], [st_b])
                ph.op("dve", lambda e: e.tensor_tensor(out=st[:, 0:1], in0=st[:, 0:1], in1=st[:, 5:6], op=ALU.add),
                      [st_b], [st_b])
                yield
        ph.op("dve", lambda e: e.tensor_scalar(out=m01[:, 0:S], in0=I[:, 0:S], scalar1=st[:, 0:1], scalar2=None,
                                               op0=ALU.is_ge), [I_b, st_b], [m01_b])
        yield
        for j0 in range(0, i + 1, 4):
            nj = min(4, i + 1 - j0)
            pt, pt_b = ptr[cnt["tr"] % 2]
            cnt["tr"] += 1
            for u in range(nj):
                j = j0 + u
                ph.op("pe", lambda e, pt=pt, u=u, j=j: e.transpose(pt[:, u, :], m01[:, j * 128:(j + 1) * 128], ident[:]),
                      [m01_b, ident_b], [pt_b])
            ph.op("dve", lambda e, pt=pt, j0=j0, nj=nj: e.tensor_copy(out=mT[:, j0:j0 + nj, :], in_=pt[:, 0:nj, :]),
                  [pt_b], [mT_b])
            yield
        ph.dma("sp", s_mT[i, :, 0:i + 1, :], mT[:, 0:i + 1, :], mT_b, reads=[mT_b])

    def merge(gens):
        state = [[g, max(n, 1), 0, False] for g, n in gens]
        while any(not st_[3] for st_ in state):
            best = None
            for st_ in state:
                if st_[3]:
                    continue
                frac = st_[2] / st_[1]
                if best is None or frac < best[0]:
                    best = (frac, st_)
            st_ = best[1]
            try:
                next(st_[0])
                st_[2] += 1
            except StopIteration:
                st_[3] = True

    def n_sel(i):
        return (NBIS + 4 if i >= 2 else 3) + (i + 4) // 4

    def n_idx(i):
        return 2 + 4 * (((i + 1) * 128 + 511) // 512)

    merge([(gen_indexer(0), n_idx(0)), (gen_indexer(1), n_idx(1))])
    for m in range(NT // 2):
        gens = [(gen_select(2 * m), n_sel(2 * m)), (gen_select(2 * m + 1), n_sel(2 * m + 1))]
        if 2 * m + 2 < NT:
            gens.append((gen_indexer(2 * m + 2), n_idx(2 * m + 2)))
            gens.append((gen_indexer(2 * m + 3), n_idx(2 * m + 3)))
        merge(gens)
    ph.run()


def phase_bias_tab(ctx, rel_bias, oh_tab, s_tab):
    ph = Phase(ctx, "btab")
    rb, rb_b = ph.sb("rb", [32, 8], F32)
    r31, r31_b = ph.sb("r31", [32, 8], F32)
    rep, rep_b = ph.sb("rep", [32, 8, 128], F32)
    oh, oh_b = ph.sb("oh", [32, 384], F32)
    tb, tb_b = ph.sb("tb", [128, 8, 384], BF16)
    pss = [ph.ps("p%d" % i) for i in range(2)]
    ph.dma("sp", rb[:], rel_bias[:, :], rb_b, writes=[rb_b])
    ph.dma("sp", r31[:], bcast_rows(rel_bias[31:32, :], 32), r31_b, writes=[r31_b])
    ph.dma("sp", oh[:], oh_tab[:, :], oh_b, writes=[oh_b])
    ph.op("dve", lambda e: e.tensor_tensor(out=rb[:], in0=rb[:], in1=r31[:], op=ALU.subtract), [rb_b, r31_b], [rb_b])
    ph.op("dve", lambda e: e.tensor_scalar(out=rb[:], in0=rb[:], scalar1=math.sqrt(128.0), scalar2=None, op0=ALU.mult),
          [rb_b], [rb_b])
    ph.op("dve", lambda e: e.tensor_copy(out=rep[:], in_=rb[:].unsqueeze(2).broadcast_to([32, 8, 128])), [rb_b], [rep_b])
    for h in range(8):
        p, p_b = pss[h % 2]
        ph.op("pe", lambda e, p=p, h=h: e.matmul(p[:, 0:384], rep[:, h, :], oh[:], start=True, stop=True),
              [rep_b, oh_b], [p_b])
        ph.op("act", lambda e, p=p, h=h: e.copy(out=tb[:, h, :], in_=p[:, 0:384]), [p_b], [tb_b])
    ph.dma("sp", s_tab[:, :], tb[:].rearrange("p h u -> p (h u)"), tb_b, reads=[tb_b])
    ph.run()


def phase_a2(ctx, hg, s_aqT, s_akT, s_av, s_mT, s_tab, s_oa):
    ph = Phase(ctx, "a2")
    ident, ident_b = make_ident(ph)
    kT, kT_b = ph.sb("kT", [128, 4, T], BF16)
    v, v_b = ph.sb("v", [128, NT, 4, 129], BF16)
    bt, bt_b = ph.sb("bt", [128, 2, 4, 128], BF16)
    r0 = hg * 512
    for h in range(4):
        ph.dma("sp", kT[:, h, :], s_akT[r0 + h * 128:r0 + (h + 1) * 128, :], kT_b, writes=[kT_b])
        ph.dma("sp", v[:, :, h, 0:128], s_av[:, r0 + h * 128:r0 + (h + 1) * 128].rearrange("(j p) d -> p j d", p=128),
               v_b, writes=[v_b])
        for pat in range(2):
            src = bass.AP(s_tab.tensor, s_tab.offset + (hg * 4 + h) * 384 + 255 - 128 * pat, [[3071, 128], [1, 128]])
            ph.dma("sp", bt[:, pat, h, :], src, bt_b, writes=[bt_b])
    ph.op("pool", lambda e: e.memset(v[:, :, :, 128:129], 1.0), [], [v_b])
    qs = [ph.sb("q%d" % i, [128, 4, 128], BF16) for i in range(2)]
    mbs = [ph.sb("mb%d" % i, [128, NT, 128], BF16) for i in range(2)]
    pts = [ph.sb("pt%d" % i, [128, 4, 128], BF16) for i in range(3)]
    sts = [ph.ps("st%d" % i) for i in range(3)]
    ops = [ph.ps("o%d" % i) for i in range(4)]
    rcs = [ph.sb("rc%d" % i, [128, 4], F32) for i in range(2)]
    outs = [ph.sb("out%d" % i, [128, 512], BF16) for i in range(2)]
    sts3 = sts
    pairs = [(i, j) for i in range(NT) for j in range(i + 1)]

    def emit_loads(i):
        q, q_b = qs[i % 2]
        mb, mb_b = mbs[i % 2]
        ph.dma("sp", q[:], s_aqT[r0:r0 + 512, i * 128:(i + 1) * 128].rearrange("(h d) t -> d h t", d=128), q_b,
               writes=[q_b])
        ph.dma("sp", mb[:, 0:i + 1, :], s_mT[i, :, 0:i + 1, :], mb_b, writes=[mb_b])

    def emit_qk(idx):
        i, j = pairs[idx]
        q, q_b = qs[i % 2]
        st, st_b = sts3[idx % 3]
        near = (i - j) <= 1
        for h in range(4):
            ph.op("pe", lambda e, h=h: e.matmul(st[:, h * 128:(h + 1) * 128], kT[:, h, j * 128:(j + 1) * 128], q[:, h, :],
                                                start=True, stop=not near), [kT_b, q_b], [st_b])
            if near:
                ph.op("pe", lambda e, h=h: e.matmul(st[:, h * 128:(h + 1) * 128], bt[:, i - j, h, :], ident[:], start=False,
                                                    stop=True), [bt_b, ident_b], [st_b])

    def emit_soft(idx):
        i, j = pairs[idx]
        mb, mb_b = mbs[i % 2]
        st, st_b = sts3[idx % 3]
        pt, pt_b = pts[idx % 3]
        ph.op("act", lambda e: e.activation(out=pt[:], in_=st[:], func=AF.Exp, scale=128.0 ** -0.5), [st_b], [pt_b])
        ph.op("dve", lambda e: e.tensor_tensor(out=pt[:], in0=pt[:], in1=mb[:, j:j + 1, :].broadcast_to([128, 4, 128]),
                                               op=ALU.mult), [pt_b, mb_b], [pt_b])

    def emit_pv(idx):
        i, j = pairs[idx]
        pt, pt_b = pts[idx % 3]
        for h in range(4):
            o, o_b = ops[h]
            ph.op("pe", lambda e, o=o, h=h: e.matmul(o[:, 0:129], pt[:, h, :], v[:, j, h, :], start=(j == 0), stop=(j == i)),
                  [pt_b, v_b], [o_b])
        if j == i:
            rc, rc_b = rcs[i % 2]
            out, out_b = outs[i % 2]
            for h in range(4):
                o, o_b = ops[h]
                ph.op("dve", lambda e, o=o, h=h: e.reciprocal(out=rc[:, h:h + 1], in_=o[:, 128:129]), [o_b], [rc_b])
                ph.op("dve", lambda e, o=o, h=h: e.tensor_scalar(out=out[:, h * 128:(h + 1) * 128], in0=o[:, 0:128],
                                                                 scalar1=rc[:, h:h + 1], scalar2=None, op0=ALU.mult),
                      [o_b, rc_b], [out_b])
            ph.dma("sp", s_oa[i * 128:(i + 1) * 128, r0:r0 + 512], out[:], out_b, reads=[out_b])

    emit_loads(0)
    emit_qk(0)
    if len(pairs) > 1:
        emit_loads(1)
        emit_qk(1)
    for idx in range(len(pairs)):
        i, j = pairs[idx]
        if j == 0 and i >= 1 and i + 1 < NT:
            emit_loads(i + 1)
        emit_soft(idx)
        if idx + 2 < len(pairs):
            emit_qk(idx + 2)
        emit_pv(idx)
    ph.run()


def phase_b(ctx, l, lb_raw, s_bqT, s_bfT, s_bi, s_bg, hgrn_norm_g, s_ob):
    ph = Phase(ctx, "hg")
    ident, ident_b = make_ident(ph)
    lbr, lbr_b = ph.sb("lbr", [128, DEPTH, 8], F32)
    lb, lb_b = ph.sb("lb", [128, 8], F32)
    oml, oml_b = ph.sb("oml", [128, 8], F32)
    ssum, ssum_b = ph.sb("ssum", [128, 8], F32)
    ph.dma("sp", lbr[:], lb_raw.rearrange("l (h p) -> p l h", p=128), lbr_b, writes=[lbr_b],
           allow_slow_non_contiguous=True)
    ph.op("act", lambda e: e.activation(out=lbr[:], in_=lbr[:], func=AF.Exp), [lbr_b], [lbr_b])
    ph.op("dve", lambda e: e.tensor_tensor(out=ssum[:], in0=lbr[:, 0, :], in1=lbr[:, 1, :], op=ALU.add), [lbr_b], [ssum_b])
    ph.op("dve", lambda e: e.tensor_tensor(out=ssum[:], in0=ssum[:], in1=lbr[:, 2, :], op=ALU.add), [lbr_b, ssum_b], [ssum_b])
    ph.op("dve", lambda e: e.tensor_tensor(out=ssum[:], in0=ssum[:], in1=lbr[:, 3, :], op=ALU.add), [lbr_b, ssum_b], [ssum_b])
    ph.op("dve", lambda e: e.reciprocal(out=ssum[:], in_=ssum[:]), [ssum_b], [ssum_b])
    ph.op("dve", lambda e: e.memset(lb[:], 0.0), [], [lb_b])
    for m in range(1, l + 1):
        ph.op("dve", lambda e, m=m: e.tensor_tensor(out=lb[:], in0=lb[:], in1=lbr[:, m, :], op=ALU.add), [lbr_b, lb_b], [lb_b])
    ph.op("dve", lambda e: e.tensor_tensor(out=lb[:], in0=lb[:], in1=ssum[:], op=ALU.mult), [lb_b, ssum_b], [lb_b])
    ph.op("dve", lambda e: e.tensor_scalar(out=oml[:], in0=lb[:], scalar1=-1.0, scalar2=1.0, op0=ALU.mult, op1=ALU.add),
          [lb_b], [oml_b])
    ng, ng_b = ph.sb("ng", [64, 128], F32)
    ph.dma("sp", ng[:], bcast_rows(hgrn_norm_g[l:l + 1, :], 64), ng_b, writes=[ng_b])
    ones, ones_b = ph.sb("ones", [128, T], BF16)
    ph.op("pool", lambda e: e.memset(ones[:], 1.0), [], [ones_b])
    zer, zer_b = ph.sb("zer", [128, 32], BF16)
    ph.op("pool", lambda e: e.memset(zer[:], 0.0), [], [zer_b])
    m01, m01_b = ph.sb("m01", [64, 64], F32)
    ph.op("pool", lambda e: e.memset(m01[:], 1.0), [], [m01_b])
    ph.op("pool", lambda e: e.affine_select(out=m01[:], in_=m01[:], pattern=[[1, 64]], compare_op=ALU.is_ge, fill=0.0,
                                            base=0, channel_multiplier=-1), [m01_b], [m01_b])
    NC = T // 64
    fT, fT_b = ph.sb("fT", [128, T], F32)
    Bc, Bc_b = ph.sb("Bc", [128, T], F32)
    E, E_b = ph.sb("E", [128, T], F32)
    qT, qT_b = ph.sb("qT", [128, T], BF16)
    qd, qd_b = ph.sb("qd", [128, T], BF16)
    kd, kd_b = ph.sb("kd", [128, T], BF16)
    kdtm, kdtm_b = ph.sb("kdtm", [64, NC, 128], BF16)
    iv, iv_b = ph.sb("iv", [64, NC, 128], BF16)
    oall, oall_b = ph.sb("oall", [64, NC, 128], F32)
    osb, osb_b = ph.sb("osb", [64, NC, 128], BF16)
    sc, sc_b = ph.sb("sc", [128, 4, NC], F32)
    S, S_b = ph.sb("S", [128, 128], F32)
    Sps = [ph.sb("Sp%d" % i, [128, 128], BF16) for i in range(2)]
    tmpu, tmpu_b = ph.sb("tmpu", [128, 128], F32)
    atms = [ph.sb("atm%d" % i, [64, 64], BF16) for i in range(2)]
    rs, rs_b = ph.sb("rs", [64, 2, NC], F32)
    psA = [ph.ps("pA%d" % i) for i in range(3)]
    psO = [ph.ps("pO%d" % i) for i in range(2)]
    psU = [ph.ps("pU%d" % i) for i in range(2)]
    psT = [ph.ps("pT%d" % i, [128, 4, 128], BF16) for i in range(1)]
    for h in range(8):
        r0 = h * 128
        ph.dma("sp", fT[:], s_bfT[r0:r0 + 128, :], fT_b, writes=[fT_b])
        ph.dma("sp", qT[:], s_bqT[r0:r0 + 128, :], qT_b, writes=[qT_b])
        ph.dma("sp", iv[:], s_bi[:, r0:r0 + 128].rearrange("(c p) v -> p c v", p=64), iv_b, writes=[iv_b])
        ph.dma("sp", osb[:], s_bg[:, r0:r0 + 128].rearrange("(c p) v -> p c v", p=64), osb_b, writes=[osb_b])
        ph.op("act", lambda e: e.activation(out=fT[:], in_=fT[:], func=AF.Sigmoid), [fT_b], [fT_b])
        ph.op("dve", lambda e, h=h: e.tensor_scalar(out=fT[:], in0=fT[:], scalar1=oml[:, h:h + 1], scalar2=lb[:, h:h + 1],
                                                    op0=ALU.mult, op1=ALU.add), [fT_b, oml_b, lb_b], [fT_b])
        ph.op("act", lambda e: e.activation(out=Bc[:], in_=fT[:], func=AF.Ln), [fT_b], [Bc_b])
        ph.op("pool", lambda e: e.tensor_scalar(out=fT[:], in0=fT[:], scalar1=-1.0, scalar2=1.0, op0=ALU.mult,
                                                op1=ALU.add), [fT_b, Bc_b], [fT_b])
        ph.op("dve", lambda e: e.tensor_tensor_scan(out=Bc[:], data0=ones[:], data1=Bc[:], initial=0.0, op0=ALU.mult,
                                                    op1=ALU.add), [Bc_b, ones_b], [Bc_b])
        Bc3 = Bc[:].rearrange("p (c s) -> p c s", s=64)
        ph.op("dve", lambda e: e.memset(sc[:, 0, 0:1], 0.0), [], [sc_b])
        ph.op("dve", lambda e, Bc3=Bc3: e.tensor_copy(out=sc[:, 0, 1:NC], in_=Bc3[:, 0:NC - 1, 63]), [Bc_b], [sc_b])
        ph.op("dve", lambda e, Bc3=Bc3: e.tensor_tensor(out=sc[:, 1, :], in0=Bc3[:, :, 63], in1=sc[:, 0, :],
                                                        op=ALU.subtract), [Bc_b, sc_b], [sc_b])
        ph.op("dve", lambda e, Bc3=Bc3: e.tensor_tensor(out=sc[:, 2, :], in0=Bc3[:, :, 31], in1=sc[:, 0, :],
                                                        op=ALU.subtract), [Bc_b, sc_b], [sc_b])
        ph.op("dve", lambda e, Bc3=Bc3: e.tensor_tensor(out=sc[:, 3, :], in0=Bc3[:, :, 63], in1=Bc3[:, :, 31],
                                                        op=ALU.subtract), [Bc_b, sc_b], [sc_b])
        ph.op("act", lambda e: e.activation(out=sc[:, 1:4, :], in_=sc[:, 1:4, :], func=AF.Exp), [sc_b], [sc_b])
        ph.op("dve", lambda e, Bc3=Bc3: e.tensor_copy(out=E[:, 0:NC], in_=Bc3[:, :, 31]), [Bc_b], [E_b])
        ph.op("dve", lambda e, Bc3=Bc3: e.tensor_tensor(out=Bc3, in0=Bc3, in1=E[:, 0:NC].unsqueeze(2).broadcast_to(
            [128, NC, 64]), op=ALU.subtract), [Bc_b, E_b], [Bc_b])
        ph.op("act", lambda e: e.activation(out=E[:], in_=Bc[:], func=AF.Exp), [Bc_b], [E_b])
        ph.op("dve", lambda e: e.tensor_tensor(out=qd[:], in0=qT[:], in1=E[:], op=ALU.mult), [qT_b, E_b], [qd_b])
        ph.op("act", lambda e: e.activation(out=E[:], in_=Bc[:], func=AF.Exp, scale=-1.0), [Bc_b, qd_b], [E_b])
        ph.op("dve", lambda e: e.tensor_tensor(out=kd[:], in0=fT[:], in1=E[:], op=ALU.mult), [fT_b, E_b], [kd_b])
        for c4 in range(NC // 4):
            pT, pT_b = psT[0]
            for u in range(4):
                c = c4 * 4 + u
                ph.op("pe", lambda e, pT=pT, u=u, c=c: e.transpose(pT[0:64, u, :], kd[:, c * 64:(c + 1) * 64], ident[:]),
                      [kd_b, ident_b], [pT_b])
            ph.op("act", lambda e, pT=pT, c4=c4: e.copy(out=kdtm[:, c4 * 4:(c4 + 1) * 4, :], in_=pT[0:64, :, :]),
                  [pT_b], [kdtm_b])
        def emit_A(c):
            pA, pA_b = psA[c % 3]
            c0 = c * 64
            ph.op("pe", lambda e: e.matmul(pA[0:32, 0:64], kd[:, c0:c0 + 32], qd[:, c0:c0 + 64], start=True, stop=True),
                  [kd_b, qd_b], [pA_b])
            ph.op("pe", lambda e: e.matmul(pA[32:64, 32:64], kd[:, c0 + 32:c0 + 64], qd[:, c0 + 32:c0 + 64], start=True,
                                           stop=True), [kd_b, qd_b], [pA_b])
            ph.op("pe", lambda e: e.matmul(pA[32:64, 0:32], zer[:], qd[:, c0:c0 + 32], start=True, stop=True),
                  [zer_b, qd_b], [pA_b])

        def emit_atm(c):
            pA, pA_b = psA[c % 3]
            atm, atm_b = atms[c % 2]
            ph.op("dve", lambda e: e.tensor_tensor(out=atm[:], in0=pA[0:64, 0:64], in1=m01[:], op=ALU.mult),
                  [pA_b, m01_b], [atm_b])

        def emit_U(c):
            pU, pU_b = psU[c % 2]
            ph.op("pe", lambda e: e.matmul(pU[:, 0:128], kdtm[:, c, :], iv[:, c, :], start=True, stop=True),
                  [kdtm_b, iv_b], [pU_b])

        emit_A(0)
        emit_A(1)
        emit_U(0)
        emit_atm(0)
        for c in range(NC):
            pO, pO_b = psO[c % 2]
            pU, pU_b = psU[c % 2]
            atm, atm_b = atms[c % 2]
            Sp, Sp_b = Sps[c % 2]
            Spn, Spn_b = Sps[(c + 1) % 2]
            cs = slice(c * 64, (c + 1) * 64)
            ph.op("pe", lambda e, pO=pO, atm=atm, c=c: e.matmul(pO[0:64, 0:128], atm[:], iv[:, c, :], start=True,
                                                               stop=(c == 0)), [atm_b, iv_b], [pO_b])
            if c > 0:
                ph.op("pe", lambda e, pO=pO, cs=cs, Sp=Sp: e.matmul(pO[0:64, 0:128], qd[:, cs], Sp[:], start=False,
                                                                   stop=True), [qd_b, Sp_b], [pO_b])
            ph.op("act", lambda e, pO=pO, c=c: e.copy(out=oall[:, c, :], in_=pO[0:64, 0:128]), [pO_b], [oall_b])
            if c + 2 < NC:
                emit_A(c + 2)
            if c + 1 < NC - 1:
                emit_U(c + 1)
            if c < NC - 1:
                if c == 0:
                    ph.op("dve", lambda e, pU=pU, c=c: e.tensor_scalar(out=S[:], in0=pU[:, 0:128], scalar1=sc[:, 3, c:c + 1],
                                                                       scalar2=None, op0=ALU.mult), [pU_b, sc_b], [S_b])
                else:
                    ph.op("dve", lambda e, pU=pU, c=c: e.tensor_scalar(out=tmpu[:], in0=pU[:, 0:128],
                                                                       scalar1=sc[:, 3, c:c + 1], scalar2=None,
                                                                       op0=ALU.mult), [pU_b, sc_b], [tmpu_b])
                    ph.op("dve", lambda e, c=c: e.scalar_tensor_tensor(out=S[:], in0=S[:], scalar=sc[:, 1, c:c + 1],
                                                                       in1=tmpu[:], op0=ALU.mult, op1=ALU.add),
                          [S_b, tmpu_b, sc_b], [S_b])
                ph.op("dve", lambda e, Spn=Spn, c=c: e.tensor_scalar(out=Spn[:], in0=S[:], scalar1=sc[:, 2, c + 1:c + 2],
                                                                     scalar2=None, op0=ALU.mult), [S_b, sc_b], [Spn_b])
                emit_atm(c + 1)
        oflat = oall[:].rearrange("p c v -> p (c v)")
        ph.op("act", lambda e: e.activation(out=kdtm[:], in_=oall[:], func=AF.Square), [oall_b], [kdtm_b])
        ph.op("dve", lambda e: e.tensor_reduce(out=rs[:, 0, :], in_=kdtm[:], axis=AX.X, op=ALU.add), [kdtm_b], [rs_b])
        ph.op("dve", lambda e: e.tensor_scalar(out=rs[:, 0, :], in0=rs[:, 0, :], scalar1=1.0 / 128, scalar2=EPS,
                                               op0=ALU.mult, op1=ALU.add), [rs_b], [rs_b])
        ph.op("act", lambda e: e.activation(out=rs[:, 0, :], in_=rs[:, 0, :], func=AF.Sqrt), [rs_b], [rs_b])
        ph.op("dve", lambda e: e.reciprocal(out=rs[:, 1, :], in_=rs[:, 0, :]), [rs_b], [rs_b])
        ph.op("dve", lambda e: e.tensor_tensor(out=oall[:], in0=oall[:], in1=rs[:, 1, :].unsqueeze(2).broadcast_to(
            [64, NC, 128]), op=ALU.mult), [oall_b, rs_b], [oall_b])
        ph.op("pool", lambda e: e.tensor_tensor(out=oall[:], in0=oall[:], in1=ng[:].unsqueeze(1).broadcast_to(
            [64, NC, 128]), op=ALU.mult), [oall_b, ng_b], [oall_b])
        ph.op("dve", lambda e: e.tensor_tensor(out=osb[:], in0=oall[:], in1=osb[:], op=ALU.mult), [oall_b, osb_b], [osb_b])
        ph.dma("sp", s_ob[:, r0:r0 + 128].rearrange("(c p) v -> p c v", p=64), osb[:], osb_b, reads=[osb_b])
    ph.run()


def phase_c(ctx, s_cq, s_ck, s_cv, s_cg, cosT, sinS, cdec, s_oc):
    ph = Phase(ctx, "ret")
    ident, ident_b = make_ident(ph)
    dec, dec_b = ph.sb("dec", [128, 8], F32)
    ph.dma("sp", dec[:], cdec[:, :], dec_b, writes=[dec_b])
    m01, m01_b = ph.sb("m01", [128, 128], F32)
    ph.op("pool", lambda e: e.memset(m01[:], 1.0), [], [m01_b])
    ph.op("pool", lambda e: e.affine_select(out=m01[:], in_=m01[:], pattern=[[1, 128]], compare_op=ALU.is_ge, fill=0.0,
                                            base=0, channel_multiplier=-1), [m01_b], [m01_b])
    qrdT, qrdT_b = ph.sb("qrdT", [128, 2, T], BF16)
    krnT, krnT_b = ph.sb("krnT", [128, 2, T], BF16)
    krn, krn_b = ph.sb("krn", [128, NT, 256], BF16)
    v, v_b = ph.sb("v", [128, NT, 512], BF16)
    ins = [[ph.sb("in%d_%d" % (a, i), [128, 256], F32) for i in range(2)] for a in range(2)]
    cs_ = [ph.sb("cos%d" % i, [128, 256], F32) for i in range(2)]
    sn_ = [ph.sb("sin%d" % i, [128, 256], F32) for i in range(2)]
    t1s = [ph.sb("t1_%d" % i, [128, 256], F32) for i in range(2)]
    t2s = [ph.sb("t2_%d" % i, [128, 256], F32) for i in range(2)]
    qtm = [ph.sb("qtm%d" % i, [128, 256], BF16) for i in range(2)]
    W = [ph.sb("W%d" % i, [128, 512], F32) for i in range(2)]
    Wb = [ph.sb("Wb%d" % i, [128, 512], BF16) for i in range(2)]
    atms = [ph.sb("atm%d" % i, [128, 128], BF16) for i in range(2)]
    gts = [ph.sb("g%d" % i, [128, 512], BF16) for i in range(2)]
    outs = [ph.sb("o%d" % i, [128, 512], BF16) for i in range(2)]
    junk, junk_b = ph.sb("junk", [128, 512], BF16)
    sts = [ph.sb("st%d" % i, [128, 4], F32) for i in range(2)]
    psT = [ph.ps("pT%d" % i, [128, 2, 128], BF16) for i in range(2)]
    psA = [ph.ps("pA%d" % i) for i in range(1)]
    psO = [ph.ps("pO%d" % i) for i in range(2)]
    psU = [ph.ps("pU%d" % i) for i in range(2)]
    for h in range(4):
        gam = 1.0 - 2.0 ** (-5.0 - h)
        g128 = gam ** 128
        ph.dma("sp", v[:], s_cv[:, h * 512:(h + 1) * 512].rearrange("(c p) d -> p c d", p=128), v_b, writes=[v_b])
        for i in range(NT):
            rows = slice(i * 128, (i + 1) * 128)
            co, co_b = cs_[i % 2]
            sn, sn_b = sn_[i % 2]
            ph.dma("sp", co[:], cosT[rows, :], co_b, writes=[co_b])
            ph.dma("sp", sn[:], sinS[rows, :], sn_b, writes=[sn_b])
            for a, src in enumerate((s_cq, s_ck)):
                x, x_b = ins[a][i % 2]
                t1, t1_b = t1s[a]
                t2, t2_b = t2s[a]
                ph.dma("sp", x[:], src[rows, h * 256:(h + 1) * 256], x_b, writes=[x_b])
                x3 = x[:].rearrange("p (i two) -> p i two", two=2)
                s3 = sn[:].rearrange("p (i two) -> p i two", two=2)
                t23 = t2[:].rearrange("p (i two) -> p i two", two=2)
                ph.op("dve", lambda e, t1=t1, x=x, co=co: e.tensor_tensor(out=t1[:], in0=x[:], in1=co[:], op=ALU.mult),
                      [x_b, co_b], [t1_b])
                ph.op("pool", lambda e, t23=t23, x3=x3, s3=s3: e.tensor_tensor(out=t23[:, :, 0], in0=x3[:, :, 1],
                                                                               in1=s3[:, :, 0], op=ALU.mult),
                      [x_b, sn_b], [t2_b])
                ph.op("pool", lambda e, t23=t23, x3=x3, s3=s3: e.tensor_tensor(out=t23[:, :, 1], in0=x3[:, :, 0],
                                                                               in1=s3[:, :, 1], op=ALU.mult),
                      [x_b, sn_b], [t2_b])
                ph.op("dve", lambda e, t1=t1, t2=t2: e.tensor_tensor(out=t1[:], in0=t1[:], in1=t2[:], op=ALU.add),
                      [t1_b, t2_b], [t1_b])
                pT, pT_b = psT[a]
                if a == 0:
                    dst, dst_b = qtm[i % 2]
                    dstap = dst[:]
                else:
                    dst_b = krn_b
                    dstap = krn[:, i, :]
                ph.op("act", lambda e, dstap=dstap, t1=t1, a=a, h=h: e.activation(out=dstap, in_=t1[:], func=AF.Copy,
                                                                                  scale=dec[:, a * 4 + h:a * 4 + h + 1]),
                      [t1_b, dec_b], [dst_b])
                for kc in range(2):
                    ph.op("pe", lambda e, pT=pT, kc=kc, dstap=dstap: e.transpose(pT[:, kc, :],
                                                                                 dstap[:, kc * 128:(kc + 1) * 128], ident[:]),
                          [dst_b, ident_b], [pT_b])
                tgt, tgt_b = (qrdT, qrdT_b) if a == 0 else (krnT, krnT_b)
                ph.op("act", lambda e, tgt=tgt, pT=pT, i=i: e.copy(out=tgt[:, :, i * 128:(i + 1) * 128], in_=pT[:]),
                      [pT_b], [tgt_b])
        for c in range(NT):
            cs = slice(c * 128, (c + 1) * 128)
            pA, pA_b = psA[0]
            pO, pO_b = psO[c % 2]
            atm, atm_b = atms[c % 2]
            g, g_b = gts[c % 2]
            out, out_b = outs[c % 2]
            st, st_b = sts[c % 2]
            ph.dma("sp", g[:], s_cg[cs, h * 512:(h + 1) * 512], g_b, writes=[g_b])
            for kc in range(2):
                ph.op("pe", lambda e, pA=pA, kc=kc, cs=cs: e.matmul(pA[:, 0:128], krnT[:, kc, cs], qrdT[:, kc, cs],
                                                                   start=(kc == 0), stop=(kc == 1)),
                      [krnT_b, qrdT_b], [pA_b])
            ph.op("dve", lambda e, pA=pA, atm=atm: e.tensor_tensor(out=atm[:], in0=pA[:, 0:128], in1=m01[:], op=ALU.mult),
                  [pA_b, m01_b], [atm_b])
            ph.op("pe", lambda e, pO=pO, atm=atm, c=c: e.matmul(pO[:], atm[:], v[:, c, :], start=True, stop=(c == 0)),
                  [atm_b, v_b], [pO_b])
            if c > 0:
                for kc in range(2):
                    ph.op("pe", lambda e, pO=pO, kc=kc, cs=cs: e.matmul(pO[:], qrdT[:, kc, cs], Wb[kc][0][:], start=False,
                                                                       stop=(kc == 1)), [qrdT_b, Wb[kc][1]], [pO_b])
            if c < NT - 1:
                for kc in range(2):
                    pU, pU_b = psU[kc]
                    ph.op("pe", lambda e, pU=pU, kc=kc, c=c: e.matmul(pU[:], krn[:, c, kc * 128:(kc + 1) * 128], v[:, c, :],
                                                                     start=True, stop=True), [krn_b, v_b], [pU_b])
                    Wk, Wk_b = W[kc]
                    if c == 0:
                        ph.op("dve", lambda e, Wk=Wk, pU=pU: e.tensor_copy(out=Wk[:], in_=pU[:]), [pU_b], [Wk_b])
                    else:
                        ph.op("dve", lambda e, Wk=Wk, pU=pU, g128=g128: e.scalar_tensor_tensor(
                            out=Wk[:], in0=Wk[:], scalar=g128, in1=pU[:], op0=ALU.mult, op1=ALU.add), [Wk_b, pU_b], [Wk_b])
                    ph.op("act", lambda e, Wk=Wk, kc=kc, g128=g128: e.activation(out=Wb[kc][0][:], in_=Wk[:], func=AF.Copy,
                                                                                 scale=g128), [Wk_b], [Wb[kc][1]])
            ph.op("act", lambda e, pO=pO, st=st: e.activation(out=junk[:], in_=pO[:], func=AF.Square,
                                                             accum_out=st[:, 0:1]), [pO_b], [junk_b, st_b])
            ph.op("dve", lambda e, st=st: e.tensor_scalar(out=st[:, 1:2], in0=st[:, 0:1], scalar1=1.0 / 512, scalar2=EPS,
                                                          op0=ALU.mult, op1=ALU.add), [st_b], [st_b])
            ph.op("act", lambda e, st=st: e.activation(out=st[:, 2:3], in_=st[:, 1:2], func=AF.Sqrt), [st_b], [st_b])
            ph.op("dve", lambda e, st=st: e.reciprocal(out=st[:, 3:4], in_=st[:, 2:3]), [st_b], [st_b])
            ph.op("dve", lambda e, pO=pO, st=st, g=g, out=out: e.scalar_tensor_tensor(
                out=out[:], in0=pO[:], scalar=st[:, 3:4], in1=g[:], op0=ALU.mult, op1=ALU.mult), [pO_b, st_b, g_b], [out_b])
            ph.dma("sp", s_oc[cs, h * 512:(h + 1) * 512], out[:], out_b, reads=[out_b])
    ph.run()


def phase_merge(ctx, l, modbc, s_oa, s_ob, s_oc, s_gT, w_a, w_b, w_c, w_out, xres):
    ph = Phase(ctx, "mrg")
    ident, ident_b = make_ident(ph)
    Wbr, Wbr_b = ph.sb("Wbr", [128, 32, D], BF16)
    wo, wo_b = ph.sb("wo", [128, 8, D], BF16)
    gt, gt_b = ph.sb("gt", [128, D], F32)
    for kc in range(8):
        ph.dma("pool", Wbr[:, kc, :], w_a[l, kc * 128:(kc + 1) * 128, :], Wbr_b, writes=[Wbr_b])
        ph.dma("pool", Wbr[:, 8 + kc, :], w_b[l, kc * 128:(kc + 1) * 128, :], Wbr_b, writes=[Wbr_b])
        ph.dma("pool", wo[:, kc, :], w_out[l, kc * 128:(kc + 1) * 128, :], wo_b, writes=[wo_b])
    for kc in range(16):
        ph.dma("pool", Wbr[:, 16 + kc, :], w_c[l, kc * 128:(kc + 1) * 128, :], Wbr_b, writes=[Wbr_b])
    ph.dma("sp", gt[:], modbc[l, :, 2 * D:3 * D], gt_b, writes=[gt_b])
    oin = [ph.sb("oin%d" % i, [128, 4096], BF16) for i in range(2)]
    oT, oT_b = ph.sb("oT", [128, 32, 512], BF16)
    gs = [ph.sb("gs%d" % i, [128, 3, 512], BF16) for i in range(2)]
    tt1 = [ph.sb("tt%d" % i, [128, 512], F32) for i in range(3)]
    mT, mT_b = ph.sb("mT", [128, 8, 512], BF16)
    xs = [ph.sb("x%d" % i, [128, D], F32) for i in range(2)]
    ty = [ph.sb("ty%d" % i, [128, 512], F32) for i in range(2)]
    psT = [ph.ps("pT%d" % i, [128, 8, 128], BF16) for i in range(2)]
    psB = [ph.ps("pB%d" % i) for i in range(3)]
    psY = [ph.ps("pY%d" % i) for i in range(2)]
    nT = 0
    ny = 0
    ng = 0
    for g in range(T // 512):
        for tt in range(4):
            rows = slice(g * 512 + tt * 128, g * 512 + (tt + 1) * 128)
            o, o_b = oin[tt % 2]
            ph.dma("sp", o[:, 0:1024], s_oa[rows, :], o_b, writes=[o_b])
            ph.dma("sp", o[:, 1024:2048], s_ob[rows, :], o_b, writes=[o_b])
            ph.dma("sp", o[:, 2048:4096], s_oc[rows, :], o_b, writes=[o_b])
            for k8 in range(4):
                pT, pT_b = psT[nT % 2]
                nT += 1
                for u in range(8):
                    kc = k8 * 8 + u
                    ph.op("pe", lambda e, pT=pT, u=u, o=o, kc=kc: e.transpose(pT[:, u, :], o[:, kc * 128:(kc + 1) * 128],
                                                                           ident[:]), [o_b, ident_b], [pT_b])
                ph.op("act", lambda e, pT=pT, k8=k8, tt=tt: e.copy(out=oT[:, k8 * 8:(k8 + 1) * 8, tt * 128:(tt + 1) * 128],
                                                                 in_=pT[:]), [pT_b], [oT_b])
        for dc in range(8):
            gsb, gsb_b = gs[ng % 2]
            ng += 1
            ph.dma("sp", gsb[:], s_gT.rearrange("(b r) t -> r b t", b=3)[dc * 128:(dc + 1) * 128, :, g * 512:(g + 1) * 512],
                   gsb_b, writes=[gsb_b])
            for br, (k0, k1) in enumerate(((0, 8), (8, 16), (16, 32))):
                pB, pB_b = psB[br]
                for kc in range(k0, k1):
                    ph.op("pe", lambda e, pB=pB, kc=kc, dc=dc, k0=k0, k1=k1: e.matmul(
                        pB[:], Wbr[:, kc, dc * 128:(dc + 1) * 128], oT[:, kc, :], start=(kc == k0), stop=(kc == k1 - 1)),
                          [Wbr_b, oT_b], [pB_b])
                t, t_b = tt1[br]
                ph.op("dve", lambda e, t=t, pB=pB, gsb=gsb, br=br: e.tensor_tensor(out=t[:], in0=pB[:], in1=gsb[:, br, :],
                                                                                  op=ALU.mult), [pB_b, gsb_b], [t_b])
            ph.op("pool", lambda e: e.tensor_tensor(out=tt1[0][0][:], in0=tt1[0][0][:], in1=tt1[1][0][:], op=ALU.add),
                  [tt1[0][1], tt1[1][1]], [tt1[0][1]])
            ph.op("pool", lambda e, dc=dc: e.tensor_tensor(out=mT[:, dc, :], in0=tt1[0][0][:], in1=tt1[2][0][:], op=ALU.add),
                  [tt1[0][1], tt1[2][1]], [mT_b])
        for tt in range(4):
            rows = slice(g * 512 + tt * 128, g * 512 + (tt + 1) * 128)
            x, x_b = xs[tt % 2]
            ph.dma("sp", x[:], xres[rows, :], x_b, writes=[x_b])
            for dh in range(2):
                pY, pY_b = psY[ny % 2]
                t, t_b = ty[ny % 2]
                ny += 1
                for dc in range(8):
                    ph.op("pe", lambda e, pY=pY, dc=dc, tt=tt, dh=dh: e.matmul(
                        pY[:], mT[:, dc, tt * 128:(tt + 1) * 128], wo[:, dc, dh * 512:(dh + 1) * 512], start=(dc == 0),
                        stop=(dc == 7)), [mT_b, wo_b], [pY_b])
                ph.op("dve", lambda e, t=t, pY=pY, dh=dh: e.tensor_tensor(out=t[:], in0=pY[:],
                                                                          in1=gt[:, dh * 512:(dh + 1) * 512], op=ALU.mult),
                      [pY_b, gt_b], [t_b])
                ph.op("pool", lambda e, t=t, x=x, dh=dh: e.tensor_tensor(out=x[:, dh * 512:(dh + 1) * 512],
                                                                         in0=x[:, dh * 512:(dh + 1) * 512], in1=t[:],
                                                                         op=ALU.add), [t_b, x_b], [x_b])
            ph.dma("sp", xres[rows, :], x[:], x_b, reads=[x_b])
    ph.run()


TH = 2048
NTH = TH // 128


def phase_route(ctx, l, hh, xres, modbc, norm_g, rg_w, rg_b, re_w, re_b, s_hT, s_gates):
    ph = Phase(ctx, "rt")
    identb, identb_b = make_ident(ph)
    idf, idf_b = ph.sb("identf32", [128, 128], F32)
    ph.op("pool", lambda e: e.memset(idf[:], 1.0), [], [idf_b])
    ph.op("pool", lambda e: e.affine_select(out=idf[:], in_=idf[:], pattern=[[-1, 128]], compare_op=ALU.is_equal,
                                            fill=0.0, base=0, channel_multiplier=1), [idf_b], [idf_b])
    G, GS_b = ph.sb("G", [128, D], F32)
    S, _ = ph.sb("S", [128, D], F32)
    ng, _ = ph.sb("ng", [128, D], F32)
    ph.dma("sp", G[:], modbc[l, :, 4 * D:5 * D], GS_b, writes=[GS_b])
    ph.dma("sp", S[:], modbc[l, :, 3 * D:4 * D], GS_b, writes=[GS_b])
    ph.dma("sp", ng[:], bcast_rows(norm_g[l:l + 1, :], 128), GS_b, writes=[GS_b])
    ph.op("dve", lambda e: e.tensor_tensor(out=G[:], in0=G[:], in1=ng[:], op=ALU.mult), [GS_b], [GS_b])
    rw, rw_b = ph.sb("rw", [128, 8, 36], F32)
    rb, rb_b = ph.sb("rb", [128, 36], F32)
    ph.dma("sp", rw[:, :, 0:4], rg_w[l].rearrange("(k p) n -> p k n", p=128), rw_b, writes=[rw_b])
    ph.dma("sp", rw[:, :, 4:36], re_w[l].rearrange("(k p) n -> p k n", p=128), rw_b, writes=[rw_b])
    ph.dma("sp", rb[:, 0:4], bcast_rows(rg_b[l:l + 1, :], 128), rb_b, writes=[rb_b])
    ph.dma("sp", rb[:, 4:36], bcast_rows(re_b[l:l + 1, :], 128), rb_b, writes=[rb_b])
    hT, hT_b = ph.sb("hT", [128, 8, TH], BF16)
    gates, gates_b = ph.sb("gates", [128, NTH, 32], F32)
    xs = [ph.sb("x%d" % i, [128, D], F32) for i in range(2)]
    sq, sq_b = ph.sb("sq", [128, D], BF16)
    hs = [ph.sb("h%d" % i, [128, D], BF16) for i in range(2)]
    st = [ph.sb("st%d" % i, [128, 4], F32) for i in range(2)]
    h32T = [ph.sb("h32T%d" % i, [128, 8, 128], F32) for i in range(2)]
    lgs = [ph.sb("lg%d" % i, [128, 36], F32) for i in range(2)]
    rts = [ph.sb("r%d" % i, [128, 64], F32) for i in range(2)]
    pts = [ph.ps("pt%d" % i, [128, 8, 128], BF16) for i in range(2)]
    pfs = [ph.ps("pf%d" % i, [128, 4, 128], F32) for i in range(2)]
    pls = [ph.ps("pl%d" % i) for i in range(2)]
    for t in range(NTH):
        x, x_b = xs[t % 2]
        h, h_b = hs[t % 2]
        s, s_b = st[t % 2]
        pt, pt_b = pts[t % 2]
        hf, hf_b = h32T[t % 2]
        pl, pl_b = pls[t % 2]
        lg, lg_b = lgs[t % 2]
        r, r_b = rts[t % 2]
        row0 = hh * TH + t * 128
        ph.dma("sp", x[:], xres[row0:row0 + 128, :], x_b, writes=[x_b])
        ph.op("act", lambda e, x=x, s=s: e.activation(out=sq[:], in_=x[:], func=AF.Square, accum_out=s[:, 0:1]),
              [x_b], [sq_b, s_b])
        ph.op("dve", lambda e, s=s: e.tensor_scalar(out=s[:, 1:2], in0=s[:, 0:1], scalar1=1.0 / D, scalar2=EPS,
                                                    op0=ALU.mult, op1=ALU.add), [s_b], [s_b])
        ph.op("act", lambda e, s=s: e.activation(out=s[:, 2:3], in_=s[:, 1:2], func=AF.Sqrt), [s_b], [s_b])
        ph.op("dve", lambda e, s=s: e.reciprocal(out=s[:, 3:4], in_=s[:, 2:3]), [s_b], [s_b])
        ph.op("dve", lambda e, x=x, s=s: e.scalar_tensor_tensor(out=x[:], in0=x[:], scalar=s[:, 3:4], in1=G[:],
                                                                op0=ALU.mult, op1=ALU.mult), [x_b, s_b, GS_b], [x_b])
        ph.op("pool", lambda e, x=x: e.tensor_tensor(out=x[:], in0=x[:], in1=S[:], op=ALU.add), [x_b, GS_b], [x_b])
        ph.op("pool", lambda e, x=x, h=h: e.tensor_copy(out=h[:], in_=x[:]), [x_b], [h_b])
        for k in range(8):
            ph.op("pe", lambda e, k=k, h=h, pt=pt: e.transpose(pt[:, k, :], h[:, k * 128:(k + 1) * 128], identb[:]),
                  [h_b, identb_b], [pt_b])
        ph.op("act", lambda e, pt=pt, t=t: e.copy(out=hT[:, :, t * 128:(t + 1) * 128], in_=pt[:]), [pt_b], [hT_b])
        for half in range(2):
            pf, pf_b = pfs[half]
            for u in range(4):
                k = half * 4 + u
                ph.op("pe", lambda e, pf=pf, u=u, k=k, x=x: e.transpose(pf[:, u, :], x[:, k * 128:(k + 1) * 128], idf[:]),
                      [x_b, idf_b], [pf_b])
            ph.op("dve", lambda e, pf=pf, hf=hf, half=half: e.tensor_copy(out=hf[:, half * 4:(half + 1) * 4, :], in_=pf[:]),
                  [pf_b], [hf_b])
        for k in range(8):
            ph.op("pe", lambda e, pl=pl, hf=hf, k=k: e.matmul(pl[:, 0:36], hf[:, k, :], rw[:, k, :], start=(k == 0),
                                                             stop=(k == 7)), [hf_b, rw_b], [pl_b])
        ph.op("dve", lambda e, lg=lg, pl=pl: e.tensor_tensor(out=lg[:], in0=pl[:, 0:36], in1=rb[:], op=ALU.add),
              [pl_b, rb_b], [lg_b])
        def dv(fn, r=r, lg=lg):
            ph.op("dve", fn, [r_b, lg_b], [r_b])
        dv(lambda e, r=r, lg=lg: e.tensor_reduce(out=r[:, 0:1], in_=lg[:, 0:4], axis=AX.X, op=ALU.max))
        dv(lambda e, r=r, lg=lg: e.tensor_scalar(out=r[:, 16:20], in0=lg[:, 0:4], scalar1=r[:, 0:1], scalar2=None,
                                                 op0=ALU.is_equal))
        dv(lambda e, r=r: e.tensor_scalar(out=r[:, 1:2], in0=r[:, 0:1], scalar1=-1.0, scalar2=None, op0=ALU.mult))
        ph.op("act", lambda e, r=r, lg=lg: e.activation(out=r[:, 20:24], in_=lg[:, 0:4], func=AF.Exp, bias=r[:, 1:2]),
              [r_b, lg_b], [r_b])
        dv(lambda e, r=r: e.tensor_reduce(out=r[:, 2:3], in_=r[:, 20:24], axis=AX.X, op=ALU.add))
        dv(lambda e, r=r: e.reciprocal(out=r[:, 3:4], in_=r[:, 2:3]))
        dv(lambda e, r=r, lg=lg: e.tensor_scalar(out=r[:, 24:32], in0=lg[:, 4:12], scalar1=r[:, 16:17], scalar2=None,
                                                 op0=ALU.mult))
        for g in range(1, 4):
            dv(lambda e, r=r, lg=lg, g=g: e.scalar_tensor_tensor(out=r[:, 24:32], in0=lg[:, 4 + 8 * g:12 + 8 * g],
                                                                 scalar=r[:, 16 + g:17 + g], in1=r[:, 24:32],
                                                                 op0=ALU.mult, op1=ALU.add))
        dv(lambda e, r=r: e.tensor_reduce(out=r[:, 4:5], in_=r[:, 24:32], axis=AX.X, op=ALU.max))
        dv(lambda e, r=r: e.tensor_scalar(out=r[:, 32:40], in0=r[:, 24:32], scalar1=r[:, 4:5], scalar2=None,
                                          op0=ALU.is_equal))
        dv(lambda e, r=r: e.scalar_tensor_tensor(out=r[:, 40:48], in0=r[:, 32:40], scalar=-1e30, in1=r[:, 24:32],
                                                 op0=ALU.mult, op1=ALU.add))
        dv(lambda e, r=r: e.tensor_reduce(out=r[:, 5:6], in_=r[:, 40:48], axis=AX.X, op=ALU.max))
        dv(lambda e, r=r: e.tensor_scalar(out=r[:, 48:56], in0=r[:, 40:48], scalar1=r[:, 5:6], scalar2=None,
                                          op0=ALU.is_equal))
        dv(lambda e, r=r: e.tensor_tensor(out=r[:, 6:7], in0=r[:, 5:6], in1=r[:, 4:5], op=ALU.subtract))
        ph.op("act", lambda e, r=r: e.activation(out=r[:, 7:8], in_=r[:, 6:7], func=AF.Exp), [r_b], [r_b])
        dv(lambda e, r=r: e.tensor_scalar(out=r[:, 8:9], in0=r[:, 7:8], scalar1=1.0, scalar2=None, op0=ALU.add))
        dv(lambda e, r=r: e.reciprocal(out=r[:, 9:10], in_=r[:, 8:9]))
        dv(lambda e, r=r: e.tensor_tensor(out=r[:, 10:11], in0=r[:, 9:10], in1=r[:, 3:4], op=ALU.mult))
        dv(lambda e, r=r: e.tensor_tensor(out=r[:, 11:12], in0=r[:, 10:11], in1=r[:, 7:8], op=ALU.mult))
        dv(lambda e, r=r: e.tensor_scalar(out=r[:, 56:64], in0=r[:, 32:40], scalar1=r[:, 10:11], scalar2=None,
                                          op0=ALU.mult))
        dv(lambda e, r=r: e.scalar_tensor_tensor(out=r[:, 56:64], in0=r[:, 48:56], scalar=r[:, 11:12], in1=r[:, 56:64],
                                                 op0=ALU.mult, op1=ALU.add))
        for g in range(4):
            ph.op("dve", lambda e, r=r, g=g, t=t: e.tensor_scalar(out=gates[:, t, g * 8:(g + 1) * 8], in0=r[:, 56:64],
                                                                  scalar1=r[:, 16 + g:17 + g], scalar2=None, op0=ALU.mult),
                  [r_b], [gates_b])
    for k in range(8):
        ph.dma("sp", s_hT[:, k, :], hT[:, k, :], hT_b, reads=[hT_b])
    ph.dma("sp", s_gates.rearrange("(t p) e -> p t e", p=128), gates[:], gates_b, reads=[gates_b])
    ph.run()


def phase_experts(ctx, l, hh, xres, modbc, s_hT, s_gates, e_gate, e_up, e_down, n_exp=32):
    ph = Phase(ctx, "ex")
    hT, hT_b = ph.sb("hT", [128, 8, TH], BF16)
    gates, gates_b = ph.sb("gates", [128, NTH, 32], F32)
    gt, gt_b = ph.sb("gt", [128, D], F32)
    yacc, yacc_b = ph.sb("yacc", [128, NTH, D], F32)
    for k in range(8):
        ph.dma("sp", hT[:, k, :], s_hT[:, k, :], hT_b, writes=[hT_b])
    ph.dma("sp", gates[:], s_gates.rearrange("(t p) e -> p t e", p=128), gates_b, writes=[gates_b])
    ph.dma("sp", gt[:], modbc[l, :, 5 * D:6 * D], gt_b, writes=[gt_b])
    wg = [ph.sb("wg%d" % i, [128, 8, 512], BF16) for i in range(2)]
    wu = [ph.sb("wu%d" % i, [128, 8, 512], BF16) for i in range(2)]
    wd = [ph.sb("wd%d" % i, [128, 4, D], BF16) for i in range(2)]
    sgs = [ph.sb("sg%d" % i, [128, 512], F32) for i in range(2)]
    hgs = [ph.sb("hg%d" % i, [128, 4, 512], BF16) for i in range(2)]
    psG = [ph.ps("pG%d" % i) for i in range(2)]
    psU = [ph.ps("pU%d" % i) for i in range(2)]
    psY = [ph.ps("pY%d" % i) for i in range(4)]
    nf = 0
    ny = 0
    nh = 0
    for ex in range(n_exp):
        g_, g_b = wg[ex % 2]
        u_, u_b = wu[ex % 2]
        d_, d_b = wd[ex % 2]
        ph.dma("pool", g_[:], e_gate[l, ex].rearrange("(k p) f -> p k f", p=128), g_b, writes=[g_b])
        ph.dma("pool", u_[:], e_up[l, ex].rearrange("(k p) f -> p k f", p=128), u_b, writes=[u_b])
        ph.dma("pool", d_[:], e_down[l, ex].rearrange("(k p) f -> p k f", p=128), d_b, writes=[d_b])
        for tg in range(TH // 512):
            ts = slice(tg * 512, (tg + 1) * 512)
            hg, hg_b = hgs[nh % 2]
            nh += 1
            for fc in range(4):
                pG, pG_b = psG[nf % 2]
                pU, pU_b = psU[nf % 2]
                sg, sg_b = sgs[nf % 2]
                nf += 1
                for k in range(8):
                    ph.op("pe", lambda e, pG=pG, g_=g_, k=k, fc=fc, ts=ts: e.matmul(
                        pG[:], g_[:, k, fc * 128:(fc + 1) * 128], hT[:, k, ts], start=(k == 0), stop=(k == 7)),
                          [g_b, hT_b], [pG_b])
                for k in range(8):
                    ph.op("pe", lambda e, pU=pU, u_=u_, k=k, fc=fc, ts=ts: e.matmul(
                        pU[:], u_[:, k, fc * 128:(fc + 1) * 128], hT[:, k, ts], start=(k == 0), stop=(k == 7)),
                          [u_b, hT_b], [pU_b])
                ph.op("act", lambda e, sg=sg, pG=pG: e.activation(out=sg[:], in_=pG[:], func=AF.Silu), [pG_b], [sg_b])
                ph.op("dve", lambda e, hg=hg, fc=fc, sg=sg, pU=pU: e.tensor_tensor(out=hg[:, fc, :], in0=pU[:], in1=sg[:],
                                                                                  op=ALU.mult), [pU_b, sg_b], [hg_b])
            for tt in range(4):
                tile = tg * 4 + tt
                for dh in range(2):
                    pY, pY_b = psY[ny % 4]
                    ny += 1
                    for fc in range(4):
                        ph.op("pe", lambda e, pY=pY, hg=hg, fc=fc, tt=tt, d_=d_, dh=dh: e.matmul(
                            pY[:], hg[:, fc, tt * 128:(tt + 1) * 128], d_[:, fc, dh * 512:(dh + 1) * 512], start=(fc == 0),
                            stop=(fc == 3)), [hg_b, d_b], [pY_b])
                    ya = yacc[:, tile, dh * 512:(dh + 1) * 512]
                    if ex == 0:
                        ph.op("dve", lambda e, ya=ya, pY=pY, tile=tile, ex=ex: e.tensor_scalar(
                            out=ya, in0=pY[:], scalar1=gates[:, tile, ex:ex + 1], scalar2=None, op0=ALU.mult),
                              [pY_b, gates_b], [yacc_b])
                    else:
                        ph.op("dve", lambda e, ya=ya, pY=pY, tile=tile, ex=ex: e.scalar_tensor_tensor(
                            out=ya, in0=pY[:], scalar=gates[:, tile, ex:ex + 1], in1=ya, op0=ALU.mult, op1=ALU.add),
                              [pY_b, gates_b, yacc_b], [yacc_b])
    xs = [ph.sb("x%d" % i, [128, D], F32) for i in range(2)]
    for t in range(NTH):
        x, x_b = xs[t % 2]
        row0 = hh * TH + t * 128
        ph.dma("sp", x[:], xres[row0:row0 + 128, :], x_b, writes=[x_b])
        ph.op("pool", lambda e, t=t: e.tensor_tensor(out=yacc[:, t, :], in0=yacc[:, t, :], in1=gt[:], op=ALU.mult),
              [yacc_b, gt_b], [yacc_b])
        ph.op("pool", lambda e, x=x, t=t: e.tensor_tensor(out=x[:], in0=x[:], in1=yacc[:, t, :], op=ALU.add),
              [x_b, yacc_b], [x_b])
        ph.dma("sp", xres[row0:row0 + 128, :], x[:], x_b, reads=[x_b])
    ph.run()


def phase_final(ctx, xres, final_g, y):
    ph = Phase(ctx, "fin")
    G, G_b = ph.sb("G", [128, D], F32)
    ph.dma("sp", G[:], bcast_rows(final_g, 128), G_b, writes=[G_b])
    xs = [ph.sb("x%d" % i, [128, D], F32) for i in range(3)]
    sq, sq_b = ph.sb("sq", [128, D], BF16)
    st = [ph.sb("st%d" % i, [128, 4], F32) for i in range(2)]
    for t in range(NT):
        x, x_b = xs[t % 3]
        s, s_b = st[t % 2]
        ph.dma("sp", x[:], xres[t * 128:(t + 1) * 128, :], x_b, writes=[x_b])
        ph.op("act", lambda e, x=x, s=s: e.activation(out=sq[:], in_=x[:], func=AF.Square, accum_out=s[:, 0:1]),
              [x_b], [sq_b, s_b])
        ph.op("dve", lambda e, s=s: e.tensor_scalar(out=s[:, 1:2], in0=s[:, 0:1], scalar1=1.0 / D, scalar2=EPS,
                                                    op0=ALU.mult, op1=ALU.add), [s_b], [s_b])
        ph.op("act", lambda e, s=s: e.activation(out=s[:, 2:3], in_=s[:, 1:2], func=AF.Sqrt), [s_b], [s_b])
        ph.op("dve", lambda e, s=s: e.reciprocal(out=s[:, 3:4], in_=s[:, 2:3]), [s_b], [s_b])
        ph.op("dve", lambda e, x=x, s=s: e.scalar_tensor_tensor(out=x[:], in0=x[:], scalar=s[:, 3:4], in1=G[:],
                                                                op0=ALU.mult, op1=ALU.mult), [x_b, s_b, G_b], [x_b])
        ph.dma("sp", y[t * 128:(t + 1) * 128, :], x[:], x_b, reads=[x_b])
    ph.run()


def make_oh_tab():
    u = np.arange(384)
    rel = np.maximum(255 - u, 0)
    relf = np.maximum(rel, 1).astype(np.float32)
    large = 16 + (np.log(relf / np.float32(16)) / np.float32(math.log(8)) * np.float32(16)).astype(np.int32)
    large = np.minimum(large, 31)
    bucket = np.where(rel < 16, rel, large)
    oh = np.zeros((32, 384), np.float32)
    oh[bucket, u] = 1.0
    return oh


def make_ret_tables():
    pos = np.arange(T, dtype=np.float32)
    theta = (np.float32(1.0) / (np.float32(10000.0) ** np.linspace(0.0, 1.0, 128, dtype=np.float32))).astype(np.float32)
    theta = np.repeat(theta, 2)
    ang = (pos[:, None] * theta[None, :]).astype(np.float32)
    cos = np.cos(ang.astype(np.float64)).astype(np.float32)
    sin = np.sin(ang.astype(np.float64)).astype(np.float32)
    sinS = sin.copy()
    sinS[:, 0::2] = -sin[:, 0::2]
    p = np.arange(128, dtype=np.float64)
    dec = np.zeros((128, 8), np.float32)
    for h in range(4):
        gam = 1.0 - 2.0 ** (-5.0 - h)
        dec[:, h] = gam ** (p + 1)
        dec[:, 4 + h] = gam ** (-(p + 1)) / 16.0
    return cos, sinS, dec


def phase_copy(ctx, src, dst, nrows):
    ph = Phase(ctx, "cp")
    b = ph.buf("cpbuf")
    step = 512
    for r in range(0, nrows, step):
        ph.dma("sp", dst[r:r + step, :], src[r:r + step, :], b)
    ph.run()
```

```python
import contextlib
import math
import numpy as np
import concourse.bass as bass
import concourse.mybir as mybir
from concourse.bass_utils import run_bass_kernel_spmd

F32 = mybir.dt.float32
BF16 = mybir.dt.bfloat16
AF = mybir.ActivationFunctionType
ALU = mybir.AluOpType
AX = mybir.AxisListType

D = 1024
T = 4096
DEPTH = 4
NT = T // 128
N_IN = 16968
EPS = 1e-6
O_AQ, O_AK, O_AV = 0, 1024, 2048
O_IQ, O_IK, O_IW = 3072, 3584, 3648
O_BQ, O_BF, O_BI, O_BG = 3656, 4680, 5704, 6728
O_CQ, O_CK, O_CV, O_CG = 7752, 8776, 9800, 11848
O_GA, O_GB, O_GC = 13896, 14920, 15944

COMPUTE = ("pe", "act", "dve", "pool")


class Buf:
    __slots__ = ("name", "writers", "readers", "sem")

    def __init__(self, name):
        self.name = name
        self.writers = []
        self.readers = []
        self.sem = None


class Op:
    __slots__ = ("eng", "emit", "deps", "is_dma", "sem", "semval", "used", "sig")

    def __init__(self, eng, emit, is_dma):
        self.eng = eng
        self.emit = emit
        self.deps = []
        self.is_dma = is_dma
        self.sem = None
        self.semval = 0
        self.used = False
        self.sig = 0


class Ctx:
    def __init__(self, nc, stack):
        self.nc = nc
        self.eng_sem = {}
        self.eng_cnt = {}
        for e in COMPUTE:
            self.eng_sem[e] = stack.enter_context(nc.semaphore("es_" + e))
            self.eng_cnt[e] = 0
        self.pool = []
        for i in range(56):
            self.pool.append([stack.enter_context(nc.semaphore("ds%d" % i)), 0])
        self.nphase = 0


class Phase:
    def __init__(self, ctx, name):
        self.ctx = ctx
        self.nc = ctx.nc
        self.name = "%s_%d" % (name, ctx.nphase)
        ctx.nphase += 1
        self.ops = []
        self.stack = contextlib.ExitStack()
        self.sems = []
        self.nbuf = 0

    def sb(self, name, shape, dtype):
        t = self.stack.enter_context(self.nc.sbuf_tensor("%s_%s" % (self.name, name), list(shape), dtype))
        return t, Buf(name)

    def ps(self, name, shape=(128, 512), dtype=F32):
        t = self.stack.enter_context(self.nc.psum_tensor("%s_%s" % (self.name, name), list(shape), dtype))
        return t, Buf(name)

    def buf(self, name):
        return Buf(name)

    def _sem_for(self, b):
        if b.sem is None:
            b.sem = self.ctx.pool.pop()
            self.sems.append(b.sem)
        return b.sem

    def _record(self, op, reads, writes):
        deps = []
        for b in reads:
            deps.extend(b.writers)
        for b in writes:
            keep = []
            for w in b.writers:
                if op.is_dma and w.is_dma and w.sem is op.sem:
                    keep.append(w)
                else:
                    deps.append(w)
            for r in b.readers:
                deps.append(r)
            b.writers = keep + [op]
            b.readers = []
        for b in reads:
            b.readers.append(op)
        seen = set()
        for d in deps:
            if (not d.is_dma) and d.eng == "pe" and op.eng == "pe":
                continue
            if id(d) not in seen and d is not op:
                seen.add(id(d))
                d.used = True
                op.deps.append(d)
        self.ops.append(op)

    def op(self, eng, emit, reads=(), writes=()):
        o = Op(eng, emit, False)
        self._record(o, reads, writes)
        return o

    def dma(self, eng, out, in_, sbuf, reads=(), writes=(), **kw):
        o = Op(eng, None, True)
        o.sem = self._sem_for(sbuf)
        o.sem[1] += 16
        o.semval = o.sem[1]
        o.emit = lambda e: e.dma_start(out=out, in_=in_, **kw)
        self._record(o, reads, writes)
        return o

    def run(self):
        ctx = self.ctx
        nc = self.nc
        for e in COMPUTE:
            for o in self.ops:
                if o.eng == e and not o.is_dma and o.used:
                    ctx.eng_cnt[e] += 1
                    o.sig = ctx.eng_cnt[e]
        engs = {"pe": [], "act": [], "dve": [], "pool": [], "sp": []}
        for o in self.ops:
            engs[o.eng].append(o)
        final_waits = [(s[0], s[1]) for s in self.sems]

        def emit_engine(e, name):
            waited = {}
            for o in engs[name]:
                need = {}
                for d in o.deps:
                    if d.is_dma:
                        key, val = d.sem[0], d.semval
                    else:
                        if d.eng == name and name == "pe":
                            continue
                        key, val = ctx.eng_sem[d.eng], d.sig
                    if need.get(key, 0) < val:
                        need[key] = val
                for key, val in need.items():
                    if waited.get(key, 0) < val:
                        e.wait_ge(key, val)
                        waited[key] = val
                ins = o.emit(e)
                if o.is_dma:
                    ins.then_inc(o.sem[0], 16)
                elif o.used:
                    ins.then_inc(ctx.eng_sem[name], 1)
            if name == "sp":
                for s, v in final_waits:
                    e.wait_ge(s, v)

        with nc.Block() as blk:
            blk.tensor(lambda e: emit_engine(e, "pe"))
            blk.scalar(lambda e: emit_engine(e, "act"))
            blk.vector(lambda e: emit_engine(e, "dve"))
            blk.gpsimd(lambda e: emit_engine(e, "pool"))
            blk.sync(lambda e: emit_engine(e, "sp"))
        for s in self.sems:
            ctx.pool.append(s)
        self.stack.close()


def bcast_rows(ap2d, nrows):
    return bass.AP(ap2d.tensor, ap2d.offset, [[0, nrows], [1, ap2d.shape[-1]]])


def phase_mod(ctx, c, ada_w, ada_b, modbc):
    nc = ctx.nc
    ph = Phase(ctx, "mod")
    cT, cT_b = ph.sb("cT", [128, 8], F32)
    cs, cs_b = ph.sb("cs", [128, 8], F32)
    crep, crep_b = ph.sb("crep", [128, 8, 128], F32)
    wts = [ph.sb("w%d" % i, [128, 8, 512], F32) for i in range(2)]
    bia, bia_b = ph.sb("bias", [128, 6144], F32)
    outs = [ph.sb("o%d" % i, [128, 512], F32) for i in range(2)]
    pss = [ph.ps("ps%d" % i) for i in range(2)]
    ph.dma("sp", cT[:], c.rearrange("o (k p) -> p (o k)", p=128), cT_b, writes=[cT_b],
           allow_slow_non_contiguous=True)
    ph.op("act", lambda e: e.activation(out=cs[:], in_=cT[:], func=AF.Silu), [cT_b], [cs_b])
    ph.op("dve", lambda e: e.tensor_copy(out=crep[:], in_=cs[:].unsqueeze(2).broadcast_to([128, 8, 128])),
          [cs_b], [crep_b])
    n = 0
    for l in range(DEPTH):
        ph.dma("sp", bia[:], bcast_rows(ada_b[l:l + 1, :], 128), bia_b, writes=[bia_b])
        for seg in (1, 4):
            ph.op("pool", lambda e, seg=seg: e.tensor_scalar_add(out=bia[:, seg * 1024:(seg + 1) * 1024],
                                                                 in0=bia[:, seg * 1024:(seg + 1) * 1024], scalar1=1.0),
                  [bia_b], [bia_b])
        for j in range(12):
            w, w_b = wts[n % 2]
            o, o_b = outs[n % 2]
            p, p_b = pss[n % 2]
            n += 1
            ph.dma("sp", w[:], ada_w[l, :, j * 512:(j + 1) * 512].rearrange("(k p) n -> p k n", p=128), w_b,
                   writes=[w_b])
            for k in range(8):
                ph.op("pe", lambda e, k=k, w=w, p=p: e.matmul(p[:], crep[:, k, :], w[:, k, :], start=(k == 0),
                                                               stop=(k == 7)),
                      [crep_b, w_b], [p_b])
            ph.op("dve", lambda e, o=o, p=p, j=j: e.tensor_tensor(out=o[:], in0=p[:], in1=bia[:, j * 512:(j + 1) * 512],
                                                                   op=ALU.add), [p_b, bia_b], [o_b])
            ph.dma("sp", modbc[l, :, j * 512:(j + 1) * 512], o[:], o_b, reads=[o_b])
    ph.run()


def emit_norm_tiles(ph, xsrc, hT, hT_b, G, S, GS_b, ident, ident_b, ntiles, h32T=None, h32T_b=None):
    xs = [ph.sb("x%d" % i, [128, D], F32) for i in range(2)]
    sq, sq_b = ph.sb("sq", [128, D], BF16)
    hs = [ph.sb("h%d" % i, [128, D], BF16) for i in range(2)]
    st = [ph.sb("st%d" % i, [128, 4], F32) for i in range(2)]
    pts = [ph.ps("pt%d" % i, [128, 8, 128], BF16) for i in range(2)]
    for t in range(ntiles):
        x, x_b = xs[t % 2]
        h, h_b = hs[t % 2]
        s, s_b = st[t % 2]
        pt, pt_b = pts[t % 2]
        ph.dma("sp", x[:], xsrc[t * 128:(t + 1) * 128, :], x_b, writes=[x_b])
        ph.op("act", lambda e, x=x, s=s: e.activation(out=sq[:], in_=x[:], func=AF.Square, accum_out=s[:, 0:1]),
              [x_b], [sq_b, s_b])
        ph.op("dve", lambda e, s=s: e.tensor_scalar(out=s[:, 1:2], in0=s[:, 0:1], scalar1=1.0 / D, scalar2=EPS,
                                                    op0=ALU.mult, op1=ALU.add), [s_b], [s_b])
        ph.op("act", lambda e, s=s: e.activation(out=s[:, 2:3], in_=s[:, 1:2], func=AF.Sqrt), [s_b], [s_b])
        ph.op("dve", lambda e, s=s: e.reciprocal(out=s[:, 3:4], in_=s[:, 2:3]), [s_b], [s_b])
        ph.op("dve", lambda e, x=x, s=s: e.scalar_tensor_tensor(out=x[:], in0=x[:], scalar=s[:, 3:4], in1=G[:],
                                                                op0=ALU.mult, op1=ALU.mult), [x_b, s_b, GS_b], [x_b])
        if S is not None:
            ph.op("pool", lambda e, x=x, h=h: e.tensor_tensor(out=h[:], in0=x[:], in1=S[:], op=ALU.add),
                  [x_b, GS_b], [h_b])
        else:
            ph.op("pool", lambda e, x=x, h=h: e.tensor_copy(out=h[:], in_=x[:]), [x_b], [h_b])
        for k in range(8):
            ph.op("pe", lambda e, k=k, h=h, pt=pt: e.transpose(pt[:, k, :], h[:, k * 128:(k + 1) * 128], ident[:]),
                  [h_b, ident_b], [pt_b])
        ph.op("act", lambda e, pt=pt, t=t: e.copy(out=hT[:, :, t * 128:(t + 1) * 128], in_=pt[:]), [pt_b], [hT_b])


def make_ident(ph, dtype=BF16):
    nc = ph.nc
    idf, idf_b = ph.sb("identf", [128, 128], F32)
    ident, ident_b = ph.sb("ident", [128, 128], dtype)
    ph.op("pool", lambda e: e.memset(idf[:], 1.0), [], [idf_b])
    ph.op("pool", lambda e: e.affine_select(out=idf[:], in_=idf[:], pattern=[[-1, 128]], compare_op=ALU.is_equal,
                                            fill=0.0, base=0, channel_multiplier=1), [idf_b], [idf_b])
    ph.op("pool", lambda e: e.tensor_copy(out=ident[:], in_=idf[:]), [idf_b], [ident_b])
    return ident, ident_b


def phase_proj(ctx, xres, modbc, l, norm_g, w_in_l, jobs, seg_scale, seg_shift):
    nc = ctx.nc
    ph = Phase(ctx, "proj")
    ident, ident_b = make_ident(ph)
    G, GS_b = ph.sb("G", [128, D], F32)
    S, _ = ph.sb("S", [128, D], F32)
    ng, _ = ph.sb("ng", [128, D], F32)
    hT, hT_b = ph.sb("hT", [128, 8, T], BF16)
    ph.dma("sp", G[:], modbc[l, :, seg_scale * D:(seg_scale + 1) * D], GS_b, writes=[GS_b])
    ph.dma("sp", S[:], modbc[l, :, seg_shift * D:(seg_shift + 1) * D], GS_b, writes=[GS_b])
    ph.dma("sp", ng[:], bcast_rows(norm_g[l:l + 1, :], 128), GS_b, writes=[GS_b])
    ph.op("dve", lambda e: e.tensor_tensor(out=G[:], in0=G[:], in1=ng[:], op=ALU.mult), [GS_b], [GS_b])
    emit_norm_tiles(ph, xres, hT, hT_b, G, S, GS_b, ident, ident_b, NT)

    wts = [ph.sb("w%d" % i, [128, 8, 512], BF16) for i in range(3)]
    pss = [ph.ps("pp%d" % i) for i in range(4)]
    stg = [ph.sb("sg%d" % i, [128, 512], F32) for i in range(4)]
    stgb = [ph.sb("sgb%d" % i, [128, 512], BF16) for i in range(4)]
    nw = 0
    ne = 0
    for (c0, ncols, kind, dst_fn, dtype, func) in jobs:
        step = 512 if kind == "tok" else 128
        for cc in range(c0, c0 + ncols, step):
            n = min(step, c0 + ncols - cc)
            w, w_b = wts[nw % 3]
            nw += 1
            ph.dma("pool", w[:, :, 0:n], w_in_l[:, cc:cc + n].rearrange("(k p) n -> p k n", p=128), w_b,
                   writes=[w_b])
            nchunk = NT if kind == "tok" else T // 512
            for ci in range(nchunk):
                p, p_b = pss[ne % 4]
                if dtype == F32:
                    sg, sg_b = stg[ne % 4]
                else:
                    sg, sg_b = stgb[ne % 4]
                evac_eng = "act" if (func is not None or ne % 2 == 0) else "dve"
                ne += 1
                if kind == "tok":
                    for k in range(8):
                        ph.op("pe", lambda e, k=k, w=w, p=p, ci=ci, n=n: e.matmul(
                            p[:, 0:n], hT[:, k, ci * 128:(ci + 1) * 128], w[:, k, 0:n], start=(k == 0), stop=(k == 7)),
                              [hT_b, w_b], [p_b])
                    po, so = p[:, 0:n], sg[:, 0:n]
                    dst = dst_fn(ci * 128, cc - c0, n)
                else:
                    for k in range(8):
                        ph.op("pe", lambda e, k=k, w=w, p=p, ci=ci, n=n: e.matmul(
                            p[0:n, :], w[:, k, 0:n], hT[:, k, ci * 512:(ci + 1) * 512], start=(k == 0), stop=(k == 7)),
                              [hT_b, w_b], [p_b])
                    po, so = p[0:n, :], sg[0:n, :]
                    dst = dst_fn(cc - c0, n, ci * 512)
                if evac_eng == "act":
                    f = func if func is not None else AF.Copy
                    ph.op("act", lambda e, po=po, so=so, f=f: e.activation(out=so, in_=po, func=f), [p_b], [sg_b])
                else:
                    ph.op("dve", lambda e, po=po, so=so: e.tensor_copy(out=so, in_=po), [p_b], [sg_b])
                ph.dma("sp", dst, so, sg_b, reads=[sg_b])
    ph.run()


INPUT_SPECS = [
    ("x", [T, D]), ("c", [1, D]), ("rel_bias", [32, 8]), ("hgrn_lb_raw", [DEPTH, D]), ("norm1_g", [DEPTH, D]),
    ("norm2_g", [DEPTH, D]), ("ada_w", [DEPTH, D, 6 * D]), ("ada_b", [DEPTH, 6 * D]), ("w_in", [DEPTH, D, N_IN]),
    ("hgrn_norm_g", [DEPTH, 128]), ("w_branch_a", [DEPTH, 1024, D]), ("w_branch_b", [DEPTH, 1024, D]),
    ("w_branch_c", [DEPTH, 2048, D]), ("w_out", [DEPTH, D, D]), ("router_group_w", [DEPTH, D, 4]),
    ("router_group_b", [DEPTH, 4]), ("router_expert_w", [DEPTH, D, 32]), ("router_expert_b", [DEPTH, 32]),
    ("expert_w_gate", [DEPTH, 32, D, 512]), ("expert_w_up", [DEPTH, 32, D, 512]), ("expert_w_down", [DEPTH, 32, 512, D]),
    ("final_norm_g", [1, D]), ("oh_tab", [32, 384]), ("cosT", [T, 256]), ("sinS", [T, 256]), ("cdec", [128, 8]),
]


def build_program(depth=DEPTH, debug=False):
    nc = bass.Bass("TRN2", target_bir_lowering=False)
    I = {}
    for name, shape in INPUT_SPECS:
        I[name] = nc.dram_tensor(name, shape, F32, kind="ExternalInput").ap()
    y = nc.dram_tensor("y", [T, D], F32, kind="ExternalOutput").ap()

    def scr(name, shape, dt):
        return nc.dram_tensor(name, shape, dt, kind="ExternalOutput" if debug else "Internal").ap()

    xres = scr("xres", [T, D], F32)
    modbc = scr("modbc", [DEPTH, 128, 6 * D], F32)
    s_aqT = scr("s_aqT", [1024, T], BF16)
    s_akT = scr("s_akT", [1024, T], BF16)
    s_av = scr("s_av", [T, 1024], BF16)
    s_iqT = scr("s_iqT", [512, T], BF16)
    s_ikT = scr("s_ikT", [64, T], BF16)
    s_iw = scr("s_iw", [T, 8], F32)
    s_bqT = scr("s_bqT", [1024, T], BF16)
    s_bfT = scr("s_bfT", [1024, T], F32)
    s_bi = scr("s_bi", [T, 1024], BF16)
    s_bg = scr("s_bg", [T, 1024], BF16)
    s_cq = scr("s_cq", [T, 1024], F32)
    s_ck = scr("s_ck", [T, 1024], F32)
    s_cv = scr("s_cv", [T, 2048], BF16)
    s_cg = scr("s_cg", [T, 2048], BF16)
    s_gT = scr("s_gT", [3072, T], BF16)
    s_mT = scr("s_mT", [NT, 128, NT, 128], BF16)
    s_tab = scr("s_tab", [128, 3072], BF16)
    s_oa = scr("s_oa", [T, 1024], BF16)
    s_ob = scr("s_ob", [T, 1024], BF16)
    s_oc = scr("s_oc", [T, 2048], BF16)
    s_hT = scr("s_hT", [128, 8, TH], BF16)
    s_gates = scr("s_gates", [T, 32], F32)

    def tok(dst):
        return lambda t0, c0, n: dst[t0:t0 + 128, c0:c0 + n]

    def feat(dst):
        return lambda c0, n, t0: dst[c0:c0 + n, t0:t0 + 512]

    jobs = [
        (O_AQ, 1024, "feat", feat(s_aqT), BF16, None),
        (O_AK, 1024, "feat", feat(s_akT), BF16, None),
        (O_AV, 1024, "tok", tok(s_av), BF16, None),
        (O_IQ, 512, "feat", feat(s_iqT), BF16, None),
        (O_IK, 64, "feat", feat(s_ikT), BF16, None),
        (O_IW, 8, "tok", tok(s_iw), F32, None),
        (O_BQ, 1024, "feat", feat(s_bqT), BF16, None),
        (O_BF, 1024, "feat", feat(s_bfT), F32, None),
        (O_BI, 1024, "tok", tok(s_bi), BF16, None),
        (O_BG, 1024, "tok", tok(s_bg), BF16, AF.Silu),
        (O_CQ, 1024, "tok", tok(s_cq), F32, None),
        (O_CK, 1024, "tok", tok(s_ck), F32, None),
        (O_CV, 2048, "tok", tok(s_cv), BF16, None),
        (O_CG, 2048, "tok", tok(s_cg), BF16, AF.Silu),
        (O_GA, 3072, "feat", feat(s_gT), BF16, AF.Sigmoid),
    ]
    with contextlib.ExitStack() as stack:
        ctx = Ctx(nc, stack)
        phase_copy(ctx, I["x"], xres, T)
        phase_mod(ctx, I["c"], I["ada_w"], I["ada_b"], modbc)
        phase_bias_tab(ctx, I["rel_bias"], I["oh_tab"], s_tab)
        for l in range(depth):
            phase_proj(ctx, xres, modbc, l, I["norm1_g"], I["w_in"][l], jobs, 1, 0)
            phase_a1(ctx, s_iqT, s_ikT, s_iw, s_mT)
            for hg in range(2):
                phase_a2(ctx, hg, s_aqT, s_akT, s_av, s_mT, s_tab, s_oa)
            phase_b(ctx, l, I["hgrn_lb_raw"], s_bqT, s_bfT, s_bi, s_bg, I["hgrn_norm_g"], s_ob)
            phase_c(ctx, s_cq, s_ck, s_cv, s_cg, I["cosT"], I["sinS"], I["cdec"], s_oc)
            phase_merge(ctx, l, modbc, s_oa, s_ob, s_oc, s_gT, I["w_branch_a"], I["w_branch_b"], I["w_branch_c"],
                        I["w_out"], xres)
            for hh in range(2):
                sg = s_gates[hh * TH:(hh + 1) * TH, :]
                phase_route(ctx, l, hh, xres, modbc, I["norm2_g"], I["router_group_w"], I["router_group_b"],
                            I["router_expert_w"], I["router_expert_b"], s_hT, sg)
                phase_experts(ctx, l, hh, xres, modbc, s_hT, sg, I["expert_w_gate"], I["expert_w_up"], I["expert_w_down"])
        phase_final(ctx, xres, I["final_norm_g"], y)
    return nc


_PROGRAM = None


def kernel(**inputs):
    global _PROGRAM
    if _PROGRAM is None:
        _PROGRAM = build_program()
    nc = _PROGRAM
    f = lambda a: np.ascontiguousarray(np.asarray(a, dtype=np.float32))
    cos, sinS, dec = make_ret_tables()
    shared = {k: f(inputs[k]) for k in ("rel_bias", "hgrn_lb_raw", "norm1_g", "norm2_g", "ada_w", "ada_b", "w_in",
                                        "hgrn_norm_g", "w_branch_a", "w_branch_b", "w_branch_c", "w_out",
                                        "router_group_w", "router_group_b", "router_expert_w", "router_expert_b",
                                        "expert_w_gate", "expert_w_up", "expert_w_down")}
    shared["final_norm_g"] = f(inputs["final_norm_g"]).reshape(1, D)
    shared["oh_tab"] = make_oh_tab()
    shared["cosT"] = cos
    shared["sinS"] = sinS
    shared["cdec"] = dec
    x = f(inputs["x"])
    c = f(inputs["c"])
    in_maps = []
    for core in range(8):
        b = core % 4
        m = dict(shared)
        m["x"] = x[b]
        m["c"] = c[b:b + 1]
        in_maps.append(m)
    res = run_bass_kernel_spmd(nc, in_maps, core_ids=list(range(8)))
    out = np.stack([np.asarray(res.results[b]["y"], dtype=np.float32) for b in range(4)], axis=0)
    return out


TOPK = 256
NBIS = 16
MASKV = -30000.0


def phase_a1(ctx, s_iqT, s_ikT, s_iw, s_mT):
    ph = Phase(ctx, "a1")
    ident, ident_b = make_ident(ph)
    ikT, ikT_b = ph.sb("ikT", [64, T], BF16)
    ph.dma("sp", ikT[:], s_ikT[:, :], ikT_b, writes=[ikT_b])
    pw, pw_b = ph.sb("pw", [128, NBIS], F32)
    for k in range(NBIS):
        ph.op("pool", lambda e, k=k: e.memset(pw[:, k:k + 1], 0.5 ** (k + 1)), [], [pw_b])
    cm, cm_b = ph.sb("cm", [128, 128], F32)
    ph.op("pool", lambda e: e.memset(cm[:], 0.0), [], [cm_b])
    ph.op("pool", lambda e: e.affine_select(out=cm[:], in_=cm[:], pattern=[[-1, 128]], compare_op=ALU.is_ge, fill=-1e30,
                                            base=0, channel_multiplier=1), [cm_b], [cm_b])
    iqs = [ph.sb("iq%d" % i, [64, 8, 128], BF16) for i in range(4)]
    ws = [ph.sb("w%d" % i, [128, 24], F32) for i in range(4)]
    Is = [ph.sb("I%d" % i, [128, T], F32) for i in range(4)]
    rls = [ph.sb("rl%d" % i, [128, 512], F32) for i in range(4)]
    junk, junk_b = ph.sb("junk", [128, T], BF16)
    junk2, junk2_b = ph.sb("junk2", [128, T], BF16)
    m01s = [ph.sb("m01_%d" % i, [128, T], BF16) for i in range(2)]
    mTs = [ph.sb("mT%d" % i, [128, NT, 128], BF16) for i in range(2)]
    sts = [ph.sb("st%d" % i, [128, 8 + NBIS], F32) for i in range(4)]
    pss = [ph.ps("ps%d" % i) for i in range(4)]
    ptr = [ph.ps("ptr%d" % i, [128, 4, 128], BF16) for i in range(2)]
    cnt = {"pz": 0, "rl": 0, "tr": 0}

    def gen_indexer(i):
        S = (i + 1) * 128
        iq, iq_b = iqs[i % 4]
        w, w_b = ws[i % 4]
        I, I_b = Is[i % 4]
        ph.dma("sp", iq[:], s_iqT[:, i * 128:(i + 1) * 128].rearrange("(h d) t -> d h t", d=64), iq_b, writes=[iq_b])
        ph.dma("sp", w[:, 0:8], s_iw[i * 128:(i + 1) * 128, :], w_b, writes=[w_b])
        ph.op("pool", lambda e: e.tensor_scalar(out=w[:, 0:8], in0=w[:, 0:8], scalar1=512.0 ** -0.5, scalar2=None,
                                                op0=ALU.mult), [w_b], [w_b])
        ph.op("pool", lambda e: e.tensor_scalar(out=w[:, 8:16], in0=w[:, 0:8], scalar1=-1.0, scalar2=None, op0=ALU.mult),
              [w_b], [w_b])
        ph.op("dve", lambda e: e.tensor_tensor(out=w[:, 8:16], in0=w[:, 8:16], in1=w[:, 0:8], op=ALU.max), [w_b], [w_b])
        ph.op("pool", lambda e: e.tensor_scalar(out=w[:, 16:24], in0=w[:, 0:8], scalar1=0.0, scalar2=2.0, op0=ALU.is_ge,
                                                op1=ALU.mult), [w_b], [w_b])
        ph.op("pool", lambda e: e.tensor_scalar(out=w[:, 16:24], in0=w[:, 16:24], scalar1=-1.0, scalar2=None, op0=ALU.add),
              [w_b], [w_b])
        yield
        for c0 in range(0, S, 512):
            n = min(512, S - c0)
            for h in range(8):
                p, p_b = pss[cnt["pz"] % 4]
                cnt["pz"] += 1
                rl, rl_b = rls[cnt["rl"] % 4]
                cnt["rl"] += 1
                ph.op("pe", lambda e, p=p, h=h, c0=c0, n=n: e.matmul(p[:, 0:n], iq[:, h, :], ikT[:, c0:c0 + n], start=True, stop=True),
                      [iq_b, ikT_b], [p_b])
                ph.op("act", lambda e, p=p, rl=rl, h=h, n=n: e.activation(out=rl[:, 0:n], in_=p[:, 0:n], func=AF.Relu,
                                                                     scale=w[:, 8 + h:9 + h]), [p_b, w_b], [rl_b])
                if h == 0:
                    ph.op("dve", lambda e, rl=rl, h=h, c0=c0, n=n: e.tensor_scalar(out=I[:, c0:c0 + n], in0=rl[:, 0:n],
                                                                       scalar1=w[:, 16 + h:17 + h], scalar2=None,
                                                                       op0=ALU.mult), [rl_b, w_b], [I_b])
                else:
                    ph.op("dve", lambda e, rl=rl, h=h, c0=c0, n=n: e.scalar_tensor_tensor(out=I[:, c0:c0 + n], in0=rl[:, 0:n],
                                                                              scalar=w[:, 16 + h:17 + h],
                                                                              in1=I[:, c0:c0 + n], op0=ALU.mult,
                                                                              op1=ALU.add), [rl_b, w_b, I_b], [I_b])
                yield
        ph.op("pool", lambda e: e.tensor_tensor(out=I[:, i * 128:(i + 1) * 128], in0=I[:, i * 128:(i + 1) * 128],
                                                in1=cm[:], op=ALU.add), [I_b, cm_b], [I_b])
        yield

    def gen_select(i):
        S = (i + 1) * 128
        I, I_b = Is[i % 4]
        m01, m01_b = m01s[i % 2]
        mT, mT_b = mTs[i % 2]
        st, st_b = sts[i % 4]
        if i < 2:
            ph.op("dve", lambda e: e.memset(st[:, 0:1], -1e29), [], [st_b])
        else:
            on_act = (i % 2 == 1)
            ph.op("dve", lambda e: e.tensor_reduce(out=st[:, 0:1], in_=I[:, 0:i * 128], axis=AX.X, op=ALU.min),
                  [I_b], [st_b])
            ph.op("dve", lambda e: e.tensor_reduce(out=st[:, 1:2], in_=I[:, 0:S], axis=AX.X, op=ALU.max), [I_b], [st_b])
            ph.op("dve", lambda e: e.tensor_tensor(out=st[:, 2:3], in0=st[:, 1:2], in1=st[:, 0:1], op=ALU.subtract),
                  [st_b], [st_b])
            ph.op("dve", lambda e: e.tensor_scalar(out=st[:, 8:8 + NBIS], in0=pw[:], scalar1=st[:, 2:3], scalar2=None,
                                                   op0=ALU.mult), [st_b, pw_b], [st_b])
            yield
            for k in range(NBIS):
                if on_act:
                    ph.op("dve", lambda e, k=k: e.tensor_scalar(out=st[:, 3:4], in0=st[:, 0:1], scalar1=st[:, 8 + k:9 + k],
                                                                scalar2=-1.0, op0=ALU.add, op1=ALU.mult), [st_b], [st_b])
                    yield
                    ph.op("act", lambda e: e.activation(out=junk2[:, 0:S], in_=I[:, 0:S], func=AF.Sign, bias=st[:, 3:4],
                                                        accum_out=st[:, 4:5]), [I_b, st_b], [junk2_b, st_b])
                    yield
                    thresh = 2.0 * (TOPK - 0.5) - S
                else:
                    ph.op("dve", lambda e, k=k: e.tensor_tensor(out=st[:, 3:4], in0=st[:, 0:1], in1=st[:, 8 + k:9 + k],
                                                                op=ALU.add), [st_b], [st_b])
                    yield
                    ph.op("dve", lambda e: e.tensor_scalar(out=junk[:, 0:S], in0=I[:, 0:S], scalar1=st[:, 3:4],
                                                           scalar2=None, op0=ALU.is_ge, op1=ALU.add,
                                                           accum_out=st[:, 4:5]), [I_b, st_b], [junk_b, st_b])
                    yield
                    thresh = TOPK - 0.5
                ph.op("dve", lambda e, k=k, thresh=thresh: e.scalar_tensor_tensor(
                    out=st[:, 5:6], in0=st[:, 4:5], scalar=thresh, in1=st[:, 8 + k:9 + k], op0=ALU.is_ge, op1=ALU.mult),
                      [st_b], [st_b])
                ph.op("dve", lambda e: e.tensor_tensor(out=st[:, 0:1], in0=st[:, 0:1], in1=st[:, 5:6], op=ALU.add),
                      [st_b], [st_b])
                yield
        ph.op("dve", lambda e: e.tensor_scalar(out=m01[:, 0:S], in0=I[:, 0:S], scalar1=st[:, 0:1], scalar2=None,
                                               op0=ALU.is_ge), [I_b, st_b], [m01_b])
        yield
        for j0 in range(0, i + 1, 4):
            nj = min(4, i + 1 - j0)
            pt, pt_b = ptr[cnt["tr"] % 2]
            cnt["tr"] += 1
            for u in range(nj):
                j = j0 + u
                ph.op("pe", lambda e, pt=pt, u=u, j=j: e.transpose(pt[:, u, :], m01[:, j * 128:(j + 1) * 128], ident[:]),
                      [m01_b, ident_b], [pt_b])
            ph.op("dve", lambda e, pt=pt, j0=j0, nj=nj: e.tensor_copy(out=mT[:, j0:j0 + nj, :], in_=pt[:, 0:nj, :]),
                  [pt_b], [mT_b])
            yield
        ph.dma("sp", s_mT[i, :, 0:i + 1, :], mT[:, 0:i + 1, :], mT_b, reads=[mT_b])

    def merge(gens):
        state = [[g, max(n, 1), 0, False] for g, n in gens]
        while any(not st_[3] for st_ in state):
            best = None
            for st_ in state:
                if st_[3]:
                    continue
                frac = st_[2] / st_[1]
                if best is None or frac < best[0]:
                    best = (frac, st_)
            st_ = best[1]
            try:
                next(st_[0])
                st_[2] += 1
            except StopIteration:
                st_[3] = True

    def n_sel(i):
        return (3 * NBIS + 4 if i >= 2 else 3) + (i + 4) // 4

    def n_idx(i):
        return 2 + 8 * (((i + 1) * 128 + 511) // 512)

    merge([(gen_indexer(0), n_idx(0)), (gen_indexer(1), n_idx(1))])
    for m in range(NT // 2):
        gens = [(gen_select(2 * m), n_sel(2 * m)), (gen_select(2 * m + 1), n_sel(2 * m + 1))]
        if 2 * m + 2 < NT:
            gens.append((gen_indexer(2 * m + 2), n_idx(2 * m + 2)))
            gens.append((gen_indexer(2 * m + 3), n_idx(2 * m + 3)))
        merge(gens)
    ph.run()


def phase_bias_tab(ctx, rel_bias, oh_tab, s_tab):
    ph = Phase(ctx, "btab")
    rb, rb_b = ph.sb("rb", [32, 8], F32)
    r31, r31_b = ph.sb("r31", [32, 8], F32)
    rep, rep_b = ph.sb("rep", [32, 8, 128], F32)
    oh, oh_b = ph.sb("oh", [32, 384], F32)
    tb, tb_b = ph.sb("tb", [128, 8, 384], BF16)
    pss = [ph.ps("p%d" % i) for i in range(2)]
    ph.dma("sp", rb[:], rel_bias[:, :], rb_b, writes=[rb_b])
    ph.dma("sp", r31[:], bcast_rows(rel_bias[31:32, :], 32), r31_b, writes=[r31_b])
    ph.dma("sp", oh[:], oh_tab[:, :], oh_b, writes=[oh_b])
    ph.op("dve", lambda e: e.tensor_tensor(out=rb[:], in0=rb[:], in1=r31[:], op=ALU.subtract), [rb_b, r31_b], [rb_b])
    ph.op("dve", lambda e: e.tensor_scalar(out=rb[:], in0=rb[:], scalar1=math.sqrt(128.0), scalar2=None, op0=ALU.mult),
          [rb_b], [rb_b])
    ph.op("dve", lambda e: e.tensor_copy(out=rep[:], in_=rb[:].unsqueeze(2).broadcast_to([32, 8, 128])), [rb_b], [rep_b])
    for h in range(8):
        p, p_b = pss[h % 2]
        ph.op("pe", lambda e, p=p, h=h: e.matmul(p[:, 0:384], rep[:, h, :], oh[:], start=True, stop=True),
              [rep_b, oh_b], [p_b])
        ph.op("act", lambda e, p=p, h=h: e.copy(out=tb[:, h, :], in_=p[:, 0:384]), [p_b], [tb_b])
    ph.dma("sp", s_tab[:, :], tb[:].rearrange("p h u -> p (h u)"), tb_b, reads=[tb_b])
    ph.run()


def phase_a2(ctx, hg, s_aqT, s_akT, s_av, s_mT, s_tab, s_oa):
    ph = Phase(ctx, "a2")
    ident, ident_b = make_ident(ph)
    kT, kT_b = ph.sb("kT", [128, 4, T], BF16)
    v, v_b = ph.sb("v", [128, NT, 4, 129], BF16)
    bt, bt_b = ph.sb("bt", [128, 2, 4, 128], BF16)
    r0 = hg * 512
    for h in range(4):
        ph.dma("sp", kT[:, h, :], s_akT[r0 + h * 128:r0 + (h + 1) * 128, :], kT_b, writes=[kT_b])
        ph.dma("sp", v[:, :, h, 0:128], s_av[:, r0 + h * 128:r0 + (h + 1) * 128].rearrange("(j p) d -> p j d", p=128),
               v_b, writes=[v_b])
        for pat in range(2):
            src = bass.AP(s_tab.tensor, s_tab.offset + (hg * 4 + h) * 384 + 255 - 128 * pat, [[3071, 128], [1, 128]])
            ph.dma("sp", bt[:, pat, h, :], src, bt_b, writes=[bt_b])
    ph.op("pool", lambda e: e.memset(v[:, :, :, 128:129], 1.0), [], [v_b])
    qs = [ph.sb("q%d" % i, [128, 4, 128], BF16) for i in range(2)]
    mbs = [ph.sb("mb%d" % i, [128, NT, 128], BF16) for i in range(2)]
    pts = [ph.sb("pt%d" % i, [128, 4, 128], BF16) for i in range(3)]
    sts = [ph.ps("st%d" % i) for i in range(3)]
    ops = [ph.ps("o%d" % i) for i in range(4)]
    rcs = [ph.sb("rc%d" % i, [128, 4], F32) for i in range(2)]
    outs = [ph.sb("out%d" % i, [128, 512], BF16) for i in range(2)]
    sts3 = sts
    pairs = [(i, j) for i in range(NT) for j in range(i + 1)]

    def emit_loads(i):
        q, q_b = qs[i % 2]
        mb, mb_b = mbs[i % 2]
        ph.dma("sp", q[:], s_aqT[r0:r0 + 512, i * 128:(i + 1) * 128].rearrange("(h d) t -> d h t", d=128), q_b,
               writes=[q_b])
        ph.dma("sp", mb[:, 0:i + 1, :], s_mT[i, :, 0:i + 1, :], mb_b, writes=[mb_b])

    def emit_qk(idx):
        i, j = pairs[idx]
        q, q_b = qs[i % 2]
        st, st_b = sts3[idx % 3]
        near = (i - j) <= 1
        for h in range(4):
            ph.op("pe", lambda e, h=h: e.matmul(st[:, h * 128:(h + 1) * 128], kT[:, h, j * 128:(j + 1) * 128], q[:, h, :],
                                                start=True, stop=not near), [kT_b, q_b], [st_b])
            if near:
                ph.op("pe", lambda e, h=h: e.matmul(st[:, h * 128:(h + 1) * 128], bt[:, i - j, h, :], ident[:], start=False,
                                                    stop=True), [bt_b, ident_b], [st_b])

    def emit_soft(idx):
        i, j = pairs[idx]
        mb, mb_b = mbs[i % 2]
        st, st_b = sts3[idx % 3]
        pt, pt_b = pts[idx % 3]
        ph.op("act", lambda e: e.activation(out=pt[:], in_=st[:], func=AF.Exp, scale=128.0 ** -0.5), [st_b], [pt_b])
        ph.op("dve", lambda e: e.tensor_tensor(out=pt[:], in0=pt[:], in1=mb[:, j:j + 1, :].broadcast_to([128, 4, 128]),
                                               op=ALU.mult), [pt_b, mb_b], [pt_b])

    def emit_pv(idx):
        i, j = pairs[idx]
        pt, pt_b = pts[idx % 3]
        for h in range(4):
            o, o_b = ops[h]
            ph.op("pe", lambda e, o=o, h=h: e.matmul(o[:, 0:129], pt[:, h, :], v[:, j, h, :], start=(j == 0), stop=(j == i)),
                  [pt_b, v_b], [o_b])
        if j == i:
            rc, rc_b = rcs[i % 2]
            out, out_b = outs[i % 2]
            for h in range(4):
                o, o_b = ops[h]
                ph.op("dve", lambda e, o=o, h=h: e.reciprocal(out=rc[:, h:h + 1], in_=o[:, 128:129]), [o_b], [rc_b])
                ph.op("dve", lambda e, o=o, h=h: e.tensor_scalar(out=out[:, h * 128:(h + 1) * 128], in0=o[:, 0:128],
                                                                 scalar1=rc[:, h:h + 1], scalar2=None, op0=ALU.mult),
                      [o_b, rc_b], [out_b])
            ph.dma("sp", s_oa[i * 128:(i + 1) * 128, r0:r0 + 512], out[:], out_b, reads=[out_b])

    emit_loads(0)
    emit_qk(0)
    if len(pairs) > 1:
        emit_loads(1)
        emit_qk(1)
    for idx in range(len(pairs)):
        i, j = pairs[idx]
        if j == 0 and i >= 1 and i + 1 < NT:
            emit_loads(i + 1)
        emit_soft(idx)
        if idx + 2 < len(pairs):
            emit_qk(idx + 2)
        emit_pv(idx)
    ph.run()


def phase_b(ctx, l, lb_raw, s_bqT, s_bfT, s_bi, s_bg, hgrn_norm_g, s_ob):
    ph = Phase(ctx, "hg")
    ident, ident_b = make_ident(ph)
    lbr, lbr_b = ph.sb("lbr", [128, DEPTH, 8], F32)
    lb, lb_b = ph.sb("lb", [128, 8], F32)
    oml, oml_b = ph.sb("oml", [128, 8], F32)
    ssum, ssum_b = ph.sb("ssum", [128, 8], F32)
    ph.dma("sp", lbr[:], lb_raw.rearrange("l (h p) -> p l h", p=128), lbr_b, writes=[lbr_b],
           allow_slow_non_contiguous=True)
    ph.op("act", lambda e: e.activation(out=lbr[:], in_=lbr[:], func=AF.Exp), [lbr_b], [lbr_b])
    ph.op("dve", lambda e: e.tensor_tensor(out=ssum[:], in0=lbr[:, 0, :], in1=lbr[:, 1, :], op=ALU.add), [lbr_b], [ssum_b])
    ph.op("dve", lambda e: e.tensor_tensor(out=ssum[:], in0=ssum[:], in1=lbr[:, 2, :], op=ALU.add), [lbr_b, ssum_b], [ssum_b])
    ph.op("dve", lambda e: e.tensor_tensor(out=ssum[:], in0=ssum[:], in1=lbr[:, 3, :], op=ALU.add), [lbr_b, ssum_b], [ssum_b])
    ph.op("dve", lambda e: e.reciprocal(out=ssum[:], in_=ssum[:]), [ssum_b], [ssum_b])
    ph.op("dve", lambda e: e.memset(lb[:], 0.0), [], [lb_b])
    for m in range(1, l + 1):
        ph.op("dve", lambda e, m=m: e.tensor_tensor(out=lb[:], in0=lb[:], in1=lbr[:, m, :], op=ALU.add), [lbr_b, lb_b], [lb_b])
    ph.op("dve", lambda e: e.tensor_tensor(out=lb[:], in0=lb[:], in1=ssum[:], op=ALU.mult), [lb_b, ssum_b], [lb_b])
    ph.op("dve", lambda e: e.tensor_scalar(out=oml[:], in0=lb[:], scalar1=-1.0, scalar2=1.0, op0=ALU.mult, op1=ALU.add),
          [lb_b], [oml_b])
    ng, ng_b = ph.sb("ng", [64, 128], F32)
    ph.dma("sp", ng[:], bcast_rows(hgrn_norm_g[l:l + 1, :], 64), ng_b, writes=[ng_b])
    ones, ones_b = ph.sb("ones", [128, T], BF16)
    ph.op("pool", lambda e: e.memset(ones[:], 1.0), [], [ones_b])
    zer, zer_b = ph.sb("zer", [128, 32], BF16)
    ph.op("pool", lambda e: e.memset(zer[:], 0.0), [], [zer_b])
    m01, m01_b = ph.sb("m01", [64, 64], F32)
    ph.op("pool", lambda e: e.memset(m01[:], 1.0), [], [m01_b])
    ph.op("pool", lambda e: e.affine_select(out=m01[:], in_=m01[:], pattern=[[1, 64]], compare_op=ALU.is_ge, fill=0.0,
                                            base=0, channel_multiplier=-1), [m01_b], [m01_b])
    NC = T // 64
    fT, fT_b = ph.sb("fT", [128, T], F32)
    Bc, Bc_b = ph.sb("Bc", [128, T], F32)
    E, E_b = ph.sb("E", [128, T], F32)
    qT, qT_b = ph.sb("qT", [128, T], BF16)
    qd, qd_b = ph.sb("qd", [128, T], BF16)
    kd, kd_b = ph.sb("kd", [128, T], BF16)
    kdtm, kdtm_b = ph.sb("kdtm", [64, NC, 128], BF16)
    iv, iv_b = ph.sb("iv", [64, NC, 128], BF16)
    oall, oall_b = ph.sb("oall", [64, NC, 128], F32)
    osb, osb_b = ph.sb("osb", [64, NC, 128], BF16)
    sc, sc_b = ph.sb("sc", [128, 4, NC], F32)
    S, S_b = ph.sb("S", [128, 128], F32)
    Sps = [ph.sb("Sp%d" % i, [128, 128], BF16) for i in range(2)]
    tmpu, tmpu_b = ph.sb("tmpu", [128, 128], F32)
    atms = [ph.sb("atm%d" % i, [64, 64], BF16) for i in range(2)]
    rs, rs_b = ph.sb("rs", [64, 2, NC], F32)
    psA = [ph.ps("pA%d" % i) for i in range(3)]
    psO = [ph.ps("pO%d" % i) for i in range(2)]
    psU = [ph.ps("pU%d" % i) for i in range(2)]
    psT = [ph.ps("pT%d" % i, [128, 4, 128], BF16) for i in range(1)]
    for h in range(8):
        r0 = h * 128
        ph.dma("sp", fT[:], s_bfT[r0:r0 + 128, :], fT_b, writes=[fT_b])
        ph.dma("sp", qT[:], s_bqT[r0:r0 + 128, :], qT_b, writes=[qT_b])
        ph.dma("sp", iv[:], s_bi[:, r0:r0 + 128].rearrange("(c p) v -> p c v", p=64), iv_b, writes=[iv_b])
        ph.dma("sp", osb[:], s_bg[:, r0:r0 + 128].rearrange("(c p) v -> p c v", p=64), osb_b, writes=[osb_b])
        ph.op("act", lambda e: e.activation(out=fT[:], in_=fT[:], func=AF.Sigmoid), [fT_b], [fT_b])
        ph.op("dve", lambda e, h=h: e.tensor_scalar(out=fT[:], in0=fT[:], scalar1=oml[:, h:h + 1], scalar2=lb[:, h:h + 1],
                                                    op0=ALU.mult, op1=ALU.add), [fT_b, oml_b, lb_b], [fT_b])
        ph.op("act", lambda e: e.activation(out=Bc[:], in_=fT[:], func=AF.Ln), [fT_b], [Bc_b])
        ph.op("pool", lambda e: e.tensor_scalar(out=fT[:], in0=fT[:], scalar1=-1.0, scalar2=1.0, op0=ALU.mult,
                                                op1=ALU.add), [fT_b, Bc_b], [fT_b])
        ph.op("dve", lambda e: e.tensor_tensor_scan(out=Bc[:], data0=ones[:], data1=Bc[:], initial=0.0, op0=ALU.mult,
                                                    op1=ALU.add), [Bc_b, ones_b], [Bc_b])
        Bc3 = Bc[:].rearrange("p (c s) -> p c s", s=64)
        ph.op("dve", lambda e: e.memset(sc[:, 0, 0:1], 0.0), [], [sc_b])
        ph.op("dve", lambda e, Bc3=Bc3: e.tensor_copy(out=sc[:, 0, 1:NC], in_=Bc3[:, 0:NC - 1, 63]), [Bc_b], [sc_b])
        ph.op("dve", lambda e, Bc3=Bc3: e.tensor_tensor(out=sc[:, 1, :], in0=Bc3[:, :, 63], in1=sc[:, 0, :],
                                                        op=ALU.subtract), [Bc_b, sc_b], [sc_b])
        ph.op("dve", lambda e, Bc3=Bc3: e.tensor_tensor(out=sc[:, 2, :], in0=Bc3[:, :, 31], in1=sc[:, 0, :],
                                                        op=ALU.subtract), [Bc_b, sc_b], [sc_b])
        ph.op("dve", lambda e, Bc3=Bc3: e.tensor_tensor(out=sc[:, 3, :], in0=Bc3[:, :, 63], in1=Bc3[:, :, 31],
                                                        op=ALU.subtract), [Bc_b, sc_b], [sc_b])
        ph.op("act", lambda e: e.activation(out=sc[:, 1:4, :], in_=sc[:, 1:4, :], func=AF.Exp), [sc_b], [sc_b])
        ph.op("dve", lambda e, Bc3=Bc3: e.tensor_copy(out=E[:, 0:NC], in_=Bc3[:, :, 31]), [Bc_b], [E_b])
        ph.op("dve", lambda e, Bc3=Bc3: e.tensor_tensor(out=Bc3, in0=Bc3, in1=E[:, 0:NC].unsqueeze(2).broadcast_to(
            [128, NC, 64]), op=ALU.subtract), [Bc_b, E_b], [Bc_b])
        ph.op("act", lambda e: e.activation(out=E[:], in_=Bc[:], func=AF.Exp), [Bc_b], [E_b])
        ph.op("dve", lambda e: e.tensor_tensor(out=qd[:], in0=qT[:], in1=E[:], op=ALU.mult), [qT_b, E_b], [qd_b])
        ph.op("act", lambda e: e.activation(out=E[:], in_=Bc[:], func=AF.Exp, scale=-1.0), [Bc_b, qd_b], [E_b])
        ph.op("dve", lambda e: e.tensor_tensor(out=kd[:], in0=fT[:], in1=E[:], op=ALU.mult), [fT_b, E_b], [kd_b])
        for c4 in range(NC // 4):
            pT, pT_b = psT[0]
            for u in range(4):
                c = c4 * 4 + u
                ph.op("pe", lambda e, pT=pT, u=u, c=c: e.transpose(pT[0:64, u, :], kd[:, c * 64:(c + 1) * 64], ident[:]),
                      [kd_b, ident_b], [pT_b])
            ph.op("act", lambda e, pT=pT, c4=c4: e.copy(out=kdtm[:, c4 * 4:(c4 + 1) * 4, :], in_=pT[0:64, :, :]),
                  [pT_b], [kdtm_b])
        def emit_A(c):
            pA, pA_b = psA[c % 3]
            c0 = c * 64
            ph.op("pe", lambda e: e.matmul(pA[0:32, 0:64], kd[:, c0:c0 + 32], qd[:, c0:c0 + 64], start=True, stop=True),
                  [kd_b, qd_b], [pA_b])
            ph.op("pe", lambda e: e.matmul(pA[32:64, 32:64], kd[:, c0 + 32:c0 + 64], qd[:, c0 + 32:c0 + 64], start=True,
                                           stop=True), [kd_b, qd_b], [pA_b])
            ph.op("pe", lambda e: e.matmul(pA[32:64, 0:32], zer[:], qd[:, c0:c0 + 32], start=True, stop=True),
                  [zer_b, qd_b], [pA_b])

        def emit_atm(c):
            pA, pA_b = psA[c % 3]
            atm, atm_b = atms[c % 2]
            ph.op("dve", lambda e: e.tensor_tensor(out=atm[:], in0=pA[0:64, 0:64], in1=m01[:], op=ALU.mult),
                  [pA_b, m01_b], [atm_b])

        def emit_U(c):
            pU, pU_b = psU[c % 2]
            ph.op("pe", lambda e: e.matmul(pU[:, 0:128], kdtm[:, c, :], iv[:, c, :], start=True, stop=True),
                  [kdtm_b, iv_b], [pU_b])

        emit_A(0)
        emit_A(1)
        emit_U(0)
        emit_atm(0)
        for c in range(NC):
            pO, pO_b = psO[c % 2]
            pU, pU_b = psU[c % 2]
            atm, atm_b = atms[c % 2]
            Sp, Sp_b = Sps[c % 2]
            Spn, Spn_b = Sps[(c + 1) % 2]
            cs = slice(c * 64, (c + 1) * 64)
            ph.op("pe", lambda e, pO=pO, atm=atm, c=c: e.matmul(pO[0:64, 0:128], atm[:], iv[:, c, :], start=True,
                                                               stop=(c == 0)), [atm_b, iv_b], [pO_b])
            if c > 0:
                ph.op("pe", lambda e, pO=pO, cs=cs, Sp=Sp: e.matmul(pO[0:64, 0:128], qd[:, cs], Sp[:], start=False,
                                                                   stop=True), [qd_b, Sp_b], [pO_b])
            ph.op("act", lambda e, pO=pO, c=c: e.copy(out=oall[:, c, :], in_=pO[0:64, 0:128]), [pO_b], [oall_b])
            if c + 2 < NC:
                emit_A(c + 2)
            if c + 1 < NC - 1:
                emit_U(c + 1)
            if c < NC - 1:
                if c == 0:
                    ph.op("dve", lambda e, pU=pU, c=c: e.tensor_scalar(out=S[:], in0=pU[:, 0:128], scalar1=sc[:, 3, c:c + 1],
                                                                       scalar2=None, op0=ALU.mult), [pU_b, sc_b], [S_b])
                else:
                    ph.op("dve", lambda e, pU=pU, c=c: e.tensor_scalar(out=tmpu[:], in0=pU[:, 0:128],
                                                                       scalar1=sc[:, 3, c:c + 1], scalar2=None,
                                                                       op0=ALU.mult), [pU_b, sc_b], [tmpu_b])
                    ph.op("dve", lambda e, c=c: e.scalar_tensor_tensor(out=S[:], in0=S[:], scalar=sc[:, 1, c:c + 1],
                                                                       in1=tmpu[:], op0=ALU.mult, op1=ALU.add),
                          [S_b, tmpu_b, sc_b], [S_b])
                ph.op("dve", lambda e, Spn=Spn, c=c: e.tensor_scalar(out=Spn[:], in0=S[:], scalar1=sc[:, 2, c + 1:c + 2],
                                                                     scalar2=None, op0=ALU.mult), [S_b, sc_b], [Spn_b])
                emit_atm(c + 1)
        oflat = oall[:].rearrange("p c v -> p (c v)")
        ph.op("act", lambda e: e.activation(out=kdtm[:], in_=oall[:], func=AF.Square), [oall_b], [kdtm_b])
        ph.op("dve", lambda e: e.tensor_reduce(out=rs[:, 0, :], in_=kdtm[:], axis=AX.X, op=ALU.add), [kdtm_b], [rs_b])
        ph.op("dve", lambda e: e.tensor_scalar(out=rs[:, 0, :], in0=rs[:, 0, :], scalar1=1.0 / 128, scalar2=EPS,
                                               op0=ALU.mult, op1=ALU.add), [rs_b], [rs_b])
        ph.op("act", lambda e: e.activation(out=rs[:, 0, :], in_=rs[:, 0, :], func=AF.Sqrt), [rs_b], [rs_b])
        ph.op("dve", lambda e: e.reciprocal(out=rs[:, 1, :], in_=rs[:, 0, :]), [rs_b], [rs_b])
        ph.op("dve", lambda e: e.tensor_tensor(out=oall[:], in0=oall[:], in1=rs[:, 1, :].unsqueeze(2).broadcast_to(
            [64, NC, 128]), op=ALU.mult), [oall_b, rs_b], [oall_b])
        ph.op("pool", lambda e: e.tensor_tensor(out=oall[:], in0=oall[:], in1=ng[:].unsqueeze(1).broadcast_to(
            [64, NC, 128]), op=ALU.mult), [oall_b, ng_b], [oall_b])
        ph.op("dve", lambda e: e.tensor_tensor(out=osb[:], in0=oall[:], in1=osb[:], op=ALU.mult), [oall_b, osb_b], [osb_b])
        ph.dma("sp", s_ob[:, r0:r0 + 128].rearrange("(c p) v -> p c v", p=64), osb[:], osb_b, reads=[osb_b])
    ph.run()


def phase_c(ctx, s_cq, s_ck, s_cv, s_cg, cosT, sinS, cdec, s_oc):
    ph = Phase(ctx, "ret")
    ident, ident_b = make_ident(ph)
    dec, dec_b = ph.sb("dec", [128, 8], F32)
    ph.dma("sp", dec[:], cdec[:, :], dec_b, writes=[dec_b])
    m01, m01_b = ph.sb("m01", [128, 128], F32)
    ph.op("pool", lambda e: e.memset(m01[:], 1.0), [], [m01_b])
    ph.op("pool", lambda e: e.affine_select(out=m01[:], in_=m01[:], pattern=[[1, 128]], compare_op=ALU.is_ge, fill=0.0,
                                            base=0, channel_multiplier=-1), [m01_b], [m01_b])
    qrdT, qrdT_b = ph.sb("qrdT", [128, 2, T], BF16)
    krnT, krnT_b = ph.sb("krnT", [128, 2, T], BF16)
    krn, krn_b = ph.sb("krn", [128, NT, 256], BF16)
    v, v_b = ph.sb("v", [128, NT, 512], BF16)
    ins = [[ph.sb("in%d_%d" % (a, i), [128, 256], F32) for i in range(2)] for a in range(2)]
    cs_ = [ph.sb("cos%d" % i, [128, 256], F32) for i in range(2)]
    sn_ = [ph.sb("sin%d" % i, [128, 256], F32) for i in range(2)]
    t1s = [ph.sb("t1_%d" % i, [128, 256], F32) for i in range(2)]
    t2s = [ph.sb("t2_%d" % i, [128, 256], F32) for i in range(2)]
    qtm = [ph.sb("qtm%d" % i, [128, 256], BF16) for i in range(2)]
    W = [ph.sb("W%d" % i, [128, 512], F32) for i in range(2)]
    Wb = [ph.sb("Wb%d" % i, [128, 512], BF16) for i in range(2)]
    atms = [ph.sb("atm%d" % i, [128, 128], BF16) for i in range(2)]
    gts = [ph.sb("g%d" % i, [128, 512], BF16) for i in range(2)]
    outs = [ph.sb("o%d" % i, [128, 512], BF16) for i in range(2)]
    junk, junk_b = ph.sb("junk", [128, 512], BF16)
    sts = [ph.sb("st%d" % i, [128, 4], F32) for i in range(2)]
    psT = [ph.ps("pT%d" % i, [128, 2, 128], BF16) for i in range(2)]
    psA = [ph.ps("pA%d" % i) for i in range(2)]
    psO = [ph.ps("pO%d" % i) for i in range(2)]
    psU = [ph.ps("pU%d" % i) for i in range(2)]
    for h in range(4):
        gam = 1.0 - 2.0 ** (-5.0 - h)
        g128 = gam ** 128
        ph.dma("sp", v[:], s_cv[:, h * 512:(h + 1) * 512].rearrange("(c p) d -> p c d", p=128), v_b, writes=[v_b])
        for i in range(NT):
            rows = slice(i * 128, (i + 1) * 128)
            co, co_b = cs_[i % 2]
            sn, sn_b = sn_[i % 2]
            ph.dma("sp", co[:], cosT[rows, :], co_b, writes=[co_b])
            ph.dma("sp", sn[:], sinS[rows, :], sn_b, writes=[sn_b])
            for a, src in enumerate((s_cq, s_ck)):
                x, x_b = ins[a][i % 2]
                t1, t1_b = t1s[a]
                t2, t2_b = t2s[a]
                ph.dma("sp", x[:], src[rows, h * 256:(h + 1) * 256], x_b, writes=[x_b])
                x3 = x[:].rearrange("p (i two) -> p i two", two=2)
                s3 = sn[:].rearrange("p (i two) -> p i two", two=2)
                t23 = t2[:].rearrange("p (i two) -> p i two", two=2)
                ph.op("dve", lambda e, t1=t1, x=x, co=co: e.tensor_tensor(out=t1[:], in0=x[:], in1=co[:], op=ALU.mult),
                      [x_b, co_b], [t1_b])
                ph.op("pool", lambda e, t23=t23, x3=x3, s3=s3: e.tensor_tensor(out=t23[:, :, 0], in0=x3[:, :, 1],
                                                                               in1=s3[:, :, 0], op=ALU.mult),
                      [x_b, sn_b], [t2_b])
                ph.op("pool", lambda e, t23=t23, x3=x3, s3=s3: e.tensor_tensor(out=t23[:, :, 1], in0=x3[:, :, 0],
                                                                               in1=s3[:, :, 1], op=ALU.mult),
                      [x_b, sn_b], [t2_b])
                ph.op("dve", lambda e, t1=t1, t2=t2: e.tensor_tensor(out=t1[:], in0=t1[:], in1=t2[:], op=ALU.add),
                      [t1_b, t2_b], [t1_b])
                pT, pT_b = psT[a]
                if a == 0:
                    dst, dst_b = qtm[i % 2]
                    dstap = dst[:]
                else:
                    dst_b = krn_b
                    dstap = krn[:, i, :]
                ph.op("act", lambda e, dstap=dstap, t1=t1, a=a, h=h: e.activation(out=dstap, in_=t1[:], func=AF.Copy,
                                                                                  scale=dec[:, a * 4 + h:a * 4 + h + 1]),
                      [t1_b, dec_b], [dst_b])
                for kc in range(2):
                    ph.op("pe", lambda e, pT=pT, kc=kc, dstap=dstap: e.transpose(pT[:, kc, :],
                                                                                 dstap[:, kc * 128:(kc + 1) * 128], ident[:]),
                          [dst_b, ident_b], [pT_b])
                tgt, tgt_b = (qrdT, qrdT_b) if a == 0 else (krnT, krnT_b)
                ph.op("act", lambda e, tgt=tgt, pT=pT, i=i: e.copy(out=tgt[:, :, i * 128:(i + 1) * 128], in_=pT[:]),
                      [pT_b], [tgt_b])
        def emit_A(c):
            cs = slice(c * 128, (c + 1) * 128)
            pA, pA_b = psA[c % 2]
            for kc in range(2):
                ph.op("pe", lambda e, kc=kc: e.matmul(pA[:, 0:128], krnT[:, kc, cs], qrdT[:, kc, cs], start=(kc == 0),
                                                      stop=(kc == 1)), [krnT_b, qrdT_b], [pA_b])

        def emit_atm(c):
            pA, pA_b = psA[c % 2]
            atm, atm_b = atms[c % 2]
            ph.op("dve", lambda e: e.tensor_tensor(out=atm[:], in0=pA[:, 0:128], in1=m01[:], op=ALU.mult),
                  [pA_b, m01_b], [atm_b])

        emit_A(0)
        emit_atm(0)
        for c in range(NT):
            cs = slice(c * 128, (c + 1) * 128)
            pO, pO_b = psO[c % 2]
            atm, atm_b = atms[c % 2]
            g, g_b = gts[c % 2]
            out, out_b = outs[c % 2]
            st, st_b = sts[c % 2]
            ph.dma("sp", g[:], s_cg[cs, h * 512:(h + 1) * 512], g_b, writes=[g_b])
            if c + 1 < NT:
                emit_A(c + 1)
            ph.op("pe", lambda e, pO=pO, atm=atm, c=c: e.matmul(pO[:], atm[:], v[:, c, :], start=True, stop=(c == 0)),
                  [atm_b, v_b], [pO_b])
            if c > 0:
                for kc in range(2):
                    ph.op("pe", lambda e, pO=pO, kc=kc, cs=cs: e.matmul(pO[:], qrdT[:, kc, cs], Wb[kc][0][:], start=False,
                                                                       stop=(kc == 1)), [qrdT_b, Wb[kc][1]], [pO_b])
            if c < NT - 1:
                for kc in range(2):
                    pU, pU_b = psU[kc]
                    ph.op("pe", lambda e, pU=pU, kc=kc, c=c: e.matmul(pU[:], krn[:, c, kc * 128:(kc + 1) * 128], v[:, c, :],
                                                                     start=True, stop=True), [krn_b, v_b], [pU_b])
                    Wk, Wk_b = W[kc]
                    if c == 0:
                        ph.op("dve", lambda e, Wk=Wk, pU=pU: e.tensor_copy(out=Wk[:], in_=pU[:]), [pU_b], [Wk_b])
                    else:
                        ph.op("dve", lambda e, Wk=Wk, pU=pU, g128=g128: e.scalar_tensor_tensor(
                            out=Wk[:], in0=Wk[:], scalar=g128, in1=pU[:], op0=ALU.mult, op1=ALU.add), [Wk_b, pU_b], [Wk_b])
                    ph.op("act", lambda e, Wk=Wk, kc=kc, g128=g128: e.activation(out=Wb[kc][0][:], in_=Wk[:], func=AF.Copy,
                                                                                 scale=g128), [Wk_b], [Wb[kc][1]])
            if c + 1 < NT:
                emit_atm(c + 1)
            ph.op("act", lambda e, pO=pO, st=st: e.activation(out=junk[:], in_=pO[:], func=AF.Square,
                                                             accum_out=st[:, 0:1]), [pO_b], [junk_b, st_b])
            ph.op("dve", lambda e, st=st: e.tensor_scalar(out=st[:, 1:2], in0=st[:, 0:1], scalar1=1.0 / 512, scalar2=EPS,
                                                          op0=ALU.mult, op1=ALU.add), [st_b], [st_b])
            ph.op("act", lambda e, st=st: e.activation(out=st[:, 2:3], in_=st[:, 1:2], func=AF.Sqrt), [st_b], [st_b])
            ph.op("dve", lambda e, st=st: e.reciprocal(out=st[:, 3:4], in_=st[:, 2:3]), [st_b], [st_b])
            ph.op("dve", lambda e, pO=pO, st=st, g=g, out=out: e.scalar_tensor_tensor(
                out=out[:], in0=pO[:], scalar=st[:, 3:4], in1=g[:], op0=ALU.mult, op1=ALU.mult), [pO_b, st_b, g_b], [out_b])
            ph.dma("sp", s_oc[cs, h * 512:(h + 1) * 512], out[:], out_b, reads=[out_b])
    ph.run()


def phase_merge(ctx, l, modbc, s_oa, s_ob, s_oc, s_gT, w_a, w_b, w_c, w_out, xres):
    ph = Phase(ctx, "mrg")
    ident, ident_b = make_ident(ph)
    Wbr, Wbr_b = ph.sb("Wbr", [128, 32, D], BF16)
    wo, wo_b = ph.sb("wo", [128, 8, D], BF16)
    gt, gt_b = ph.sb("gt", [128, D], F32)
    for kc in range(8):
        ph.dma("pool", Wbr[:, kc, :], w_a[l, kc * 128:(kc + 1) * 128, :], Wbr_b, writes=[Wbr_b])
        ph.dma("pool", Wbr[:, 8 + kc, :], w_b[l, kc * 128:(kc + 1) * 128, :], Wbr_b, writes=[Wbr_b])
        ph.dma("pool", wo[:, kc, :], w_out[l, kc * 128:(kc + 1) * 128, :], wo_b, writes=[wo_b])
    for kc in range(16):
        ph.dma("pool", Wbr[:, 16 + kc, :], w_c[l, kc * 128:(kc + 1) * 128, :], Wbr_b, writes=[Wbr_b])
    ph.dma("sp", gt[:], modbc[l, :, 2 * D:3 * D], gt_b, writes=[gt_b])
    oin = [ph.sb("oin%d" % i, [128, 4096], BF16) for i in range(2)]
    oT, oT_b = ph.sb("oT", [128, 32, 512], BF16)
    gs = [ph.sb("gs%d" % i, [128, 3, 512], BF16) for i in range(2)]
    tt1 = [ph.sb("tt%d" % i, [128, 512], F32) for i in range(3)]
    mT, mT_b = ph.sb("mT", [128, 8, 512], BF16)
    xs = [ph.sb("x%d" % i, [128, D], F32) for i in range(2)]
    ty = [ph.sb("ty%d" % i, [128, 512], F32) for i in range(2)]
    psT = [ph.ps("pT%d" % i, [128, 8, 128], BF16) for i in range(2)]
    psB = [ph.ps("pB%d" % i) for i in range(3)]
    psY = [ph.ps("pY%d" % i) for i in range(2)]
    nT = 0
    ny = 0
    ng = 0
    for g in range(T // 512):
        for tt in range(4):
            rows = slice(g * 512 + tt * 128, g * 512 + (tt + 1) * 128)
            o, o_b = oin[tt % 2]
            ph.dma("sp", o[:, 0:1024], s_oa[rows, :], o_b, writes=[o_b])
            ph.dma("sp", o[:, 1024:2048], s_ob[rows, :], o_b, writes=[o_b])
            ph.dma("sp", o[:, 2048:4096], s_oc[rows, :], o_b, writes=[o_b])
            for k8 in range(4):
                pT, pT_b = psT[nT % 2]
                nT += 1
                for u in range(8):
                    kc = k8 * 8 + u
                    ph.op("pe", lambda e, pT=pT, u=u, o=o, kc=kc: e.transpose(pT[:, u, :], o[:, kc * 128:(kc + 1) * 128],
                                                                           ident[:]), [o_b, ident_b], [pT_b])
                ph.op("act", lambda e, pT=pT, k8=k8, tt=tt: e.copy(out=oT[:, k8 * 8:(k8 + 1) * 8, tt * 128:(tt + 1) * 128],
                                                                 in_=pT[:]), [pT_b], [oT_b])
        for dc in range(8):
            gsb, gsb_b = gs[ng % 2]
            ng += 1
            ph.dma("sp", gsb[:], s_gT.rearrange("(b r) t -> r b t", b=3)[dc * 128:(dc + 1) * 128, :, g * 512:(g + 1) * 512],
                   gsb_b, writes=[gsb_b])
            for br, (k0, k1) in enumerate(((0, 8), (8, 16), (16, 32))):
                pB, pB_b = psB[br]
                for kc in range(k0, k1):
                    ph.op("pe", lambda e, pB=pB, kc=kc, dc=dc, k0=k0, k1=k1: e.matmul(
                        pB[:], Wbr[:, kc, dc * 128:(dc + 1) * 128], oT[:, kc, :], start=(kc == k0), stop=(kc == k1 - 1)),
                          [Wbr_b, oT_b], [pB_b])
                t, t_b = tt1[br]
                ph.op("dve", lambda e, t=t, pB=pB, gsb=gsb, br=br: e.tensor_tensor(out=t[:], in0=pB[:], in1=gsb[:, br, :],
                                                                                  op=ALU.mult), [pB_b, gsb_b], [t_b])
            ph.op("pool", lambda e: e.tensor_tensor(out=tt1[0][0][:], in0=tt1[0][0][:], in1=tt1[1][0][:], op=ALU.add),
                  [tt1[0][1], tt1[1][1]], [tt1[0][1]])
            ph.op("pool", lambda e, dc=dc: e.tensor_tensor(out=mT[:, dc, :], in0=tt1[0][0][:], in1=tt1[2][0][:], op=ALU.add),
                  [tt1[0][1], tt1[2][1]], [mT_b])
        for tt in range(4):
            rows = slice(g * 512 + tt * 128, g * 512 + (tt + 1) * 128)
            x, x_b = xs[tt % 2]
            ph.dma("sp", x[:], xres[rows, :], x_b, writes=[x_b])
            for dh in range(2):
                pY, pY_b = psY[ny % 2]
                t, t_b = ty[ny % 2]
                ny += 1
                for dc in range(8):
                    ph.op("pe", lambda e, pY=pY, dc=dc, tt=tt, dh=dh: e.matmul(
                        pY[:], mT[:, dc, tt * 128:(tt + 1) * 128], wo[:, dc, dh * 512:(dh + 1) * 512], start=(dc == 0),
                        stop=(dc == 7)), [mT_b, wo_b], [pY_b])
                ph.op("dve", lambda e, t=t, pY=pY, dh=dh: e.tensor_tensor(out=t[:], in0=pY[:],
                                                                          in1=gt[:, dh * 512:(dh + 1) * 512], op=ALU.mult),
                      [pY_b, gt_b], [t_b])
                ph.op("pool", lambda e, t=t, x=x, dh=dh: e.tensor_tensor(out=x[:, dh * 512:(dh + 1) * 512],
                                                                         in0=x[:, dh * 512:(dh + 1) * 512], in1=t[:],
                                                                         op=ALU.add), [t_b, x_b], [x_b])
            ph.dma("sp", xres[rows, :], x[:], x_b, reads=[x_b])
    ph.run()


TH = 2048
NTH = TH // 128


def phase_route(ctx, l, hh, xres, modbc, norm_g, rg_w, rg_b, re_w, re_b, s_hT, s_gates):
    ph = Phase(ctx, "rt")
    identb, identb_b = make_ident(ph)
    idf, idf_b = ph.sb("identf32", [128, 128], F32)
    ph.op("pool", lambda e: e.memset(idf[:], 1.0), [], [idf_b])
    ph.op("pool", lambda e: e.affine_select(out=idf[:], in_=idf[:], pattern=[[-1, 128]], compare_op=ALU.is_equal,
                                            fill=0.0, base=0, channel_multiplier=1), [idf_b], [idf_b])
    G, GS_b = ph.sb("G", [128, D], F32)
    S, _ = ph.sb("S", [128, D], F32)
    ng, _ = ph.sb("ng", [128, D], F32)
    ph.dma("sp", G[:], modbc[l, :, 4 * D:5 * D], GS_b, writes=[GS_b])
    ph.dma("sp", S[:], modbc[l, :, 3 * D:4 * D], GS_b, writes=[GS_b])
    ph.dma("sp", ng[:], bcast_rows(norm_g[l:l + 1, :], 128), GS_b, writes=[GS_b])
    ph.op("dve", lambda e: e.tensor_tensor(out=G[:], in0=G[:], in1=ng[:], op=ALU.mult), [GS_b], [GS_b])
    rw, rw_b = ph.sb("rw", [128, 8, 36], F32)
    rb, rb_b = ph.sb("rb", [128, 36], F32)
    ph.dma("sp", rw[:, :, 0:4], rg_w[l].rearrange("(k p) n -> p k n", p=128), rw_b, writes=[rw_b])
    ph.dma("sp", rw[:, :, 4:36], re_w[l].rearrange("(k p) n -> p k n", p=128), rw_b, writes=[rw_b])
    ph.dma("sp", rb[:, 0:4], bcast_rows(rg_b[l:l + 1, :], 128), rb_b, writes=[rb_b])
    ph.dma("sp", rb[:, 4:36], bcast_rows(re_b[l:l + 1, :], 128), rb_b, writes=[rb_b])
    hT, hT_b = ph.sb("hT", [128, 8, TH], BF16)
    gates, gates_b = ph.sb("gates", [128, NTH, 32], F32)
    xs = [ph.sb("x%d" % i, [128, D], F32) for i in range(2)]
    sq, sq_b = ph.sb("sq", [128, D], BF16)
    hs = [ph.sb("h%d" % i, [128, D], BF16) for i in range(2)]
    st = [ph.sb("st%d" % i, [128, 4], F32) for i in range(2)]
    h32T = [ph.sb("h32T%d" % i, [128, 8, 128], F32) for i in range(2)]
    lgs = [ph.sb("lg%d" % i, [128, 36], F32) for i in range(2)]
    rts = [ph.sb("r%d" % i, [128, 64], F32) for i in range(2)]
    pts = [ph.ps("pt%d" % i, [128, 8, 128], BF16) for i in range(2)]
    pfs = [ph.ps("pf%d" % i, [128, 4, 128], F32) for i in range(2)]
    pls = [ph.ps("pl%d" % i) for i in range(2)]
    for t in range(NTH):
        x, x_b = xs[t % 2]
        h, h_b = hs[t % 2]
        s, s_b = st[t % 2]
        pt, pt_b = pts[t % 2]
        hf, hf_b = h32T[t % 2]
        pl, pl_b = pls[t % 2]
        lg, lg_b = lgs[t % 2]
        r, r_b = rts[t % 2]
        row0 = hh * TH + t * 128
        ph.dma("sp", x[:], xres[row0:row0 + 128, :], x_b, writes=[x_b])
        ph.op("act", lambda e, x=x, s=s: e.activation(out=sq[:], in_=x[:], func=AF.Square, accum_out=s[:, 0:1]),
              [x_b], [sq_b, s_b])
        ph.op("dve", lambda e, s=s: e.tensor_scalar(out=s[:, 1:2], in0=s[:, 0:1], scalar1=1.0 / D, scalar2=EPS,
                                                    op0=ALU.mult, op1=ALU.add), [s_b], [s_b])
        ph.op("act", lambda e, s=s: e.activation(out=s[:, 2:3], in_=s[:, 1:2], func=AF.Sqrt), [s_b], [s_b])
        ph.op("dve", lambda e, s=s: e.reciprocal(out=s[:, 3:4], in_=s[:, 2:3]), [s_b], [s_b])
        ph.op("dve", lambda e, x=x, s=s: e.scalar_tensor_tensor(out=x[:], in0=x[:], scalar=s[:, 3:4], in1=G[:],
                                                                op0=ALU.mult, op1=ALU.mult), [x_b, s_b, GS_b], [x_b])
        ph.op("pool", lambda e, x=x: e.tensor_tensor(out=x[:], in0=x[:], in1=S[:], op=ALU.add), [x_b, GS_b], [x_b])
        ph.op("pool", lambda e, x=x, h=h: e.tensor_copy(out=h[:], in_=x[:]), [x_b], [h_b])
        for k in range(8):
            ph.op("pe", lambda e, k=k, h=h, pt=pt: e.transpose(pt[:, k, :], h[:, k * 128:(k + 1) * 128], identb[:]),
                  [h_b, identb_b], [pt_b])
        ph.op("act", lambda e, pt=pt, t=t: e.copy(out=hT[:, :, t * 128:(t + 1) * 128], in_=pt[:]), [pt_b], [hT_b])
        for half in range(2):
            pf, pf_b = pfs[half]
            for u in range(4):
                k = half * 4 + u
                ph.op("pe", lambda e, pf=pf, u=u, k=k, x=x: e.transpose(pf[:, u, :], x[:, k * 128:(k + 1) * 128], idf[:]),
                      [x_b, idf_b], [pf_b])
            ph.op("dve", lambda e, pf=pf, hf=hf, half=half: e.tensor_copy(out=hf[:, half * 4:(half + 1) * 4, :], in_=pf[:]),
                  [pf_b], [hf_b])
        for k in range(8):
            ph.op("pe", lambda e, pl=pl, hf=hf, k=k: e.matmul(pl[:, 0:36], hf[:, k, :], rw[:, k, :], start=(k == 0),
                                                             stop=(k == 7)), [hf_b, rw_b], [pl_b])
        ph.op("dve", lambda e, lg=lg, pl=pl: e.tensor_tensor(out=lg[:], in0=pl[:, 0:36], in1=rb[:], op=ALU.add),
              [pl_b, rb_b], [lg_b])
        def dv(fn, r=r, lg=lg):
            ph.op("dve", fn, [r_b, lg_b], [r_b])
        dv(lambda e, r=r, lg=lg: e.tensor_reduce(out=r[:, 0:1], in_=lg[:, 0:4], axis=AX.X, op=ALU.max))
        dv(lambda e, r=r, lg=lg: e.tensor_scalar(out=r[:, 16:20], in0=lg[:, 0:4], scalar1=r[:, 0:1], scalar2=None,
                                                 op0=ALU.is_equal))
        dv(lambda e, r=r: e.tensor_scalar(out=r[:, 1:2], in0=r[:, 0:1], scalar1=-1.0, scalar2=None, op0=ALU.mult))
        ph.op("act", lambda e, r=r, lg=lg: e.activation(out=r[:, 20:24], in_=lg[:, 0:4], func=AF.Exp, bias=r[:, 1:2]),
              [r_b, lg_b], [r_b])
        dv(lambda e, r=r: e.tensor_reduce(out=r[:, 2:3], in_=r[:, 20:24], axis=AX.X, op=ALU.add))
        dv(lambda e, r=r: e.reciprocal(out=r[:, 3:4], in_=r[:, 2:3]))
        dv(lambda e, r=r, lg=lg: e.tensor_scalar(out=r[:, 24:32], in0=lg[:, 4:12], scalar1=r[:, 16:17], scalar2=None,
                                                 op0=ALU.mult))
        for g in range(1, 4):
            dv(lambda e, r=r, lg=lg, g=g: e.scalar_tensor_tensor(out=r[:, 24:32], in0=lg[:, 4 + 8 * g:12 + 8 * g],
                                                                 scalar=r[:, 16 + g:17 + g], in1=r[:, 24:32],
                                                                 op0=ALU.mult, op1=ALU.add))
        dv(lambda e, r=r: e.tensor_reduce(out=r[:, 4:5], in_=r[:, 24:32], axis=AX.X, op=ALU.max))
        dv(lambda e, r=r: e.tensor_scalar(out=r[:, 32:40], in0=r[:, 24:32], scalar1=r[:, 4:5], scalar2=None,
                                          op0=ALU.is_equal))
        dv(lambda e, r=r: e.scalar_tensor_tensor(out=r[:, 40:48], in0=r[:, 32:40], scalar=-1e30, in1=r[:, 24:32],
                                                 op0=ALU.mult, op1=ALU.add))
        dv(lambda e, r=r: e.tensor_reduce(out=r[:, 5:6], in_=r[:, 40:48], axis=AX.X, op=ALU.max))
        dv(lambda e, r=r: e.tensor_scalar(out=r[:, 48:56], in0=r[:, 40:48], scalar1=r[:, 5:6], scalar2=None,
                                          op0=ALU.is_equal))
        dv(lambda e, r=r: e.tensor_tensor(out=r[:, 6:7], in0=r[:, 5:6], in1=r[:, 4:5], op=ALU.subtract))
        ph.op("act", lambda e, r=r: e.activation(out=r[:, 7:8], in_=r[:, 6:7], func=AF.Exp), [r_b], [r_b])
        dv(lambda e, r=r: e.tensor_scalar(out=r[:, 8:9], in0=r[:, 7:8], scalar1=1.0, scalar2=None, op0=ALU.add))
        dv(lambda e, r=r: e.reciprocal(out=r[:, 9:10], in_=r[:, 8:9]))
        dv(lambda e, r=r: e.tensor_tensor(out=r[:, 10:11], in0=r[:, 9:10], in1=r[:, 3:4], op=ALU.mult))
        dv(lambda e, r=r: e.tensor_tensor(out=r[:, 11:12], in0=r[:, 10:11], in1=r[:, 7:8], op=ALU.mult))
        dv(lambda e, r=r: e.tensor_scalar(out=r[:, 56:64], in0=r[:, 32:40], scalar1=r[:, 10:11], scalar2=None,
                                          op0=ALU.mult))
        dv(lambda e, r=r: e.scalar_tensor_tensor(out=r[:, 56:64], in0=r[:, 48:56], scalar=r[:, 11:12], in1=r[:, 56:64],
                                                 op0=ALU.mult, op1=ALU.add))
        for g in range(4):
            ph.op("dve", lambda e, r=r, g=g, t=t: e.tensor_scalar(out=gates[:, t, g * 8:(g + 1) * 8], in0=r[:, 56:64],
                                                                  scalar1=r[:, 16 + g:17 + g], scalar2=None, op0=ALU.mult),
                  [r_b], [gates_b])
    for k in range(8):
        ph.dma("sp", s_hT[:, k, :], hT[:, k, :], hT_b, reads=[hT_b])
    ph.dma("sp", s_gates.rearrange("(t p) e -> p t e", p=128), gates[:], gates_b, reads=[gates_b])
    ph.run()


def phase_experts(ctx, l, hh, xres, modbc, s_hT, s_gates, e_gate, e_up, e_down, n_exp=32):
    ph = Phase(ctx, "ex")
    hT, hT_b = ph.sb("hT", [128, 8, TH], BF16)
    gates, gates_b = ph.sb("gates", [128, NTH, 32], F32)
    gt, gt_b = ph.sb("gt", [128, D], F32)
    yacc, yacc_b = ph.sb("yacc", [128, NTH, D], F32)
    for k in range(8):
        ph.dma("sp", hT[:, k, :], s_hT[:, k, :], hT_b, writes=[hT_b])
    ph.dma("sp", gates[:], s_gates.rearrange("(t p) e -> p t e", p=128), gates_b, writes=[gates_b])
    ph.dma("sp", gt[:], modbc[l, :, 5 * D:6 * D], gt_b, writes=[gt_b])
    wg = [ph.sb("wg%d" % i, [128, 8, 512], BF16) for i in range(2)]
    wu = [ph.sb("wu%d" % i, [128, 8, 512], BF16) for i in range(2)]
    wd = [ph.sb("wd%d" % i, [128, 4, D], BF16) for i in range(2)]
    sgs = [ph.sb("sg%d" % i, [128, 512], F32) for i in range(2)]
    hgs = [ph.sb("hg%d" % i, [128, 4, 512], BF16) for i in range(2)]
    psG = [ph.ps("pG%d" % i) for i in range(2)]
    psU = [ph.ps("pU%d" % i) for i in range(2)]
    psY = [ph.ps("pY%d" % i) for i in range(4)]
    nf = 0
    ny = 0
    nh = 0
    for ex in range(n_exp):
        g_, g_b = wg[ex % 2]
        u_, u_b = wu[ex % 2]
        d_, d_b = wd[ex % 2]
        ph.dma("pool", g_[:], e_gate[l, ex].rearrange("(k p) f -> p k f", p=128), g_b, writes=[g_b])
        ph.dma("pool", u_[:], e_up[l, ex].rearrange("(k p) f -> p k f", p=128), u_b, writes=[u_b])
        ph.dma("pool", d_[:], e_down[l, ex].rearrange("(k p) f -> p k f", p=128), d_b, writes=[d_b])
        for tg in range(TH // 512):
            ts = slice(tg * 512, (tg + 1) * 512)
            hg, hg_b = hgs[nh % 2]
            nh += 1
            for fc in range(4):
                pG, pG_b = psG[nf % 2]
                pU, pU_b = psU[nf % 2]
                sg, sg_b = sgs[nf % 2]
                nf += 1
                for k in range(8):
                    ph.op("pe", lambda e, pG=pG, g_=g_, k=k, fc=fc, ts=ts: e.matmul(
                        pG[:], g_[:, k, fc * 128:(fc + 1) * 128], hT[:, k, ts], start=(k == 0), stop=(k == 7)),
                          [g_b, hT_b], [pG_b])
                for k in range(8):
                    ph.op("pe", lambda e, pU=pU, u_=u_, k=k, fc=fc, ts=ts: e.matmul(
                        pU[:], u_[:, k, fc * 128:(fc + 1) * 128], hT[:, k, ts], start=(k == 0), stop=(k == 7)),
                          [u_b, hT_b], [pU_b])
                ph.op("act", lambda e, sg=sg, pG=pG: e.activation(out=sg[:], in_=pG[:], func=AF.Silu), [pG_b], [sg_b])
                ph.op("dve", lambda e, hg=hg, fc=fc, sg=sg, pU=pU: e.tensor_tensor(out=hg[:, fc, :], in0=pU[:], in1=sg[:],
                                                                                  op=ALU.mult), [pU_b, sg_b], [hg_b])
            for tt in range(4):
                tile = tg * 4 + tt
                for dh in range(2):
                    pY, pY_b = psY[ny % 4]
                    ny += 1
                    for fc in range(4):
                        ph.op("pe", lambda e, pY=pY, hg=hg, fc=fc, tt=tt, d_=d_, dh=dh: e.matmul(
                            pY[:], hg[:, fc, tt * 128:(tt + 1) * 128], d_[:, fc, dh * 512:(dh + 1) * 512], start=(fc == 0),
                            stop=(fc == 3)), [hg_b, d_b], [pY_b])
                    ya = yacc[:, tile, dh * 512:(dh + 1) * 512]
                    if ex == 0:
                        ph.op("dve", lambda e, ya=ya, pY=pY, tile=tile, ex=ex: e.tensor_scalar(
                            out=ya, in0=pY[:], scalar1=gates[:, tile, ex:ex + 1], scalar2=None, op0=ALU.mult),
                              [pY_b, gates_b], [yacc_b])
                    else:
                        ph.op("dve", lambda e, ya=ya, pY=pY, tile=tile, ex=ex: e.scalar_tensor_tensor(
                            out=ya, in0=pY[:], scalar=gates[:, tile, ex:ex + 1], in1=ya, op0=ALU.mult, op1=ALU.add),
                              [pY_b, gates_b, yacc_b], [yacc_b])
    xs = [ph.sb("x%d" % i, [128, D], F32) for i in range(2)]
    for t in range(NTH):
        x, x_b = xs[t % 2]
        row0 = hh * TH + t * 128
        ph.dma("sp", x[:], xres[row0:row0 + 128, :], x_b, writes=[x_b])
        ph.op("pool", lambda e, t=t: e.tensor_tensor(out=yacc[:, t, :], in0=yacc[:, t, :], in1=gt[:], op=ALU.mult),
              [yacc_b, gt_b], [yacc_b])
        ph.op("pool", lambda e, x=x, t=t: e.tensor_tensor(out=x[:], in0=x[:], in1=yacc[:, t, :], op=ALU.add),
              [x_b, yacc_b], [x_b])
        ph.dma("sp", xres[row0:row0 + 128, :], x[:], x_b, reads=[x_b])
    ph.run()


def phase_final(ctx, xres, final_g, y):
    ph = Phase(ctx, "fin")
    G, G_b = ph.sb("G", [128, D], F32)
    ph.dma("sp", G[:], bcast_rows(final_g, 128), G_b, writes=[G_b])
    xs = [ph.sb("x%d" % i, [128, D], F32) for i in range(3)]
    sq, sq_b = ph.sb("sq", [128, D], BF16)
    st = [ph.sb("st%d" % i, [128, 4], F32) for i in range(2)]
    for t in range(NT):
        x, x_b = xs[t % 3]
        s, s_b = st[t % 2]
        ph.dma("sp", x[:], xres[t * 128:(t + 1) * 128, :], x_b, writes=[x_b])
        ph.op("act", lambda e, x=x, s=s: e.activation(out=sq[:], in_=x[:], func=AF.Square, accum_out=s[:, 0:1]),
              [x_b], [sq_b, s_b])
        ph.op("dve", lambda e, s=s: e.tensor_scalar(out=s[:, 1:2], in0=s[:, 0:1], scalar1=1.0 / D, scalar2=EPS,
                                                    op0=ALU.mult, op1=ALU.add), [s_b], [s_b])
        ph.op("act", lambda e, s=s: e.activation(out=s[:, 2:3], in_=s[:, 1:2], func=AF.Sqrt), [s_b], [s_b])
        ph.op("dve", lambda e, s=s: e.reciprocal(out=s[:, 3:4], in_=s[:, 2:3]), [s_b], [s_b])
        ph.op("dve", lambda e, x=x, s=s: e.scalar_tensor_tensor(out=x[:], in0=x[:], scalar=s[:, 3:4], in1=G[:],
                                                                op0=ALU.mult, op1=ALU.mult), [x_b, s_b, G_b], [x_b])
        ph.dma("sp", y[t * 128:(t + 1) * 128, :], x[:], x_b, reads=[x_b])
    ph.run()


def make_oh_tab():
    u = np.arange(384)
    rel = np.maximum(255 - u, 0)
    relf = np.maximum(rel, 1).astype(np.float32)
    large = 16 + (np.log(relf / np.float32(16)) / np.float32(math.log(8)) * np.float32(16)).astype(np.int32)
    large = np.minimum(large, 31)
    bucket = np.where(rel < 16, rel, large)
    oh = np.zeros((32, 384), np.float32)
    oh[bucket, u] = 1.0
    return oh


def make_ret_tables():
    pos = np.arange(T, dtype=np.float32)
    theta = (np.float32(1.0) / (np.float32(10000.0) ** np.linspace(0.0, 1.0, 128, dtype=np.float32))).astype(np.float32)
    theta = np.repeat(theta, 2)
    ang = (pos[:, None] * theta[None, :]).astype(np.float32)
    cos = np.cos(ang.astype(np.float64)).astype(np.float32)
    sin = np.sin(ang.astype(np.float64)).astype(np.float32)
    sinS = sin.copy()
    sinS[:, 0::2] = -sin[:, 0::2]
    p = np.arange(128, dtype=np.float64)
    dec = np.zeros((128, 8), np.float32)
    for h in range(4):
        gam = 1.0 - 2.0 ** (-5.0 - h)
        dec[:, h] = gam ** (p + 1)
        dec[:, 4 + h] = gam ** (-(p + 1)) / 16.0
    return cos, sinS, dec


def phase_copy(ctx, src, dst, nrows):
    ph = Phase(ctx, "cp")
    b = ph.buf("cpbuf")
    step = 512
    for r in range(0, nrows, step):
        ph.dma("sp", dst[r:r + step, :], src[r:r + step, :], b)
    ph.run()
```

```python
import contextlib
import math
import numpy as np
import concourse.bass as bass
import concourse.mybir as mybir
from concourse.bass_utils import run_bass_kernel_spmd

F32 = mybir.dt.float32
BF16 = mybir.dt.bfloat16
AF = mybir.ActivationFunctionType
ALU = mybir.AluOpType
AX = mybir.AxisListType

D = 1024
T = 4096
DEPTH = 4
NT = T // 128
N_IN = 16968
EPS = 1e-6
O_AQ, O_AK, O_AV = 0, 1024, 2048
O_IQ, O_IK, O_IW = 3072, 3584, 3648
O_BQ, O_BF, O_BI, O_BG = 3656, 4680, 5704, 6728
O_CQ, O_CK, O_CV, O_CG = 7752, 8776, 9800, 11848
O_GA, O_GB, O_GC = 13896, 14920, 15944

COMPUTE = ("pe", "act", "dve", "pool")


class Buf:
    __slots__ = ("name", "writers", "readers", "sem")

    def __init__(self, name):
        self.name = name
        self.writers = []
        self.readers = []
        self.sem = None


class Op:
    __slots__ = ("eng", "emit", "deps", "is_dma", "sem", "semval", "used", "sig")

    def __init__(self, eng, emit, is_dma):
        self.eng = eng
        self.emit = emit
        self.deps = []
        self.is_dma = is_dma
        self.sem = None
        self.semval = 0
        self.used = False
        self.sig = 0


class Ctx:
    def __init__(self, nc, stack):
        self.nc = nc
        self.eng_sem = {}
        self.eng_cnt = {}
        for e in COMPUTE:
            self.eng_sem[e] = stack.enter_context(nc.semaphore("es_" + e))
            self.eng_cnt[e] = 0
        self.pool = []
        for i in range(56):
            self.pool.append([stack.enter_context(nc.semaphore("ds%d" % i)), 0])
        self.nphase = 0


class Phase:
    def __init__(self, ctx, name):
        self.ctx = ctx
        self.nc = ctx.nc
        self.name = "%s_%d" % (name, ctx.nphase)
        ctx.nphase += 1
        self.ops = []
        self.stack = contextlib.ExitStack()
        self.sems = []
        self.nbuf = 0

    def sb(self, name, shape, dtype):
        t = self.stack.enter_context(self.nc.sbuf_tensor("%s_%s" % (self.name, name), list(shape), dtype))
        return t, Buf(name)

    def ps(self, name, shape=(128, 512), dtype=F32):
        t = self.stack.enter_context(self.nc.psum_tensor("%s_%s" % (self.name, name), list(shape), dtype))
        return t, Buf(name)

    def buf(self, name):
        return Buf(name)

    def _sem_for(self, b):
        if b.sem is None:
            b.sem = self.ctx.pool.pop()
            self.sems.append(b.sem)
        return b.sem

    def _record(self, op, reads, writes):
        deps = []
        for b in reads:
            deps.extend(b.writers)
        for b in writes:
            keep = []
            for w in b.writers:
                if op.is_dma and w.is_dma and w.sem is op.sem:
                    keep.append(w)
                else:
                    deps.append(w)
            for r in b.readers:
                deps.append(r)
            b.writers = keep + [op]
            b.readers = []
        for b in reads:
            b.readers.append(op)
        seen = set()
        for d in deps:
            if (not d.is_dma) and d.eng == "pe" and op.eng == "pe":
                continue
            if id(d) not in seen and d is not op:
                seen.add(id(d))
                d.used = True
                op.deps.append(d)
        self.ops.append(op)

    def op(self, eng, emit, reads=(), writes=()):
        o = Op(eng, emit, False)
        self._record(o, reads, writes)
        return o

    def dma(self, eng, out, in_, sbuf, reads=(), writes=(), **kw):
        o = Op(eng, None, True)
        o.sem = self._sem_for(sbuf)
        o.sem[1] += 16
        o.semval = o.sem[1]
        o.emit = lambda e: e.dma_start(out=out, in_=in_, **kw)
        self._record(o, reads, writes)
        return o

    def run(self):
        ctx = self.ctx
        nc = self.nc
        for e in COMPUTE:
            for o in self.ops:
                if o.eng == e and not o.is_dma and o.used:
                    ctx.eng_cnt[e] += 1
                    o.sig = ctx.eng_cnt[e]
        engs = {"pe": [], "act": [], "dve": [], "pool": [], "sp": []}
        for o in self.ops:
            engs[o.eng].append(o)
        final_waits = [(s[0], s[1]) for s in self.sems]

        def emit_engine(e, name):
            waited = {}
            for o in engs[name]:
                need = {}
                for d in o.deps:
                    if d.is_dma:
                        key, val = d.sem[0], d.semval
                    else:
                        if d.eng == name and name == "pe":
                            continue
                        key, val = ctx.eng_sem[d.eng], d.sig
                    if need.get(key, 0) < val:
                        need[key] = val
                for key, val in need.items():
                    if waited.get(key, 0) < val:
                        e.wait_ge(key, val)
                        waited[key] = val
                ins = o.emit(e)
                if o.is_dma:
                    ins.then_inc(o.sem[0], 16)
                elif o.used:
                    ins.then_inc(ctx.eng_sem[name], 1)
            if name == "sp":
                for s, v in final_waits:
                    e.wait_ge(s, v)

        with nc.Block() as blk:
            blk.tensor(lambda e: emit_engine(e, "pe"))
            blk.scalar(lambda e: emit_engine(e, "act"))
            blk.vector(lambda e: emit_engine(e, "dve"))
            blk.gpsimd(lambda e: emit_engine(e, "pool"))
            blk.sync(lambda e: emit_engine(e, "sp"))
        for s in self.sems:
            ctx.pool.append(s)
        self.stack.close()


def bcast_rows(ap2d, nrows):
    return bass.AP(ap2d.tensor, ap2d.offset, [[0, nrows], [1, ap2d.shape[-1]]])


def phase_mod(ctx, c, ada_w, ada_b, modbc):
    nc = ctx.nc
    ph = Phase(ctx, "mod")
    cT, cT_b = ph.sb("cT", [128, 8], F32)
    cs, cs_b = ph.sb("cs", [128, 8], F32)
    crep, crep_b = ph.sb("crep", [128, 8, 128], F32)
    wts = [ph.sb("w%d" % i, [128, 8, 512], F32) for i in range(2)]
    bia, bia_b = ph.sb("bias", [128, 6144], F32)
    outs = [ph.sb("o%d" % i, [128, 512], F32) for i in range(2)]
    pss = [ph.ps("ps%d" % i) for i in range(2)]
    ph.dma("sp", cT[:], c.rearrange("o (k p) -> p (o k)", p=128), cT_b, writes=[cT_b],
           allow_slow_non_contiguous=True)
    ph.op("act", lambda e: e.activation(out=cs[:], in_=cT[:], func=AF.Silu), [cT_b], [cs_b])
    ph.op("dve", lambda e: e.tensor_copy(out=crep[:], in_=cs[:].unsqueeze(2).broadcast_to([128, 8, 128])),
          [cs_b], [crep_b])
    n = 0
    for l in range(DEPTH):
        ph.dma("sp", bia[:], bcast_rows(ada_b[l:l + 1, :], 128), bia_b, writes=[bia_b])
        for seg in (1, 4):
            ph.op("pool", lambda e, seg=seg: e.tensor_scalar_add(out=bia[:, seg * 1024:(seg + 1) * 1024],
                                                                 in0=bia[:, seg * 1024:(seg + 1) * 1024], scalar1=1.0),
                  [bia_b], [bia_b])
        for j in range(12):
            w, w_b = wts[n % 2]
            o, o_b = outs[n % 2]
            p, p_b = pss[n % 2]
            n += 1
            ph.dma("sp", w[:], ada_w[l, :, j * 512:(j + 1) * 512].rearrange("(k p) n -> p k n", p=128), w_b,
                   writes=[w_b])
            for k in range(8):
                ph.op("pe", lambda e, k=k, w=w, p=p: e.matmul(p[:], crep[:, k, :], w[:, k, :], start=(k == 0),
                                                               stop=(k == 7)),
                      [crep_b, w_b], [p_b])
            ph.op("dve", lambda e, o=o, p=p, j=j: e.tensor_tensor(out=o[:], in0=p[:], in1=bia[:, j * 512:(j + 1) * 512],
                                                                   op=ALU.add), [p_b, bia_b], [o_b])
            ph.dma("sp", modbc[l, :, j * 512:(j + 1) * 512], o[:], o_b, reads=[o_b])
    ph.run()


def emit_norm_tiles(ph, xsrc, hT, hT_b, G, S, GS_b, ident, ident_b, ntiles, h32T=None, h32T_b=None):
    xs = [ph.sb("x%d" % i, [128, D], F32) for i in range(2)]
    sq, sq_b = ph.sb("sq", [128, D], BF16)
    hs = [ph.sb("h%d" % i, [128, D], BF16) for i in range(2)]
    st = [ph.sb("st%d" % i, [128, 4], F32) for i in range(2)]
    pts = [ph.ps("pt%d" % i, [128, 8, 128], BF16) for i in range(2)]
    for t in range(ntiles):
        x, x_b = xs[t % 2]
        h, h_b = hs[t % 2]
        s, s_b = st[t % 2]
        pt, pt_b = pts[t % 2]
        ph.dma("sp", x[:], xsrc[t * 128:(t + 1) * 128, :], x_b, writes=[x_b])
        ph.op("act", lambda e, x=x, s=s: e.activation(out=sq[:], in_=x[:], func=AF.Square, accum_out=s[:, 0:1]),
              [x_b], [sq_b, s_b])
        ph.op("dve", lambda e, s=s: e.tensor_scalar(out=s[:, 1:2], in0=s[:, 0:1], scalar1=1.0 / D, scalar2=EPS,
                                                    op0=ALU.mult, op1=ALU.add), [s_b], [s_b])
        ph.op("act", lambda e, s=s: e.activation(out=s[:, 2:3], in_=s[:, 1:2], func=AF.Sqrt), [s_b], [s_b])
        ph.op("dve", lambda e, s=s: e.reciprocal(out=s[:, 3:4], in_=s[:, 2:3]), [s_b], [s_b])
        ph.op("dve", lambda e, x=x, s=s: e.scalar_tensor_tensor(out=x[:], in0=x[:], scalar=s[:, 3:4], in1=G[:],
                                                                op0=ALU.mult, op1=ALU.mult), [x_b, s_b, GS_b], [x_b])
        if S is not None:
            ph.op("pool", lambda e, x=x, h=h: e.tensor_tensor(out=h[:], in0=x[:], in1=S[:], op=ALU.add),
                  [x_b, GS_b], [h_b])
        else:
            ph.op("pool", lambda e, x=x, h=h: e.tensor_copy(out=h[:], in_=x[:]), [x_b], [h_b])
        for k in range(8):
            ph.op("pe", lambda e, k=k, h=h, pt=pt: e.transpose(pt[:, k, :], h[:, k * 128:(k + 1) * 128], ident[:]),
                  [h_b, ident_b], [pt_b])
        ph.op("act", lambda e, pt=pt, t=t: e.copy(out=hT[:, :, t * 128:(t + 1) * 128], in_=pt[:]), [pt_b], [hT_b])


def make_ident(ph, dtype=BF16):
    nc = ph.nc
    idf, idf_b = ph.sb("identf", [128, 128], F32)
    ident, ident_b = ph.sb("ident", [128, 128], dtype)
    ph.op("pool", lambda e: e.memset(idf[:], 1.0), [], [idf_b])
    ph.op("pool", lambda e: e.affine_select(out=idf[:], in_=idf[:], pattern=[[-1, 128]], compare_op=ALU.is_equal,
                                            fill=0.0, base=0, channel_multiplier=1), [idf_b], [idf_b])
    ph.op("pool", lambda e: e.tensor_copy(out=ident[:], in_=idf[:]), [idf_b], [ident_b])
    return ident, ident_b


def phase_proj(ctx, xres, modbc, l, norm_g, w_in_l, jobs, seg_scale, seg_shift):
    nc = ctx.nc
    ph = Phase(ctx, "proj")
    ident, ident_b = make_ident(ph)
    G, GS_b = ph.sb("G", [128, D], F32)
    S, _ = ph.sb("S", [128, D], F32)
    ng, _ = ph.sb("ng", [128, D], F32)
    hT, hT_b = ph.sb("hT", [128, 8, T], BF16)
    ph.dma("sp", G[:], modbc[l, :, seg_scale * D:(seg_scale + 1) * D], GS_b, writes=[GS_b])
    ph.dma("sp", S[:], modbc[l, :, seg_shift * D:(seg_shift + 1) * D], GS_b, writes=[GS_b])
    ph.dma("sp", ng[:], bcast_rows(norm_g[l:l + 1, :], 128), GS_b, writes=[GS_b])
    ph.op("dve", lambda e: e.tensor_tensor(out=G[:], in0=G[:], in1=ng[:], op=ALU.mult), [GS_b], [GS_b])
    emit_norm_tiles(ph, xres, hT, hT_b, G, S, GS_b, ident, ident_b, NT)

    wts = [ph.sb("w%d" % i, [128, 8, 512], BF16) for i in range(3)]
    pss = [ph.ps("pp%d" % i) for i in range(4)]
    stg = [ph.sb("sg%d" % i, [128, 512], F32) for i in range(4)]
    stgb = [ph.sb("sgb%d" % i, [128, 512], BF16) for i in range(4)]
    nw = 0
    ne = 0
    for (c0, ncols, kind, dst_fn, dtype, func) in jobs:
        step = 512 if kind == "tok" else 128
        for cc in range(c0, c0 + ncols, step):
            n = min(step, c0 + ncols - cc)
            w, w_b = wts[nw % 3]
            nw += 1
            ph.dma("pool", w[:, :, 0:n], w_in_l[:, cc:cc + n].rearrange("(k p) n -> p k n", p=128), w_b,
                   writes=[w_b])
            nchunk = NT if kind == "tok" else T // 512
            for ci in range(nchunk):
                p, p_b = pss[ne % 4]
                if dtype == F32:
                    sg, sg_b = stg[ne % 4]
                else:
                    sg, sg_b = stgb[ne % 4]
                evac_eng = "act" if (func is not None or ne % 2 == 0) else "dve"
                ne += 1
                if kind == "tok":
                    for k in range(8):
                        ph.op("pe", lambda e, k=k, w=w, p=p, ci=ci, n=n: e.matmul(
                            p[:, 0:n], hT[:, k, ci * 128:(ci + 1) * 128], w[:, k, 0:n], start=(k == 0), stop=(k == 7)),
                              [hT_b, w_b], [p_b])
                    po, so = p[:, 0:n], sg[:, 0:n]
                    dst = dst_fn(ci * 128, cc - c0, n)
                else:
                    for k in range(8):
                        ph.op("pe", lambda e, k=k, w=w, p=p, ci=ci, n=n: e.matmul(
                            p[0:n, :], w[:, k, 0:n], hT[:, k, ci * 512:(ci + 1) * 512], start=(k == 0), stop=(k == 7)),
                              [hT_b, w_b], [p_b])
                    po, so = p[0:n, :], sg[0:n, :]
                    dst = dst_fn(cc - c0, n, ci * 512)
                if evac_eng == "act":
                    f = func if func is not None else AF.Copy
                    ph.op("act", lambda e, po=po, so=so, f=f: e.activation(out=so, in_=po, func=f), [p_b], [sg_b])
                else:
                    ph.op("dve", lambda e, po=po, so=so: e.tensor_copy(out=so, in_=po), [p_b], [sg_b])
                ph.dma("sp", dst, so, sg_b, reads=[sg_b])
    ph.run()


INPUT_SPECS = [
    ("x", [T, D]), ("c", [1, D]), ("rel_bias", [32, 8]), ("hgrn_lb_raw", [DEPTH, D]), ("norm1_g", [DEPTH, D]),
    ("norm2_g", [DEPTH, D]), ("ada_w", [DEPTH, D, 6 * D]), ("ada_b", [DEPTH, 6 * D]), ("w_in", [DEPTH, D, N_IN]),
    ("hgrn_norm_g", [DEPTH, 128]), ("w_branch_a", [DEPTH, 1024, D]), ("w_branch_b", [DEPTH, 1024, D]),
    ("w_branch_c", [DEPTH, 2048, D]), ("w_out", [DEPTH, D, D]), ("router_group_w", [DEPTH, D, 4]),
    ("router_group_b", [DEPTH, 4]), ("router_expert_w", [DEPTH, D, 32]), ("router_expert_b", [DEPTH, 32]),
    ("expert_w_gate", [DEPTH, 32, D, 512]), ("expert_w_up", [DEPTH, 32, D, 512]), ("expert_w_down", [DEPTH, 32, 512, D]),
    ("final_norm_g", [1, D]), ("oh_tab", [32, 384]), ("cosT", [T, 256]), ("sinS", [T, 256]), ("cdec", [128, 8]),
]


def build_program(depth=DEPTH, debug=False):
    nc = bass.Bass("TRN2", target_bir_lowering=False)
    I = {}
    for name, shape in INPUT_SPECS:
        I[name] = nc.dram_tensor(name, shape, F32, kind="ExternalInput").ap()
    y = nc.dram_tensor("y", [T, D], F32, kind="ExternalOutput").ap()

    def scr(name, shape, dt):
        return nc.dram_tensor(name, shape, dt, kind="ExternalOutput" if debug else "Internal").ap()

    xres = scr("xres", [T, D], F32)
    modbc = scr("modbc", [DEPTH, 128, 6 * D], F32)
    s_aqT = scr("s_aqT", [1024, T], BF16)
    s_akT = scr("s_akT", [1024, T], BF16)
    s_av = scr("s_av", [T, 1024], BF16)
    s_iqT = scr("s_iqT", [512, T], BF16)
    s_ikT = scr("s_ikT", [64, T], BF16)
    s_iw = scr("s_iw", [T, 8], F32)
    s_bqT = scr("s_bqT", [1024, T], BF16)
    s_bfT = scr("s_bfT", [1024, T], F32)
    s_bi = scr("s_bi", [T, 1024], BF16)
    s_bg = scr("s_bg", [T, 1024], BF16)
    s_cq = scr("s_cq", [T, 1024], F32)
    s_ck = scr("s_ck", [T, 1024], F32)
    s_cv = scr("s_cv", [T, 2048], BF16)
    s_cg = scr("s_cg", [T, 2048], BF16)
    s_gT = scr("s_gT", [3072, T], BF16)
    s_mT = scr("s_mT", [NT, 128, NT, 128], BF16)
    s_tab = scr("s_tab", [128, 3072], BF16)
    s_oa = scr("s_oa", [T, 1024], BF16)
    s_ob = scr("s_ob", [T, 1024], BF16)
    s_oc = scr("s_oc", [T, 2048], BF16)
    s_hT = scr("s_hT", [128, 8, TH], BF16)
    s_gates = scr("s_gates", [T, 32], F32)

    def tok(dst):
        return lambda t0, c0, n: dst[t0:t0 + 128, c0:c0 + n]

    def feat(dst):
        return lambda c0, n, t0: dst[c0:c0 + n, t0:t0 + 512]

    jobs = [
        (O_AQ, 1024, "feat", feat(s_aqT), BF16, None),
        (O_AK, 1024, "feat", feat(s_akT), BF16, None),
        (O_AV, 1024, "tok", tok(s_av), BF16, None),
        (O_IQ, 512, "feat", feat(s_iqT), BF16, None),
        (O_IK, 64, "feat", feat(s_ikT), BF16, None),
        (O_IW, 8, "tok", tok(s_iw), F32, None),
        (O_BQ, 1024, "feat", feat(s_bqT), BF16, None),
        (O_BF, 1024, "feat", feat(s_bfT), F32, None),
        (O_BI, 1024, "tok", tok(s_bi), BF16, None),
        (O_BG, 1024, "tok", tok(s_bg), BF16, AF.Silu),
        (O_CQ, 1024, "tok", tok(s_cq), F32, None),
        (O_CK, 1024, "tok", tok(s_ck), F32, None),
        (O_CV, 2048, "tok", tok(s_cv), BF16, None),
        (O_CG, 2048, "tok", tok(s_cg), BF16, AF.Silu),
        (O_GA, 3072, "feat", feat(s_gT), BF16, AF.Sigmoid),
    ]
    with contextlib.ExitStack() as stack:
        ctx = Ctx(nc, stack)
        phase_copy(ctx, I["x"], xres, T)
        phase_mod(ctx, I["c"], I["ada_w"], I["ada_b"], modbc)
        phase_bias_tab(ctx, I["rel_bias"], I["oh_tab"], s_tab)
        for l in range(depth):
            phase_proj(ctx, xres, modbc, l, I["norm1_g"], I["w_in"][l], jobs, 1, 0)
            phase_a1(ctx, s_iqT, s_ikT, s_iw, s_mT)
            for hg in range(2):
                phase_a2(ctx, hg, s_aqT, s_akT, s_av, s_mT, s_tab, s_oa)
            phase_b(ctx, l, I["hgrn_lb_raw"], s_bqT, s_bfT, s_bi, s_bg, I["hgrn_norm_g"], s_ob)
            phase_c(ctx, s_cq, s_ck, s_cv, s_cg, I["cosT"], I["sinS"], I["cdec"], s_oc)
            phase_merge(ctx, l, modbc, s_oa, s_ob, s_oc, s_gT, I["w_branch_a"], I["w_branch_b"], I["w_branch_c"],
                        I["w_out"], xres)
            for hh in range(2):
                sg = s_gates[hh * TH:(hh + 1) * TH, :]
                phase_route(ctx, l, hh, xres, modbc, I["norm2_g"], I["router_group_w"], I["router_group_b"],
                            I["router_expert_w"], I["router_expert_b"], s_hT, sg)
                phase_experts(ctx, l, hh, xres, modbc, s_hT, sg, I["expert_w_gate"], I["expert_w_up"], I["expert_w_down"])
        phase_final(ctx, xres, I["final_norm_g"], y)
    return nc


_PROGRAM = None


def kernel(**inputs):
    global _PROGRAM
    if _PROGRAM is None:
        _PROGRAM = build_program()
    nc = _PROGRAM
    f = lambda a: np.ascontiguousarray(np.asarray(a, dtype=np.float32))
    cos, sinS, dec = make_ret_tables()
    shared = {k: f(inputs[k]) for k in ("rel_bias", "hgrn_lb_raw", "norm1_g", "norm2_g", "ada_w", "ada_b", "w_in",
                                        "hgrn_norm_g", "w_branch_a", "w_branch_b", "w_branch_c", "w_out",
                                        "router_group_w", "router_group_b", "router_expert_w", "router_expert_b",
                                        "expert_w_gate", "expert_w_up", "expert_w_down")}
    shared["final_norm_g"] = f(inputs["final_norm_g"]).reshape(1, D)
    shared["oh_tab"] = make_oh_tab()
    shared["cosT"] = cos
    shared["sinS"] = sinS
    shared["cdec"] = dec
    x = f(inputs["x"])
    c = f(inputs["c"])
    in_maps = []
    for core in range(8):
        b = core % 4
        m = dict(shared)
        m["x"] = x[b]
        m["c"] = c[b:b + 1]
        in_maps.append(m)
    res = run_bass_kernel_spmd(nc, in_maps, core_ids=list(range(8)))
    out = np.stack([np.asarray(res.results[b]["y"], dtype=np.float32) for b in range(4)], axis=0)
    return out


TOPK = 256
NBIS = 16
MASKV = -30000.0


def phase_a1(ctx, s_iqT, s_ikT, s_iw, s_mT):
    ph = Phase(ctx, "a1")
    ident, ident_b = make_ident(ph)
    ikT, ikT_b = ph.sb("ikT", [64, T], BF16)
    ph.dma("sp", ikT[:], s_ikT[:, :], ikT_b, writes=[ikT_b])
    pw, pw_b = ph.sb("pw", [128, NBIS], F32)
    for k in range(NBIS):
        ph.op("pool", lambda e, k=k: e.memset(pw[:, k:k + 1], 0.5 ** (k + 1)), [], [pw_b])
    cm, cm_b = ph.sb("cm", [128, 128], F32)
    ph.op("pool", lambda e: e.memset(cm[:], 0.0), [], [cm_b])
    ph.op("pool", lambda e: e.affine_select(out=cm[:], in_=cm[:], pattern=[[-1, 128]], compare_op=ALU.is_ge, fill=-1e30,
                                            base=0, channel_multiplier=1), [cm_b], [cm_b])
    iqs = [ph.sb("iq%d" % i, [64, 8, 128], BF16) for i in range(4)]
    ws = [ph.sb("w%d" % i, [128, 24], F32) for i in range(4)]
    Is = [ph.sb("I%d" % i, [128, T], F32) for i in range(4)]
    rls = [ph.sb("rl%d" % i, [128, 512], F32) for i in range(4)]
    junk, junk_b = ph.sb("junk", [128, T], BF16)
    junk2, junk2_b = ph.sb("junk2", [128, T], BF16)
    m01s = [ph.sb("m01_%d" % i, [128, T], BF16) for i in range(2)]
    mTs = [ph.sb("mT%d" % i, [128, NT, 128], BF16) for i in range(2)]
    sts = [ph.sb("st%d" % i, [128, 8 + NBIS], F32) for i in range(4)]
    pss = [ph.ps("ps%d" % i) for i in range(4)]
    ptr = [ph.ps("ptr%d" % i, [128, 4, 128], BF16) for i in range(2)]
    cnt = {"pz": 0, "rl": 0, "tr": 0}

    def gen_indexer(i):
        S = (i + 1) * 128
        iq, iq_b = iqs[i % 4]
        w, w_b = ws[i % 4]
        I, I_b = Is[i % 4]
        ph.dma("sp", iq[:], s_iqT[:, i * 128:(i + 1) * 128].rearrange("(h d) t -> d h t", d=64), iq_b, writes=[iq_b])
        ph.dma("sp", w[:, 0:8], s_iw[i * 128:(i + 1) * 128, :], w_b, writes=[w_b])
        ph.op("pool", lambda e: e.tensor_scalar(out=w[:, 0:8], in0=w[:, 0:8], scalar1=512.0 ** -0.5, scalar2=None,
                                                op0=ALU.mult), [w_b], [w_b])
        ph.op("pool", lambda e: e.tensor_scalar(out=w[:, 8:16], in0=w[:, 0:8], scalar1=-1.0, scalar2=None, op0=ALU.mult),
              [w_b], [w_b])
        ph.op("dve", lambda e: e.tensor_tensor(out=w[:, 8:16], in0=w[:, 8:16], in1=w[:, 0:8], op=ALU.max), [w_b], [w_b])
        ph.op("pool", lambda e: e.tensor_scalar(out=w[:, 16:24], in0=w[:, 0:8], scalar1=0.0, scalar2=2.0, op0=ALU.is_ge,
                                                op1=ALU.mult), [w_b], [w_b])
        ph.op("pool", lambda e: e.tensor_scalar(out=w[:, 16:24], in0=w[:, 16:24], scalar1=-1.0, scalar2=None, op0=ALU.add),
              [w_b], [w_b])
        yield
        for c0 in range(0, S, 512):
            n = min(512, S - c0)
            for h in range(8):
                p, p_b = pss[cnt["pz"] % 4]
                cnt["pz"] += 1
                rl, rl_b = rls[cnt["rl"] % 4]
                cnt["rl"] += 1
                ph.op("pe", lambda e, p=p, h=h, c0=c0, n=n: e.matmul(p[:, 0:n], iq[:, h, :], ikT[:, c0:c0 + n], start=True, stop=True),
                      [iq_b, ikT_b], [p_b])
                ph.op("act", lambda e, p=p, rl=rl, h=h, n=n: e.activation(out=rl[:, 0:n], in_=p[:, 0:n], func=AF.Relu,
                                                                     scale=w[:, 8 + h:9 + h]), [p_b, w_b], [rl_b])
                if h == 0:
                    ph.op("dve", lambda e, rl=rl, h=h, c0=c0, n=n: e.tensor_scalar(out=I[:, c0:c0 + n], in0=rl[:, 0:n],
                                                                       scalar1=w[:, 16 + h:17 + h], scalar2=None,
                                                                       op0=ALU.mult), [rl_b, w_b], [I_b])
                else:
                    ph.op("dve", lambda e, rl=rl, h=h, c0=c0, n=n: e.scalar_tensor_tensor(out=I[:, c0:c0 + n], in0=rl[:, 0:n],
                                                                              scalar=w[:, 16 + h:17 + h],
                                                                              in1=I[:, c0:c0 + n], op0=ALU.mult,
                                                                              op1=ALU.add), [rl_b, w_b, I_b], [I_b])
                yield
        ph.op("pool", lambda e: e.tensor_tensor(out=I[:, i * 128:(i + 1) * 128], in0=I[:, i * 128:(i + 1) * 128],
                                                in1=cm[:], op=ALU.add), [I_b, cm_b], [I_b])
        yield

    def gen_select(i):
        S = (i + 1) * 128
        I, I_b = Is[i % 4]
        m01, m01_b = m01s[i % 2]
        mT, mT_b = mTs[i % 2]
        st, st_b = sts[i % 4]
        if i < 2:
            ph.op("dve", lambda e: e.memset(st[:, 0:1], -1e29), [], [st_b])
        else:
            on_act = (i % 2 == 1)
            ph.op("dve", lambda e: e.tensor_reduce(out=st[:, 0:1], in_=I[:, 0:i * 128], axis=AX.X, op=ALU.min),
                  [I_b], [st_b])
            ph.op("dve", lambda e: e.tensor_reduce(out=st[:, 1:2], in_=I[:, 0:S], axis=AX.X, op=ALU.max), [I_b], [st_b])
            ph.op("dve", lambda e: e.tensor_tensor(out=st[:, 2:3], in0=st[:, 1:2], in1=st[:, 0:1], op=ALU.subtract),
                  [st_b], [st_b])
            ph.op("dve", lambda e: e.tensor_scalar(out=st[:, 8:8 + NBIS], in0=pw[:], scalar1=st[:, 2:3], scalar2=None,
                                                   op0=ALU.mult), [st_b, pw_b], [st_b])
            yield
            for k in range(NBIS):
                if on_act:
                    ph.op("dve", lambda e, k=k: e.tensor_scalar(out=st[:, 3:4], in0=st[:, 0:1], scalar1=st[:, 8 + k:9 + k],
                                                                scalar2=-1.0, op0=ALU.add, op1=ALU.mult), [st_b], [st_b])
                    yield
                    ph.op("act", lambda e: e.activation(out=junk2[:, 0:S], in_=I[:, 0:S], func=AF.Sign, bias=st[:, 3:4],
                                                        accum_out=st[:, 4:5]), [I_b, st_b], [junk2_b, st_b])
                    yield
                    thresh = 2.0 * (TOPK - 0.5) - S
                else:
                    ph.op("dve", lambda e, k=k: e.tensor_tensor(out=st[:, 3:4], in0=st[:, 0:1], in1=st[:, 8 + k:9 + k],
                                                                op=ALU.add), [st_b], [st_b])
                    yield
                    ph.op("dve", lambda e: e.tensor_scalar(out=junk[:, 0:S], in0=I[:, 0:S], scalar1=st[:, 3:4],
                                                           scalar2=None, op0=ALU.is_ge, op1=ALU.add,
                                                           accum_out=st[:, 4:5]), [I_b, st_b], [junk_b, st_b])
                    yield
                    thresh = TOPK - 0.5
                ph.op("dve", lambda e, k=k, thresh=thresh: e.scalar_tensor_tensor(
                    out=st[:, 5:6], in0=st[:, 4:5], scalar=thresh, in1=st[:, 8 + k:9 + k], op0=ALU.is_ge, op1=ALU.mult),
                      [st_b], [st_b])
                ph.op("dve", lambda e: e.tensor_tensor(out=st[:, 0:1], in0=st[:, 0:1], in1=st[:, 5:6], op=ALU.add),
                      [st_b], [st_b])
                yield
        ph.op("dve", lambda e: e.tensor_scalar(out=m01[:, 0:S], in0=I[:, 0:S], scalar1=st[:, 0:1], scalar2=None,
                                               op0=ALU.is_ge), [I_b, st_b], [m01_b])
        yield
        for j0 in range(0, i + 1, 4):
            nj = min(4, i + 1 - j0)
            pt, pt_b = ptr[cnt["tr"] % 2]
            cnt["tr"] += 1
            for u in range(nj):
                j = j0 + u
                ph.op("pe", lambda e, pt=pt, u=u, j=j: e.transpose(pt[:, u, :], m01[:, j * 128:(j + 1) * 128], ident[:]),
                      [m01_b, ident_b], [pt_b])
            ph.op("dve", lambda e, pt=pt, j0=j0, nj=nj: e.tensor_copy(out=mT[:, j0:j0 + nj, :], in_=pt[:, 0:nj, :]),
                  [pt_b], [mT_b])
            yield
        ph.dma("sp", s_mT[i, :, 0:i + 1, :], mT[:, 0:i + 1, :], mT_b, reads=[mT_b])

    def merge(gens):
        state = [[g, max(n, 1), 0, False] for g, n in gens]
        while any(not st_[3] for st_ in state):
            best = None
            for st_ in state:
                if st_[3]:
                    continue
                frac = st_[2] / st_[1]
                if best is None or frac < best[0]:
                    best = (frac, st_)
            st_ = best[1]
            try:
                next(st_[0])
                st_[2] += 1
            except StopIteration:
                st_[3] = True

    def n_sel(i):
        return (3 * NBIS + 4 if i >= 2 else 3) + (i + 4) // 4

    def n_idx(i):
        return 2 + 8 * (((i + 1) * 128 + 511) // 512)

    merge([(gen_indexer(0), n_idx(0)), (gen_indexer(1), n_idx(1))])
    for m in range(NT // 2):
        gens = [(gen_select(2 * m), n_sel(2 * m)), (gen_select(2 * m + 1), n_sel(2 * m + 1))]
        if 2 * m + 2 < NT:
            gens.append((gen_indexer(2 * m + 2), n_idx(2 * m + 2)))
            gens.append((gen_indexer(2 * m + 3), n_idx(2 * m + 3)))
        merge(gens)
    ph.run()


def phase_bias_tab(ctx, rel_bias, oh_tab, s_tab):
    ph = Phase(ctx, "btab")
    rb, rb_b = ph.sb("rb", [32, 8], F32)
    r31, r31_b = ph.sb("r31", [32, 8], F32)
    rep, rep_b = ph.sb("rep", [32, 8, 128], F32)
    oh, oh_b = ph.sb("oh", [32, 384], F32)
    tb, tb_b = ph.sb("tb", [128, 8, 384], BF16)
    pss = [ph.ps("p%d" % i) for i in range(2)]
    ph.dma("sp", rb[:], rel_bias[:, :], rb_b, writes=[rb_b])
    ph.dma("sp", r31[:], bcast_rows(rel_bias[31:32, :], 32), r31_b, writes=[r31_b])
    ph.dma("sp", oh[:], oh_tab[:, :], oh_b, writes=[oh_b])
    ph.op("dve", lambda e: e.tensor_tensor(out=rb[:], in0=rb[:], in1=r31[:], op=ALU.subtract), [rb_b, r31_b], [rb_b])
    ph.op("dve", lambda e: e.tensor_scalar(out=rb[:], in0=rb[:], scalar1=math.sqrt(128.0), scalar2=None, op0=ALU.mult),
          [rb_b], [rb_b])
    ph.op("dve", lambda e: e.tensor_copy(out=rep[:], in_=rb[:].unsqueeze(2).broadcast_to([32, 8, 128])), [rb_b], [rep_b])
    for h in range(8):
        p, p_b = pss[h % 2]
        ph.op("pe", lambda e, p=p, h=h: e.matmul(p[:, 0:384], rep[:, h, :], oh[:], start=True, stop=True),
              [rep_b, oh_b], [p_b])
        ph.op("act", lambda e, p=p, h=h: e.copy(out=tb[:, h, :], in_=p[:, 0:384]), [p_b], [tb_b])
    ph.dma("sp", s_tab[:, :], tb[:].rearrange("p h u -> p (h u)"), tb_b, reads=[tb_b])
    ph.run()


def phase_a2(ctx, hg, s_aqT, s_akT, s_av, s_mT, s_tab, s_oa):
    ph = Phase(ctx, "a2")
    ident, ident_b = make_ident(ph)
    kT, kT_b = ph.sb("kT", [128, 4, T], BF16)
    v, v_b = ph.sb("v", [128, NT, 4, 129], BF16)
    bt, bt_b = ph.sb("bt", [128, 2, 4, 128], BF16)
    r0 = hg * 512
    for h in range(4):
        ph.dma("sp", kT[:, h, :], s_akT[r0 + h * 128:r0 + (h + 1) * 128, :], kT_b, writes=[kT_b])
        ph.dma("sp", v[:, :, h, 0:128], s_av[:, r0 + h * 128:r0 + (h + 1) * 128].rearrange("(j p) d -> p j d", p=128),
               v_b, writes=[v_b])
        for pat in range(2):
            src = bass.AP(s_tab.tensor, s_tab.offset + (hg * 4 + h) * 384 + 255 - 128 * pat, [[3071, 128], [1, 128]])
            ph.dma("sp", bt[:, pat, h, :], src, bt_b, writes=[bt_b])
    ph.op("pool", lambda e: e.memset(v[:, :, :, 128:129], 1.0), [], [v_b])
    qs = [ph.sb("q%d" % i, [128, 4, 128], BF16) for i in range(2)]
    mbs = [ph.sb("mb%d" % i, [128, NT, 128], BF16) for i in range(2)]
    pts = [ph.sb("pt%d" % i, [128, 4, 128], BF16) for i in range(3)]
    sts = [ph.ps("st%d" % i) for i in range(3)]
    ops = [ph.ps("o%d" % i) for i in range(4)]
    rcs = [ph.sb("rc%d" % i, [128, 4], F32) for i in range(2)]
    outs = [ph.sb("out%d" % i, [128, 512], BF16) for i in range(2)]
    sts3 = sts
    pairs = [(i, j) for i in range(NT) for j in range(i + 1)]

    def emit_loads(i):
        q, q_b = qs[i % 2]
        mb, mb_b = mbs[i % 2]
        ph.dma("sp", q[:], s_aqT[r0:r0 + 512, i * 128:(i + 1) * 128].rearrange("(h d) t -> d h t", d=128), q_b,
               writes=[q_b])
        ph.dma("sp", mb[:, 0:i + 1, :], s_mT[i, :, 0:i + 1, :], mb_b, writes=[mb_b])

    def emit_qk(idx):
        i, j = pairs[idx]
        q, q_b = qs[i % 2]
        st, st_b = sts3[idx % 3]
        near = (i - j) <= 1
        for h in range(4):
            ph.op("pe", lambda e, h=h: e.matmul(st[:, h * 128:(h + 1) * 128], kT[:, h, j * 128:(j + 1) * 128], q[:, h, :],
                                                start=True, stop=not near), [kT_b, q_b], [st_b])
            if near:
                ph.op("pe", lambda e, h=h: e.matmul(st[:, h * 128:(h + 1) * 128], bt[:, i - j, h, :], ident[:], start=False,
                                                    stop=True), [bt_b, ident_b], [st_b])

    def emit_soft(idx):
        i, j = pairs[idx]
        mb, mb_b = mbs[i % 2]
        st, st_b = sts3[idx % 3]
        pt, pt_b = pts[idx % 3]
        ph.op("act", lambda e: e.activation(out=pt[:], in_=st[:], func=AF.Exp, scale=128.0 ** -0.5), [st_b], [pt_b])
        ph.op("dve", lambda e: e.tensor_tensor(out=pt[:], in0=pt[:], in1=mb[:, j:j + 1, :].broadcast_to([128, 4, 128]),
                                               op=ALU.mult), [pt_b, mb_b], [pt_b])

    def emit_pv(idx):
        i, j = pairs[idx]
        pt, pt_b = pts[idx % 3]
        for h in range(4):
            o, o_b = ops[h]
            ph.op("pe", lambda e, o=o, h=h: e.matmul(o[:, 0:129], pt[:, h, :], v[:, j, h, :], start=(j == 0), stop=(j == i)),
                  [pt_b, v_b], [o_b])
        if j == i:
            rc, rc_b = rcs[i % 2]
            out, out_b = outs[i % 2]
            for h in range(4):
                o, o_b = ops[h]
                ph.op("dve", lambda e, o=o, h=h: e.reciprocal(out=rc[:, h:h + 1], in_=o[:, 128:129]), [o_b], [rc_b])
                ph.op("dve", lambda e, o=o, h=h: e.tensor_scalar(out=out[:, h * 128:(h + 1) * 128], in0=o[:, 0:128],
                                                                 scalar1=rc[:, h:h + 1], scalar2=None, op0=ALU.mult),
                      [o_b, rc_b], [out_b])
            ph.dma("sp", s_oa[i * 128:(i + 1) * 128, r0:r0 + 512], out[:], out_b, reads=[out_b])

    emit_loads(0)
    emit_qk(0)
    if len(pairs) > 1:
        emit_loads(1)
        emit_qk(1)
    for idx in range(len(pairs)):
        i, j = pairs[idx]
        if j == 0 and i >= 1 and i + 1 < NT:
            emit_loads(i + 1)
        emit_soft(idx)
        if idx + 2 < len(pairs):
            emit_qk(idx + 2)
        emit_pv(idx)
    ph.run()


def phase_b(ctx, l, lb_raw, s_bqT, s_bfT, s_bi, s_bg, hgrn_norm_g, s_ob):
    ph = Phase(ctx, "hg")
    ident, ident_b = make_ident(ph)
    lbr, lbr_b = ph.sb("lbr", [128, DEPTH, 8], F32)
    lb, lb_b = ph.sb("lb", [128, 8], F32)
    oml, oml_b = ph.sb("oml", [128, 8], F32)
    ssum, ssum_b = ph.sb("ssum", [128, 8], F32)
    ph.dma("sp", lbr[:], lb_raw.rearrange("l (h p) -> p l h", p=128), lbr_b, writes=[lbr_b],
           allow_slow_non_contiguous=True)
    ph.op("act", lambda e: e.activation(out=lbr[:], in_=lbr[:], func=AF.Exp), [lbr_b], [lbr_b])
    ph.op("dve", lambda e: e.tensor_tensor(out=ssum[:], in0=lbr[:, 0, :], in1=lbr[:, 1, :], op=ALU.add), [lbr_b], [ssum_b])
    ph.op("dve", lambda e: e.tensor_tensor(out=ssum[:], in0=ssum[:], in1=lbr[:, 2, :], op=ALU.add), [lbr_b, ssum_b], [ssum_b])
    ph.op("dve", lambda e: e.tensor_tensor(out=ssum[:], in0=ssum[:], in1=lbr[:, 3, :], op=ALU.add), [lbr_b, ssum_b], [ssum_b])
    ph.op("dve", lambda e: e.reciprocal(out=ssum[:], in_=ssum[:]), [ssum_b], [ssum_b])
    ph.op("dve", lambda e: e.memset(lb[:], 0.0), [], [lb_b])
    for m in range(1, l + 1):
        ph.op("dve", lambda e, m=m: e.tensor_tensor(out=lb[:], in0=lb[:], in1=lbr[:, m, :], op=ALU.add), [lbr_b, lb_b], [lb_b])
    ph.op("dve", lambda e: e.tensor_tensor(out=lb[:], in0=lb[:], in1=ssum[:], op=ALU.mult), [lb_b, ssum_b], [lb_b])
    ph.op("dve", lambda e: e.tensor_scalar(out=oml[:], in0=lb[:], scalar1=-1.0, scalar2=1.0, op0=ALU.mult, op1=ALU.add),
          [lb_b], [oml_b])
    ng, ng_b = ph.sb("ng", [64, 128], F32)
    ph.dma("sp", ng[:], bcast_rows(hgrn_norm_g[l:l + 1, :], 64), ng_b, writes=[ng_b])
    ones, ones_b = ph.sb("ones", [128, T], BF16)
    ph.op("pool", lambda e: e.memset(ones[:], 1.0), [], [ones_b])
    zer, zer_b = ph.sb("zer", [128, 32], BF16)
    ph.op("pool", lambda e: e.memset(zer[:], 0.0), [], [zer_b])
    m01, m01_b = ph.sb("m01", [64, 64], F32)
    ph.op("pool", lambda e: e.memset(m01[:], 1.0), [], [m01_b])
    ph.op("pool", lambda e: e.affine_select(out=m01[:], in_=m01[:], pattern=[[1, 64]], compare_op=ALU.is_ge, fill=0.0,
                                            base=0, channel_multiplier=-1), [m01_b], [m01_b])
    NC = T // 64
    fT, fT_b = ph.sb("fT", [128, T], F32)
    Bc, Bc_b = ph.sb("Bc", [128, T], F32)
    E, E_b = ph.sb("E", [128, T], F32)
    qT, qT_b = ph.sb("qT", [128, T], BF16)
    qd, qd_b = ph.sb("qd", [128, T], BF16)
    kd, kd_b = ph.sb("kd", [128, T], BF16)
    kdtm, kdtm_b = ph.sb("kdtm", [64, NC, 128], BF16)
    iv, iv_b = ph.sb("iv", [64, NC, 128], BF16)
    oall, oall_b = ph.sb("oall", [64, NC, 128], F32)
    osb, osb_b = ph.sb("osb", [64, NC, 128], BF16)
    sc, sc_b = ph.sb("sc", [128, 4, NC], F32)
    S, S_b = ph.sb("S", [128, 128], F32)
    Sps = [ph.sb("Sp%d" % i, [128, 128], BF16) for i in range(2)]
    tmpu, tmpu_b = ph.sb("tmpu", [128, 128], F32)
    atms = [ph.sb("atm%d" % i, [64, 64], BF16) for i in range(2)]
    rs, rs_b = ph.sb("rs", [64, 2, NC], F32)
    psA = [ph.ps("pA%d" % i) for i in range(3)]
    psO = [ph.ps("pO%d" % i) for i in range(2)]
    psU = [ph.ps("pU%d" % i) for i in range(2)]
    psT = [ph.ps("pT%d" % i, [128, 4, 128], BF16) for i in range(1)]
    ph.dma("sp", fT[:], s_bfT[0:128, :], fT_b, writes=[fT_b])
    ph.dma("sp", qT[:], s_bqT[0:128, :], qT_b, writes=[qT_b])
    for h in range(8):
        r0 = h * 128
        ph.dma("sp", iv[:], s_bi[:, r0:r0 + 128].rearrange("(c p) v -> p c v", p=64), iv_b, writes=[iv_b])
        ph.dma("sp", osb[:], s_bg[:, r0:r0 + 128].rearrange("(c p) v -> p c v", p=64), osb_b, writes=[osb_b])
        ph.op("act", lambda e: e.activation(out=fT[:], in_=fT[:], func=AF.Sigmoid), [fT_b], [fT_b])
        ph.op("dve", lambda e, h=h: e.tensor_scalar(out=fT[:], in0=fT[:], scalar1=oml[:, h:h + 1], scalar2=lb[:, h:h + 1],
                                                    op0=ALU.mult, op1=ALU.add), [fT_b, oml_b, lb_b], [fT_b])
        ph.op("act", lambda e: e.activation(out=Bc[:], in_=fT[:], func=AF.Ln), [fT_b], [Bc_b])
        ph.op("pool", lambda e: e.tensor_scalar(out=fT[:], in0=fT[:], scalar1=-1.0, scalar2=1.0, op0=ALU.mult,
                                                op1=ALU.add), [fT_b, Bc_b], [fT_b])
        ph.op("dve", lambda e: e.tensor_tensor_scan(out=Bc[:], data0=ones[:], data1=Bc[:], initial=0.0, op0=ALU.mult,
                                                    op1=ALU.add), [Bc_b, ones_b], [Bc_b])
        Bc3 = Bc[:].rearrange("p (c s) -> p c s", s=64)
        ph.op("dve", lambda e: e.memset(sc[:, 0, 0:1], 0.0), [], [sc_b])
        ph.op("dve", lambda e, Bc3=Bc3: e.tensor_copy(out=sc[:, 0, 1:NC], in_=Bc3[:, 0:NC - 1, 63]), [Bc_b], [sc_b])
        ph.op("dve", lambda e, Bc3=Bc3: e.tensor_tensor(out=sc[:, 1, :], in0=Bc3[:, :, 63], in1=sc[:, 0, :],
                                                        op=ALU.subtract), [Bc_b, sc_b], [sc_b])
        ph.op("dve", lambda e, Bc3=Bc3: e.tensor_tensor(out=sc[:, 2, :], in0=Bc3[:, :, 31], in1=sc[:, 0, :],
                                                        op=ALU.subtract), [Bc_b, sc_b], [sc_b])
        ph.op("dve", lambda e, Bc3=Bc3: e.tensor_tensor(out=sc[:, 3, :], in0=Bc3[:, :, 63], in1=Bc3[:, :, 31],
                                                        op=ALU.subtract), [Bc_b, sc_b], [sc_b])
        ph.op("act", lambda e: e.activation(out=sc[:, 1:4, :], in_=sc[:, 1:4, :], func=AF.Exp), [sc_b], [sc_b])
        ph.op("dve", lambda e, Bc3=Bc3: e.tensor_copy(out=E[:, 0:NC], in_=Bc3[:, :, 31]), [Bc_b], [E_b])
        ph.op("dve", lambda e, Bc3=Bc3: e.tensor_tensor(out=Bc3, in0=Bc3, in1=E[:, 0:NC].unsqueeze(2).broadcast_to(
            [128, NC, 64]), op=ALU.subtract), [Bc_b, E_b], [Bc_b])
        ph.op("act", lambda e: e.activation(out=E[:], in_=Bc[:], func=AF.Exp), [Bc_b], [E_b])
        ph.op("dve", lambda e: e.tensor_tensor(out=qd[:], in0=qT[:], in1=E[:], op=ALU.mult), [qT_b, E_b], [qd_b])
        ph.op("act", lambda e: e.activation(out=E[:], in_=Bc[:], func=AF.Exp, scale=-1.0), [Bc_b, qd_b], [E_b])
        ph.op("dve", lambda e: e.tensor_tensor(out=kd[:], in0=fT[:], in1=E[:], op=ALU.mult), [fT_b, E_b], [kd_b])
        for c4 in range(NC // 4):
            pT, pT_b = psT[0]
            for u in range(4):
                c = c4 * 4 + u
                ph.op("pe", lambda e, pT=pT, u=u, c=c: e.transpose(pT[0:64, u, :], kd[:, c * 64:(c + 1) * 64], ident[:]),
                      [kd_b, ident_b], [pT_b])
            ph.op("act", lambda e, pT=pT, c4=c4: e.copy(out=kdtm[:, c4 * 4:(c4 + 1) * 4, :], in_=pT[0:64, :, :]),
                  [pT_b], [kdtm_b])
        def emit_A(c):
            pA, pA_b = psA[c % 3]
            c0 = c * 64
            ph.op("pe", lambda e: e.matmul(pA[0:32, 0:64], kd[:, c0:c0 + 32], qd[:, c0:c0 + 64], start=True, stop=True),
                  [kd_b, qd_b], [pA_b])
            ph.op("pe", lambda e: e.matmul(pA[32:64, 32:64], kd[:, c0 + 32:c0 + 64], qd[:, c0 + 32:c0 + 64], start=True,
                                           stop=True), [kd_b, qd_b], [pA_b])
            ph.op("pe", lambda e: e.matmul(pA[32:64, 0:32], zer[:], qd[:, c0:c0 + 32], start=True, stop=True),
                  [zer_b, qd_b], [pA_b])

        def emit_atm(c):
            pA, pA_b = psA[c % 3]
            atm, atm_b = atms[c % 2]
            ph.op("dve", lambda e: e.tensor_tensor(out=atm[:], in0=pA[0:64, 0:64], in1=m01[:], op=ALU.mult),
                  [pA_b, m01_b], [atm_b])

        def emit_U(c):
            pU, pU_b = psU[c % 2]
            ph.op("pe", lambda e: e.matmul(pU[:, 0:128], kdtm[:, c, :], iv[:, c, :], start=True, stop=True),
                  [kdtm_b, iv_b], [pU_b])

        if h + 1 < 8:
            ph.dma("sp", fT[:], s_bfT[r0 + 128:r0 + 256, :], fT_b, writes=[fT_b])
            ph.dma("sp", qT[:], s_bqT[r0 + 128:r0 + 256, :], qT_b, writes=[qT_b])
        emit_A(0)
        emit_A(1)
        emit_U(0)
        emit_atm(0)
        for c in range(NC):
            pO, pO_b = psO[c % 2]
            pU, pU_b = psU[c % 2]
            atm, atm_b = atms[c % 2]
            Sp, Sp_b = Sps[c % 2]
            Spn, Spn_b = Sps[(c + 1) % 2]
            cs = slice(c * 64, (c + 1) * 64)
            ph.op("pe", lambda e, pO=pO, atm=atm, c=c: e.matmul(pO[0:64, 0:128], atm[:], iv[:, c, :], start=True,
                                                               stop=(c == 0)), [atm_b, iv_b], [pO_b])
            if c > 0:
                ph.op("pe", lambda e, pO=pO, cs=cs, Sp=Sp: e.matmul(pO[0:64, 0:128], qd[:, cs], Sp[:], start=False,
                                                                   stop=True), [qd_b, Sp_b], [pO_b])
            ph.op("act", lambda e, pO=pO, c=c: e.copy(out=oall[:, c, :], in_=pO[0:64, 0:128]), [pO_b], [oall_b])
            if c + 2 < NC:
                emit_A(c + 2)
            if c + 1 < NC - 1:
                emit_U(c + 1)
            if c < NC - 1:
                if c == 0:
                    ph.op("dve", lambda e, pU=pU, c=c: e.tensor_scalar(out=S[:], in0=pU[:, 0:128], scalar1=sc[:, 3, c:c + 1],
                                                                       scalar2=None, op0=ALU.mult), [pU_b, sc_b], [S_b])
                else:
                    ph.op("dve", lambda e, pU=pU, c=c: e.tensor_scalar(out=tmpu[:], in0=pU[:, 0:128],
                                                                       scalar1=sc[:, 3, c:c + 1], scalar2=None,
                                                                       op0=ALU.mult), [pU_b, sc_b], [tmpu_b])
                    ph.op("dve", lambda e, c=c: e.scalar_tensor_tensor(out=S[:], in0=S[:], scalar=sc[:, 1, c:c + 1],
                                                                       in1=tmpu[:], op0=ALU.mult, op1=ALU.add),
                          [S_b, tmpu_b, sc_b], [S_b])
                ph.op("dve", lambda e, Spn=Spn, c=c: e.tensor_scalar(out=Spn[:], in0=S[:], scalar1=sc[:, 2, c + 1:c + 2],
                                                                     scalar2=None, op0=ALU.mult), [S_b, sc_b], [Spn_b])
                emit_atm(c + 1)
        oflat = oall[:].rearrange("p c v -> p (c v)")
        ph.op("act", lambda e: e.activation(out=kdtm[:], in_=oall[:], func=AF.Square), [oall_b], [kdtm_b])
        ph.op("dve", lambda e: e.tensor_reduce(out=rs[:, 0, :], in_=kdtm[:], axis=AX.X, op=ALU.add), [kdtm_b], [rs_b])
        ph.op("dve", lambda e: e.tensor_scalar(out=rs[:, 0, :], in0=rs[:, 0, :], scalar1=1.0 / 128, scalar2=EPS,
                                               op0=ALU.mult, op1=ALU.add), [rs_b], [rs_b])
        ph.op("act", lambda e: e.activation(out=rs[:, 0, :], in_=rs[:, 0, :], func=AF.Sqrt), [rs_b], [rs_b])
        ph.op("dve", lambda e: e.reciprocal(out=rs[:, 1, :], in_=rs[:, 0, :]), [rs_b], [rs_b])
        ph.op("dve", lambda e: e.tensor_tensor(out=oall[:], in0=oall[:], in1=rs[:, 1, :].unsqueeze(2).broadcast_to(
            [64, NC, 128]), op=ALU.mult), [oall_b, rs_b], [oall_b])
        ph.op("pool", lambda e: e.tensor_tensor(out=oall[:], in0=oall[:], in1=ng[:].unsqueeze(1).broadcast_to(
            [64, NC, 128]), op=ALU.mult), [oall_b, ng_b], [oall_b])
        ph.op("dve", lambda e: e.tensor_tensor(out=osb[:], in0=oall[:], in1=osb[:], op=ALU.mult), [oall_b, osb_b], [osb_b])
        ph.dma("sp", s_ob[:, r0:r0 + 128].rearrange("(c p) v -> p c v", p=64), osb[:], osb_b, reads=[osb_b])
    ph.run()


def phase_c(ctx, s_cq, s_ck, s_cv, s_cg, cosT, sinS, cdec, s_oc):
    ph = Phase(ctx, "ret")
    ident, ident_b = make_ident(ph)
    dec, dec_b = ph.sb("dec", [128, 8], F32)
    ph.dma("sp", dec[:], cdec[:, :], dec_b, writes=[dec_b])
    m01, m01_b = ph.sb("m01", [128, 128], F32)
    ph.op("pool", lambda e: e.memset(m01[:], 1.0), [], [m01_b])
    ph.op("pool", lambda e: e.affine_select(out=m01[:], in_=m01[:], pattern=[[1, 128]], compare_op=ALU.is_ge, fill=0.0,
                                            base=0, channel_multiplier=-1), [m01_b], [m01_b])
    qrdT, qrdT_b = ph.sb("qrdT", [128, 2, T], BF16)
    krnT, krnT_b = ph.sb("krnT", [128, 2, T], BF16)
    krn, krn_b = ph.sb("krn", [128, NT, 256], BF16)
    v, v_b = ph.sb("v", [128, NT, 512], BF16)
    ins = [[ph.sb("in%d_%d" % (a, i), [128, 256], F32) for i in range(2)] for a in range(2)]
    cs_ = [ph.sb("cos%d" % i, [128, 256], F32) for i in range(2)]
    sn_ = [ph.sb("sin%d" % i, [128, 256], F32) for i in range(2)]
    t1s = [ph.sb("t1_%d" % i, [128, 256], F32) for i in range(2)]
    t2s = [ph.sb("t2_%d" % i, [128, 256], F32) for i in range(2)]
    qtm = [ph.sb("qtm%d" % i, [128, 256], BF16) for i in range(2)]
    W = [ph.sb("W%d" % i, [128, 512], F32) for i in range(2)]
    Wb = [ph.sb("Wb%d" % i, [128, 512], BF16) for i in range(2)]
    atms = [ph.sb("atm%d" % i, [128, 128], BF16) for i in range(2)]
    gts = [ph.sb("g%d" % i, [128, 512], BF16) for i in range(2)]
    outs = [ph.sb("o%d" % i, [128, 512], BF16) for i in range(2)]
    junk, junk_b = ph.sb("junk", [128, 512], BF16)
    sts = [ph.sb("st%d" % i, [128, 4], F32) for i in range(2)]
    psT = [ph.ps("pT%d" % i, [128, 2, 128], BF16) for i in range(2)]
    psA = [ph.ps("pA%d" % i) for i in range(2)]
    psO = [ph.ps("pO%d" % i) for i in range(2)]
    psU = [ph.ps("pU%d" % i) for i in range(2)]
    for h in range(4):
        gam = 1.0 - 2.0 ** (-5.0 - h)
        g128 = gam ** 128
        ph.dma("sp", v[:], s_cv[:, h * 512:(h + 1) * 512].rearrange("(c p) d -> p c d", p=128), v_b, writes=[v_b])
        for i in range(NT):
            rows = slice(i * 128, (i + 1) * 128)
            co, co_b = cs_[i % 2]
            sn, sn_b = sn_[i % 2]
            ph.dma("sp", co[:], cosT[rows, :], co_b, writes=[co_b])
            ph.dma("sp", sn[:], sinS[rows, :], sn_b, writes=[sn_b])
            for a, src in enumerate((s_cq, s_ck)):
                x, x_b = ins[a][i % 2]
                t1, t1_b = t1s[a]
                t2, t2_b = t2s[a]
                ph.dma("sp", x[:], src[rows, h * 256:(h + 1) * 256], x_b, writes=[x_b])
                x3 = x[:].rearrange("p (i two) -> p i two", two=2)
                s3 = sn[:].rearrange("p (i two) -> p i two", two=2)
                t23 = t2[:].rearrange("p (i two) -> p i two", two=2)
                ph.op("dve", lambda e, t1=t1, x=x, co=co: e.tensor_tensor(out=t1[:], in0=x[:], in1=co[:], op=ALU.mult),
                      [x_b, co_b], [t1_b])
                ph.op("pool", lambda e, t23=t23, x3=x3, s3=s3: e.tensor_tensor(out=t23[:, :, 0], in0=x3[:, :, 1],
                                                                               in1=s3[:, :, 0], op=ALU.mult),
                      [x_b, sn_b], [t2_b])
                ph.op("pool", lambda e, t23=t23, x3=x3, s3=s3: e.tensor_tensor(out=t23[:, :, 1], in0=x3[:, :, 0],
                                                                               in1=s3[:, :, 1], op=ALU.mult),
                      [x_b, sn_b], [t2_b])
                ph.op("dve", lambda e, t1=t1, t2=t2: e.tensor_tensor(out=t1[:], in0=t1[:], in1=t2[:], op=ALU.add),
                      [t1_b, t2_b], [t1_b])
                pT, pT_b = psT[a]
                if a == 0:
                    dst, dst_b = qtm[i % 2]
                    dstap = dst[:]
                else:
                    dst_b = krn_b
                    dstap = krn[:, i, :]
                ph.op("act", lambda e, dstap=dstap, t1=t1, a=a, h=h: e.activation(out=dstap, in_=t1[:], func=AF.Copy,
                                                                                  scale=dec[:, a * 4 + h:a * 4 + h + 1]),
                      [t1_b, dec_b], [dst_b])
                for kc in range(2):
                    ph.op("pe", lambda e, pT=pT, kc=kc, dstap=dstap: e.transpose(pT[:, kc, :],
                                                                                 dstap[:, kc * 128:(kc + 1) * 128], ident[:]),
                          [dst_b, ident_b], [pT_b])
                tgt, tgt_b = (qrdT, qrdT_b) if a == 0 else (krnT, krnT_b)
                ph.op("act", lambda e, tgt=tgt, pT=pT, i=i: e.copy(out=tgt[:, :, i * 128:(i + 1) * 128], in_=pT[:]),
                      [pT_b], [tgt_b])
        def emit_A(c):
            cs = slice(c * 128, (c + 1) * 128)
            pA, pA_b = psA[c % 2]
            for kc in range(2):
                ph.op("pe", lambda e, kc=kc: e.matmul(pA[:, 0:128], krnT[:, kc, cs], qrdT[:, kc, cs], start=(kc == 0),
                                                      stop=(kc == 1)), [krnT_b, qrdT_b], [pA_b])

        def emit_atm(c):
            pA, pA_b = psA[c % 2]
            atm, atm_b = atms[c % 2]
            ph.op("dve", lambda e: e.tensor_tensor(out=atm[:], in0=pA[:, 0:128], in1=m01[:], op=ALU.mult),
                  [pA_b, m01_b], [atm_b])

        emit_A(0)
        emit_atm(0)
        for c in range(NT):
            cs = slice(c * 128, (c + 1) * 128)
            pO, pO_b = psO[c % 2]
            atm, atm_b = atms[c % 2]
            g, g_b = gts[c % 2]
            out, out_b = outs[c % 2]
            st, st_b = sts[c % 2]
            ph.dma("sp", g[:], s_cg[cs, h * 512:(h + 1) * 512], g_b, writes=[g_b])
            if c + 1 < NT:
                emit_A(c + 1)
            ph.op("pe", lambda e, pO=pO, atm=atm, c=c: e.matmul(pO[:], atm[:], v[:, c, :], start=True, stop=(c == 0)),
                  [atm_b, v_b], [pO_b])
            if c > 0:
                for kc in range(2):
                    ph.op("pe", lambda e, pO=pO, kc=kc, cs=cs: e.matmul(pO[:], qrdT[:, kc, cs], Wb[kc][0][:], start=False,
                                                                       stop=(kc == 1)), [qrdT_b, Wb[kc][1]], [pO_b])
            if c < NT - 1:
                for kc in range(2):
                    pU, pU_b = psU[kc]
                    ph.op("pe", lambda e, pU=pU, kc=kc, c=c: e.matmul(pU[:], krn[:, c, kc * 128:(kc + 1) * 128], v[:, c, :],
                                                                     start=True, stop=True), [krn_b, v_b], [pU_b])
                    Wk, Wk_b = W[kc]
                    if c == 0:
                        ph.op("dve", lambda e, Wk=Wk, pU=pU: e.tensor_copy(out=Wk[:], in_=pU[:]), [pU_b], [Wk_b])
                    else:
                        ph.op("dve", lambda e, Wk=Wk, pU=pU, g128=g128: e.scalar_tensor_tensor(
                            out=Wk[:], in0=Wk[:], scalar=g128, in1=pU[:], op0=ALU.mult, op1=ALU.add), [Wk_b, pU_b], [Wk_b])
                    ph.op("act", lambda e, Wk=Wk, kc=kc, g128=g128: e.activation(out=Wb[kc][0][:], in_=Wk[:], func=AF.Copy,
                                                                                 scale=g128), [Wk_b], [Wb[kc][1]])
            if c + 1 < NT:
                emit_atm(c + 1)
            ph.op("act", lambda e, pO=pO, st=st: e.activation(out=junk[:], in_=pO[:], func=AF.Square,
                                                             accum_out=st[:, 0:1]), [pO_b], [junk_b, st_b])
            ph.op("dve", lambda e, st=st: e.tensor_scalar(out=st[:, 1:2], in0=st[:, 0:1], scalar1=1.0 / 512, scalar2=EPS,
                                                          op0=ALU.mult, op1=ALU.add), [st_b], [st_b])
            ph.op("act", lambda e, st=st: e.activation(out=st[:, 2:3], in_=st[:, 1:2], func=AF.Sqrt), [st_b], [st_b])
            ph.op("dve", lambda e, st=st: e.reciprocal(out=st[:, 3:4], in_=st[:, 2:3]), [st_b], [st_b])
            ph.op("dve", lambda e, pO=pO, st=st, g=g, out=out: e.scalar_tensor_tensor(
                out=out[:], in0=pO[:], scalar=st[:, 3:4], in1=g[:], op0=ALU.mult, op1=ALU.mult), [pO_b, st_b, g_b], [out_b])
            ph.dma("sp", s_oc[cs, h * 512:(h + 1) * 512], out[:], out_b, reads=[out_b])
    ph.run()


def phase_merge(ctx, l, modbc, s_oa, s_ob, s_oc, s_gT, w_a, w_b, w_c, w_out, xres):
    ph = Phase(ctx, "mrg")
    ident, ident_b = make_ident(ph)
    Wbr, Wbr_b = ph.sb("Wbr", [128, 32, D], BF16)
    wo, wo_b = ph.sb("wo", [128, 8, D], BF16)
    gt, gt_b = ph.sb("gt", [128, D], F32)
    for kc in range(8):
        ph.dma("pool", Wbr[:, kc, :], w_a[l, kc * 128:(kc + 1) * 128, :], Wbr_b, writes=[Wbr_b])
        ph.dma("pool", Wbr[:, 8 + kc, :], w_b[l, kc * 128:(kc + 1) * 128, :], Wbr_b, writes=[Wbr_b])
        ph.dma("pool", wo[:, kc, :], w_out[l, kc * 128:(kc + 1) * 128, :], wo_b, writes=[wo_b])
    for kc in range(16):
        ph.dma("pool", Wbr[:, 16 + kc, :], w_c[l, kc * 128:(kc + 1) * 128, :], Wbr_b, writes=[Wbr_b])
    ph.dma("sp", gt[:], modbc[l, :, 2 * D:3 * D], gt_b, writes=[gt_b])
    oin = [ph.sb("oin%d" % i, [128, 4096], BF16) for i in range(2)]
    oT, oT_b = ph.sb("oT", [128, 32, 512], BF16)
    gs = [ph.sb("gs%d" % i, [128, 3, 512], BF16) for i in range(2)]
    tt1 = [ph.sb("tt%d" % i, [128, 512], F32) for i in range(3)]
    mT, mT_b = ph.sb("mT", [128, 8, 512], BF16)
    xs = [ph.sb("x%d" % i, [128, D], F32) for i in range(2)]
    ty = [ph.sb("ty%d" % i, [128, 512], F32) for i in range(2)]
    psT = [ph.ps("pT%d" % i, [128, 8, 128], BF16) for i in range(2)]
    psB = [ph.ps("pB%d" % i) for i in range(3)]
    psY = [ph.ps("pY%d" % i) for i in range(2)]
    nT = 0
    ny = 0
    ng = 0
    for g in range(T // 512):
        for tt in range(4):
            rows = slice(g * 512 + tt * 128, g * 512 + (tt + 1) * 128)
            o, o_b = oin[tt % 2]
            ph.dma("sp", o[:, 0:1024], s_oa[rows, :], o_b, writes=[o_b])
            ph.dma("sp", o[:, 1024:2048], s_ob[rows, :], o_b, writes=[o_b])
            ph.dma("sp", o[:, 2048:4096], s_oc[rows, :], o_b, writes=[o_b])
            for k8 in range(4):
                pT, pT_b = psT[nT % 2]
                nT += 1
                for u in range(8):
                    kc = k8 * 8 + u
                    ph.op("pe", lambda e, pT=pT, u=u, o=o, kc=kc: e.transpose(pT[:, u, :], o[:, kc * 128:(kc + 1) * 128],
                                                                           ident[:]), [o_b, ident_b], [pT_b])
                ph.op("act", lambda e, pT=pT, k8=k8, tt=tt: e.copy(out=oT[:, k8 * 8:(k8 + 1) * 8, tt * 128:(tt + 1) * 128],
                                                                 in_=pT[:]), [pT_b], [oT_b])
        for dc in range(8):
            gsb, gsb_b = gs[ng % 2]
            ng += 1
            ph.dma("sp", gsb[:], s_gT.rearrange("(b r) t -> r b t", b=3)[dc * 128:(dc + 1) * 128, :, g * 512:(g + 1) * 512],
                   gsb_b, writes=[gsb_b])
            for br, (k0, k1) in enumerate(((0, 8), (8, 16), (16, 32))):
                pB, pB_b = psB[br]
                for kc in range(k0, k1):
                    ph.op("pe", lambda e, pB=pB, kc=kc, dc=dc, k0=k0, k1=k1: e.matmul(
                        pB[:], Wbr[:, kc, dc * 128:(dc + 1) * 128], oT[:, kc, :], start=(kc == k0), stop=(kc == k1 - 1)),
                          [Wbr_b, oT_b], [pB_b])
                t, t_b = tt1[br]
                ph.op("dve", lambda e, t=t, pB=pB, gsb=gsb, br=br: e.tensor_tensor(out=t[:], in0=pB[:], in1=gsb[:, br, :],
                                                                                  op=ALU.mult), [pB_b, gsb_b], [t_b])
            ph.op("pool", lambda e: e.tensor_tensor(out=tt1[0][0][:], in0=tt1[0][0][:], in1=tt1[1][0][:], op=ALU.add),
                  [tt1[0][1], tt1[1][1]], [tt1[0][1]])
            ph.op("pool", lambda e, dc=dc: e.tensor_tensor(out=mT[:, dc, :], in0=tt1[0][0][:], in1=tt1[2][0][:], op=ALU.add),
                  [tt1[0][1], tt1[2][1]], [mT_b])
        for tt in range(4):
            rows = slice(g * 512 + tt * 128, g * 512 + (tt + 1) * 128)
            x, x_b = xs[tt % 2]
            ph.dma("sp", x[:], xres[rows, :], x_b, writes=[x_b])
            for dh in range(2):
                pY, pY_b = psY[ny % 2]
                t, t_b = ty[ny % 2]
                ny += 1
                for dc in range(8):
                    ph.op("pe", lambda e, pY=pY, dc=dc, tt=tt, dh=dh: e.matmul(
                        pY[:], mT[:, dc, tt * 128:(tt + 1) * 128], wo[:, dc, dh * 512:(dh + 1) * 512], start=(dc == 0),
                        stop=(dc == 7)), [mT_b, wo_b], [pY_b])
                ph.op("dve", lambda e, t=t, pY=pY, dh=dh: e.tensor_tensor(out=t[:], in0=pY[:],
                                                                          in1=gt[:, dh * 512:(dh + 1) * 512], op=ALU.mult),
                      [pY_b, gt_b], [t_b])
                ph.op("pool", lambda e, t=t, x=x, dh=dh: e.tensor_tensor(out=x[:, dh * 512:(dh + 1) * 512],
                                                                         in0=x[:, dh * 512:(dh + 1) * 512], in1=t[:],
                                                                         op=ALU.add), [t_b, x_b], [x_b])
            ph.dma("sp", xres[rows, :], x[:], x_b, reads=[x_b])
    ph.run()


TH = 2048
NTH = TH // 128


def phase_route(ctx, l, hh, xres, modbc, norm_g, rg_w, rg_b, re_w, re_b, s_hT, s_gates):
    ph = Phase(ctx, "rt")
    identb, identb_b = make_ident(ph)
    idf, idf_b = ph.sb("identf32", [128, 128], F32)
    ph.op("pool", lambda e: e.memset(idf[:], 1.0), [], [idf_b])
    ph.op("pool", lambda e: e.affine_select(out=idf[:], in_=idf[:], pattern=[[-1, 128]], compare_op=ALU.is_equal,
                                            fill=0.0, base=0, channel_multiplier=1), [idf_b], [idf_b])
    G, GS_b = ph.sb("G", [128, D], F32)
    S, _ = ph.sb("S", [128, D], F32)
    ng, _ = ph.sb("ng", [128, D], F32)
    ph.dma("sp", G[:], modbc[l, :, 4 * D:5 * D], GS_b, writes=[GS_b])
    ph.dma("sp", S[:], modbc[l, :, 3 * D:4 * D], GS_b, writes=[GS_b])
    ph.dma("sp", ng[:], bcast_rows(norm_g[l:l + 1, :], 128), GS_b, writes=[GS_b])
    ph.op("dve", lambda e: e.tensor_tensor(out=G[:], in0=G[:], in1=ng[:], op=ALU.mult), [GS_b], [GS_b])
    rw, rw_b = ph.sb("rw", [128, 8, 36], F32)
    rb, rb_b = ph.sb("rb", [128, 36], F32)
    ph.dma("sp", rw[:, :, 0:4], rg_w[l].rearrange("(k p) n -> p k n", p=128), rw_b, writes=[rw_b])
    ph.dma("sp", rw[:, :, 4:36], re_w[l].rearrange("(k p) n -> p k n", p=128), rw_b, writes=[rw_b])
    ph.dma("sp", rb[:, 0:4], bcast_rows(rg_b[l:l + 1, :], 128), rb_b, writes=[rb_b])
    ph.dma("sp", rb[:, 4:36], bcast_rows(re_b[l:l + 1, :], 128), rb_b, writes=[rb_b])
    hT, hT_b = ph.sb("hT", [128, 8, TH], BF16)
    gates, gates_b = ph.sb("gates", [128, NTH, 32], F32)
    xs = [ph.sb("x%d" % i, [128, D], F32) for i in range(2)]
    sq, sq_b = ph.sb("sq", [128, D], BF16)
    hs = [ph.sb("h%d" % i, [128, D], BF16) for i in range(2)]
    st = [ph.sb("st%d" % i, [128, 4], F32) for i in range(2)]
    h32T = [ph.sb("h32T%d" % i, [128, 8, 128], F32) for i in range(2)]
    lgs = [ph.sb("lg%d" % i, [128, 36], F32) for i in range(2)]
    rts = [ph.sb("r%d" % i, [128, 64], F32) for i in range(2)]
    pts = [ph.ps("pt%d" % i, [128, 8, 128], BF16) for i in range(2)]
    pfs = [ph.ps("pf%d" % i, [128, 4, 128], F32) for i in range(2)]
    pls = [ph.ps("pl%d" % i) for i in range(2)]
    for t in range(NTH):
        x, x_b = xs[t % 2]
        h, h_b = hs[t % 2]
        s, s_b = st[t % 2]
        pt, pt_b = pts[t % 2]
        hf, hf_b = h32T[t % 2]
        pl, pl_b = pls[t % 2]
        lg, lg_b = lgs[t % 2]
        r, r_b = rts[t % 2]
        row0 = hh * TH + t * 128
        ph.dma("sp", x[:], xres[row0:row0 + 128, :], x_b, writes=[x_b])
        ph.op("act", lambda e, x=x, s=s: e.activation(out=sq[:], in_=x[:], func=AF.Square, accum_out=s[:, 0:1]),
              [x_b], [sq_b, s_b])
        ph.op("dve", lambda e, s=s: e.tensor_scalar(out=s[:, 1:2], in0=s[:, 0:1], scalar1=1.0 / D, scalar2=EPS,
                                                    op0=ALU.mult, op1=ALU.add), [s_b], [s_b])
        ph.op("act", lambda e, s=s: e.activation(out=s[:, 2:3], in_=s[:, 1:2], func=AF.Sqrt), [s_b], [s_b])
        ph.op("dve", lambda e, s=s: e.reciprocal(out=s[:, 3:4], in_=s[:, 2:3]), [s_b], [s_b])
        ph.op("dve", lambda e, x=x, s=s: e.scalar_tensor_tensor(out=x[:], in0=x[:], scalar=s[:, 3:4], in1=G[:],
                                                                op0=ALU.mult, op1=ALU.mult), [x_b, s_b, GS_b], [x_b])
        ph.op("pool", lambda e, x=x: e.tensor_tensor(out=x[:], in0=x[:], in1=S[:], op=ALU.add), [x_b, GS_b], [x_b])
        ph.op("pool", lambda e, x=x, h=h: e.tensor_copy(out=h[:], in_=x[:]), [x_b], [h_b])
        for k in range(8):
            ph.op("pe", lambda e, k=k, h=h, pt=pt: e.transpose(pt[:, k, :], h[:, k * 128:(k + 1) * 128], identb[:]),
                  [h_b, identb_b], [pt_b])
        ph.op("act", lambda e, pt=pt, t=t: e.copy(out=hT[:, :, t * 128:(t + 1) * 128], in_=pt[:]), [pt_b], [hT_b])
        for half in range(2):
            pf, pf_b = pfs[half]
            for u in range(4):
                k = half * 4 + u
                ph.op("pe", lambda e, pf=pf, u=u, k=k, x=x: e.transpose(pf[:, u, :], x[:, k * 128:(k + 1) * 128], idf[:]),
                      [x_b, idf_b], [pf_b])
            ph.op("dve", lambda e, pf=pf, hf=hf, half=half: e.tensor_copy(out=hf[:, half * 4:(half + 1) * 4, :], in_=pf[:]),
                  [pf_b], [hf_b])
        for k in range(8):
            ph.op("pe", lambda e, pl=pl, hf=hf, k=k: e.matmul(pl[:, 0:36], hf[:, k, :], rw[:, k, :], start=(k == 0),
                                                             stop=(k == 7)), [hf_b, rw_b], [pl_b])
        ph.op("dve", lambda e, lg=lg, pl=pl: e.tensor_tensor(out=lg[:], in0=pl[:, 0:36], in1=rb[:], op=ALU.add),
              [pl_b, rb_b], [lg_b])
        def dv(fn, r=r, lg=lg):
            ph.op("dve", fn, [r_b, lg_b], [r_b])
        dv(lambda e, r=r, lg=lg: e.tensor_reduce(out=r[:, 0:1], in_=lg[:, 0:4], axis=AX.X, op=ALU.max))
        dv(lambda e, r=r, lg=lg: e.tensor_scalar(out=r[:, 16:20], in0=lg[:, 0:4], scalar1=r[:, 0:1], scalar2=None,
                                                 op0=ALU.is_equal))
        dv(lambda e, r=r: e.tensor_scalar(out=r[:, 1:2], in0=r[:, 0:1], scalar1=-1.0, scalar2=None, op0=ALU.mult))
        ph.op("act", lambda e, r=r, lg=lg: e.activation(out=r[:, 20:24], in_=lg[:, 0:4], func=AF.Exp, bias=r[:, 1:2]),
              [r_b, lg_b], [r_b])
        dv(lambda e, r=r: e.tensor_reduce(out=r[:, 2:3], in_=r[:, 20:24], axis=AX.X, op=ALU.add))
        dv(lambda e, r=r: e.reciprocal(out=r[:, 3:4], in_=r[:, 2:3]))
        dv(lambda e, r=r, lg=lg: e.tensor_scalar(out=r[:, 24:32], in0=lg[:, 4:12], scalar1=r[:, 16:17], scalar2=None,
                                                 op0=ALU.mult))
        for g in range(1, 4):
            dv(lambda e, r=r, lg=lg, g=g: e.scalar_tensor_tensor(out=r[:, 24:32], in0=lg[:, 4 + 8 * g:12 + 8 * g],
                                                                 scalar=r[:, 16 + g:17 + g], in1=r[:, 24:32],
                                                                 op0=ALU.mult, op1=ALU.add))
        dv(lambda e, r=r: e.tensor_reduce(out=r[:, 4:5], in_=r[:, 24:32], axis=AX.X, op=ALU.max))
        dv(lambda e, r=r: e.tensor_scalar(out=r[:, 32:40], in0=r[:, 24:32], scalar1=r[:, 4:5], scalar2=None,
                                          op0=ALU.is_equal))
        dv(lambda e, r=r: e.scalar_tensor_tensor(out=r[:, 40:48], in0=r[:, 32:40], scalar=-1e30, in1=r[:, 24:32],
                                                 op0=ALU.mult, op1=ALU.add))
        dv(lambda e, r=r: e.tensor_reduce(out=r[:, 5:6], in_=r[:, 40:48], axis=AX.X, op=ALU.max))
        dv(lambda e, r=r: e.tensor_scalar(out=r[:, 48:56], in0=r[:, 40:48], scalar1=r[:, 5:6], scalar2=None,
                                          op0=ALU.is_equal))
        dv(lambda e, r=r: e.tensor_tensor(out=r[:, 6:7], in0=r[:, 5:6], in1=r[:, 4:5], op=ALU.subtract))
        ph.op("act", lambda e, r=r: e.activation(out=r[:, 7:8], in_=r[:, 6:7], func=AF.Exp), [r_b], [r_b])
        dv(lambda e, r=r: e.tensor_scalar(out=r[:, 8:9], in0=r[:, 7:8], scalar1=1.0, scalar2=None, op0=ALU.add))
        dv(lambda e, r=r: e.reciprocal(out=r[:, 9:10], in_=r[:, 8:9]))
        dv(lambda e, r=r: e.tensor_tensor(out=r[:, 10:11], in0=r[:, 9:10], in1=r[:, 3:4], op=ALU.mult))
        dv(lambda e, r=r: e.tensor_tensor(out=r[:, 11:12], in0=r[:, 10:11], in1=r[:, 7:8], op=ALU.mult))
        dv(lambda e, r=r: e.tensor_scalar(out=r[:, 56:64], in0=r[:, 32:40], scalar1=r[:, 10:11], scalar2=None,
                                          op0=ALU.mult))
        dv(lambda e, r=r: e.scalar_tensor_tensor(out=r[:, 56:64], in0=r[:, 48:56], scalar=r[:, 11:12], in1=r[:, 56:64],
                                                 op0=ALU.mult, op1=ALU.add))
        for g in range(4):
            ph.op("dve", lambda e, r=r, g=g, t=t: e.tensor_scalar(out=gates[:, t, g * 8:(g + 1) * 8], in0=r[:, 56:64],
                                                                  scalar1=r[:, 16 + g:17 + g], scalar2=None, op0=ALU.mult),
                  [r_b], [gates_b])
    for k in range(8):
        ph.dma("sp", s_hT[:, k, :], hT[:, k, :], hT_b, reads=[hT_b])
    ph.dma("sp", s_gates.rearrange("(t p) e -> p t e", p=128), gates[:], gates_b, reads=[gates_b])
    ph.run()


def phase_experts(ctx, l, hh, xres, modbc, s_hT, s_gates, e_gate, e_up, e_down, n_exp=32):
    ph = Phase(ctx, "ex")
    hT, hT_b = ph.sb("hT", [128, 8, TH], BF16)
    gates, gates_b = ph.sb("gates", [128, NTH, 32], F32)
    gt, gt_b = ph.sb("gt", [128, D], F32)
    yacc, yacc_b = ph.sb("yacc", [128, NTH, D], F32)
    for k in range(8):
        ph.dma("sp", hT[:, k, :], s_hT[:, k, :], hT_b, writes=[hT_b])
    ph.dma("sp", gates[:], s_gates.rearrange("(t p) e -> p t e", p=128), gates_b, writes=[gates_b])
    ph.dma("sp", gt[:], modbc[l, :, 5 * D:6 * D], gt_b, writes=[gt_b])
    wg = [ph.sb("wg%d" % i, [128, 8, 512], BF16) for i in range(2)]
    wu = [ph.sb("wu%d" % i, [128, 8, 512], BF16) for i in range(2)]
    wd = [ph.sb("wd%d" % i, [128, 4, D], BF16) for i in range(2)]
    sgs = [ph.sb("sg%d" % i, [128, 512], F32) for i in range(2)]
    hgs = [ph.sb("hg%d" % i, [128, 4, 512], BF16) for i in range(2)]
    psG = [ph.ps("pG%d" % i) for i in range(2)]
    psU = [ph.ps("pU%d" % i) for i in range(2)]
    psY = [ph.ps("pY%d" % i) for i in range(4)]
    nf = 0
    ny = 0
    nh = 0
    for ex in range(n_exp):
        g_, g_b = wg[ex % 2]
        u_, u_b = wu[ex % 2]
        d_, d_b = wd[ex % 2]
        ph.dma("pool", g_[:], e_gate[l, ex].rearrange("(k p) f -> p k f", p=128), g_b, writes=[g_b])
        ph.dma("pool", u_[:], e_up[l, ex].rearrange("(k p) f -> p k f", p=128), u_b, writes=[u_b])
        ph.dma("pool", d_[:], e_down[l, ex].rearrange("(k p) f -> p k f", p=128), d_b, writes=[d_b])
        for tg in range(TH // 512):
            ts = slice(tg * 512, (tg + 1) * 512)
            hg, hg_b = hgs[nh % 2]
            nh += 1
            for fc in range(4):
                pG, pG_b = psG[nf % 2]
                pU, pU_b = psU[nf % 2]
                sg, sg_b = sgs[nf % 2]
                nf += 1
                for k in range(8):
                    ph.op("pe", lambda e, pG=pG, g_=g_, k=k, fc=fc, ts=ts: e.matmul(
                        pG[:], g_[:, k, fc * 128:(fc + 1) * 128], hT[:, k, ts], start=(k == 0), stop=(k == 7)),
                          [g_b, hT_b], [pG_b])
                for k in range(8):
                    ph.op("pe", lambda e, pU=pU, u_=u_, k=k, fc=fc, ts=ts: e.matmul(
                        pU[:], u_[:, k, fc * 128:(fc + 1) * 128], hT[:, k, ts], start=(k == 0), stop=(k == 7)),
                          [u_b, hT_b], [pU_b])
                ph.op("act", lambda e, sg=sg, pG=pG: e.activation(out=sg[:], in_=pG[:], func=AF.Silu), [pG_b], [sg_b])
                ph.op("dve", lambda e, hg=hg, fc=fc, sg=sg, pU=pU: e.tensor_tensor(out=hg[:, fc, :], in0=pU[:], in1=sg[:],
                                                                                  op=ALU.mult), [pU_b, sg_b], [hg_b])
            for tt in range(4):
                tile = tg * 4 + tt
                for dh in range(2):
                    pY, pY_b = psY[ny % 4]
                    ny += 1
                    for fc in range(4):
                        ph.op("pe", lambda e, pY=pY, hg=hg, fc=fc, tt=tt, d_=d_, dh=dh: e.matmul(
                            pY[:], hg[:, fc, tt * 128:(tt + 1) * 128], d_[:, fc, dh * 512:(dh + 1) * 512], start=(fc == 0),
                            stop=(fc == 3)), [hg_b, d_b], [pY_b])
                    ya = yacc[:, tile, dh * 512:(dh + 1) * 512]
                    if ex == 0:
                        ph.op("dve", lambda e, ya=ya, pY=pY, tile=tile, ex=ex: e.tensor_scalar(
                            out=ya, in0=pY[:], scalar1=gates[:, tile, ex:ex + 1], scalar2=None, op0=ALU.mult),
                              [pY_b, gates_b], [yacc_b])
                    else:
                        ph.op("dve", lambda e, ya=ya, pY=pY, tile=tile, ex=ex: e.scalar_tensor_tensor(
                            out=ya, in0=pY[:], scalar=gates[:, tile, ex:ex + 1], in1=ya, op0=ALU.mult, op1=ALU.add),
                              [pY_b, gates_b, yacc_b], [yacc_b])
    xs = [ph.sb("x%d" % i, [128, D], F32) for i in range(2)]
    for t in range(NTH):
        x, x_b = xs[t % 2]
        row0 = hh * TH + t * 128
        ph.dma("sp", x[:], xres[row0:row0 + 128, :], x_b, writes=[x_b])
        ph.op("pool", lambda e, t=t: e.tensor_tensor(out=yacc[:, t, :], in0=yacc[:, t, :], in1=gt[:], op=ALU.mult),
              [yacc_b, gt_b], [yacc_b])
        ph.op("pool", lambda e, x=x, t=t: e.tensor_tensor(out=x[:], in0=x[:], in1=yacc[:, t, :], op=ALU.add),
              [x_b, yacc_b], [x_b])
        ph.dma("sp", xres[row0:row0 + 128, :], x[:], x_b, reads=[x_b])
    ph.run()


def phase_final(ctx, xres, final_g, y):
    ph = Phase(ctx, "fin")
    G, G_b = ph.sb("G", [128, D], F32)
    ph.dma("sp", G[:], bcast_rows(final_g, 128), G_b, writes=[G_b])
    xs = [ph.sb("x%d" % i, [128, D], F32) for i in range(3)]
    sq, sq_b = ph.sb("sq", [128, D], BF16)
    st = [ph.sb("st%d" % i, [128, 4], F32) for i in range(2)]
    for t in range(NT):
        x, x_b = xs[t % 3]
        s, s_b = st[t % 2]
        ph.dma("sp", x[:], xres[t * 128:(t + 1) * 128, :], x_b, writes=[x_b])
        ph.op("act", lambda e, x=x, s=s: e.activation(out=sq[:], in_=x[:], func=AF.Square, accum_out=s[:, 0:1]),
              [x_b], [sq_b, s_b])
        ph.op("dve", lambda e, s=s: e.tensor_scalar(out=s[:, 1:2], in0=s[:, 0:1], scalar1=1.0 / D, scalar2=EPS,
                                                    op0=ALU.mult, op1=ALU.add), [s_b], [s_b])
        ph.op("act", lambda e, s=s: e.activation(out=s[:, 2:3], in_=s[:, 1:2], func=AF.Sqrt), [s_b], [s_b])
        ph.op("dve", lambda e, s=s: e.reciprocal(out=s[:, 3:4], in_=s[:, 2:3]), [s_b], [s_b])
        ph.op("dve", lambda e, x=x, s=s: e.scalar_tensor_tensor(out=x[:], in0=x[:], scalar=s[:, 3:4], in1=G[:],
                                                                op0=ALU.mult, op1=ALU.mult), [x_b, s_b, G_b], [x_b])
        ph.dma("sp", y[t * 128:(t + 1) * 128, :], x[:], x_b, reads=[x_b])
    ph.run()


def make_oh_tab():
    u = np.arange(384)
    rel = np.maximum(255 - u, 0)
    relf = np.maximum(rel, 1).astype(np.float32)
    large = 16 + (np.log(relf / np.float32(16)) / np.float32(math.log(8)) * np.float32(16)).astype(np.int32)
    large = np.minimum(large, 31)
    bucket = np.where(rel < 16, rel, large)
    oh = np.zeros((32, 384), np.float32)
    oh[bucket, u] = 1.0
    return oh


def make_ret_tables():
    pos = np.arange(T, dtype=np.float32)
    theta = (np.float32(1.0) / (np.float32(10000.0) ** np.linspace(0.0, 1.0, 128, dtype=np.float32))).astype(np.float32)
    theta = np.repeat(theta, 2)
    ang = (pos[:, None] * theta[None, :]).astype(np.float32)
    cos = np.cos(ang.astype(np.float64)).astype(np.float32)
    sin = np.sin(ang.astype(np.float64)).astype(np.float32)
    sinS = sin.copy()
    sinS[:, 0::2] = -sin[:, 0::2]
    p = np.arange(128, dtype=np.float64)
    dec = np.zeros((128, 8), np.float32)
    for h in range(4):
        gam = 1.0 - 2.0 ** (-5.0 - h)
        dec[:, h] = gam ** (p + 1)
        dec[:, 4 + h] = gam ** (-(p + 1)) / 16.0
    return cos, sinS, dec


def phase_copy(ctx, src, dst, nrows):
    ph = Phase(ctx, "cp")
    b = ph.buf("cpbuf")
    step = 512
    for r in range(0, nrows, step):
        ph.dma("sp", dst[r:r + step, :], src[r:r + step, :], b)
    ph.run()
```
